# Optimizing a Trainium2 kernel written in Bass

```python
import math
import jax, jax.numpy as jnp
from jax import lax
import numpy as np

D_MODEL = 1024
BATCH = 2
SEQ = 16384
DEPTH = 2

GRID_W = 64
CTX_LEN = 256
HEAD_DIM = 64
EPS = 1e-6
NEG_INF = -1e30

A_HEADS = D_MODEL // 256
A_WIDTH = A_HEADS * HEAD_DIM
CHUNK = 128
B_HEADS = (3 * D_MODEL) // 512
B_WIDTH = B_HEADS * HEAD_DIM
NA_ROWS = 8
NA_COLS = 16
C_HEADS = (3 * D_MODEL) // 512
C_NOPE = 64
C_ROPE = 32
C_QK = C_NOPE + C_ROPE
C_VDIM = 64
C_Q_LORA = (3 * D_MODEL) // 8
C_KV_LORA = D_MODEL // 4
C_WIDTH = C_HEADS * C_VDIM
Q_BLOCK = 128
ROPE_BASE = 10000.0

MIX_WIDTH = A_WIDTH + B_WIDTH + C_WIDTH
IN_A = 2 * A_WIDTH
IN_B = 3 * B_WIDTH
IN_C = C_Q_LORA + C_KV_LORA + C_ROPE
IN_WIDTH = IN_A + IN_B + IN_C

N_GROUPS = 8
EXPERTS_PER_GROUP = 8
N_EXPERTS = N_GROUPS * EXPERTS_PER_GROUP
TOP_K = 2
D_EXPERT = D_MODEL // 2
MOE_BLOCK = 128

kernel_name = 'hybrid_dit_gmlp_natten_mla_hmoe'


def rmsnorm(x, g):
    xf = x.astype(jnp.float32)
    y = xf * lax.rsqrt(jnp.mean(xf * xf, axis=-1, keepdims=True) + EPS)
    return (y * g.astype(jnp.float32)).astype(x.dtype)


def modulate(h, shift, scale):
    return h * (1 + scale) + shift


def rope_table(pos):
    half = C_ROPE // 2
    inv = ROPE_BASE ** (-jnp.arange(0, half, 2, dtype=jnp.float32) / half)
    ang = pos.astype(jnp.float32)[:, None] * inv[None, :]
    return jnp.cos(ang), jnp.sin(ang)


def rope_1d(x, cos, sin):
    x1, x2 = jnp.split(x, 2, axis=-1)
    return jnp.concatenate([x1 * cos - x2 * sin, x2 * cos + x1 * sin], axis=-1)


def axial_rope(x, cos_r, sin_r, cos_c, sin_c):
    half = C_ROPE // 2
    return jnp.concatenate([rope_1d(x[..., :half], cos_r, sin_r), rope_1d(x[..., half:], cos_c, sin_c)], axis=-1)


def chunk_gmlp(z, v_norm, w_s, b_s):
    bn, t, _ = z.shape
    u, v = jnp.split(jax.nn.gelu(z), 2, axis=-1)
    v = rmsnorm(v, v_norm).reshape(bn, t // CHUNK, CHUNK, A_HEADS, HEAD_DIM)
    v = jnp.einsum('hpq,bnqhd->bnphd', w_s, v) + b_s.T[:, :, None]
    return u * v.reshape(bn, t, A_WIDTH)


def heads_qkv(z, q_norm, k_norm):
    bn, t, _ = z.shape
    q, k, v = jnp.split(z, 3, axis=-1)
    q = rmsnorm(q.reshape(bn, t, B_HEADS, HEAD_DIM), q_norm)
    k = rmsnorm(k.reshape(bn, t, B_HEADS, HEAD_DIM), k_norm)
    return q, k, v.reshape(bn, t, B_HEADS, HEAD_DIM)


def dense_attend(q, k, v):
    s = jnp.einsum('bqhd,bkhd->bhqk', q, k).astype(jnp.float32) * (HEAD_DIM ** -0.5)
    p = jax.nn.softmax(s, axis=-1).astype(v.dtype)
    o = jnp.einsum('bhqk,bkhd->bqhd', p, v)
    return o.reshape(o.shape[0], o.shape[1], -1)


def neighbourhood_attention(q, k, v, k_ctx, v_ctx, rpb):
    bn, l, h, dh = q.shape
    rows = l // GRID_W
    kh = min(NA_ROWS, rows)
    r = jnp.arange(rows)
    r0 = jnp.clip(r - kh // 2, 0, rows - kh)
    key_rows = r0[:, None] + jnp.arange(kh)[None, :]
    cq = jnp.arange(GRID_W)
    c0 = jnp.clip(cq - NA_COLS // 2, 0, GRID_W - NA_COLS)
    in_win = (cq[None, :] >= c0[:, None]) & (cq[None, :] < c0[:, None] + NA_COLS)
    dc = jnp.clip(cq[None, :] - cq[:, None], -(NA_COLS - 1), NA_COLS - 1) + NA_COLS - 1
    dr = key_rows - r[:, None] + NA_ROWS - 1
    bias = jnp.take(rpb[:, dr], dc, axis=-1)
    bias = bias.transpose(0, 1, 3, 2, 4).astype(jnp.float32)
    qg = q.reshape(bn, rows, GRID_W, h, dh)
    kg = k.reshape(bn, rows, GRID_W, h, dh)[:, key_rows]
    vg = v.reshape(bn, rows, GRID_W, h, dh)[:, key_rows]
    scale = dh ** -0.5
    s_loc = jnp.einsum('brqhd,brkwhd->bhrqkw', qg, kg).astype(jnp.float32) * scale + bias[None]
    s_loc = jnp.where(in_win[:, None, :], s_loc, NEG_INF)
    s_ctx = jnp.einsum('brqhd,bchd->bhrqc', qg, k_ctx).astype(jnp.float32) * scale
    s = jnp.concatenate([s_loc.reshape(bn, h, rows, GRID_W, kh * GRID_W), s_ctx], axis=-1)
    p = jax.nn.softmax(s, axis=-1).astype(v.dtype)
    p_loc = p[..., :kh * GRID_W].reshape(bn, h, rows, GRID_W, kh, GRID_W)
    p_ctx = p[..., kh * GRID_W:]
    o = jnp.einsum('bhrqkw,brkwhd->brqhd', p_loc, vg) + jnp.einsum('bhrqc,bchd->brqhd', p_ctx, v_ctx)
    return o.reshape(bn, l, h * dh)


def mla_project(z, q_a_norm, w_q_up, kv_a_norm, w_kv_up, q_norm, k_norm):
    bn, t, _ = z.shape
    q_lat, kv_lat, k_rope = jnp.split(z, [C_Q_LORA, C_Q_LORA + C_KV_LORA], axis=-1)
    q = (rmsnorm(q_lat, q_a_norm) @ w_q_up).reshape(bn, t, C_HEADS, C_QK)
    kv = (rmsnorm(kv_lat, kv_a_norm) @ w_kv_up).reshape(bn, t, C_HEADS, C_NOPE + C_VDIM)
    k_nope, v = jnp.split(kv, [C_NOPE], axis=-1)
    q_nope = rmsnorm(q[..., :C_NOPE], q_norm[:C_NOPE])
    q_rope = rmsnorm(q[..., C_NOPE:], q_norm[C_NOPE:])
    k_nope = rmsnorm(k_nope, k_norm[:C_NOPE])
    k_rope = rmsnorm(k_rope, k_norm[C_NOPE:])
    return q_nope, q_rope, k_nope, k_rope, v


def mla_attend(q_nope, q_rope, k_nope, k_rope, v):
    s = jnp.einsum('bqhd,bkhd->bhqk', q_nope, k_nope) + jnp.einsum('bqhd,bkd->bhqk', q_rope, k_rope)
    p = jax.nn.softmax(s.astype(jnp.float32) * (C_QK ** -0.5), axis=-1).astype(v.dtype)
    o = jnp.einsum('bhqk,bkhd->bqhd', p, v)
    return o.reshape(o.shape[0], o.shape[1], -1)


def mla_latent_attention(q_nope, q_rope, k_nope, k_rope, v):
    bn, l, h, _ = q_nope.shape
    nb = l // Q_BLOCK
    qn = q_nope.reshape(bn, nb, Q_BLOCK, h, C_NOPE).transpose(1, 0, 2, 3, 4)
    qr = q_rope.reshape(bn, nb, Q_BLOCK, h, C_ROPE).transpose(1, 0, 2, 3, 4)
    o = lax.map(lambda a: mla_attend(a[0], a[1], k_nope, k_rope, v), (qn, qr))
    return o.transpose(1, 0, 2, 3).reshape(bn, l, h * C_VDIM)


def hier_moe(t, w_group, w_router, w1, w3, w2):
    n, d = t.shape
    tok = jnp.arange(n)
    g_logits = (t @ w_group).astype(jnp.float32)
    g_idx = jnp.argmax(g_logits, axis=-1)
    g_gate = jax.nn.softmax(g_logits, axis=-1)[tok, g_idx]
    e_logits = (t @ w_router).astype(jnp.float32).reshape(n, N_GROUPS, EXPERTS_PER_GROUP)[tok, g_idx]
    top_l, top_j = lax.top_k(e_logits, TOP_K)
    weights = g_gate[:, None] * jax.nn.softmax(top_l, axis=-1)
    expert = g_idx[:, None] * EXPERTS_PER_GROUP + top_j
    a = n * TOP_K
    flat_e = expert.reshape(a)
    flat_tok = jnp.arange(a) // TOP_K
    flat_w = weights.reshape(a)
    order = jnp.argsort(flat_e)
    se, stok, sw = flat_e[order], flat_tok[order], flat_w[order]
    counts = jnp.bincount(flat_e, length=N_EXPERTS)
    starts = jnp.cumsum(counts) - counts
    pcounts = (counts + MOE_BLOCK - 1) // MOE_BLOCK * MOE_BLOCK
    pends = jnp.cumsum(pcounts)
    pstarts = pends - pcounts
    dest = pstarts[se] + (jnp.arange(a) - starts[se])
    n_blocks = -(-a // MOE_BLOCK) + N_EXPERTS
    p_slots = n_blocks * MOE_BLOCK
    slot_tok = jnp.zeros((p_slots,), jnp.int32).at[dest].set(stok.astype(jnp.int32))
    slot_w = jnp.zeros((p_slots,), t.dtype).at[dest].set(sw.astype(t.dtype))
    block_e = jnp.minimum(jnp.searchsorted(pends, jnp.arange(n_blocks) * MOE_BLOCK, side='right'), N_EXPERTS - 1)
    xs = t[slot_tok].reshape(n_blocks, MOE_BLOCK, d)

    def expert_block(args):
        xb, e = args
        hb = jax.nn.silu(xb @ w1[e]) * (xb @ w3[e])
        return hb @ w2[e]

    ys = lax.map(expert_block, (xs, block_e)).reshape(p_slots, d)
    return jnp.zeros_like(t).at[slot_tok].add(ys * slot_w[:, None])


def setup_inputs(seed: int = 0) -> dict:
    key = jax.random.key(seed)
    ks = jax.random.split(key, 27)
    f32 = jnp.float32

    def nrm(i, shape, s):
        return jax.random.normal(ks[i], shape, f32) * s

    def gain(i, shape):
        return 1.0 + 0.01 * jax.random.normal(ks[i], shape, f32)

    L, D = DEPTH, D_MODEL
    return {
        'x': nrm(0, (BATCH, SEQ, D), 1.0),
        'c': nrm(1, (BATCH, D), 1.0),
        'ctx': nrm(2, (BATCH, CTX_LEN, D), 1.0),
        'c_ctx': nrm(3, (D,), 1.0),
        'w_ada': nrm(4, (L, D, 6 * D), 0.5 * D ** -0.5),
        'b_ada': nrm(5, (L, 6 * D), 0.02),
        'norm_mix': gain(6, (L, D)),
        'w_in': nrm(7, (L, D, IN_WIDTH), D ** -0.5),
        'a_v_norm': gain(8, (L, A_WIDTH)),
        'a_w_s': nrm(9, (L, A_HEADS, CHUNK, CHUNK), CHUNK ** -0.5),
        'a_b_s': 1.0 + nrm(10, (L, A_HEADS, CHUNK), 0.02),
        'b_q_norm': gain(11, (L, HEAD_DIM)),
        'b_k_norm': gain(12, (L, HEAD_DIM)),
        'b_rpb': nrm(13, (L, B_HEADS, 2 * NA_ROWS - 1, 2 * NA_COLS - 1), 0.1),
        'c_q_a_norm': gain(14, (L, C_Q_LORA)),
        'c_w_q_up': nrm(15, (L, C_Q_LORA, C_HEADS * C_QK), C_Q_LORA ** -0.5),
        'c_kv_a_norm': gain(16, (L, C_KV_LORA)),
        'c_w_kv_up': nrm(17, (L, C_KV_LORA, C_HEADS * (C_NOPE + C_VDIM)), C_KV_LORA ** -0.5),
        'c_q_norm': gain(18, (L, C_QK)),
        'c_k_norm': gain(19, (L, C_QK)),
        'w_out': nrm(20, (L, MIX_WIDTH, D), MIX_WIDTH ** -0.5),
        'norm_ffn': gain(21, (L, D)),
        'moe_w_group': nrm(22, (L, D, N_GROUPS), D ** -0.5),
        'moe_w_router': nrm(23, (L, D, N_EXPERTS), D ** -0.5),
        'moe_w1': nrm(24, (L, N_EXPERTS, D, D_EXPERT), D ** -0.5),
        'moe_w3': nrm(25, (L, N_EXPERTS, D, D_EXPERT), D ** -0.5),
        'moe_w2': nrm(26, (L, N_EXPERTS, D_EXPERT, D), D_EXPERT ** -0.5),
    }


def reference(x, c, ctx, c_ctx, w_ada, b_ada, norm_mix, w_in, a_v_norm, a_w_s, a_b_s, b_q_norm, b_k_norm, b_rpb,
              c_q_a_norm, c_w_q_up, c_kv_a_norm, c_w_kv_up, c_q_norm, c_k_norm, w_out, norm_ffn,
              moe_w_group, moe_w_router, moe_w1, moe_w3, moe_w2):
    bn, l, d = x.shape
    nc = ctx.shape[1]
    pos = jnp.arange(l)
    cos_r, sin_r = rope_table(pos // GRID_W)
    cos_c, sin_c = rope_table(pos % GRID_W)
    cos_r, sin_r, cos_c, sin_c = (a.astype(x.dtype) for a in (cos_r, sin_r, cos_c, sin_c))
    silu_c = jax.nn.silu(c)
    silu_cc = jax.nn.silu(c_ctx)
    xc = ctx
    for i in range(DEPTH):
        need_ctx = i < DEPTH - 1
        mod = silu_c @ w_ada[i] + b_ada[i]
        mod_c = silu_cc @ w_ada[i] + b_ada[i]
        sh1, s1, g1, sh2, s2, g2 = [m[:, None, :] for m in jnp.split(mod, 6, axis=-1)]
        sh1c, s1c, g1c, sh2c, s2c, g2c = jnp.split(mod_c, 6, axis=-1)

        h = modulate(rmsnorm(x, norm_mix[i]), sh1, s1)
        hc = modulate(rmsnorm(xc, norm_mix[i]), sh1c, s1c)
        z_a, z_b, z_c = jnp.split(h @ w_in[i], [IN_A, IN_A + IN_B], axis=-1)
        zc_a, zc_b, zc_c = jnp.split(hc @ w_in[i], [IN_A, IN_A + IN_B], axis=-1)
        o_a = chunk_gmlp(z_a, a_v_norm[i], a_w_s[i], a_b_s[i])
        q_b, k_b, v_b = heads_qkv(z_b, b_q_norm[i], b_k_norm[i])
        qc_b, kc_b, vc_b = heads_qkv(zc_b, b_q_norm[i], b_k_norm[i])
        o_b = neighbourhood_attention(q_b, k_b, v_b, kc_b, vc_b, b_rpb[i])
        qn, qr, kn, kr, v_c = mla_project(z_c, c_q_a_norm[i], c_w_q_up[i], c_kv_a_norm[i], c_w_kv_up[i], c_q_norm[i], c_k_norm[i])
        qr = axial_rope(qr, cos_r[:, None], sin_r[:, None], cos_c[:, None], sin_c[:, None])
        kr = axial_rope(kr, cos_r, sin_r, cos_c, sin_c)
        cqn, cqr, ckn, ckr, cv = mla_project(zc_c, c_q_a_norm[i], c_w_q_up[i], c_kv_a_norm[i], c_w_kv_up[i], c_q_norm[i], c_k_norm[i])
        o_c = mla_latent_attention(qn, qr, jnp.concatenate([kn, ckn], axis=1), jnp.concatenate([kr, ckr], axis=1),
                                   jnp.concatenate([v_c, cv], axis=1))
        x = x + g1 * (jnp.concatenate([o_a, o_b, o_c], axis=-1) @ w_out[i])
        if need_ctx:
            oc_a = chunk_gmlp(zc_a, a_v_norm[i], a_w_s[i], a_b_s[i])
            oc_b = dense_attend(qc_b, kc_b, vc_b)
            oc_c = mla_attend(cqn, cqr, ckn, ckr, cv)
            xc = xc + g1c * (jnp.concatenate([oc_a, oc_b, oc_c], axis=-1) @ w_out[i])

        h2 = modulate(rmsnorm(x, norm_ffn[i]), sh2, s2).reshape(bn * l, d)
        if need_ctx:
            h2c = modulate(rmsnorm(xc, norm_ffn[i]), sh2c, s2c).reshape(bn * nc, d)
            y = hier_moe(jnp.concatenate([h2, h2c], axis=0), moe_w_group[i], moe_w_router[i], moe_w1[i], moe_w3[i], moe_w2[i])
            x = x + g2 * y[:bn * l].reshape(bn, l, d)
            xc = xc + g2c * y[bn * l:].reshape(bn, nc, d)
        else:
            y = hier_moe(h2, moe_w_group[i], moe_w_router[i], moe_w1[i], moe_w3[i], moe_w2[i])
            x = x + g2 * y.reshape(bn, l, d)
    return x
```

```python
import contextlib
import numpy as np
import ml_dtypes
import concourse.bass as bass
import concourse.mybir as mybir
from concourse.bass_utils import run_bass_kernel_spmd

F32 = mybir.dt.float32
BF16 = mybir.dt.bfloat16
I32 = mybir.dt.int32
AF = mybir.ActivationFunctionType
ALU = mybir.AluOpType
AX = mybir.AxisListType
NPBF = ml_dtypes.bfloat16

D = 1024
GRID_W = 64
CTX = 256
EPS = 1e-6
INW = 2336
NEG = -30000.0


class _Op:
    __slots__ = ("eng", "fn", "deps", "needed", "semkey", "val", "dma", "idx")

    def __init__(self, eng, fn, dma):
        self.eng = eng
        self.fn = fn
        self.deps = []
        self.needed = False
        self.semkey = None
        self.val = 0
        self.dma = dma


class Sched:
    ENGS = ("pe", "act", "dve", "pool", "sp")
    NLANES = 6

    def __init__(self, nc):
        self.nc = nc
        self.ops = {e: [] for e in self.ENGS}
        self.bufs = {}
        self.phase = 0
        self.lane_ops = {}
        self.lane_n = {e: 0 for e in self.ENGS}
        self.pending = {e: [] for e in self.ENGS}
        self.last = {e: None for e in self.ENGS}

    def _add(self, eng, fn, r, w, dma):
        op = _Op(eng, fn, dma)
        deps = []
        for k in r:
            st = self.bufs.setdefault(k, [None, []])
            if st[0] is not None:
                deps.append(st[0])
        for k in w:
            st = self.bufs.setdefault(k, [None, []])
            if st[0] is not None:
                deps.append(st[0])
            deps.extend(st[1])
        deps.extend(self.pending[eng])
        self.pending[eng] = []
        if dma:
            lane = self.lane_n[eng] % self.NLANES
            self.lane_n[eng] += 1
            key = ("lane", eng, lane)
            prev = self.lane_ops.get(key)
            if prev is not None:
                deps.append(prev)
            self.lane_ops[key] = op
            op.semkey = key
            op.val = (prev.val if prev is not None else 0) + 16
            op.needed = True
        else:
            op.semkey = ("eng", eng, self.phase)
        seen = set()
        for d in deps:
            if d is op or id(d) in seen:
                continue
            seen.add(id(d))
            if (not d.dma) and d.eng == eng and eng == "pe":
                continue
            d.needed = True
            op.deps.append(d)
        for k in r:
            self.bufs[k][1].append(op)
        for k in w:
            self.bufs[k] = [op, []]
        self.ops[eng].append(op)
        self.last[eng] = op
        return op

    def op(self, eng, fn, r=(), w=()):
        return self._add(eng, fn, r, w, False)

    def dma(self, eng, out, in_, r=(), w=()):
        return self._add(eng, lambda e: e.dma_start(out=out, in_=in_), r, w, True)

    def barrier(self):
        lasts = [o for o in self.last.values() if o is not None] + list(self.lane_ops.values())
        for e in self.ENGS:
            self.pending[e] = list(lasts)
        self.bufs = {}
        self.phase += 1

    def emit(self, stack):
        nc = self.nc
        cnt = {}
        for e in self.ENGS:
            for op in self.ops[e]:
                if not op.dma and op.needed:
                    cnt[op.semkey] = cnt.get(op.semkey, 0) + 1
                    op.val = cnt[op.semkey]
        keys = set()
        for e in self.ENGS:
            for op in self.ops[e]:
                if op.needed:
                    keys.add(op.semkey)
        sems = {k: stack.enter_context(nc.semaphore("s_" + "_".join(str(x) for x in k))) for k in sorted(keys, key=str)}
        finals = {}
        for op in self.lane_ops.values():
            finals[op.semkey] = op.val

        def run(engname, e):
            waited = {}
            for op in self.ops[engname]:
                for d in op.deps:
                    if waited.get(d.semkey, 0) >= d.val:
                        continue
                    e.wait_ge(sems[d.semkey], d.val)
                    waited[d.semkey] = d.val
                ins = op.fn(e)
                if op.needed:
                    ins.then_inc(sems[op.semkey], 16 if op.dma else 1)
            for k, v in finals.items():
                if k[1] == engname and waited.get(k, 0) < v:
                    e.wait_ge(sems[k], v)

        with nc.Block() as block:
            @block.tensor
            def _(e):
                run("pe", e)

            @block.scalar
            def _(e):
                run("act", e)

            @block.vector
            def _(e):
                run("dve", e)

            @block.gpsimd
            def _(e):
                run("pool", e)

            @block.sync
            def _(e):
                run("sp", e)


class KB:
    def __init__(self, nc, stack):
        self.nc = nc
        self.stack = stack
        self.S = Sched(nc)
        self.nps = 0

    def sb(self, name, shape, dt):
        return self.stack.enter_context(self.nc.sbuf_tensor(name, list(shape), dt))

    def ps(self, name, shape, dt):
        return self.stack.enter_context(self.nc.psum_tensor(name, list(shape), dt))

    def dram(self, name, shape, dt, kind):
        return self.nc.dram_tensor(name, list(shape), dt, kind=kind).ap()

    def mm(self, out, lhsT, rhs, start, stop, r, w):
        self.S.op("pe", lambda e: e.matmul(out, lhsT, rhs, start=start, stop=stop), r, w)

    def tr(self, out, in_, ident, r, w):
        self.S.op("pe", lambda e: e.transpose(out, in_, ident), r, w)

    def act(self, out, in_, func, r, w, **kw):
        self.S.op("act", lambda e: e.activation(out, in_, func, **kw), r, w)

    def ts(self, eng, out, in0, s1, s2, op0, op1, r, w):
        if op1 is None:
            self.S.op(eng, lambda e: e.tensor_scalar(out, in0, s1, None, op0), r, w)
        else:
            self.S.op(eng, lambda e: e.tensor_scalar(out, in0, s1, s2, op0, op1), r, w)

    def tt(self, eng, out, in0, in1, op, r, w):
        self.S.op(eng, lambda e: e.tensor_tensor(out, in0, in1, op), r, w)

    def stt(self, eng, out, in0, scalar, in1, op0, op1, r, w):
        self.S.op(eng, lambda e: e.scalar_tensor_tensor(out, in0, scalar, in1, op0, op1), r, w)

    def cp(self, eng, out, in_, r, w):
        if eng == "act":
            self.S.op("act", lambda e: e.copy(out, in_), r, w)
        else:
            self.S.op(eng, lambda e: e.tensor_copy(out, in_), r, w)

    def rsum(self, eng, out, in_, r, w):
        self.S.op(eng, lambda e: e.reduce_sum(out, in_, AX.X), r, w)

    def memset(self, eng, ap, v, r, w):
        self.S.op(eng, lambda e: e.memset(ap, v), r, w)

    def dma(self, eng, out, in_, r, w):
        self.S.dma(eng, out, in_, r, w)


G_AV, G_BQ, G_BK, G_CQA, G_CKVA, G_CQN, G_CKN, G_CKR, G_TOT = 0, 256, 640, 1024, 1408, 1664, 2240, 2624, 2656


def v3(ap, g):
    return ap.rearrange("p (g d) -> p g d", g=g)


def bc3(ap2, d):
    p, g = ap2.shape
    return ap2.unsqueeze(2).to_broadcast([p, g, d])


def build_A(NT_OWN):
    NT = NT_OWN + 2
    T = NT * 128
    nc = bass.Bass("TRN2", target_bir_lowering=False)
    stack = contextlib.ExitStack()
    with stack:
        K = KB(nc, stack)
        S = K.S
        x_d = K.dram("x", [T, D], F32, "ExternalInput")
        cT_d = K.dram("cT", [128, 16], F32, "ExternalInput")
        wada_d = K.dram("w_ada", [128, 8, 6144], F32, "ExternalInput")
        bada_d = K.dram("b_adaT", [128, 48], F32, "ExternalInput")
        nmix_d = K.dram("norm_mixT", [128, 8], F32, "ExternalInput")
        win_d = K.dram("w_in", [128, 8, INW], F32, "ExternalInput")
        wsT_d = K.dram("w_sT", [128, 4, 128], F32, "ExternalInput")
        bs_d = K.dram("b_s", [128, 4], F32, "ExternalInput")
        gains_d = K.dram("gains", [128, G_TOT], F32, "ExternalInput")
        wq_d = K.dram("wq", [128, 3, 576], F32, "ExternalInput")
        wkv_d = K.dram("wkv", [128, 2, 768], F32, "ExternalInput")
        rope_d = K.dram("rope", [T, 32], F32, "ExternalInput")
        ident_d = K.dram("ident", [128, 128], F32, "ExternalInput")
        oa_d = K.dram("oa", [T, 256], BF16, "ExternalOutput")
        qbT_d = K.dram("qbT", [3, 128, T], BF16, "ExternalOutput")
        kvb_d = K.dram("kvb", [T, 768], BF16, "ExternalOutput")
        qcT_d = K.dram("qcT", [6, 96, T], BF16, "ExternalOutput")
        kcT_d = K.dram("kcT", [6, 96, T], BF16, "ExternalOutput")
        vc_d = K.dram("vc", [T, 390], BF16, "ExternalOutput")
        modT_d = K.dram("modT", [128, 96], F32, "ExternalOutput")
        ident = K.sb("ident_b", [128, 128], BF16)
        identf = K.sb("identf", [128, 128], F32)
        cT = K.sb("cTs", [128, 16], F32)
        scT = K.sb("scT", [128, 16], F32)
        bada = K.sb("bada", [128, 48], F32)
        nmix = K.sb("nmix", [128, 8], F32)
        modT = K.sb("modTs", [128, 96], F32)
        A1 = K.sb("A1", [128, 16], F32)
        wst = [K.sb(f"wst{i}", [128, 8, 512], F32) for i in range(2)]
        win = K.sb("win", [128, 8, INW], BF16)
        wsT = K.sb("wsT", [128, 4, 128], BF16)
        bs = K.sb("bs", [128, 4], F32)
        gains = K.sb("gainss", [128, G_TOT], F32)
        wq = K.sb("wqs", [128, 3, 576], BF16)
        wkv = K.sb("wkvs", [128, 2, 768], BF16)
        xt = [K.sb(f"xt{i}", [128, D], F32) for i in range(2)]
        ropet = [K.sb(f"ropet{i}", [128, 32], F32) for i in range(2)]
        sqj = K.sb("sqj", [128, D], F32)
        st = K.sb("st", [128, 64], F32)
        xn = K.sb("xn", [128, D], BF16)
        hT = K.sb("hT", [128, 8, 128], BF16)
        z = K.sb("z", [128, INW], F32)
        g1 = K.sb("g1", [128, 512], F32)
        g2 = K.sb("g2", [128, 512], F32)
        gg = K.sb("gg", [128, 512], F32)
        vnb = K.sb("vnb", [128, 256], BF16)
        oa = K.sb("oas", [128, 256], BF16)
        t384 = K.sb("t384", [128, 384], F32)
        u384 = K.sb("u384", [128, 384], F32)
        qnb = K.sb("qnb", [128, 384], BF16)
        qbTs = K.sb("qbTs", [128, 3, 128], BF16)
        kvbs = K.sb("kvbs", [128, 768], BF16)
        qab = K.sb("qab", [128, 384], BF16)
        qaT = K.sb("qaT", [128, 3, 128], BF16)
        qf = K.sb("qf", [128, 576], F32)
        qs = K.sb("qs", [128, 576], F32)
        qc = K.sb("qc", [128, 6, 96], BF16)
        rt = [K.sb(f"rt{i}", [128, 48], F32) for i in range(4)]
        qcTs = K.sb("qcTs", [96, 6, 128], BF16)
        kvab = K.sb("kvab", [128, 256], BF16)
        kvaT = K.sb("kvaT", [128, 2, 128], BF16)
        kvf = K.sb("kvf", [128, 768], F32)
        kc = K.sb("kc", [128, 6, 96], BF16)
        kr = K.sb("kr", [128, 32], F32)
        krr = K.sb("krr", [128, 32], F32)
        vcs = K.sb("vcs", [128, 6, 65], BF16)
        kcTs = K.sb("kcTs", [96, 6, 128], BF16)
        PB = [K.ps(f"pb{i}", [128, 512], F32) for i in range(8)]
        PB0b = PB[0].bitcast(BF16)

        K.dma("sp", identf[:, :], ident_d[:, :], [], ["identf"])
        K.cp("dve", ident[:, :], identf[:, :], ["identf"], ["ident"])
        K.dma("sp", cT[:, :], cT_d[:, :], [], ["cT"])
        K.dma("sp", bada[:, :], bada_d[:, :], [], ["bada"])
        K.dma("sp", nmix[:, :], nmix_d[:, :], [], ["nmix"])
        K.dma("sp", bs[:, :], bs_d[:, :], [], ["bs"])
        K.dma("sp", gains[:, :], gains_d[:, :], [], ["gains"])
        K.dma("pool", win[:, :, :], win_d[:, :, :], [], ["win"])
        K.dma("pool", wsT[:, :, :], wsT_d[:, :, :], [], ["wsT"])
        K.dma("pool", wq[:, :, :], wq_d[:, :, :], [], ["wq"])
        K.dma("pool", wkv[:, :, :], wkv_d[:, :, :], [], ["wkv"])
        K.memset("pool", vcs[:, :, :], 1.0, [], ["vcs"])
        K.act(scT[:, :], cT[:, :], AF.Silu, ["cT"], ["scT"])
        for grp in range(12):
            b = grp % 2
            K.dma("sp", wst[b][:, :, :], wada_d[:, :, grp * 512:(grp + 1) * 512], [], [("wst", b)])
            for jj in range(4):
                j = grp * 4 + jj
                for c in range(8):
                    K.mm(PB[1][:, 2 * j:2 * j + 2], wst[b][:, c, jj * 128:(jj + 1) * 128], scT[:, 2 * c:2 * c + 2],
                         c == 0, c == 7, [("wst", b), "scT"], ["pb1"])
        K.tt("dve", v3(modT[:, :], 48), v3(PB[1][:, 0:96], 48), bc3(bada[:, :], 2), ALU.add, ["pb1", "bada"], ["modT"])
        K.dma("sp", modT_d[:, :], modT[:, :], ["modT"], [])
        K.stt("dve", v3(A1[:, :], 8), v3(modT[:, 16:32], 8), 1.0, bc3(nmix[:, :], 2), ALU.add, ALU.mult,
              ["modT", "nmix"], ["A1"])

        def rstd_of(ss, n, dim, extra=None):
            K.ts("dve", ss, ss, 1.0 / dim, EPS, ALU.mult, ALU.add, ["st"], ["st"])
            K.act(ss, ss, AF.Sqrt, ["st"], ["st"])
            K.S.op("dve", lambda e, a=ss: e.reciprocal(a, a), ["st"], ["st"])
            if extra is not None:
                K.ts("dve", ss, ss, extra, None, ALU.mult, None, ["st"], ["st"])

        def rope(src3, dst3, G, rp, rkeys, wkeys):
            for a in range(2):
                o = 16 * a
                cos = rp[:, 16 * a:16 * a + 8].unsqueeze(1).to_broadcast([128, G, 8])
                sin = rp[:, 16 * a + 8:16 * a + 16].unsqueeze(1).to_broadcast([128, G, 8])
                x1 = src3[:, :, o:o + 8]
                x2 = src3[:, :, o + 8:o + 16]
                t = [v3(rt[i][:, 0:G * 8], G) for i in range(4)]
                K.tt("pool", t[0], x1, cos, ALU.mult, rkeys, ["rt0"])
                K.tt("pool", t[1], x2, sin, ALU.mult, rkeys, ["rt1"])
                K.tt("dve", dst3[:, :, o:o + 8], t[0], t[1], ALU.subtract, ["rt0", "rt1"], wkeys)
                K.tt("pool", t[2], x2, cos, ALU.mult, rkeys, ["rt2"])
                K.tt("pool", t[3], x1, sin, ALU.mult, rkeys, ["rt3"])
                K.tt("dve", dst3[:, :, o + 8:o + 16], t[2], t[3], ALU.add, ["rt2", "rt3"], wkeys)

        for t in range(NT):
            b = t % 2
            wh = 0 if t < NT_OWN else 1
            rows = slice(t * 128, (t + 1) * 128)
            X = xt[b]
            K.dma("sp", X[:, :], x_d[rows, :], [], [("xt", b)])
            K.dma("sp", ropet[b][:, :], rope_d[rows, :], [], [("rope", b)])
            K.tt("dve", sqj[:, :], X[:, :], X[:, :], ALU.mult, [("xt", b)], ["sqj"])
            K.rsum("dve", st[:, 0:1], sqj[:, :], ["sqj"], ["st"])
            rstd_of(st[:, 0:1], 1, D)
            K.act(xn[:, :], X[:, :], AF.Copy, [("xt", b), "st"], ["xn"], scale=st[:, 0:1])
            for c in range(8):
                K.tr(PB0b[:, c * 128:(c + 1) * 128], xn[:, c * 128:(c + 1) * 128], ident[:, :], ["xn", "ident"], ["pb0"])
            for c in range(8):
                K.ts("dve", hT[:, c, :], PB0b[:, c * 128:(c + 1) * 128], A1[:, 2 * c + wh:2 * c + wh + 1],
                     modT[:, 2 * c + wh:2 * c + wh + 1], ALU.mult, ALU.add, ["pb0", "A1", "modT"], ["hT"])
            for k5 in range(5):
                n0 = k5 * 512
                n1 = min(INW, n0 + 512)
                pb = 1 + k5 % 3
                for c in range(8):
                    K.mm(PB[pb][:, 0:n1 - n0], hT[:, c, :], win[:, c, n0:n1], c == 0, c == 7, ["hT", "win"], [f"pb{pb}"])
                K.cp("act", z[:, n0:n1], PB[pb][:, 0:n1 - n0], [f"pb{pb}"], ["z"])
            za = z[:, 0:512]
            K.tt("pool", g1[:, :], za, za, ALU.mult, ["z"], ["g1"])
            K.ts("dve", g1[:, :], g1[:, :], 0.044715, 1.0, ALU.mult, ALU.add, ["g1"], ["g1"])
            K.tt("pool", g1[:, :], g1[:, :], za, ALU.mult, ["g1", "z"], ["g1"])
            K.act(g2[:, :], g1[:, :], AF.Sigmoid, ["g1"], ["g2"], scale=1.5957691216057308)
            K.tt("dve", gg[:, :], g2[:, :], za, ALU.mult, ["g2", "z"], ["gg"])
            K.tt("pool", g1[:, 0:256], gg[:, 256:512], gg[:, 256:512], ALU.mult, ["gg"], ["g1"])
            K.rsum("dve", st[:, 0:1], g1[:, 0:256], ["g1"], ["st"])
            rstd_of(st[:, 0:1], 1, 256)
            K.ts("dve", g1[:, 256:512], gg[:, 256:512], st[:, 0:1], None, ALU.mult, None, ["gg", "st", "g1"], ["g1"])
            K.tt("pool", vnb[:, :], g1[:, 256:512], gains[:, G_AV:G_AV + 256], ALU.mult, ["g1", "gains"], ["vnb"])
            for hd in range(4):
                K.mm(PB[4][:, hd * 64:(hd + 1) * 64], wsT[:, hd, :], vnb[:, hd * 64:(hd + 1) * 64], True, True,
                     ["wsT", "vnb"], ["pb4"])
            for hd in range(4):
                K.stt("dve", oa[:, hd * 64:(hd + 1) * 64], PB[4][:, hd * 64:(hd + 1) * 64], bs[:, hd:hd + 1],
                      gg[:, hd * 64:(hd + 1) * 64], ALU.add, ALU.mult, ["pb4", "bs", "gg"], ["oa"])
            K.dma("sp", oa_d[rows, :], oa[:, :], ["oa"], [])
            for which, (o0, gofs, extra) in enumerate(((512, G_BQ, 0.125), (896, G_BK, None))):
                src = z[:, o0:o0 + 384]
                K.tt("pool", t384[:, :], src, src, ALU.mult, ["z"], ["t384"])
                K.rsum("dve", st[:, 0:6], v3(t384[:, :], 6), ["t384"], ["st"])
                rstd_of(st[:, 0:6], 6, 64, extra)
                K.tt("dve", v3(u384[:, :], 6), v3(src, 6), bc3(st[:, 0:6], 64), ALU.mult, ["z", "st"], ["u384"])
                dst = qnb[:, :] if which == 0 else kvbs[:, 0:384]
                K.tt("pool", dst, u384[:, :], gains[:, gofs:gofs + 384], ALU.mult, ["u384", "gains"],
                     ["qnb" if which == 0 else "kvbs"])
            for pr in range(3):
                K.tr(PB0b[:, pr * 128:(pr + 1) * 128], qnb[:, pr * 128:(pr + 1) * 128], ident[:, :], ["qnb", "ident"], ["pb0"])
            K.cp("dve", qbTs[:, :, :], v3(PB0b[:, 0:384], 3), ["pb0"], ["qbTs"])
            K.dma("sp", qbT_d[:, :, rows].rearrange("a p t -> p a t"), qbTs[:, :, :], ["qbTs"], [])
            K.cp("act", kvbs[:, 384:768], z[:, 1280:1664], ["z"], ["kvbs"])
            K.dma("sp", kvb_d[rows, :], kvbs[:, :], ["kvbs"], [])
            src = z[:, 1664:2048]
            K.tt("pool", t384[:, :], src, src, ALU.mult, ["z"], ["t384"])
            K.rsum("dve", st[:, 0:1], t384[:, :], ["t384"], ["st"])
            rstd_of(st[:, 0:1], 1, 384)
            K.ts("dve", u384[:, :], src, st[:, 0:1], None, ALU.mult, None, ["z", "st"], ["u384"])
            K.tt("pool", qab[:, :], u384[:, :], gains[:, G_CQA:G_CQA + 384], ALU.mult, ["u384", "gains"], ["qab"])
            for c in range(3):
                K.tr(PB0b[:, c * 128:(c + 1) * 128], qab[:, c * 128:(c + 1) * 128], ident[:, :], ["qab", "ident"], ["pb0"])
            K.cp("dve", qaT[:, :, :], v3(PB0b[:, 0:384], 3), ["pb0"], ["qaT"])
            for c in range(3):
                K.mm(PB[5][:, 0:512], qaT[:, c, :], wq[:, c, 0:512], c == 0, c == 2, ["qaT", "wq"], ["pb5"])
            for c in range(3):
                K.mm(PB[7][:, 256:320], qaT[:, c, :], wq[:, c, 512:576], c == 0, c == 2, ["qaT", "wq"], ["pb7b"])
            K.cp("act", qf[:, 0:512], PB[5][:, 0:512], ["pb5"], ["qf"])
            K.cp("act", qf[:, 512:576], PB[7][:, 256:320], ["pb7b"], ["qf"])
            qf3 = v3(qf[:, :], 6)
            qs3 = v3(qs[:, :], 6)
            K.tt("pool", qs[:, :], qf[:, :], qf[:, :], ALU.mult, ["qf"], ["qs"])
            K.rsum("dve", st[:, 0:6], qs3[:, :, 0:64], ["qs"], ["st"])
            K.rsum("dve", st[:, 8:14], qs3[:, :, 64:96], ["qs"], ["st"])
            rstd_of(st[:, 0:6], 6, 64)
            rstd_of(st[:, 8:14], 6, 32)
            K.tt("dve", qs3[:, :, 0:64], qf3[:, :, 0:64], bc3(st[:, 0:6], 64), ALU.mult, ["qf", "st", "qs"], ["qs"])
            K.tt("dve", qs3[:, :, 64:96], qf3[:, :, 64:96], bc3(st[:, 8:14], 32), ALU.mult, ["qf", "st", "qs"], ["qs"])
            K.tt("pool", qf[:, :], qs[:, :], gains[:, G_CQN:G_CQN + 576], ALU.mult, ["qs", "gains"], ["qf"])
            K.cp("act", qc[:, :, 0:64], qf3[:, :, 0:64], ["qf"], ["qc"])
            rope(qf3[:, :, 64:96], qc[:, :, 64:96], 6, ropet[b], ["qf", ("rope", b)], ["qc"])
            for h in range(6):
                K.tr(PB0b[0:96, h * 128:(h + 1) * 128], qc[:, h, :], ident[:, :], ["qc", "ident"], ["pb0"])
            K.cp("dve", qcTs[:, :, :], v3(PB0b[0:96, 0:768], 6), ["pb0"], ["qcTs"])
            K.dma("sp", qcT_d[:, :, rows].rearrange("h p t -> p h t"), qcTs[:, :, :], ["qcTs"], [])
            src = z[:, 2048:2304]
            K.tt("pool", t384[:, 0:256], src, src, ALU.mult, ["z"], ["t384"])
            K.rsum("dve", st[:, 0:1], t384[:, 0:256], ["t384"], ["st"])
            rstd_of(st[:, 0:1], 1, 256)
            K.ts("dve", u384[:, 0:256], src, st[:, 0:1], None, ALU.mult, None, ["z", "st"], ["u384"])
            K.tt("pool", kvab[:, :], u384[:, 0:256], gains[:, G_CKVA:G_CKVA + 256], ALU.mult, ["u384", "gains"], ["kvab"])
            for c in range(2):
                K.tr(PB0b[:, c * 128:(c + 1) * 128], kvab[:, c * 128:(c + 1) * 128], ident[:, :], ["kvab", "ident"], ["pb0"])
            K.cp("dve", kvaT[:, :, :], v3(PB0b[:, 0:256], 2), ["pb0"], ["kvaT"])
            for c in range(2):
                K.mm(PB[6][:, 0:512], kvaT[:, c, :], wkv[:, c, 0:512], c == 0, c == 1, ["kvaT", "wkv"], ["pb6"])
            for c in range(2):
                K.mm(PB[7][:, 0:256], kvaT[:, c, :], wkv[:, c, 512:768], c == 0, c == 1, ["kvaT", "wkv"], ["pb7a"])
            K.cp("act", kvf[:, 0:512], PB[6][:, 0:512], ["pb6"], ["kvf"])
            K.cp("act", kvf[:, 512:768], PB[7][:, 0:256], ["pb7a"], ["kvf"])
            kvf3 = v3(kvf[:, :], 6)
            K.cp("act", vcs[:, :, 0:64], kvf3[:, :, 64:128], ["kvf"], ["vcs"])
            K.dma("sp", vc_d[rows, :], vcs[:, :, :].rearrange("p h d -> p (h d)"), ["vcs"], [])
            t3 = v3(t384[:, :], 6)
            u3 = v3(u384[:, :], 6)
            K.tt("pool", t3, kvf3[:, :, 0:64], kvf3[:, :, 0:64], ALU.mult, ["kvf"], ["t384"])
            K.rsum("dve", st[:, 0:6], t3, ["t384"], ["st"])
            rstd_of(st[:, 0:6], 6, 64)
            K.tt("dve", u3, kvf3[:, :, 0:64], bc3(st[:, 0:6], 64), ALU.mult, ["kvf", "st"], ["u384"])
            K.tt("pool", kc[:, :, 0:64], u3, v3(gains[:, G_CKN:G_CKN + 384], 6), ALU.mult, ["u384", "gains"], ["kc"])
            src = z[:, 2304:2336]
            K.tt("pool", kr[:, :], src, src, ALU.mult, ["z"], ["kr"])
            K.rsum("dve", st[:, 0:1], kr[:, :], ["kr"], ["st"])
            rstd_of(st[:, 0:1], 1, 32)
            K.ts("dve", kr[:, :], src, st[:, 0:1], None, ALU.mult, None, ["z", "st", "kr"], ["kr"])
            K.tt("pool", kr[:, :], kr[:, :], gains[:, G_CKR:G_CKR + 32], ALU.mult, ["kr", "gains"], ["kr"])
            rope(v3(kr[:, :], 1), v3(krr[:, :], 1), 1, ropet[b], ["kr", ("rope", b)], ["krr"])
            K.cp("dve", kc[:, :, 64:96], krr[:, :].unsqueeze(1).to_broadcast([128, 6, 32]), ["krr"], ["kc"])
            for h in range(6):
                K.tr(PB0b[0:96, h * 128:(h + 1) * 128], kc[:, h, :], ident[:, :], ["kc", "ident"], ["pb0"])
            K.cp("dve", kcTs[:, :, :], v3(PB0b[0:96, 0:768], 6), ["pb0"], ["kcTs"])
            K.dma("sp", kcT_d[:, :, rows].rearrange("h p t -> p h t"), kcTs[:, :, :], ["kcTs"], [])
        S.emit(stack)
    return nc


def _kmaj(w, kc):
    return np.ascontiguousarray(w.reshape(kc, 128, -1).transpose(1, 0, 2))


def _colT(v):
    return np.ascontiguousarray(v.reshape(-1, 128).T)


def _bcrow(v):
    return np.ascontiguousarray(np.broadcast_to(v[None, :], (128, v.shape[0])))


def rope_tables(NT_OWN, rank):
    half = 16
    inv = (np.float32(10000.0) ** (-(np.arange(0, half, 2, dtype=np.float32)) / np.float32(half))).astype(np.float32)
    pos = np.arange(NT_OWN * 128) + rank * NT_OWN * 128
    out = np.zeros(((NT_OWN + 2) * 128, 32), np.float32)
    ar = (pos // GRID_W).astype(np.float32)[:, None] * inv[None, :]
    ac = (pos % GRID_W).astype(np.float32)[:, None] * inv[None, :]
    n = NT_OWN * 128
    out[:n, 0:8] = np.cos(ar)
    out[:n, 8:16] = np.sin(ar)
    out[:n, 16:24] = np.cos(ac)
    out[:n, 24:32] = np.sin(ac)
    out[n:, 0:8] = 1.0
    out[n:, 16:24] = 1.0
    return out


def prep_A(P, i, x_cur, xc_cur, NT_OWN, ranks_per_batch=4):
    B = x_cur.shape[0]
    gains = np.concatenate([
        P["a_v_norm"][i], np.tile(P["b_q_norm"][i], 6), np.tile(P["b_k_norm"][i], 6), P["c_q_a_norm"][i],
        P["c_kv_a_norm"][i], np.tile(P["c_q_norm"][i], 6), np.tile(P["c_k_norm"][i][:64], 6), P["c_k_norm"][i][64:]])
    shared = {
        "w_ada": _kmaj(P["w_ada"][i], 8),
        "b_adaT": _colT(P["b_ada"][i]),
        "norm_mixT": _colT(P["norm_mix"][i]),
        "w_in": _kmaj(P["w_in"][i], 8),
        "w_sT": np.ascontiguousarray(P["a_w_s"][i].transpose(2, 0, 1)),
        "b_s": np.ascontiguousarray(P["a_b_s"][i].T),
        "gains": _bcrow(gains.astype(np.float32)),
        "wq": _kmaj(P["c_w_q_up"][i], 3),
        "wkv": _kmaj(P["c_w_kv_up"][i], 2),
        "ident": np.eye(128, dtype=np.float32),
    }
    maps = []
    n = NT_OWN * 128
    for b in range(B):
        cT = np.stack([_colT(P["c"][b]), _colT(P["c_ctx"])], axis=2).reshape(128, 16)
        for r in range(ranks_per_batch):
            m = dict(shared)
            m["x"] = np.ascontiguousarray(np.concatenate([x_cur[b, r * n:(r + 1) * n], xc_cur[b]], axis=0))
            m["cT"] = np.ascontiguousarray(cT)
            m["rope"] = rope_tables(NT_OWN, r)
            maps.append(m)
    return maps


def _specials(NT_OWN):
    return sorted(set(t for t in (0, 1, NT_OWN - 2, NT_OWN - 1) if 0 <= t < NT_OWN))


def build_B(NT_OWN, TB=4, NE=64):
    NT = NT_OWN + 2
    T = NT * 128
    NW = NT_OWN + 8
    NKT = 4 * NT_OWN + 2
    TK = NKT * 128
    QB = min(4, NT_OWN)
    CH = 16
    specials = _specials(NT_OWN)
    NCLS = 1 + len(specials)
    BIG = 1.0e30
    nc = bass.Bass("TRN2", target_bir_lowering=False)
    stack = contextlib.ExitStack()
    with stack:
        K = KB(nc, stack)
        S = K.S
        x_d = K.dram("x", [T, D], F32, "ExternalInput")
        oa_d = K.dram("oa", [T, 256], BF16, "ExternalInput")
        qbT_d = K.dram("qbT", [3, 128, T], BF16, "ExternalInput")
        qcT_d = K.dram("qcT", [6, 96, T], BF16, "ExternalInput")
        kvbw_d = K.dram("kvbw", [NW * 128, 768], BF16, "ExternalInput")
        kcTa_d = K.dram("kcT_all", [6, 96, TK], BF16, "ExternalInput")
        vca_d = K.dram("vc_all", [TK, 390], BF16, "ExternalInput")
        modT_d = K.dram("modT", [128, 96], F32, "ExternalInput")
        btab_d = K.dram("btab", [NCLS, 128, 6, 1024], BF16, "ExternalInput")
        wout_d = K.dram("w_out", [128, 8, D], F32, "ExternalInput")
        nffn_d = K.dram("norm_ffnT", [128, 8], F32, "ExternalInput")
        wgr_d = K.dram("wgr", [128, 8, 72], F32, "ExternalInput")
        w1_d = K.dram("w1", [NE, 128, 8, 512], F32, "ExternalInput")
        w3_d = K.dram("w3", [NE, 128, 8, 512], F32, "ExternalInput")
        w2_d = K.dram("w2", [NE, 128, 4, D], F32, "ExternalInput")
        ident_d = K.dram("ident", [128, 128], F32, "ExternalInput")
        mix_d = K.dram("mixd", [T, D], BF16, "Internal")
        xo_d = K.dram("xo", [T, D], F32, "ExternalOutput")

        AR = 45056
        arena = K.sb("arena", [128, AR], BF16)
        identb = K.sb("ident_b", [128, 128], BF16)
        identf = K.sb("identf", [128, 128], F32)
        ones = K.sb("ones", [128, 128], F32)
        modT = K.sb("modTs", [128, 96], F32)
        nffn = K.sb("nffn", [128, 8], F32)
        A2 = K.sb("A2", [128, 16], F32)
        G = [K.sb(f"G{i}", [128, D], F32) for i in range(4)]
        gbc = K.sb("gbc", [128, 128], F32)
        wgr = K.sb("wgrs", [128, 8, 72], F32)
        kvt = [K.sb(f"kvt{i}", [128, 768], BF16) for i in range(2)]
        qbt = [K.sb(f"qbt{i}", [128, 3, 128], BF16) for i in range(2)]
        PT = [K.sb(f"PT{i}", [128, 512], BF16) for i in range(3)]
        st = K.sb("st", [128, 64], F32)
        ob = K.sb("ob", [128, 384], BF16)
        oc = K.sb("oc", [128, 4, 64], BF16)
        ocT = K.sb("ocT", [65, 512], F32)
        qT = [K.sb(f"qT{i}", [96, 512], BF16) for i in range(2)]
        xt = [K.sb(f"xt{i}", [128, D], F32) for i in range(2)]
        mixrow = K.sb("mixrow", [128, D], BF16)
        mixT = K.sb("mixT", [128, 8, 128], BF16)
        tmpf = K.sb("tmpf", [128, D], F32)
        h2n = K.sb("h2n", [128, D], F32)
        h2Tf = K.sb("h2Tf", [128, 8, 128], F32)
        x1 = K.sb("x1", [128, TB, D], F32)
        yacc = K.sb("yacc", [128, TB, D], F32)
        gate = K.sb("gate", [128, TB, 64], F32)
        lg = K.sb("lg", [128, 72], F32)
        r64 = [K.sb(f"r64_{i}", [128, 64], F32) for i in range(4)]
        sil = [K.sb(f"sil{i}", [128, 512], F32) for i in range(2)]
        hT = K.sb("hTe", [128, 4, 512], BF16)
        PB = [K.ps(f"pb{i}", [128, 512], F32) for i in range(8)]
        PB0b = PB[0].bitcast(BF16)

        K.dma("sp", identf[:, :], ident_d[:, :], [], ["identf"])
        K.cp("dve", identb[:, :], identf[:, :], ["identf"], ["identb"])
        K.memset("dve", ones[:, :], 1.0, [], ["ones"])
        K.dma("sp", modT[:, :], modT_d[:, :], [], ["modT"])
        K.dma("sp", nffn[:, :], nffn_d[:, :], [], ["nffn"])
        K.dma("sp", wgr[:, :, :], wgr_d[:, :, :], [], ["wgr"])
        K.stt("dve", v3(A2[:, :], 8), v3(modT[:, 64:80], 8), 1.0, bc3(nffn[:, :], 2), ALU.add, ALU.mult,
              ["modT", "nffn"], ["A2"])
        for gi, (j0, wh) in enumerate(((16, 0), (16, 1), (40, 0), (40, 1))):
            for c in range(8):
                col = 2 * (j0 + c) + wh
                K.ts("dve", gbc[:, :], ones[:, :], modT[:, col:col + 1], None, ALU.mult, None, ["ones", "modT"], ["gbc"])
                K.mm(PB[7][:, (c % 4) * 128:(c % 4 + 1) * 128], gbc[:, :], identf[:, :], True, True, ["gbc", "identf"], ["pb7"])
                K.cp("act", G[gi][:, c * 128:(c + 1) * 128], PB[7][:, (c % 4) * 128:(c % 4 + 1) * 128], ["pb7"], [("G", gi)])

        o1 = 3 * NW * 128
        o2 = o1 + NW * 390
        KbT = arena[:, 0:o1].rearrange("p (a t) -> p a t", a=3)
        Vb = arena[:, o1:o2].rearrange("p (s h d) -> p s h d", s=NW, h=6)
        bt0 = arena[:, o2:o2 + 6144].rearrange("p (h e) -> p h e", h=6)
        btS = arena[:, o2 + 6144:o2 + 12288].rearrange("p (h e) -> p h e", h=6)
        assert o2 + 12288 <= AR
        K.memset("pool", arena[:, o1:o2], 1.0, [], ["Vb"])
        K.dma("sp", bt0, btab_d[0], [], ["bt0"])
        for s in range(NW):
            b = s % 2
            K.dma("sp", kvt[b][:, :], kvbw_d[s * 128:(s + 1) * 128, :], [], [("kvt", b)])
            for pr in range(3):
                K.tr(PB0b[:, pr * 128:(pr + 1) * 128], kvt[b][:, pr * 128:(pr + 1) * 128], identb[:, :], [("kvt", b), "identb"], ["pb0"])
            K.cp("dve", KbT[:, :, s * 128:(s + 1) * 128], v3(PB0b[:, 0:384], 3), ["pb0"], ["KbT"])
            K.cp("pool", Vb[:, s, :, 0:64], v3(kvt[b][:, 384:768], 6), [("kvt", b), "Vb"], ["Vb"])
        gcount = 0
        for t in range(NT):
            b = t % 2
            own = t < NT_OWN
            rows = slice(t * 128, (t + 1) * 128)
            K.dma("sp", qbt[b][:, :, :], qbT_d[:, :, rows].rearrange("a p t -> p a t"), [], [("qbt", b)])
            bt, btk = bt0, "bt0"
            if own and t in specials:
                K.dma("sp", btS, btab_d[1 + specials.index(t)], [], ["btS"])
                bt, btk = btS, "btS"
            if own:
                kts = [(t + j + 3, j) for j in range(-3, 4)] + [(NW - 2, None), (NW - 1, None)]
            else:
                kts = [(NW - 2, None), (NW - 1, None)]
            groups = [kts[i:i + 3] for i in range(0, len(kts), 3)]
            ob_bank = 4 + t % 2
            OB = PB[ob_bank]
            for h in range(6):
                pr, pb = h // 2, (h % 2) * 64
                nk = 0
                for grp in groups:
                    bank = 1 + gcount % 3
                    pi = gcount % 3
                    gcount += 1
                    for i, (s, j) in enumerate(grp):
                        K.mm(PB[bank][:, i * 128:(i + 1) * 128], KbT[pb:pb + 64, pr, s * 128:(s + 1) * 128],
                             qbt[b][pb:pb + 64, pr, :], True, j is None, ["KbT", ("qbt", b)], [f"pb{bank}"])
                        if j is not None:
                            e0 = (7 - 2 * j) * 64
                            K.mm(PB[bank][:, i * 128:(i + 1) * 128], identb[:, :], bt[:, h, e0:e0 + 128], False, True,
                                 ["identb", btk], [f"pb{bank}"])
                    n = len(grp) * 128
                    K.act(PT[pi][:, 0:n], PB[bank][:, 0:n], AF.Exp, [f"pb{bank}"], [("PT", pi)])
                    for i, (s, j) in enumerate(grp):
                        K.mm(OB[:, h * 65:(h + 1) * 65], PT[pi][:, i * 128:(i + 1) * 128], Vb[:, s, h, :],
                             nk == 0, nk == len(kts) - 1, [("PT", pi), "Vb"], [f"pb{ob_bank}"])
                        nk += 1
            O3 = v3(OB[:, 0:390], 6)
            K.S.op("dve", lambda e, O3=O3: e.reciprocal(st[:, 0:6], O3[:, :, 64]), [f"pb{ob_bank}"], ["st"])
            K.tt("dve", v3(ob[:, :], 6), O3[:, :, 0:64], bc3(st[:, 0:6], 64), ALU.mult, [f"pb{ob_bank}", "st"], ["ob"])
            K.dma("sp", mix_d[rows, 256:640], ob[:, :], ["ob"], [("mixd", t)])

        S.barrier()
        Kc = [arena[:, i * 2048:(i + 1) * 2048] for i in range(3)]
        Vc = [arena[:, 6144 + i * 1040:6144 + (i + 1) * 1040].rearrange("p (k d) -> p k d", d=65) for i in range(3)]
        qblocks = [(t0, QB, list(range(NKT))) for t0 in range(0, NT_OWN, QB)] + [(NT_OWN, 2, [NKT - 2, NKT - 1])]
        sc = 0
        cc = 0
        qh = 0
        SCALE_C = 96.0 ** -0.5
        for (t0, ntl, klist) in qblocks:
            nq = ntl * 128
            for h in range(6):
                b2 = qh % 2
                oc_bank = 4 + qh % 2
                qh += 1
                OC = PB[oc_bank]
                K.dma("sp", qT[b2][:, 0:nq], qcT_d[h, :, t0 * 128:t0 * 128 + nq], [], [("qT", b2)])
                chunks = [klist[i:i + CH] for i in range(0, len(klist), CH)]
                nk = 0
                for chk in chunks:
                    cb = cc % 3
                    cc += 1
                    k0, n_k = chk[0], len(chk)
                    K.dma("sp", Kc[cb][0:96, 0:n_k * 128], kcTa_d[h, :, k0 * 128:(k0 + n_k) * 128], [], [("Kc", cb)])
                    K.dma("act", Vc[cb][:, 0:n_k, :],
                          vca_d[k0 * 128:(k0 + n_k) * 128, h * 65:(h + 1) * 65].rearrange("(k p) d -> p k d", p=128),
                          [], [("Vc", cb)])
                    for kt in range(n_k):
                        bank = 1 + sc % 3
                        pi = sc % 3
                        sc += 1
                        K.mm(PB[bank][:, 0:nq], Kc[cb][0:96, kt * 128:(kt + 1) * 128], qT[b2][:, 0:nq], True, True,
                             [("Kc", cb), ("qT", b2)], [f"pb{bank}"])
                        K.act(PT[pi][:, 0:nq], PB[bank][:, 0:nq], AF.Exp, [f"pb{bank}"], [("PT", pi)], scale=SCALE_C)
                        K.mm(OC[0:65, 0:nq], Vc[cb][:, kt, :], PT[pi][:, 0:nq], nk == 0, nk == len(klist) - 1,
                             [("PT", pi), ("Vc", cb)], [f"pb{oc_bank}"])
                        nk += 1
                K.cp("dve", ocT[:, 0:nq], OC[0:65, 0:nq], [f"pb{oc_bank}"], ["ocT"])
                for qi in range(ntl):
                    K.tr(PB[0][:, qi * 65:(qi + 1) * 65], ocT[:, qi * 128:(qi + 1) * 128], identf[0:65, 0:65], ["ocT", "identf"], ["pb0"])
                O3 = v3(PB[0][:, 0:ntl * 65], ntl)
                K.S.op("dve", lambda e, O3=O3, ntl=ntl: e.reciprocal(st[:, 0:ntl], O3[:, :, 64]), ["pb0"], ["st"])
                K.tt("dve", oc[:, 0:ntl, :], O3[:, :, 0:64], bc3(st[:, 0:ntl], 64), ALU.mult, ["pb0", "st"], ["oc"])
                K.dma("sp", mix_d[t0 * 128:t0 * 128 + nq, 640 + h * 64:704 + h * 64].rearrange("(q p) d -> p q d", p=128),
                      oc[:, 0:ntl, :], ["oc"], [("mixd", t0 + i) for i in range(ntl)])

        S.barrier()
        WSZ = 12288
        Wb = [arena[:, i * WSZ:(i + 1) * WSZ] for i in range(2)]
        h2T = arena[:, 2 * WSZ:2 * WSZ + TB * 1024].rearrange("p (c t) -> p c t", c=8)
        wout = arena[:, 2 * WSZ + TB * 1024:2 * WSZ + TB * 1024 + 8192].rearrange("p (c n) -> p c n", c=8)
        assert 2 * WSZ + TB * 1024 + 8192 <= AR
        K.dma("pool", wout, wout_d[:, :, :], [], ["wout"])

        def rstd_of(ss, dim):
            K.ts("dve", ss, ss, 1.0 / dim, EPS, ALU.mult, ALU.add, ["st"], ["st"])
            K.act(ss, ss, AF.Sqrt, ["st"], ["st"])
            K.S.op("dve", lambda e, a=ss: e.reciprocal(a, a), ["st"], ["st"])

        tiles_all = list(range(NT))
        blocks = [tiles_all[i:i + TB] for i in range(0, NT, TB)]
        wcount = 0
        for blk in blocks:
            for ti, t in enumerate(blk):
                b = t % 2
                wh = 0 if t < NT_OWN else 1
                rows = slice(t * 128, (t + 1) * 128)
                K.dma("sp", xt[b][:, :], x_d[rows, :], [], [("xt", b)])
                K.dma("sp", mixrow[:, 256:1024], mix_d[rows, 256:1024], [("mixd", t)], ["mixrow"])
                K.dma("sp", mixrow[:, 0:256], oa_d[rows, :], [], ["mixrow"])
                for c in range(8):
                    K.tr(PB0b[:, c * 128:(c + 1) * 128], mixrow[:, c * 128:(c + 1) * 128], identb[:, :], ["mixrow", "identb"], ["pb0"])
                K.cp("dve", mixT[:, :, :], v3(PB0b[:, :], 8), ["pb0"], ["mixT"])
                for half in range(2):
                    pb = 1 + half
                    for c in range(8):
                        K.mm(PB[pb][:, :], mixT[:, c, :], wout[:, c, half * 512:(half + 1) * 512], c == 0, c == 7,
                             ["mixT", "wout"], [f"pb{pb}"])
                    hs = slice(half * 512, (half + 1) * 512)
                    K.tt("dve", tmpf[:, hs], PB[pb][:, :], G[wh][:, hs], ALU.mult, [f"pb{pb}", ("G", wh)], ["tmpf"])
                    K.tt("pool", x1[:, ti, hs], tmpf[:, hs], xt[b][:, hs], ALU.add, ["tmpf", ("xt", b)], [("x1", ti)])
                K.tt("pool", tmpf[:, :], x1[:, ti, :], x1[:, ti, :], ALU.mult, [("x1", ti), "tmpf"], ["tmpf"])
                K.rsum("dve", st[:, 0:1], tmpf[:, :], ["tmpf"], ["st"])
                rstd_of(st[:, 0:1], D)
                K.act(h2n[:, :], x1[:, ti, :], AF.Copy, [("x1", ti), "st"], ["h2n"], scale=st[:, 0:1])
                for c in range(8):
                    pb = 6 + c // 4
                    K.tr(PB[pb][:, (c % 4) * 128:(c % 4 + 1) * 128], h2n[:, c * 128:(c + 1) * 128], identf[:, :], ["h2n", "identf"], [f"pb{pb}"])
                for c in range(8):
                    pb = 6 + c // 4
                    K.ts("dve", h2Tf[:, c, :], PB[pb][:, (c % 4) * 128:(c % 4 + 1) * 128], A2[:, 2 * c + wh:2 * c + wh + 1],
                         modT[:, 2 * (24 + c) + wh:2 * (24 + c) + wh + 1], ALU.mult, ALU.add, [f"pb{pb}", "A2", "modT"], ["h2Tf"])
                K.cp("pool", h2T[:, :, ti * 128:(ti + 1) * 128], h2Tf[:, :, :], ["h2Tf"], [("h2T", ti)])
                for c in range(8):
                    K.mm(PB[3][:, 0:72], h2Tf[:, c, :], wgr[:, c, :], c == 0, c == 7, ["h2Tf", "wgr"], ["pb3"])
                K.cp("act", lg[:, :], PB[3][:, 0:72], ["pb3"], ["lg"])
                gl = lg[:, 0:8]
                rl3 = v3(lg[:, 8:72], 8)
                s_gmax, s_ngmax, s_gsum, s_m1, s_m2, s_d, s_e2, s_wa, s_wb = [st[:, 16 + i:17 + i] for i in range(9)]
                goh, gex, pen = st[:, 32:40], st[:, 40:48], st[:, 48:56]
                RK = ["lg", "st", "r64"]
                K.S.op("dve", lambda e, a=s_gmax, g=gl: e.reduce_max(a, g, AX.X), ["lg"], ["st"])
                K.ts("dve", goh, gl, s_gmax, None, ALU.is_equal, None, RK, ["st"])
                K.ts("dve", s_ngmax, s_gmax, -1.0, None, ALU.mult, None, RK, ["st"])
                K.act(gex, gl, AF.Exp, RK, ["st"], bias=s_ngmax)
                K.rsum("dve", s_gsum, gex, RK, ["st"])
                K.S.op("dve", lambda e, a=s_gsum: e.reciprocal(a, a), RK, ["st"])
                K.ts("dve", pen, goh, BIG, -BIG, ALU.mult, ALU.add, RK, ["st"])
                rm, oh1, rm2, oh2 = [r[:, :] for r in r64]
                K.tt("dve", v3(rm, 8), rl3, bc3(pen, 8), ALU.add, RK, ["r64"])
                K.S.op("dve", lambda e, a=s_m1, g=rm: e.reduce_max(a, g, AX.X), RK, ["st"])
                K.ts("dve", oh1, rm, s_m1, None, ALU.is_equal, None, RK, ["r64"])
                K.stt("dve", rm2, oh1, -BIG, rm, ALU.mult, ALU.add, RK, ["r64"])
                K.S.op("dve", lambda e, a=s_m2, g=rm2: e.reduce_max(a, g, AX.X), RK, ["st"])
                K.ts("dve", oh2, rm2, s_m2, None, ALU.is_equal, None, RK, ["r64"])
                K.tt("dve", s_d, s_m2, s_m1, ALU.subtract, RK, ["st"])
                K.act(s_e2, s_d, AF.Exp, RK, ["st"])
                K.ts("dve", s_wa, s_e2, 1.0, None, ALU.add, None, RK, ["st"])
                K.S.op("dve", lambda e, a=s_wa: e.reciprocal(a, a), RK, ["st"])
                K.tt("dve", s_wa, s_wa, s_gsum, ALU.mult, RK, ["st"])
                K.tt("dve", s_wb, s_wa, s_e2, ALU.mult, RK, ["st"])
                K.ts("dve", gate[:, ti, :], oh1, s_wa, None, ALU.mult, None, RK, [("gate", ti)])
                K.stt("dve", gate[:, ti, :], oh2, s_wb, gate[:, ti, :], ALU.mult, ALU.add, RK + [("gate", ti)], [("gate", ti)])
                K.memset("pool", yacc[:, ti, :], 0.0, [], [("yacc", ti)])
            nblk = len(blk)
            subs = [list(range(i, min(i + 4, nblk))) for i in range(0, nblk, 4)]
            for e_ in range(NE):
                wb = wcount % 2
                wcount += 1
                W = Wb[wb]
                w1e = W[:, 0:4096].rearrange("p (c n) -> p c n", c=8)
                w3e = W[:, 4096:8192].rearrange("p (c n) -> p c n", c=8)
                w2e = W[:, 8192:12288].rearrange("p (c n) -> p c n", c=4)
                K.dma("pool", w1e, w1_d[e_], [], [("W", wb)])
                K.dma("pool", w3e, w3_d[e_], [], [("W", wb)])
                K.dma("pool", w2e, w2_d[e_], [], [("W", wb)])
                for sub in subs:
                    ns = len(sub) * 128
                    c0 = sub[0] * 128
                    hk = [("h2T", ti) for ti in sub]
                    for m in range(4):
                        p1, p3 = 1 + m % 2, 4 + m % 2
                        for c in range(8):
                            K.mm(PB[p1][:, 0:ns], w1e[:, c, m * 128:(m + 1) * 128], h2T[:, c, c0:c0 + ns], c == 0, c == 7,
                                 [("W", wb)] + hk, [f"pb{p1}"])
                        for c in range(8):
                            K.mm(PB[p3][:, 0:ns], w3e[:, c, m * 128:(m + 1) * 128], h2T[:, c, c0:c0 + ns], c == 0, c == 7,
                                 [("W", wb)] + hk, [f"pb{p3}"])
                        K.act(sil[m % 2][:, 0:ns], PB[p1][:, 0:ns], AF.Silu, [f"pb{p1}"], [("sil", m % 2)])
                        K.tt("dve", hT[:, m, 0:ns], PB[p3][:, 0:ns], sil[m % 2][:, 0:ns], ALU.mult, [f"pb{p3}", ("sil", m % 2)], ["hTe"])
                    for ii, ti in enumerate(sub):
                        for half in range(2):
                            py = 6 + half
                            for kc in range(4):
                                K.mm(PB[py][:, :], hT[:, kc, ii * 128:(ii + 1) * 128], w2e[:, kc, half * 512:(half + 1) * 512],
                                     kc == 0, kc == 3, ["hTe", ("W", wb)], [f"pb{py}"])
                            hs = slice(half * 512, (half + 1) * 512)
                            K.stt("dve", yacc[:, ti, hs], PB[py][:, :], gate[:, ti, e_:e_ + 1], yacc[:, ti, hs], ALU.mult, ALU.add,
                                  [f"pb{py}", ("gate", ti), ("yacc", ti)], [("yacc", ti)])
            for ti, t in enumerate(blk):
                wh = 0 if t < NT_OWN else 1
                rows = slice(t * 128, (t + 1) * 128)
                K.tt("pool", yacc[:, ti, :], yacc[:, ti, :], G[2 + wh][:, :], ALU.mult, [("yacc", ti), ("G", 2 + wh)], [("yacc", ti)])
                K.tt("pool", yacc[:, ti, :], yacc[:, ti, :], x1[:, ti, :], ALU.add, [("yacc", ti), ("x1", ti)], [("yacc", ti)])
                K.dma("sp", xo_d[rows, :], yacc[:, ti, :], [("yacc", ti)], [])
        S.emit(stack)
    return nc


def build_btab(rpb, NT_OWN, rank):
    rows_total = 8 * NT_OWN
    specials = _specials(NT_OWN)
    gts = [rows_total // 4] + [rank * NT_OWN + t for t in specials]
    qc = np.arange(64)
    kc = np.arange(64)
    c0 = np.clip(qc - 8, 0, 48)
    colok = (kc[:, None] >= c0[None, :]) & (kc[:, None] < c0[None, :] + 16)
    dc = np.clip(kc[:, None] - qc[None, :], -15, 15) + 15
    tab = np.full((len(gts), 2, 64, 6, 16, 64), NEG, np.float32)
    for ci, gt in enumerate(gts):
        for e in range(16):
            b = (e + 1) % 2
            j = (7 + b - e) // 2
            qrow = 2 * gt + b
            r0 = min(max(qrow - 4, 0), rows_total - 8)
            for a in range(2):
                krow = 2 * (gt + j) + a
                if krow < 0 or krow >= rows_total or krow < r0 or krow >= r0 + 8:
                    continue
                dr = krow - qrow + 7
                vals = rpb[:, dr, :][:, dc]
                vals = np.where(colok[None], vals, np.float32(NEG))
                tab[ci, a, :, :, e, :] = vals.transpose(1, 0, 2)
    return tab.reshape(len(gts), 128, 6, 1024).astype(NPBF)


def prep_B(P, i, x_cur, xc_cur, NT_OWN, outsA, ranks_per_batch=4, NE=64):
    B = x_cur.shape[0]
    n = NT_OWN * 128
    NTB = ranks_per_batch * NT_OWN
    shared = {
        "w_out": _kmaj(P["w_out"][i], 8),
        "norm_ffnT": _colT(P["norm_ffn"][i]),
        "wgr": _kmaj(np.concatenate([P["moe_w_group"][i], P["moe_w_router"][i]], axis=1), 8),
        "w1": np.ascontiguousarray(P["moe_w1"][i][:NE].reshape(NE, 8, 128, 512).transpose(0, 2, 1, 3)),
        "w3": np.ascontiguousarray(P["moe_w3"][i][:NE].reshape(NE, 8, 128, 512).transpose(0, 2, 1, 3)),
        "w2": np.ascontiguousarray(P["moe_w2"][i][:NE].reshape(NE, 4, 128, 1024).transpose(0, 2, 1, 3)),
        "ident": np.eye(128, dtype=np.float32),
    }
    maps = []
    for b in range(B):
        cores = [outsA[b * ranks_per_batch + r] for r in range(ranks_per_batch)]
        kv_tiles = [c["kvb"][:n].reshape(NT_OWN, 128, 768) for c in cores]
        kv_all = np.concatenate(kv_tiles, axis=0)
        for r in range(ranks_per_batch):
            me = cores[r]
            m = dict(shared)
            m["x"] = np.ascontiguousarray(np.concatenate([x_cur[b, r * n:(r + 1) * n], xc_cur[b]], axis=0))
            m["oa"] = me["oa"]
            m["qbT"] = me["qbT"]
            m["qcT"] = me["qcT"]
            m["modT"] = me["modT"]
            win = np.zeros((NT_OWN + 8, 128, 768), NPBF)
            for s in range(NT_OWN + 6):
                g = r * NT_OWN - 3 + s
                if 0 <= g < NTB:
                    win[s] = kv_all[g]
            win[NT_OWN + 6:] = me["kvb"][n:].reshape(2, 128, 768)
            m["kvbw"] = win.reshape(-1, 768)
            m["kcT_all"] = np.ascontiguousarray(np.concatenate([c["kcT"][:, :, :n] for c in cores] + [me["kcT"][:, :, n:]], axis=2))
            m["vc_all"] = np.ascontiguousarray(np.concatenate([c["vc"][:n] for c in cores] + [me["vc"][n:]], axis=0))
            m["btab"] = build_btab(P["b_rpb"][i], NT_OWN, r)
            maps.append(m)
    return maps


_PROG = {}


def _device_runner(name, builder, in_maps):
    if name not in _PROG:
        _PROG[name] = builder()
    res = run_bass_kernel_spmd(_PROG[name], in_maps, core_ids=list(range(len(in_maps))))
    return res.results


def run_model(P, NT_OWN, runner, depth=2, ranks_per_batch=4, TB=4):
    x_cur = np.asarray(P["x"], np.float32)
    xc_cur = np.asarray(P["ctx"], np.float32)
    B = x_cur.shape[0]
    n = NT_OWN * 128
    for i in range(depth):
        mapsA = prep_A(P, i, x_cur, xc_cur, NT_OWN, ranks_per_batch)
        outsA = runner("A", lambda: build_A(NT_OWN), mapsA)
        mapsB = prep_B(P, i, x_cur, xc_cur, NT_OWN, outsA, ranks_per_batch)
        del mapsA
        outsB = runner("B", lambda: build_B(NT_OWN, TB), mapsB)
        del mapsB
        x_new = np.empty_like(x_cur)
        xc_new = np.empty_like(xc_cur)
        for b in range(B):
            for r in range(ranks_per_batch):
                xo = outsB[b * ranks_per_batch + r]["xo"]
                x_new[b, r * n:(r + 1) * n] = xo[:n]
                if r == 0:
                    xc_new[b] = xo[n:]
        x_cur, xc_cur = x_new, xc_new
    return x_cur


def kernel(**inputs):
    P = {k: np.asarray(v) for k, v in inputs.items()}
    return run_model(P, 32, _device_runner).astype(np.float32)
```

```python
import contextlib
import numpy as np
import ml_dtypes
import concourse.bass as bass
import concourse.mybir as mybir
from concourse.bass_utils import run_bass_kernel_spmd

F32 = mybir.dt.float32
BF16 = mybir.dt.bfloat16
I32 = mybir.dt.int32
AF = mybir.ActivationFunctionType
ALU = mybir.AluOpType
AX = mybir.AxisListType
NPBF = ml_dtypes.bfloat16

D = 1024
GRID_W = 64
CTX = 256
EPS = 1e-6
INW = 2336
NEG = -30000.0


class _Op:
    __slots__ = ("eng", "fn", "deps", "needed", "semkey", "val", "dma", "idx")

    def __init__(self, eng, fn, dma):
        self.eng = eng
        self.fn = fn
        self.deps = []
        self.needed = False
        self.semkey = None
        self.val = 0
        self.dma = dma


class Sched:
    ENGS = ("pe", "act", "dve", "pool", "sp")
    NLANES = 6

    def __init__(self, nc, gc):
        self.nc = nc
        self.gc = gc
        self.ops = {e: [] for e in self.ENGS}
        self.bufs = {}
        self.phase = 0
        self.lane_ops = {}
        self.lane_n = {e: 0 for e in self.ENGS}
        self.pending = {e: [] for e in self.ENGS}
        self.last = {e: None for e in self.ENGS}

    def _add(self, eng, fn, r, w, dma):
        op = _Op(eng, fn, dma)
        deps = []
        for k in r:
            st = self.bufs.setdefault(k, [None, []])
            if st[0] is not None:
                deps.append(st[0])
        for k in w:
            st = self.bufs.setdefault(k, [None, []])
            if st[0] is not None:
                deps.append(st[0])
            deps.extend(st[1])
        deps.extend(self.pending[eng])
        self.pending[eng] = []
        if dma:
            lane = self.lane_n[eng] % self.NLANES
            self.lane_n[eng] += 1
            key = ("lane", eng, lane)
            prev = self.lane_ops.get(key)
            if prev is not None:
                deps.append(prev)
            self.lane_ops[key] = op
            op.semkey = key
            op.val = (prev.val if prev is not None else self.gc.lane_vals.get(key, 0)) + 16
            op.needed = True
        else:
            op.semkey = ("eng", eng, self.phase)
        seen = set()
        for d in deps:
            if d is op or id(d) in seen:
                continue
            seen.add(id(d))
            if (not d.dma) and d.eng == eng and eng == "pe":
                continue
            d.needed = True
            op.deps.append(d)
        for k in r:
            self.bufs[k][1].append(op)
        for k in w:
            self.bufs[k] = [op, []]
        self.ops[eng].append(op)
        self.last[eng] = op
        return op

    def op(self, eng, fn, r=(), w=()):
        return self._add(eng, fn, r, w, False)

    def dma(self, eng, out, in_, r=(), w=()):
        return self._add(eng, lambda e: e.dma_start(out=out, in_=in_), r, w, True)

    def dmafn(self, eng, fn, r=(), w=()):
        return self._add(eng, fn, r, w, True)

    def cc(self, fn, r=(), w=()):
        op = self._add("pool", fn, r, w, False)
        op.semkey = ("cc", self.gc.next_uid())
        op.needed = True
        op.val = 1
        op.dma = True
        return op

    def barrier(self):
        lasts = [o for o in self.last.values() if o is not None] + list(self.lane_ops.values())
        for e in self.ENGS:
            self.pending[e] = list(lasts)
        self.bufs = {}
        self.phase += 1

    def emit(self):
        nc = self.nc
        gc = self.gc
        for e in self.ENGS:
            if self.ops[e] and not self.ops[e][-1].dma:
                self.ops[e][-1].needed = True
        cnt = {}
        for e in self.ENGS:
            for op in self.ops[e]:
                if not op.dma and op.needed:
                    cnt[op.semkey] = cnt.get(op.semkey, 0) + 1
                    op.val = cnt[op.semkey]
        sems = {}
        finals = {}
        for e in self.ENGS:
            for op in self.ops[e]:
                if not op.needed:
                    continue
                k = op.semkey
                if k not in sems:
                    if k[0] == "lane":
                        if k not in gc.lane_sems:
                            gc.lane_sems[k] = gc.stack.enter_context(nc.semaphore("l_" + "_".join(str(x) for x in k[1:])))
                        sems[k] = gc.lane_sems[k]
                    else:
                        sems[k] = gc.stack.enter_context(nc.semaphore(f"s{gc.next_uid()}_" + "_".join(str(x) for x in k)))
                finals[k] = max(finals.get(k, 0), op.val)
        for k, v in finals.items():
            if k[0] == "lane":
                gc.lane_vals[k] = v

        def run(engname, e):
            waited = {}
            for op in self.ops[engname]:
                for d in op.deps:
                    if waited.get(d.semkey, 0) >= d.val:
                        continue
                    e.wait_ge(sems[d.semkey], d.val)
                    waited[d.semkey] = d.val
                ins = op.fn(e)
                if op.needed:
                    if op.semkey[0] == "cc":
                        ins.then_inc(sems[op.semkey])
                    else:
                        ins.then_inc(sems[op.semkey], 16 if op.dma else 1)
            for k, v in finals.items():
                if waited.get(k, 0) < v:
                    e.wait_ge(sems[k], v)

        with nc.Block() as block:
            @block.tensor
            def _(e):
                run("pe", e)

            @block.scalar
            def _(e):
                run("act", e)

            @block.vector
            def _(e):
                run("dve", e)

            @block.gpsimd
            def _(e):
                run("pool", e)

            @block.sync
            def _(e):
                run("sp", e)


class GC:
    def __init__(self, stack):
        self.stack = stack
        self.lane_sems = {}
        self.lane_vals = {}
        self.uid = 0

    def next_uid(self):
        self.uid += 1
        return self.uid


class KB:
    def __init__(self, nc, stack, gc):
        self.nc = nc
        self.stack = stack
        self.gc = gc
        self.S = Sched(nc, gc)
        self.tag = f"_u{gc.next_uid()}"

    def sb(self, name, shape, dt):
        return self.stack.enter_context(self.nc.sbuf_tensor(name + self.tag, list(shape), dt))

    def ps(self, name, shape, dt):
        return self.stack.enter_context(self.nc.psum_tensor(name + self.tag, list(shape), dt))

    def dram(self, name, shape, dt, kind):
        return self.nc.dram_tensor(name, list(shape), dt, kind=kind).ap()

    def mm(self, out, lhsT, rhs, start, stop, r, w):
        self.S.op("pe", lambda e: e.matmul(out, lhsT, rhs, start=start, stop=stop), r, w)

    def tr(self, out, in_, ident, r, w):
        self.S.op("pe", lambda e: e.transpose(out, in_, ident), r, w)

    def act(self, out, in_, func, r, w, **kw):
        self.S.op("act", lambda e: e.activation(out, in_, func, **kw), r, w)

    def ts(self, eng, out, in0, s1, s2, op0, op1, r, w):
        if op1 is None:
            self.S.op(eng, lambda e: e.tensor_scalar(out, in0, s1, None, op0), r, w)
        else:
            self.S.op(eng, lambda e: e.tensor_scalar(out, in0, s1, s2, op0, op1), r, w)

    def tt(self, eng, out, in0, in1, op, r, w):
        self.S.op(eng, lambda e: e.tensor_tensor(out, in0, in1, op), r, w)

    def stt(self, eng, out, in0, scalar, in1, op0, op1, r, w):
        self.S.op(eng, lambda e: e.scalar_tensor_tensor(out, in0, scalar, in1, op0, op1), r, w)

    def cp(self, eng, out, in_, r, w):
        if eng == "act":
            self.S.op("act", lambda e: e.copy(out, in_), r, w)
        else:
            self.S.op(eng, lambda e: e.tensor_copy(out, in_), r, w)

    def rsum(self, eng, out, in_, r, w):
        self.S.op(eng, lambda e: e.reduce_sum(out, in_, AX.X), r, w)

    def memset(self, eng, ap, v, r, w):
        self.S.op(eng, lambda e: e.memset(ap, v), r, w)

    def dma(self, eng, out, in_, r, w):
        self.S.dma(eng, out, in_, r, w)


G_AV, G_BQ, G_BK, G_CQA, G_CKVA, G_CQN, G_CKN, G_CKR, G_TOT = 0, 256, 640, 1024, 1408, 1664, 2240, 2624, 2656


def v3(ap, g):
    return ap.rearrange("p (g d) -> p g d", g=g)


def bc3(ap2, d):
    p, g = ap2.shape
    return ap2.unsqueeze(2).to_broadcast([p, g, d])


def emit_A(nc, gc, d, NT_OWN, L):
    NT = NT_OWN + 2
    T = NT * 128
    n = NT_OWN * 128
    stack = contextlib.ExitStack()
    with stack:
        K = KB(nc, stack, gc)
        S = K.S
        x_d = d["x"] if L == 0 else d["xs_0"]
        cT_d, rope_d, ident_d = d["cT"], d["rope"], d["ident"]
        wada_d, bada_d, nmix_d, win_d = d[f"w_ada_{L}"], d[f"b_adaT_{L}"], d[f"norm_mixT_{L}"], d[f"w_in_{L}"]
        wsT_d, bs_d, gains_d, wq_d, wkv_d = d[f"w_sT_{L}"], d[f"b_s_{L}"], d[f"gains_{L}"], d[f"wq_{L}"], d[f"wkv_{L}"]
        oa_d, qbT_d, qcT_d, modT_d = d[f"oa_{L}"], d[f"qbT_{L}"], d[f"qcT_{L}"], d[f"modT_{L}"]
        PT = min(4, NT_OWN)
        HT = min(3, NT_OWN)
        kcT_ctx, vc_ctx = d[f"kcT_ctx_{L}"], d[f"vc_ctx_{L}"]
        kvb_own, kvb_ctx, kvb_lo, kvb_hi = d[f"kvb_own_{L}"], d[f"kvb_ctx_{L}"], d[f"kvb_lo_{L}"], d[f"kvb_hi_{L}"]
        ident = K.sb("ident_b", [128, 128], BF16)
        identf = K.sb("identf", [128, 128], F32)
        cT = K.sb("cTs", [128, 16], F32)
        scT = K.sb("scT", [128, 16], F32)
        bada = K.sb("bada", [128, 48], F32)
        nmix = K.sb("nmix", [128, 8], F32)
        modT = K.sb("modTs", [128, 96], F32)
        A1 = K.sb("A1", [128, 16], F32)
        wst = [K.sb(f"wst{i}", [128, 8, 512], F32) for i in range(2)]
        win = K.sb("win", [128, 8, INW], BF16)
        wsT = K.sb("wsT", [128, 4, 128], BF16)
        bs = K.sb("bs", [128, 4], F32)
        gains = K.sb("gainss", [128, G_TOT], F32)
        wq = K.sb("wqs", [128, 3, 576], BF16)
        wkv = K.sb("wkvs", [128, 2, 768], BF16)
        xt = [K.sb(f"xt{i}", [128, D], F32) for i in range(2)]
        ropet = [K.sb(f"ropet{i}", [128, 32], F32) for i in range(2)]
        sqj = K.sb("sqj", [128, D], F32)
        st = K.sb("st", [128, 64], F32)
        xn = K.sb("xn", [128, D], BF16)
        hT = K.sb("hT", [128, 8, 128], BF16)
        z = K.sb("z", [128, INW], F32)
        g1 = K.sb("g1", [128, 512], F32)
        g2 = K.sb("g2", [128, 512], F32)
        gg = K.sb("gg", [128, 512], F32)
        vnb = K.sb("vnb", [128, 256], BF16)
        oa = K.sb("oas", [128, 256], BF16)
        t384 = K.sb("t384", [128, 384], F32)
        u384 = K.sb("u384", [128, 384], F32)
        qnb = K.sb("qnb", [128, 384], BF16)
        qbTs = K.sb("qbTs", [128, 3, 128], BF16)
        kvbs = K.sb("kvbs", [128, 768], BF16)
        qab = K.sb("qab", [128, 384], BF16)
        qaT = K.sb("qaT", [128, 3, 128], BF16)
        qf = K.sb("qf", [128, 576], F32)
        qs = K.sb("qs", [128, 576], F32)
        qc = K.sb("qc", [128, 6, 96], BF16)
        rt = [K.sb(f"rt{i}", [128, 48], F32) for i in range(4)]
        qcTs = K.sb("qcTs", [96, 6, 128], BF16)
        kvab = K.sb("kvab", [128, 256], BF16)
        kvaT = K.sb("kvaT", [128, 2, 128], BF16)
        kvf = K.sb("kvf", [128, 768], F32)
        kc = K.sb("kc", [128, 6, 96], BF16)
        kr = K.sb("kr", [128, 32], F32)
        krr = K.sb("krr", [128, 32], F32)
        vcs = K.sb("vcs", [128, 6, 65], BF16)
        kcTs = K.sb("kcTs", [96, 6, 128], BF16)
        PB = [K.ps(f"pb{i}", [128, 512], F32) for i in range(8)]
        PB0b = PB[0].bitcast(BF16)

        K.dma("sp", identf[:, :], ident_d[:, :], [], ["identf"])
        K.cp("dve", ident[:, :], identf[:, :], ["identf"], ["ident"])
        K.dma("sp", cT[:, :], cT_d[:, :], [], ["cT"])
        K.dma("sp", bada[:, :], bada_d[:, :], [], ["bada"])
        K.dma("sp", nmix[:, :], nmix_d[:, :], [], ["nmix"])
        K.dma("sp", bs[:, :], bs_d[:, :], [], ["bs"])
        K.dma("sp", gains[:, :], gains_d[:, :], [], ["gains"])
        K.dma("pool", win[:, :, :], win_d[:, :, :], [], ["win"])
        K.dma("pool", wsT[:, :, :], wsT_d[:, :, :], [], ["wsT"])
        K.dma("pool", wq[:, :, :], wq_d[:, :, :], [], ["wq"])
        K.dma("pool", wkv[:, :, :], wkv_d[:, :, :], [], ["wkv"])
        K.memset("pool", vcs[:, :, :], 1.0, [], ["vcs"])
        K.act(scT[:, :], cT[:, :], AF.Silu, ["cT"], ["scT"])
        for grp in range(12):
            b = grp % 2
            K.dma("sp", wst[b][:, :, :], wada_d[:, :, grp * 512:(grp + 1) * 512], [], [("wst", b)])
            for jj in range(4):
                j = grp * 4 + jj
                for c in range(8):
                    K.mm(PB[1][:, 2 * j:2 * j + 2], wst[b][:, c, jj * 128:(jj + 1) * 128], scT[:, 2 * c:2 * c + 2],
                         c == 0, c == 7, [("wst", b), "scT"], ["pb1"])
        K.tt("dve", v3(modT[:, :], 48), v3(PB[1][:, 0:96], 48), bc3(bada[:, :], 2), ALU.add, ["pb1", "bada"], ["modT"])
        K.dma("sp", modT_d[:, :], modT[:, :], ["modT"], [])
        K.stt("dve", v3(A1[:, :], 8), v3(modT[:, 16:32], 8), 1.0, bc3(nmix[:, :], 2), ALU.add, ALU.mult,
              ["modT", "nmix"], ["A1"])

        def rstd_of(ss, n, dim, extra=None):
            K.ts("dve", ss, ss, 1.0 / dim, EPS, ALU.mult, ALU.add, ["st"], ["st"])
            K.act(ss, ss, AF.Sqrt, ["st"], ["st"])
            K.S.op("dve", lambda e, a=ss: e.reciprocal(a, a), ["st"], ["st"])
            if extra is not None:
                K.ts("dve", ss, ss, extra, None, ALU.mult, None, ["st"], ["st"])

        def rope(src3, dst3, G, rp, rkeys, wkeys):
            for a in range(2):
                o = 16 * a
                cos = rp[:, 16 * a:16 * a + 8].unsqueeze(1).to_broadcast([128, G, 8])
                sin = rp[:, 16 * a + 8:16 * a + 16].unsqueeze(1).to_broadcast([128, G, 8])
                x1 = src3[:, :, o:o + 8]
                x2 = src3[:, :, o + 8:o + 16]
                t = [v3(rt[i][:, 0:G * 8], G) for i in range(4)]
                K.tt("pool", t[0], x1, cos, ALU.mult, rkeys, ["rt0"])
                K.tt("pool", t[1], x2, sin, ALU.mult, rkeys, ["rt1"])
                K.tt("dve", dst3[:, :, o:o + 8], t[0], t[1], ALU.subtract, ["rt0", "rt1"], wkeys)
                K.tt("pool", t[2], x2, cos, ALU.mult, rkeys, ["rt2"])
                K.tt("pool", t[3], x1, sin, ALU.mult, rkeys, ["rt3"])
                K.tt("dve", dst3[:, :, o + 8:o + 16], t[2], t[3], ALU.add, ["rt2", "rt3"], wkeys)

        for t in range(NT):
            b = t % 2
            wh = 0 if t < NT_OWN else 1
            rows = slice(t * 128, (t + 1) * 128)
            X = xt[b]
            K.dma("sp", X[:, :], x_d[rows, :], [], [("xt", b)])
            K.dma("sp", ropet[b][:, :], rope_d[rows, :], [], [("rope", b)])
            K.tt("dve", sqj[:, :], X[:, :], X[:, :], ALU.mult, [("xt", b)], ["sqj"])
            K.rsum("dve", st[:, 0:1], sqj[:, :], ["sqj"], ["st"])
            rstd_of(st[:, 0:1], 1, D)
            K.act(xn[:, :], X[:, :], AF.Copy, [("xt", b), "st"], ["xn"], scale=st[:, 0:1])
            for c in range(8):
                K.tr(PB0b[:, c * 128:(c + 1) * 128], xn[:, c * 128:(c + 1) * 128], ident[:, :], ["xn", "ident"], ["pb0"])
            for c in range(8):
                K.ts("dve", hT[:, c, :], PB0b[:, c * 128:(c + 1) * 128], A1[:, 2 * c + wh:2 * c + wh + 1],
                     modT[:, 2 * c + wh:2 * c + wh + 1], ALU.mult, ALU.add, ["pb0", "A1", "modT"], ["hT"])
            for k5 in range(5):
                n0 = k5 * 512
                n1 = min(INW, n0 + 512)
                pb = 1 + k5 % 3
                for c in range(8):
                    K.mm(PB[pb][:, 0:n1 - n0], hT[:, c, :], win[:, c, n0:n1], c == 0, c == 7, ["hT", "win"], [f"pb{pb}"])
                K.cp("act", z[:, n0:n1], PB[pb][:, 0:n1 - n0], [f"pb{pb}"], ["z"])
            za = z[:, 0:512]
            K.tt("pool", g1[:, :], za, za, ALU.mult, ["z"], ["g1"])
            K.ts("dve", g1[:, :], g1[:, :], 0.044715, 1.0, ALU.mult, ALU.add, ["g1"], ["g1"])
            K.tt("pool", g1[:, :], g1[:, :], za, ALU.mult, ["g1", "z"], ["g1"])
            K.act(g2[:, :], g1[:, :], AF.Sigmoid, ["g1"], ["g2"], scale=1.5957691216057308)
            K.tt("dve", gg[:, :], g2[:, :], za, ALU.mult, ["g2", "z"], ["gg"])
            K.tt("pool", g1[:, 0:256], gg[:, 256:512], gg[:, 256:512], ALU.mult, ["gg"], ["g1"])
            K.rsum("dve", st[:, 0:1], g1[:, 0:256], ["g1"], ["st"])
            rstd_of(st[:, 0:1], 1, 256)
            K.ts("dve", g1[:, 256:512], gg[:, 256:512], st[:, 0:1], None, ALU.mult, None, ["gg", "st", "g1"], ["g1"])
            K.tt("pool", vnb[:, :], g1[:, 256:512], gains[:, G_AV:G_AV + 256], ALU.mult, ["g1", "gains"], ["vnb"])
            for hd in range(4):
                K.mm(PB[4][:, hd * 64:(hd + 1) * 64], wsT[:, hd, :], vnb[:, hd * 64:(hd + 1) * 64], True, True,
                     ["wsT", "vnb"], ["pb4"])
            for hd in range(4):
                K.stt("dve", oa[:, hd * 64:(hd + 1) * 64], PB[4][:, hd * 64:(hd + 1) * 64], bs[:, hd:hd + 1],
                      gg[:, hd * 64:(hd + 1) * 64], ALU.add, ALU.mult, ["pb4", "bs", "gg"], ["oa"])
            K.dma("sp", oa_d[rows, :], oa[:, :], ["oa"], [])
            for which, (o0, gofs, extra) in enumerate(((512, G_BQ, 0.125), (896, G_BK, None))):
                src = z[:, o0:o0 + 384]
                K.tt("pool", t384[:, :], src, src, ALU.mult, ["z"], ["t384"])
                K.rsum("dve", st[:, 0:6], v3(t384[:, :], 6), ["t384"], ["st"])
                rstd_of(st[:, 0:6], 6, 64, extra)
                K.tt("dve", v3(u384[:, :], 6), v3(src, 6), bc3(st[:, 0:6], 64), ALU.mult, ["z", "st"], ["u384"])
                dst = qnb[:, :] if which == 0 else kvbs[:, 0:384]
                K.tt("pool", dst, u384[:, :], gains[:, gofs:gofs + 384], ALU.mult, ["u384", "gains"],
                     ["qnb" if which == 0 else "kvbs"])
            for pr in range(3):
                K.tr(PB0b[:, pr * 128:(pr + 1) * 128], qnb[:, pr * 128:(pr + 1) * 128], ident[:, :], ["qnb", "ident"], ["pb0"])
            K.cp("dve", qbTs[:, :, :], v3(PB0b[:, 0:384], 3), ["pb0"], ["qbTs"])
            K.dma("sp", qbT_d[:, :, rows].rearrange("a p t -> p a t"), qbTs[:, :, :], ["qbTs"], [])
            K.cp("act", kvbs[:, 384:768], z[:, 1280:1664], ["z"], ["kvbs"])
            if t < NT_OWN:
                K.dma("sp", kvb_own[rows, :], kvbs[:, :], ["kvbs"], ["kvb_x"])
                if t < HT:
                    K.dma("sp", kvb_lo[t * 128:(t + 1) * 128, :], kvbs[:, :], ["kvbs"], ["kvb_x"])
                if t >= NT_OWN - HT:
                    t2 = t - (NT_OWN - HT)
                    K.dma("sp", kvb_hi[t2 * 128:(t2 + 1) * 128, :], kvbs[:, :], ["kvbs"], ["kvb_x"])
            else:
                K.dma("sp", kvb_ctx[(t - NT_OWN) * 128:(t - NT_OWN + 1) * 128, :], kvbs[:, :], ["kvbs"], ["kvb_x"])
            src = z[:, 1664:2048]
            K.tt("pool", t384[:, :], src, src, ALU.mult, ["z"], ["t384"])
            K.rsum("dve", st[:, 0:1], t384[:, :], ["t384"], ["st"])
            rstd_of(st[:, 0:1], 1, 384)
            K.ts("dve", u384[:, :], src, st[:, 0:1], None, ALU.mult, None, ["z", "st"], ["u384"])
            K.tt("pool", qab[:, :], u384[:, :], gains[:, G_CQA:G_CQA + 384], ALU.mult, ["u384", "gains"], ["qab"])
            for c in range(3):
                K.tr(PB0b[:, c * 128:(c + 1) * 128], qab[:, c * 128:(c + 1) * 128], ident[:, :], ["qab", "ident"], ["pb0"])
            K.cp("dve", qaT[:, :, :], v3(PB0b[:, 0:384], 3), ["pb0"], ["qaT"])
            for c in range(3):
                K.mm(PB[5][:, 0:512], qaT[:, c, :], wq[:, c, 0:512], c == 0, c == 2, ["qaT", "wq"], ["pb5"])
            for c in range(3):
                K.mm(PB[7][:, 256:320], qaT[:, c, :], wq[:, c, 512:576], c == 0, c == 2, ["qaT", "wq"], ["pb7b"])
            K.cp("act", qf[:, 0:512], PB[5][:, 0:512], ["pb5"], ["qf"])
            K.cp("act", qf[:, 512:576], PB[7][:, 256:320], ["pb7b"], ["qf"])
            qf3 = v3(qf[:, :], 6)
            qs3 = v3(qs[:, :], 6)
            K.tt("pool", qs[:, :], qf[:, :], qf[:, :], ALU.mult, ["qf"], ["qs"])
            K.rsum("dve", st[:, 0:6], qs3[:, :, 0:64], ["qs"], ["st"])
            K.rsum("dve", st[:, 8:14], qs3[:, :, 64:96], ["qs"], ["st"])
            rstd_of(st[:, 0:6], 6, 64)
            rstd_of(st[:, 8:14], 6, 32)
            K.tt("dve", qs3[:, :, 0:64], qf3[:, :, 0:64], bc3(st[:, 0:6], 64), ALU.mult, ["qf", "st", "qs"], ["qs"])
            K.tt("dve", qs3[:, :, 64:96], qf3[:, :, 64:96], bc3(st[:, 8:14], 32), ALU.mult, ["qf", "st", "qs"], ["qs"])
            K.tt("pool", qf[:, :], qs[:, :], gains[:, G_CQN:G_CQN + 576], ALU.mult, ["qs", "gains"], ["qf"])
            K.cp("act", qc[:, :, 0:64], qf3[:, :, 0:64], ["qf"], ["qc"])
            rope(qf3[:, :, 64:96], qc[:, :, 64:96], 6, ropet[b], ["qf", ("rope", b)], ["qc"])
            for h in range(6):
                K.tr(PB0b[0:96, h * 128:(h + 1) * 128], qc[:, h, :], ident[:, :], ["qc", "ident"], ["pb0"])
            K.cp("dve", qcTs[:, :, :], v3(PB0b[0:96, 0:768], 6), ["pb0"], ["qcTs"])
            K.dma("sp", qcT_d[:, :, rows].rearrange("h p t -> p h t"), qcTs[:, :, :], ["qcTs"], [])
            src = z[:, 2048:2304]
            K.tt("pool", t384[:, 0:256], src, src, ALU.mult, ["z"], ["t384"])
            K.rsum("dve", st[:, 0:1], t384[:, 0:256], ["t384"], ["st"])
            rstd_of(st[:, 0:1], 1, 256)
            K.ts("dve", u384[:, 0:256], src, st[:, 0:1], None, ALU.mult, None, ["z", "st"], ["u384"])
            K.tt("pool", kvab[:, :], u384[:, 0:256], gains[:, G_CKVA:G_CKVA + 256], ALU.mult, ["u384", "gains"], ["kvab"])
            for c in range(2):
                K.tr(PB0b[:, c * 128:(c + 1) * 128], kvab[:, c * 128:(c + 1) * 128], ident[:, :], ["kvab", "ident"], ["pb0"])
            K.cp("dve", kvaT[:, :, :], v3(PB0b[:, 0:256], 2), ["pb0"], ["kvaT"])
            for c in range(2):
                K.mm(PB[6][:, 0:512], kvaT[:, c, :], wkv[:, c, 0:512], c == 0, c == 1, ["kvaT", "wkv"], ["pb6"])
            for c in range(2):
                K.mm(PB[7][:, 0:256], kvaT[:, c, :], wkv[:, c, 512:768], c == 0, c == 1, ["kvaT", "wkv"], ["pb7a"])
            K.cp("act", kvf[:, 0:512], PB[6][:, 0:512], ["pb6"], ["kvf"])
            K.cp("act", kvf[:, 512:768], PB[7][:, 0:256], ["pb7a"], ["kvf"])
            kvf3 = v3(kvf[:, :], 6)
            K.cp("act", vcs[:, :, 0:64], kvf3[:, :, 64:128], ["kvf"], ["vcs"])
            if t < NT_OWN:
                K.dma("sp", d[f"vc_own_{L}_{t // PT}"][(t % PT) * 128:(t % PT + 1) * 128, :],
                      vcs[:, :, :].rearrange("p h d -> p (h d)"), ["vcs"], ["vc_x"])
            else:
                K.dma("sp", vc_ctx[(t - NT_OWN) * 128:(t - NT_OWN + 1) * 128, :], vcs[:, :, :].rearrange("p h d -> p (h d)"), ["vcs"], ["vc_x"])
            t3 = v3(t384[:, :], 6)
            u3 = v3(u384[:, :], 6)
            K.tt("pool", t3, kvf3[:, :, 0:64], kvf3[:, :, 0:64], ALU.mult, ["kvf"], ["t384"])
            K.rsum("dve", st[:, 0:6], t3, ["t384"], ["st"])
            rstd_of(st[:, 0:6], 6, 64)
            K.tt("dve", u3, kvf3[:, :, 0:64], bc3(st[:, 0:6], 64), ALU.mult, ["kvf", "st"], ["u384"])
            K.tt("pool", kc[:, :, 0:64], u3, v3(gains[:, G_CKN:G_CKN + 384], 6), ALU.mult, ["u384", "gains"], ["kc"])
            src = z[:, 2304:2336]
            K.tt("pool", kr[:, :], src, src, ALU.mult, ["z"], ["kr"])
            K.rsum("dve", st[:, 0:1], kr[:, :], ["kr"], ["st"])
            rstd_of(st[:, 0:1], 1, 32)
            K.ts("dve", kr[:, :], src, st[:, 0:1], None, ALU.mult, None, ["z", "st", "kr"], ["kr"])
            K.tt("pool", kr[:, :], kr[:, :], gains[:, G_CKR:G_CKR + 32], ALU.mult, ["kr", "gains"], ["kr"])
            rope(v3(kr[:, :], 1), v3(krr[:, :], 1), 1, ropet[b], ["kr", ("rope", b)], ["krr"])
            K.cp("dve", kc[:, :, 64:96], krr[:, :].unsqueeze(1).to_broadcast([128, 6, 32]), ["krr"], ["kc"])
            for h in range(6):
                K.tr(PB0b[0:96, h * 128:(h + 1) * 128], kc[:, h, :], ident[:, :], ["kc", "ident"], ["pb0"])
            K.cp("dve", kcTs[:, :, :], v3(PB0b[0:96, 0:768], 6), ["pb0"], ["kcTs"])
            if t < NT_OWN:
                K.dma("sp", d[f"kcT_own_{L}_{t // PT}"].rearrange("(h p) t -> p h t", h=6)[:, :, (t % PT) * 128:(t % PT + 1) * 128],
                      kcTs[:, :, :], ["kcTs"], ["kc_x"])
            else:
                K.dma("sp", kcT_ctx.rearrange("(h p) t -> p h t", h=6)[:, :, (t - NT_OWN) * 128:(t - NT_OWN + 1) * 128],
                      kcTs[:, :, :], ["kcTs"], ["kc_x"])
        S.emit()


def _kmaj(w, kc):
    return np.ascontiguousarray(w.reshape(kc, 128, -1).transpose(1, 0, 2))


def _colT(v):
    return np.ascontiguousarray(v.reshape(-1, 128).T)


def _bcrow(v):
    return np.ascontiguousarray(np.broadcast_to(v[None, :], (128, v.shape[0])))


def rope_tables(NT_OWN, rank):
    half = 16
    inv = (np.float32(10000.0) ** (-(np.arange(0, half, 2, dtype=np.float32)) / np.float32(half))).astype(np.float32)
    pos = np.arange(NT_OWN * 128) + rank * NT_OWN * 128
    out = np.zeros(((NT_OWN + 2) * 128, 32), np.float32)
    ar = (pos // GRID_W).astype(np.float32)[:, None] * inv[None, :]
    ac = (pos % GRID_W).astype(np.float32)[:, None] * inv[None, :]
    n = NT_OWN * 128
    out[:n, 0:8] = np.cos(ar)
    out[:n, 8:16] = np.sin(ar)
    out[:n, 16:24] = np.cos(ac)
    out[:n, 24:32] = np.sin(ac)
    out[n:, 0:8] = 1.0
    out[n:, 16:24] = 1.0
    return out


def prep_A(P, i, x_cur, xc_cur, NT_OWN, ranks_per_batch=4):
    B = x_cur.shape[0]
    gains = np.concatenate([
        P["a_v_norm"][i], np.tile(P["b_q_norm"][i], 6), np.tile(P["b_k_norm"][i], 6), P["c_q_a_norm"][i],
        P["c_kv_a_norm"][i], np.tile(P["c_q_norm"][i], 6), np.tile(P["c_k_norm"][i][:64], 6), P["c_k_norm"][i][64:]])
    shared = {
        "w_ada": _kmaj(P["w_ada"][i], 8),
        "b_adaT": _colT(P["b_ada"][i]),
        "norm_mixT": _colT(P["norm_mix"][i]),
        "w_in": _kmaj(P["w_in"][i], 8),
        "w_sT": np.ascontiguousarray(P["a_w_s"][i].transpose(2, 0, 1)),
        "b_s": np.ascontiguousarray(P["a_b_s"][i].T),
        "gains": _bcrow(gains.astype(np.float32)),
        "wq": _kmaj(P["c_w_q_up"][i], 3),
        "wkv": _kmaj(P["c_w_kv_up"][i], 2),
        "ident": np.eye(128, dtype=np.float32),
    }
    maps = []
    n = NT_OWN * 128
    for b in range(B):
        cT = np.stack([_colT(P["c"][b]), _colT(P["c_ctx"])], axis=2).reshape(128, 16)
        for r in range(ranks_per_batch):
            m = dict(shared)
            m["x"] = np.ascontiguousarray(np.concatenate([x_cur[b, r * n:(r + 1) * n], xc_cur[b]], axis=0))
            m["cT"] = np.ascontiguousarray(cT)
            m["rope"] = rope_tables(NT_OWN, r)
            maps.append(m)
    return maps


def _specials(NT_OWN):
    return sorted(set(t for t in (0, 1, NT_OWN - 2, NT_OWN - 1) if 0 <= t < NT_OWN))


def emit_B(nc, gc, d, NT_OWN, L, TB=4, NE=64, NR=4):
    NT = NT_OWN + 2
    T = NT * 128
    n = NT_OWN * 128
    NW = NT_OWN + 8
    NKT = NR * NT_OWN + 2
    QB = min(4, NT_OWN)
    CH = min(4, NT_OWN)
    specials = _specials(NT_OWN)
    BIG = 1.0e30
    stack = contextlib.ExitStack()
    with stack:
        K = KB(nc, stack, gc)
        S = K.S
        x_d = d["x"] if L == 0 else d["xs_0"]
        xo_d = d["xs_0"] if L == 0 else d["xo"]
        oa_d, qbT_d, qcT_d, modT_d, mix_d = d[f"oa_{L}"], d[f"qbT_{L}"], d[f"qcT_{L}"], d[f"modT_{L}"], d[f"mixd_{L}"]
        PCT = min(4, NT_OWN)
        HT = min(3, NT_OWN)
        kcT_ctx, vc_ctx = d[f"kcT_ctx_{L}"], d[f"vc_ctx_{L}"]
        kvb_own, kvb_ctx, kvb_glo, kvb_ghi = d[f"kvb_own_{L}"], d[f"kvb_ctx_{L}"], d[f"kvb_glo_{L}"], d[f"kvb_ghi_{L}"]
        btab_d, wout_d, nffn_d, wgr_d = d[f"btab_{L}"], d[f"w_out_{L}"], d[f"norm_ffnT_{L}"], d[f"wgr_{L}"]
        w1_d, w3_d, w2_d = d[f"w1_{L}"], d[f"w3_{L}"], d[f"w2_{L}"]
        ident_d, widx_d = d["ident"], d["widx"]

        AR = 45056
        arena = K.sb("arena", [128, AR], BF16)
        identb = K.sb("ident_b", [128, 128], BF16)
        identf = K.sb("identf", [128, 128], F32)
        ones = K.sb("ones", [128, 128], F32)
        modT = K.sb("modTs", [128, 96], F32)
        nffn = K.sb("nffn", [128, 8], F32)
        A2 = K.sb("A2", [128, 16], F32)
        G = [K.sb(f"G{i}", [128, D], F32) for i in range(4)]
        gbc = K.sb("gbc", [128, 128], F32)
        wgr = K.sb("wgrs", [128, 8, 72], F32)
        kvt = [K.sb(f"kvt{i}", [128, 768], BF16) for i in range(2)]
        qbt = [K.sb(f"qbt{i}", [128, 3, 128], BF16) for i in range(2)]
        PT = [K.sb(f"PT{i}", [128, 512], BF16) for i in range(3)]
        st = K.sb("st", [128, 64], F32)
        ob = K.sb("ob", [128, 384], BF16)
        oc = K.sb("oc", [128, 4, 64], BF16)
        ocT = K.sb("ocT", [65, 512], F32)
        qT = [K.sb(f"qT{i}", [96, 512], BF16) for i in range(2)]
        xt = [K.sb(f"xt{i}", [128, D], F32) for i in range(2)]
        mixrow = K.sb("mixrow", [128, D], BF16)
        mixT = K.sb("mixT", [128, 8, 128], BF16)
        tmpf = K.sb("tmpf", [128, D], F32)
        h2n = K.sb("h2n", [128, D], F32)
        h2Tf = K.sb("h2Tf", [128, 8, 128], F32)
        lg = K.sb("lg", [128, 72], F32)
        r64 = [K.sb(f"r64_{i}", [128, 64], F32) for i in range(4)]
        sil = [K.sb(f"sil{i}", [128, 128], F32) for i in range(2)]
        hT = K.sb("hTe", [128, 4, 128], BF16)
        PB = [K.ps(f"pb{i}", [128, 512], F32) for i in range(8)]
        PB0b = PB[0].bitcast(BF16)

        K.dma("sp", identf[:, :], ident_d[:, :], [], ["identf"])
        K.cp("dve", identb[:, :], identf[:, :], ["identf"], ["identb"])
        K.memset("dve", ones[:, :], 1.0, [], ["ones"])
        K.dma("sp", modT[:, :], modT_d[:, :], [], ["modT"])
        K.dma("sp", nffn[:, :], nffn_d[:, :], [], ["nffn"])
        K.dma("sp", wgr[:, :, :], wgr_d[:, :, :], [], ["wgr"])
        K.stt("dve", v3(A2[:, :], 8), v3(modT[:, 64:80], 8), 1.0, bc3(nffn[:, :], 2), ALU.add, ALU.mult,
              ["modT", "nffn"], ["A2"])
        for gi, (j0, wh) in enumerate(((16, 0), (16, 1), (40, 0), (40, 1))):
            for c in range(8):
                col = 2 * (j0 + c) + wh
                K.ts("dve", gbc[:, :], ones[:, :], modT[:, col:col + 1], None, ALU.mult, None, ["ones", "modT"], ["gbc"])
                K.mm(PB[7][:, (c % 4) * 128:(c % 4 + 1) * 128], gbc[:, :], identf[:, :], True, True, ["gbc", "identf"], ["pb7"])
                K.cp("act", G[gi][:, c * 128:(c + 1) * 128], PB[7][:, (c % 4) * 128:(c % 4 + 1) * 128], ["pb7"], [("G", gi)])

        o1 = 3 * NW * 128
        o2 = o1 + NW * 390
        KbT = arena[:, 0:o1].rearrange("p (a t) -> p a t", a=3)
        Vb = arena[:, o1:o2].rearrange("p (s h d) -> p s h d", s=NW, h=6)
        bt0 = arena[:, o2:o2 + 6144].rearrange("p (h e) -> p h e", h=6)
        btS = arena[:, o2 + 6144:o2 + 12288].rearrange("p (h e) -> p h e", h=6)
        assert o2 + 12288 <= AR
        K.memset("pool", arena[:, o1:o2], 1.0, [], ["Vb"])
        K.dma("sp", bt0, btab_d[0], [], ["bt0"])
        widx = K.sb("widx", [128, 6], I32)
        K.dma("sp", widx[:, :], widx_d[:, :], [], ["widx"])
        for s in range(NW):
            b = s % 2
            if s < 3 or NT_OWN + 3 <= s < NT_OWN + 6:
                srcg = kvb_ghi if s < 3 else kvb_glo
                col = s if s < 3 else s - NT_OWN
                K.S.dmafn("pool", lambda e, o=kvt[b][:, :], ix=widx[:, col:col + 1], sg=srcg: e.indirect_dma_start(
                    out=o, out_offset=None, in_=sg[:, :], in_offset=bass.IndirectOffsetOnAxis(ap=ix, axis=0)),
                    ["widx"], [("kvt", b)])
            elif s < NT_OWN + 3:
                K.dma("sp", kvt[b][:, :], kvb_own[(s - 3) * 128:(s - 2) * 128, :], [], [("kvt", b)])
            else:
                K.dma("sp", kvt[b][:, :], kvb_ctx[(s - NT_OWN - 6) * 128:(s - NT_OWN - 5) * 128, :], [], [("kvt", b)])
            for pr in range(3):
                K.tr(PB0b[:, pr * 128:(pr + 1) * 128], kvt[b][:, pr * 128:(pr + 1) * 128], identb[:, :], [("kvt", b), "identb"], ["pb0"])
            K.cp("dve", KbT[:, :, s * 128:(s + 1) * 128], v3(PB0b[:, 0:384], 3), ["pb0"], ["KbT"])
            K.cp("pool", Vb[:, s, :, 0:64], v3(kvt[b][:, 384:768], 6), [("kvt", b), "Vb"], ["Vb"])
        DSK = 2
        itemsB = []
        for t in range(NT):
            b = t % 2
            own = t < NT_OWN
            rows = slice(t * 128, (t + 1) * 128)
            special = own and t in specials
            bt, btk = (btS, "btS") if special else (bt0, "bt0")
            if own:
                kts = [(t + j + 3, j) for j in range(-3, 4)] + [(NW - 2, None), (NW - 1, None)]
            else:
                kts = [(NW - 2, None), (NW - 1, None)]
            groups = [kts[i:i + 3] for i in range(0, len(kts), 3)]
            ob_bank = 4 + t % 2
            for h in range(6):
                nk0 = 0
                for gi, grp in enumerate(groups):
                    itemsB.append(dict(t=t, b=b, h=h, grp=grp, nk0=nk0, nkt=len(kts), ob_bank=ob_bank, bt=bt, btk=btk,
                                       first=(h == 0 and gi == 0), last=(h == 5 and gi == len(groups) - 1),
                                       special=special, rows=rows))
                    nk0 += len(grp)

        def qkB(i, it):
            b, h, t = it["b"], it["h"], it["t"]
            if it["first"]:
                K.dma("sp", qbt[b][:, :, :], qbT_d[:, :, it["rows"]].rearrange("a p t -> p a t"), [], [("qbt", b)])
                if it["special"]:
                    K.dma("sp", btS, btab_d[1 + specials.index(t)], [], ["btS"])
            pr, pb = h // 2, (h % 2) * 64
            bank = 1 + i % 3
            pi = i % 3
            for ii, (s_, j) in enumerate(it["grp"]):
                K.mm(PB[bank][:, ii * 128:(ii + 1) * 128], KbT[pb:pb + 64, pr, s_ * 128:(s_ + 1) * 128],
                     qbt[b][pb:pb + 64, pr, :], True, j is None, ["KbT", ("qbt", b)], [f"pb{bank}"])
                if j is not None:
                    e0 = (7 - 2 * j) * 64
                    K.mm(PB[bank][:, ii * 128:(ii + 1) * 128], identb[:, :], it["bt"][:, h, e0:e0 + 128], False, True,
                         ["identb", it["btk"]], [f"pb{bank}"])
            n_ = len(it["grp"]) * 128
            K.act(PT[pi][:, 0:n_], PB[bank][:, 0:n_], AF.Exp, [f"pb{bank}"], [("PT", pi)])

        def pvB(i, it):
            h = it["h"]
            pi = i % 3
            OB = PB[it["ob_bank"]]
            for ii, (s_, j) in enumerate(it["grp"]):
                nk = it["nk0"] + ii
                K.mm(OB[:, h * 65:(h + 1) * 65], PT[pi][:, ii * 128:(ii + 1) * 128], Vb[:, s_, h, :],
                     nk == 0, nk == it["nkt"] - 1, [("PT", pi), "Vb"], [f"pb{it['ob_bank']}"])
            if it["last"]:
                O3 = v3(OB[:, 0:390], 6)
                K.S.op("dve", lambda e, O3=O3: e.reciprocal(st[:, 0:6], O3[:, :, 64]), [f"pb{it['ob_bank']}"], ["st"])
                K.tt("dve", v3(ob[:, :], 6), O3[:, :, 0:64], bc3(st[:, 0:6], 64), ALU.mult, [f"pb{it['ob_bank']}", "st"], ["ob"])
                K.dma("sp", mix_d[it["rows"], 256:640], ob[:, :], ["ob"], [("mixd", it["t"])])

        for i in range(len(itemsB) + DSK):
            if i < len(itemsB):
                qkB(i, itemsB[i])
            if i - DSK >= 0:
                pvB(i - DSK, itemsB[i - DSK])

        S.barrier()
        Kc = [arena[:, i * 2048:(i + 1) * 2048] for i in range(3)]
        Vc = [arena[:, 6144 + i * 1040:6144 + (i + 1) * 1040].rearrange("p (k d) -> p k d", d=65) for i in range(3)]
        qblocks = [(t0, QB, list(range(NKT))) for t0 in range(0, NT_OWN, QB)] + [(NT_OWN, 2, [NKT - 2, NKT - 1])]
        SCALE_C = 96.0 ** -0.5
        itemsC = []
        qh = 0
        cc = 0
        for (t0, ntl, klist) in qblocks:
            nq = ntl * 128
            for h in range(6):
                b2 = qh % 2
                oc_bank = 4 + qh % 2
                qh += 1
                own_k = [k for k in klist if k < NR * NT_OWN]
                ctx_k = [k for k in klist if k >= NR * NT_OWN]
                chunks = [own_k[i:i + CH] for i in range(0, len(own_k), CH)] + ([ctx_k] if ctx_k else [])
                nk = 0
                for chk in chunks:
                    cb = cc % 3
                    cc += 1
                    for kt in range(len(chk)):
                        itemsC.append(dict(t0=t0, ntl=ntl, nq=nq, h=h, b2=b2, oc_bank=oc_bank, cb=cb, chk=chk, kt=kt,
                                           newq=(nk == 0), newchunk=(kt == 0), nk=nk, nkt=len(klist)))
                        nk += 1

        def qkC(i, it):
            h, b2, cb, nq = it["h"], it["b2"], it["cb"], it["nq"]
            if it["newq"]:
                K.dma("sp", qT[b2][:, 0:nq], qcT_d[h, :, it["t0"] * 128:it["t0"] * 128 + nq], [], [("qT", b2)])
            if it["newchunk"]:
                chk = it["chk"]
                k0, n_k = chk[0], len(chk)
                if k0 < NR * NT_OWN:
                    rk, l0 = k0 // NT_OWN, k0 % NT_OWN
                    pj, lt = l0 // PCT, l0 % PCT
                    assert lt + n_k <= PCT
                    ksrc = d[f"kcT_g_{L}_{pj}"].rearrange("(r h p) t -> r h p t", r=NR, h=6)[rk, h, :, lt * 128:(lt + n_k) * 128]
                    v0 = (rk * PCT + lt) * 128
                    vsrc = d[f"vc_g_{L}_{pj}"][v0:v0 + n_k * 128, h * 65:(h + 1) * 65]
                else:
                    c0_ = (k0 - NR * NT_OWN) * 128
                    ksrc = kcT_ctx.rearrange("(h p) t -> h p t", h=6)[h, :, c0_:c0_ + n_k * 128]
                    vsrc = vc_ctx[c0_:c0_ + n_k * 128, h * 65:(h + 1) * 65]
                K.dma("sp", Kc[cb][0:96, 0:n_k * 128], ksrc, [], [("Kc", cb)])
                K.dma("act", Vc[cb][:, 0:n_k, :], vsrc.rearrange("(k p) d -> p k d", p=128), [], [("Vc", cb)])
            bank = 1 + i % 3
            pi = i % 3
            kt = it["kt"]
            K.mm(PB[bank][:, 0:nq], Kc[cb][0:96, kt * 128:(kt + 1) * 128], qT[b2][:, 0:nq], True, True,
                 [("Kc", cb), ("qT", b2)], [f"pb{bank}"])
            K.act(PT[pi][:, 0:nq], PB[bank][:, 0:nq], AF.Exp, [f"pb{bank}"], [("PT", pi)], scale=SCALE_C)

        def pvC(i, it):
            pi = i % 3
            nq, ntl, oc_bank, cb, kt, h = it["nq"], it["ntl"], it["oc_bank"], it["cb"], it["kt"], it["h"]
            OC = PB[oc_bank]
            K.mm(OC[0:65, 0:nq], Vc[cb][:, kt, :], PT[pi][:, 0:nq], it["nk"] == 0, it["nk"] == it["nkt"] - 1,
                 [("PT", pi), ("Vc", cb)], [f"pb{oc_bank}"])
            if it["nk"] == it["nkt"] - 1:
                t0 = it["t0"]
                K.cp("dve", ocT[:, 0:nq], OC[0:65, 0:nq], [f"pb{oc_bank}"], ["ocT"])
                for qi in range(ntl):
                    K.tr(PB[0][:, qi * 65:(qi + 1) * 65], ocT[:, qi * 128:(qi + 1) * 128], identf[0:65, 0:65], ["ocT", "identf"], ["pb0"])
                O3 = v3(PB[0][:, 0:ntl * 65], ntl)
                K.S.op("dve", lambda e, O3=O3, ntl=ntl: e.reciprocal(st[:, 0:ntl], O3[:, :, 64]), ["pb0"], ["st"])
                K.tt("dve", oc[:, 0:ntl, :], O3[:, :, 0:64], bc3(st[:, 0:ntl], 64), ALU.mult, ["pb0", "st"], ["oc"])
                K.dma("sp", mix_d[t0 * 128:t0 * 128 + nq, 640 + h * 64:704 + h * 64].rearrange("(q p) d -> p q d", p=128),
                      oc[:, 0:ntl, :], ["oc"], [("mixd", t0 + i_) for i_ in range(ntl)])

        for i in range(len(itemsC) + DSK):
            if i < len(itemsC):
                qkC(i, itemsC[i])
            if i - DSK >= 0:
                pvC(i - DSK, itemsC[i - DSK])

        S.barrier()
        NB = (2 * T) // 128 + 64
        h2_d, x1_d, xs_d, ys_d = d[f"h2_{L}"], d[f"x1_{L}"], d[f"xsl_{L}"], d[f"ys_{L}"]
        w1r = w1_d.rearrange("e p c n -> (e p) (c n)")
        w3r = w3_d.rearrange("e p c n -> (e p) (c n)")
        w2r = w2_d.rearrange("e p c n -> (e p) (c n)")
        WSZ = 12288
        Wb = [arena[:, i * WSZ:(i + 1) * WSZ] for i in range(2)]
        wout = arena[:, 2 * WSZ:2 * WSZ + 8192].rearrange("p (c n) -> p c n", c=8)
        ohb = arena[:, 2 * WSZ + 8192:2 * WSZ + 8192 + NT * 128].rearrange("p (t e) -> p t e", t=NT)
        assert 2 * WSZ + 8192 + NT * 128 <= AR
        K.dma("pool", wout, wout_d[:, :, :], [], ["wout"])
        ut = K.sb("ut", [128, 128], F32)
        iop = K.sb("iop", [128, 1], F32)
        K.dma("sp", ut[:, :], d["ut"][:, :], [], ["ut"])
        K.dma("sp", iop[:, :], d["iop"][:, :], [], ["iop"])
        base = K.sb("base", [128, 64], F32)
        K.memset("dve", base[:, :], 0.0, [], ["base"])
        rk = K.sb("rk", [128, NT, 2], F32)
        wts = K.sb("wts", [128, NT, 2], F32)
        dstf = K.sb("dstf", [128, NT, 2], F32)
        dsti = K.sb("dsti", [128, NT * 2], I32)
        h2s = [K.sb(f"h2s{i}", [128, D], BF16) for i in range(2)]
        MB = [K.sb(f"MB{i}", [128, D], F32) for i in range(4)]
        for gi, (src, wh) in enumerate((("A2", 0), ("A2", 1), ("B2", 0), ("B2", 1))):
            for c in range(8):
                col = (A2[:, 2 * c + wh:2 * c + wh + 1] if src == "A2"
                       else modT[:, 2 * (24 + c) + wh:2 * (24 + c) + wh + 1])
                K.ts("dve", gbc[:, :], ones[:, :], col, None, ALU.mult, None, ["ones", "modT", "A2"], ["gbc"])
                K.mm(PB[7][:, (c % 4) * 128:(c % 4 + 1) * 128], gbc[:, :], identf[:, :], True, True, ["gbc", "identf"], ["pb7"])
                K.cp("act", MB[gi][:, c * 128:(c + 1) * 128], PB[7][:, (c % 4) * 128:(c % 4 + 1) * 128], ["pb7"], [("MB", gi)])

        def rstd_of(ss, dim):
            K.ts("dve", ss, ss, 1.0 / dim, EPS, ALU.mult, ALU.add, ["st"], ["st"])
            K.act(ss, ss, AF.Sqrt, ["st"], ["st"])
            K.S.op("dve", lambda e, a=ss: e.reciprocal(a, a), ["st"], ["st"])

        x1t = [K.sb(f"x1t{i}", [128, D], F32) for i in range(2)]
        for t in range(NT):
            b = t % 2
            wh = 0 if t < NT_OWN else 1
            rows = slice(t * 128, (t + 1) * 128)
            X1 = x1t[b]
            K.dma("sp", xt[b][:, :], x_d[rows, :], [], [("xt", b)])
            K.dma("sp", mixrow[:, 256:1024], mix_d[rows, 256:1024], [("mixd", t)], ["mixrow"])
            K.dma("sp", mixrow[:, 0:256], oa_d[rows, :], [], ["mixrow"])
            for c in range(8):
                K.tr(PB0b[:, c * 128:(c + 1) * 128], mixrow[:, c * 128:(c + 1) * 128], identb[:, :], ["mixrow", "identb"], ["pb0"])
            K.cp("dve", mixT[:, :, :], v3(PB0b[:, :], 8), ["pb0"], ["mixT"])
            for half in range(2):
                pb = 1 + half
                for c in range(8):
                    K.mm(PB[pb][:, :], mixT[:, c, :], wout[:, c, half * 512:(half + 1) * 512], c == 0, c == 7,
                         ["mixT", "wout"], [f"pb{pb}"])
                hs = slice(half * 512, (half + 1) * 512)
                K.tt("dve", tmpf[:, hs], PB[pb][:, :], G[wh][:, hs], ALU.mult, [f"pb{pb}", ("G", wh)], ["tmpf"])
                K.tt("pool", X1[:, hs], tmpf[:, hs], xt[b][:, hs], ALU.add, ["tmpf", ("xt", b)], [("x1", b)])
            K.dma("sp", x1_d[rows, :], X1[:, :], [("x1", b)], [])
            K.tt("pool", tmpf[:, :], X1[:, :], X1[:, :], ALU.mult, [("x1", b), "tmpf"], ["tmpf"])
            K.rsum("dve", st[:, 0:1], tmpf[:, :], ["tmpf"], ["st"])
            rstd_of(st[:, 0:1], D)
            K.act(h2n[:, :], X1[:, :], AF.Copy, [("x1", b), "st"], ["h2n"], scale=st[:, 0:1])
            K.tt("pool", tmpf[:, :], h2n[:, :], MB[wh][:, :], ALU.mult, ["h2n", ("MB", wh), "tmpf"], ["tmpf"])
            K.tt("pool", h2s[b][:, :], tmpf[:, :], MB[2 + wh][:, :], ALU.add, ["tmpf", ("MB", 2 + wh)], [("h2s", b)])
            K.dma("sp", h2_d[rows, :], h2s[b][:, :], [("h2s", b)], [])
            for c in range(8):
                pb = 6 + c // 4
                K.tr(PB[pb][:, (c % 4) * 128:(c % 4 + 1) * 128], h2n[:, c * 128:(c + 1) * 128], identf[:, :], ["h2n", "identf"], [f"pb{pb}"])
            for c in range(8):
                pb = 6 + c // 4
                K.ts("dve", h2Tf[:, c, :], PB[pb][:, (c % 4) * 128:(c % 4 + 1) * 128], A2[:, 2 * c + wh:2 * c + wh + 1],
                     modT[:, 2 * (24 + c) + wh:2 * (24 + c) + wh + 1], ALU.mult, ALU.add, [f"pb{pb}", "A2", "modT"], ["h2Tf"])
            for c in range(8):
                K.mm(PB[3][:, 0:72], h2Tf[:, c, :], wgr[:, c, :], c == 0, c == 7, ["h2Tf", "wgr"], ["pb3"])
            K.cp("act", lg[:, :], PB[3][:, 0:72], ["pb3"], ["lg"])
            gl = lg[:, 0:8]
            rl3 = v3(lg[:, 8:72], 8)
            s_gmax, s_ngmax, s_gsum, s_m1, s_m2, s_d, s_e2, s_wa, s_wb = [st[:, 16 + i:17 + i] for i in range(9)]
            goh, gex, pen = st[:, 32:40], st[:, 40:48], st[:, 48:56]
            RK = ["lg", "st", "r64"]
            K.S.op("dve", lambda e, a=s_gmax, g=gl: e.reduce_max(a, g, AX.X), ["lg"], ["st"])
            K.ts("dve", goh, gl, s_gmax, None, ALU.is_equal, None, RK, ["st"])
            K.ts("dve", s_ngmax, s_gmax, -1.0, None, ALU.mult, None, RK, ["st"])
            K.act(gex, gl, AF.Exp, RK, ["st"], bias=s_ngmax)
            K.rsum("dve", s_gsum, gex, RK, ["st"])
            K.S.op("dve", lambda e, a=s_gsum: e.reciprocal(a, a), RK, ["st"])
            K.ts("dve", pen, goh, BIG, -BIG, ALU.mult, ALU.add, RK, ["st"])
            rm, oh1, rm2, oh2 = [r[:, :] for r in r64]
            K.tt("dve", v3(rm, 8), rl3, bc3(pen, 8), ALU.add, RK, ["r64"])
            K.S.op("dve", lambda e, a=s_m1, g=rm: e.reduce_max(a, g, AX.X), RK, ["st"])
            K.ts("dve", oh1, rm, s_m1, None, ALU.is_equal, None, RK, ["r64"])
            K.stt("dve", rm2, oh1, -BIG, rm, ALU.mult, ALU.add, RK, ["r64"])
            K.S.op("dve", lambda e, a=s_m2, g=rm2: e.reduce_max(a, g, AX.X), RK, ["st"])
            K.ts("dve", oh2, rm2, s_m2, None, ALU.is_equal, None, RK, ["r64"])
            K.tt("dve", s_d, s_m2, s_m1, ALU.subtract, RK, ["st"])
            K.act(s_e2, s_d, AF.Exp, RK, ["st"])
            K.ts("dve", s_wa, s_e2, 1.0, None, ALU.add, None, RK, ["st"])
            K.S.op("dve", lambda e, a=s_wa: e.reciprocal(a, a), RK, ["st"])
            K.tt("dve", wts[:, t, 0:1], s_wa, s_gsum, ALU.mult, RK, ["wts"])
            K.tt("dve", wts[:, t, 1:2], wts[:, t, 0:1], s_e2, ALU.mult, RK + ["wts"], ["wts"])
            K.cp("dve", ohb[:, t, 0:64], oh1, RK, ["ohb"])
            K.cp("dve", ohb[:, t, 64:128], oh2, RK, ["ohb"])
            K.tt("dve", rm, oh1, oh2, ALU.add, RK, ["r64"])
            K.mm(PB[3][:, 128:192], ut[:, :], rm, True, True, ["ut", "r64"], ["pb3"])
            K.mm(PB[3][:, 192:256], ones[:, :], rm, True, True, ["ones", "r64"], ["pb3"])
            K.tt("dve", rm2, PB[3][:, 128:192], base[:, :], ALU.add, ["pb3", "base", "r64"], ["r64"])
            K.tt("dve", rm, oh1, rm2, ALU.mult, RK, ["r64"])
            K.rsum("dve", rk[:, t, 0:1], rm, RK, ["rk"])
            K.tt("dve", rm, oh2, rm2, ALU.mult, RK, ["r64"])
            K.rsum("dve", rk[:, t, 1:2], rm, RK, ["rk"])
            K.tt("dve", base[:, :], PB[3][:, 192:256], base[:, :], ALU.add, ["pb3", "base", "r64"], ["base"])
        cs = [r64[0][:, :], r64[1][:, :]]
        pc = r64[2][:, :]
        pst = r64[3][:, :]
        KS = ["base", "r64"]
        K.ts("dve", pc, base[:, :], 127.0, None, ALU.add, None, KS, ["r64"])
        pci = K.sb("pci", [128, 64], I32)
        K.cp("dve", pci[:, :], pc, KS, ["pci"])
        K.ts("dve", pci[:, :], pci[:, :], 7, 7, ALU.arith_shift_right, ALU.logical_shift_left, ["pci"], ["pci"])
        K.cp("dve", pc, pci[:, :], ["pci"] + KS, ["r64"])
        K.cp("dve", cs[0], pc, KS, ["r64"])
        cur = 0
        for sh in (1, 2, 4, 8, 16, 32):
            K.cp("dve", cs[1 - cur][:, 0:sh], cs[cur][:, 0:sh], KS, ["r64"])
            K.tt("dve", cs[1 - cur][:, sh:64], cs[cur][:, sh:64], cs[cur][:, 0:64 - sh], ALU.add, KS, ["r64"])
            cur = 1 - cur
        pend = cs[cur]
        K.tt("dve", pst, pend, pc, ALU.subtract, KS, ["r64"])
        other = cs[1 - cur]
        for t in range(NT):
            for k in range(2):
                K.tt("dve", other, ohb[:, t, 64 * k:64 * k + 64], pst, ALU.mult, KS + ["ohb"], ["r64"])
                K.rsum("dve", dstf[:, t, k:k + 1], other, KS, ["dstf"])
        K.tt("dve", dstf[:, :, :], dstf[:, :, :], rk[:, :, :], ALU.add, ["dstf", "rk"], ["dstf"])
        K.cp("dve", dsti[:, :], dstf[:, :, :].rearrange("p t k -> p (t k)"), ["dstf"], ["dsti"])
        zt = K.sb("zt", [128, D], BF16)
        K.memset("pool", zt[:, :], 0.0, [], ["zt"])
        for bk in range(NB):
            K.dma("sp" if bk % 2 == 0 else "act", xs_d[bk * 128:(bk + 1) * 128, :], zt[:, :], ["zt"], ["xs"])
        S.barrier()
        for t in range(NT):
            b = t % 2
            K.dma("sp", h2s[b][:, :], h2_d[t * 128:(t + 1) * 128, :], [], [("h2s", b)])
            for k in range(2):
                K.S.dmafn("pool", lambda e, src=h2s[b][:, :], ix=dsti[:, 2 * t + k:2 * t + k + 1]: e.indirect_dma_start(
                    out=xs_d[:, :], out_offset=bass.IndirectOffsetOnAxis(ap=ix, axis=0), in_=src, in_offset=None),
                    [("h2s", b), "dsti"], ["xs"])
        S.barrier()
        xsb = [K.sb(f"xsb{i}", [128, D], BF16) for i in range(2)]
        xT = K.sb("xTb", [128, 8, 128], BF16)
        ysb = [K.sb(f"ysb{i}", [128, D], F32) for i in range(2)]
        widx2 = K.sb("widx2", [128, 2], I32)
        for bk in range(NB):
            wb = bk % 2
            W = Wb[wb]
            w1e = W[:, 0:4096].rearrange("p (c n) -> p c n", c=8)
            w3e = W[:, 4096:8192].rearrange("p (c n) -> p c n", c=8)
            w2e = W[:, 8192:12288].rearrange("p (c n) -> p c n", c=4)
            ef = st[:, 60:61]
            K.ts("dve", other, pend, float(128 * bk), None, ALU.is_le, None, ["r64"], ["r64b"])
            K.rsum("dve", ef, other, ["r64b"], ["st"])
            K.ts("dve", ef, ef, float(NE - 1), 128.0, ALU.min, ALU.mult, ["st"], ["st"])
            K.tt("dve", ef, ef, iop[:, :], ALU.add, ["st", "iop"], ["st"])
            K.cp("dve", widx2[:, wb:wb + 1], ef, ["st"], [("widx2", wb)])
            for (dst, srcw) in ((W[:, 0:4096], w1r), (W[:, 4096:8192], w3r), (W[:, 8192:12288], w2r)):
                K.S.dmafn("pool", lambda e, o=dst, sw=srcw, ix=widx2[:, wb:wb + 1]: e.indirect_dma_start(
                    out=o, out_offset=None, in_=sw[:, :], in_offset=bass.IndirectOffsetOnAxis(ap=ix, axis=0)),
                    [("widx2", wb)], [("W", wb)])
            K.dma("sp", xsb[wb][:, :], xs_d[bk * 128:(bk + 1) * 128, :], [], [("xsb", wb)])
            for c in range(8):
                K.tr(PB0b[:, c * 128:(c + 1) * 128], xsb[wb][:, c * 128:(c + 1) * 128], identb[:, :], [("xsb", wb), "identb"], ["pb0"])
            K.cp("dve", xT[:, :, :], v3(PB0b[:, :], 8), ["pb0"], ["xT"])
            for m in range(4):
                p1, p3 = 1 + m % 2, 4 + m % 2
                for c in range(8):
                    K.mm(PB[p1][:, 0:128], w1e[:, c, m * 128:(m + 1) * 128], xT[:, c, :], c == 0, c == 7, [("W", wb), "xT"], [f"pb{p1}"])
                for c in range(8):
                    K.mm(PB[p3][:, 0:128], w3e[:, c, m * 128:(m + 1) * 128], xT[:, c, :], c == 0, c == 7, [("W", wb), "xT"], [f"pb{p3}"])
                K.act(sil[m % 2][:, 0:128], PB[p1][:, 0:128], AF.Silu, [f"pb{p1}"], [("sil", m % 2)])
                K.tt("dve", hT[:, m, 0:128], PB[p3][:, 0:128], sil[m % 2][:, 0:128], ALU.mult, [f"pb{p3}", ("sil", m % 2)], ["hTe"])
            for half in range(2):
                py = 6 + half
                for kc in range(4):
                    K.mm(PB[py][:, :], hT[:, kc, 0:128], w2e[:, kc, half * 512:(half + 1) * 512], kc == 0, kc == 3,
                         ["hTe", ("W", wb)], [f"pb{py}"])
                K.cp("act", ysb[wb][:, half * 512:(half + 1) * 512], PB[py][:, :], [f"pb{py}"], [("ysb", wb)])
            K.dma("sp", ys_d[bk * 128:(bk + 1) * 128, :], ysb[wb][:, :], [("ysb", wb)], [])
        S.barrier()
        for t in range(NT):
            b = t % 2
            wh = 0 if t < NT_OWN else 1
            rows = slice(t * 128, (t + 1) * 128)
            K.dma("sp", x1t[b][:, :], x1_d[rows, :], [], [("x1", b)])
            for k in range(2):
                K.S.dmafn("pool", lambda e, o=ysb[k][:, :], ix=dsti[:, 2 * t + k:2 * t + k + 1]: e.indirect_dma_start(
                    out=o, out_offset=None, in_=ys_d[:, :], in_offset=bass.IndirectOffsetOnAxis(ap=ix, axis=0)),
                    ["dsti"], [("ysb", k)])
            K.ts("dve", tmpf[:, :], ysb[0][:, :], wts[:, t, 0:1], None, ALU.mult, None, [("ysb", 0), "wts"], ["tmpf"])
            K.stt("dve", tmpf[:, :], ysb[1][:, :], wts[:, t, 1:2], tmpf[:, :], ALU.mult, ALU.add, [("ysb", 1), "wts", "tmpf"], ["tmpf"])
            K.tt("pool", tmpf[:, :], tmpf[:, :], G[2 + wh][:, :], ALU.mult, ["tmpf", ("G", 2 + wh)], ["tmpf"])
            K.tt("pool", h2n[:, :], tmpf[:, :], x1t[b][:, :], ALU.add, ["tmpf", ("x1", b)], ["h2n"])
            K.dma("sp", xo_d[rows, :], h2n[:, :], ["h2n"], [])
        S.emit()


def build_btab(rpb, NT_OWN, rank):
    rows_total = 8 * NT_OWN
    specials = _specials(NT_OWN)
    gts = [rows_total // 4] + [rank * NT_OWN + t for t in specials]
    qc = np.arange(64)
    kc = np.arange(64)
    c0 = np.clip(qc - 8, 0, 48)
    colok = (kc[:, None] >= c0[None, :]) & (kc[:, None] < c0[None, :] + 16)
    dc = np.clip(kc[:, None] - qc[None, :], -15, 15) + 15
    tab = np.full((len(gts), 2, 64, 6, 16, 64), NEG, np.float32)
    for ci, gt in enumerate(gts):
        for e in range(16):
            b = (e + 1) % 2
            j = (7 + b - e) // 2
            qrow = 2 * gt + b
            r0 = min(max(qrow - 4, 0), rows_total - 8)
            for a in range(2):
                krow = 2 * (gt + j) + a
                if krow < 0 or krow >= rows_total or krow < r0 or krow >= r0 + 8:
                    continue
                dr = krow - qrow + 7
                vals = rpb[:, dr, :][:, dc]
                vals = np.where(colok[None], vals, np.float32(NEG))
                tab[ci, a, :, :, e, :] = vals.transpose(1, 0, 2)
    return tab.reshape(len(gts), 128, 6, 1024).astype(NPBF)


def prep_B(P, i, x_cur, xc_cur, NT_OWN, outsA, ranks_per_batch=4, NE=64):
    B = x_cur.shape[0]
    n = NT_OWN * 128
    NTB = ranks_per_batch * NT_OWN
    shared = {
        "w_out": _kmaj(P["w_out"][i], 8),
        "norm_ffnT": _colT(P["norm_ffn"][i]),
        "wgr": _kmaj(np.concatenate([P["moe_w_group"][i], P["moe_w_router"][i]], axis=1), 8),
        "w1": np.ascontiguousarray(P["moe_w1"][i][:NE].reshape(NE, 8, 128, 512).transpose(0, 2, 1, 3)),
        "w3": np.ascontiguousarray(P["moe_w3"][i][:NE].reshape(NE, 8, 128, 512).transpose(0, 2, 1, 3)),
        "w2": np.ascontiguousarray(P["moe_w2"][i][:NE].reshape(NE, 4, 128, 1024).transpose(0, 2, 1, 3)),
        "ident": np.eye(128, dtype=np.float32),
    }
    maps = []
    for b in range(B):
        cores = [outsA[b * ranks_per_batch + r] for r in range(ranks_per_batch)]
        kv_tiles = [c["kvb"][:n].reshape(NT_OWN, 128, 768) for c in cores]
        kv_all = np.concatenate(kv_tiles, axis=0)
        for r in range(ranks_per_batch):
            me = cores[r]
            m = dict(shared)
            m["x"] = np.ascontiguousarray(np.concatenate([x_cur[b, r * n:(r + 1) * n], xc_cur[b]], axis=0))
            m["oa"] = me["oa"]
            m["qbT"] = me["qbT"]
            m["qcT"] = me["qcT"]
            m["modT"] = me["modT"]
            win = np.zeros((NT_OWN + 8, 128, 768), NPBF)
            for s in range(NT_OWN + 6):
                g = r * NT_OWN - 3 + s
                if 0 <= g < NTB:
                    win[s] = kv_all[g]
            win[NT_OWN + 6:] = me["kvb"][n:].reshape(2, 128, 768)
            m["kvbw"] = win.reshape(-1, 768)
            m["kcT_all"] = np.ascontiguousarray(np.concatenate([c["kcT"][:, :, :n] for c in cores] + [me["kcT"][:, :, n:]], axis=2))
            m["vc_all"] = np.ascontiguousarray(np.concatenate([c["vc"][:n] for c in cores] + [me["vc"][n:]], axis=0))
            m["btab"] = build_btab(P["b_rpb"][i], NT_OWN, r)
            maps.append(m)
    return maps


def declare_dram(nc, NT_OWN, NR=4, NE=64):
    NT = NT_OWN + 2
    T = NT * 128
    n = NT_OWN * 128
    NCLS = 1 + len(_specials(NT_OWN))
    d = {}

    def t(name, shape, dt, kind="Internal"):
        d[name] = nc.dram_tensor(name, list(shape), dt, kind=kind).ap()

    EI = "ExternalInput"
    t("x", [T, D], F32, EI)
    t("cT", [128, 16], F32, EI)
    t("rope", [T, 32], F32, EI)
    t("ident", [128, 128], F32, EI)
    t("widx", [128, 6], I32, EI)
    t("ut", [128, 128], F32, EI)
    t("iop", [128, 1], F32, EI)
    t("xo", [T, D], F32, "ExternalOutput")
    t("xs_0", [T, D], F32)
    for L in range(2):
        t(f"w_ada_{L}", [128, 8, 6144], F32, EI)
        t(f"b_adaT_{L}", [128, 48], F32, EI)
        t(f"norm_mixT_{L}", [128, 8], F32, EI)
        t(f"w_in_{L}", [128, 8, INW], F32, EI)
        t(f"w_sT_{L}", [128, 4, 128], F32, EI)
        t(f"b_s_{L}", [128, 4], F32, EI)
        t(f"gains_{L}", [128, G_TOT], F32, EI)
        t(f"wq_{L}", [128, 3, 576], F32, EI)
        t(f"wkv_{L}", [128, 2, 768], F32, EI)
        t(f"btab_{L}", [NCLS, 128, 6, 1024], BF16, EI)
        t(f"w_out_{L}", [128, 8, D], F32, EI)
        t(f"norm_ffnT_{L}", [128, 8], F32, EI)
        t(f"wgr_{L}", [128, 8, 72], F32, EI)
        t(f"w1_{L}", [NE, 128, 8, 512], F32, EI)
        t(f"w3_{L}", [NE, 128, 8, 512], F32, EI)
        t(f"w2_{L}", [NE, 128, 4, D], F32, EI)
        t(f"oa_{L}", [T, 256], BF16)
        t(f"qbT_{L}", [3, 128, T], BF16)
        t(f"qcT_{L}", [6, 96, T], BF16)
        t(f"modT_{L}", [128, 96], F32)
        t(f"mixd_{L}", [T, D], BF16)
        PT = min(4, NT_OWN)
        for j in range(NT_OWN // PT):
            t(f"kcT_own_{L}_{j}", [576, PT * 128], BF16)
            t(f"kcT_g_{L}_{j}", [NR * 576, PT * 128], BF16)
            t(f"vc_own_{L}_{j}", [PT * 128, 390], BF16)
            t(f"vc_g_{L}_{j}", [NR * PT * 128, 390], BF16)
        t(f"kcT_ctx_{L}", [576, 256], BF16)
        t(f"vc_ctx_{L}", [256, 390], BF16)
        t(f"kvb_own_{L}", [n, 768], BF16)
        t(f"kvb_ctx_{L}", [256, 768], BF16)
        HT = min(3, NT_OWN)
        t(f"kvb_lo_{L}", [HT * 128, 768], BF16)
        t(f"kvb_hi_{L}", [HT * 128, 768], BF16)
        t(f"kvb_glo_{L}", [NR * HT * 128, 768], BF16)
        t(f"kvb_ghi_{L}", [NR * HT * 128, 768], BF16)
        t(f"h2_{L}", [T, D], BF16)
        t(f"x1_{L}", [T, D], F32)
        t(f"xsl_{L}", [((2 * T) // 128 + 64) * 128, D], BF16)
        t(f"ys_{L}", [((2 * T) // 128 + 64) * 128, D], F32)
    return d


def emit_X(nc, gc, d, L, groups):
    stack = contextlib.ExitStack()
    with stack:
        K = KB(nc, stack, gc)
        pairs = [(d[f"kvb_lo_{L}"], d[f"kvb_glo_{L}"]), (d[f"kvb_hi_{L}"], d[f"kvb_ghi_{L}"])]
        j = 0
        while f"vc_own_{L}_{j}" in d:
            pairs.append((d[f"vc_own_{L}_{j}"], d[f"vc_g_{L}_{j}"]))
            pairs.append((d[f"kcT_own_{L}_{j}"], d[f"kcT_g_{L}_{j}"]))
            j += 1
        for nm, (src, dst) in enumerate(pairs):
            K.S.cc(lambda e, src=src, dst=dst: e.collective_compute(
                "AllGather", ALU.bypass, replica_groups=groups, ins=[src[:, :]], outs=[dst[:, :]]), [], [nm])
        K.S.emit()


def build_fused(NT_OWN, ngroups=2, NR=4, TB=4, NE=64, do_x=True):
    nc = bass.Bass("TRN2", target_bir_lowering=False)
    gstack = contextlib.ExitStack()
    with gstack:
        gc = GC(gstack)
        d = declare_dram(nc, NT_OWN, NR, NE)
        groups = [list(range(g * NR, (g + 1) * NR)) for g in range(ngroups)]
        for L in range(2):
            emit_A(nc, gc, d, NT_OWN, L)
            if do_x:
                emit_X(nc, gc, d, L, groups)
            emit_B(nc, gc, d, NT_OWN, L, TB=TB, NR=NR, NE=NE)
    return nc


def halo_index(NT_OWN, r, NR):
    HT = min(3, NT_OWN)
    idx = np.zeros((128, 6), np.int32)
    for col in range(6):
        g = r * NT_OWN - 3 + col if col < 3 else (r + 1) * NT_OWN + (col - 3)
        if not (0 <= g < NR * NT_OWN):
            continue
        rk, l = g // NT_OWN, g % NT_OWN
        if col < 3:
            t2 = l - (NT_OWN - HT)
        else:
            t2 = l
        if not (0 <= t2 < HT):
            continue
        idx[:, col] = (rk * HT + t2) * 128 + np.arange(128)
    return idx


def prep_fused(P, NT_OWN, NR=4):
    x = np.asarray(P["x"], np.float32)
    ctx = np.asarray(P["ctx"], np.float32)
    B = x.shape[0]
    n = NT_OWN * 128
    shared = {"ident": np.eye(128, dtype=np.float32),
              "ut": np.triu(np.ones((128, 128), np.float32), 1),
              "iop": np.arange(128, dtype=np.float32).reshape(128, 1)}
    per_rank = [dict() for _ in range(NR)]
    for L in range(2):
        gains = np.concatenate([
            P["a_v_norm"][L], np.tile(P["b_q_norm"][L], 6), np.tile(P["b_k_norm"][L], 6), P["c_q_a_norm"][L],
            P["c_kv_a_norm"][L], np.tile(P["c_q_norm"][L], 6), np.tile(P["c_k_norm"][L][:64], 6), P["c_k_norm"][L][64:]])
        shared.update({
            f"w_ada_{L}": _kmaj(P["w_ada"][L], 8),
            f"b_adaT_{L}": _colT(P["b_ada"][L]),
            f"norm_mixT_{L}": _colT(P["norm_mix"][L]),
            f"w_in_{L}": _kmaj(P["w_in"][L], 8),
            f"w_sT_{L}": np.ascontiguousarray(P["a_w_s"][L].transpose(2, 0, 1)),
            f"b_s_{L}": np.ascontiguousarray(P["a_b_s"][L].T),
            f"gains_{L}": _bcrow(gains.astype(np.float32)),
            f"wq_{L}": _kmaj(P["c_w_q_up"][L], 3),
            f"wkv_{L}": _kmaj(P["c_w_kv_up"][L], 2),
            f"w_out_{L}": _kmaj(P["w_out"][L], 8),
            f"norm_ffnT_{L}": _colT(P["norm_ffn"][L]),
            f"wgr_{L}": _kmaj(np.concatenate([P["moe_w_group"][L], P["moe_w_router"][L]], axis=1), 8),
            f"w1_{L}": np.ascontiguousarray(P["moe_w1"][L].reshape(64, 8, 128, 512).transpose(0, 2, 1, 3)),
            f"w3_{L}": np.ascontiguousarray(P["moe_w3"][L].reshape(64, 8, 128, 512).transpose(0, 2, 1, 3)),
            f"w2_{L}": np.ascontiguousarray(P["moe_w2"][L].reshape(64, 4, 128, 1024).transpose(0, 2, 1, 3)),
        })
        for r in range(NR):
            per_rank[r][f"btab_{L}"] = build_btab(P["b_rpb"][L], NT_OWN, r)
    for r in range(NR):
        per_rank[r]["rope"] = rope_tables(NT_OWN, r)
        per_rank[r]["widx"] = halo_index(NT_OWN, r, NR)
    maps = []
    for b in range(B):
        cT = np.ascontiguousarray(np.stack([_colT(P["c"][b]), _colT(P["c_ctx"])], axis=2).reshape(128, 16))
        for r in range(NR):
            m = dict(shared)
            m.update(per_rank[r])
            m["x"] = np.ascontiguousarray(np.concatenate([x[b, r * n:(r + 1) * n], ctx[b]], axis=0))
            m["cT"] = cT
            maps.append(m)
    return maps


_PROG = {}


def kernel(**inputs):
    P = {k: np.asarray(v) for k, v in inputs.items()}
    NT_OWN = 32
    n = NT_OWN * 128
    if "fused" not in _PROG:
        _PROG["fused"] = build_fused(NT_OWN)
    maps = prep_fused(P, NT_OWN)
    res = run_bass_kernel_spmd(_PROG["fused"], maps, core_ids=list(range(8))).results
    out = np.empty((2, 4 * n, D), np.float32)
    for b in range(2):
        for r in range(4):
            out[b, r * n:(r + 1) * n] = res[b * 4 + r]["xo"][:n]
    return out
```

```python
import contextlib
import numpy as np
import ml_dtypes
import concourse.bass as bass
import concourse.mybir as mybir
from concourse.bass_utils import run_bass_kernel_spmd

F32 = mybir.dt.float32
BF16 = mybir.dt.bfloat16
I32 = mybir.dt.int32
AF = mybir.ActivationFunctionType
ALU = mybir.AluOpType
AX = mybir.AxisListType
NPBF = ml_dtypes.bfloat16

D = 1024
GRID_W = 64
CTX = 256
EPS = 1e-6
INW = 2336
NEG = -30000.0


class _Op:
    __slots__ = ("eng", "fn", "deps", "needed", "semkey", "val", "dma", "idx")

    def __init__(self, eng, fn, dma):
        self.eng = eng
        self.fn = fn
        self.deps = []
        self.needed = False
        self.semkey = None
        self.val = 0
        self.dma = dma


class Sched:
    ENGS = ("pe", "act", "dve", "pool", "sp")
    NLANES = 6

    def __init__(self, nc, gc):
        self.nc = nc
        self.gc = gc
        self.ops = {e: [] for e in self.ENGS}
        self.bufs = {}
        self.phase = 0
        self.lane_ops = {}
        self.lane_n = {e: 0 for e in self.ENGS}
        self.pending = {e: [] for e in self.ENGS}
        self.last = {e: None for e in self.ENGS}

    def _add(self, eng, fn, r, w, dma):
        op = _Op(eng, fn, dma)
        deps = []
        for k in r:
            st = self.bufs.setdefault(k, [None, []])
            if st[0] is not None:
                deps.append(st[0])
        for k in w:
            st = self.bufs.setdefault(k, [None, []])
            if st[0] is not None:
                deps.append(st[0])
            deps.extend(st[1])
        deps.extend(self.pending[eng])
        self.pending[eng] = []
        if dma:
            lane = self.lane_n[eng] % self.NLANES
            self.lane_n[eng] += 1
            key = ("lane", eng, lane)
            prev = self.lane_ops.get(key)
            if prev is not None:
                deps.append(prev)
            self.lane_ops[key] = op
            op.semkey = key
            op.val = (prev.val if prev is not None else self.gc.lane_vals.get(key, 0)) + 16
            op.needed = True
        else:
            op.semkey = ("eng", eng, self.phase)
        seen = set()
        for d in deps:
            if d is op or id(d) in seen:
                continue
            seen.add(id(d))
            if (not d.dma) and d.eng == eng and eng == "pe":
                continue
            d.needed = True
            op.deps.append(d)
        for k in r:
            self.bufs[k][1].append(op)
        for k in w:
            self.bufs[k] = [op, []]
        self.ops[eng].append(op)
        self.last[eng] = op
        return op

    def op(self, eng, fn, r=(), w=()):
        return self._add(eng, fn, r, w, False)

    def dma(self, eng, out, in_, r=(), w=()):
        return self._add(eng, lambda e: e.dma_start(out=out, in_=in_), r, w, True)

    def dmafn(self, eng, fn, r=(), w=()):
        return self._add(eng, fn, r, w, True)

    def cc(self, fn, r=(), w=()):
        op = self._add("pool", fn, r, w, False)
        op.semkey = ("cc", self.gc.next_uid())
        op.needed = True
        op.val = 1
        op.dma = True
        return op

    def barrier(self):
        lasts = [o for o in self.last.values() if o is not None] + list(self.lane_ops.values())
        for e in self.ENGS:
            self.pending[e] = list(lasts)
        self.bufs = {}
        self.phase += 1

    def emit(self):
        nc = self.nc
        gc = self.gc
        for e in self.ENGS:
            if self.ops[e] and not self.ops[e][-1].dma:
                self.ops[e][-1].needed = True
        cnt = {}
        for e in self.ENGS:
            for op in self.ops[e]:
                if not op.dma and op.needed:
                    cnt[op.semkey] = cnt.get(op.semkey, 0) + 1
                    op.val = cnt[op.semkey]
        sems = {}
        finals = {}
        for e in self.ENGS:
            for op in self.ops[e]:
                if not op.needed:
                    continue
                k = op.semkey
                if k not in sems:
                    if k[0] == "lane":
                        if k not in gc.lane_sems:
                            gc.lane_sems[k] = gc.stack.enter_context(nc.semaphore("l_" + "_".join(str(x) for x in k[1:])))
                        sems[k] = gc.lane_sems[k]
                    else:
                        sems[k] = gc.stack.enter_context(nc.semaphore(f"s{gc.next_uid()}_" + "_".join(str(x) for x in k)))
                finals[k] = max(finals.get(k, 0), op.val)
        for k, v in finals.items():
            if k[0] == "lane":
                gc.lane_vals[k] = v

        def run(engname, e):
            waited = {}
            for op in self.ops[engname]:
                for d in op.deps:
                    if waited.get(d.semkey, 0) >= d.val:
                        continue
                    e.wait_ge(sems[d.semkey], d.val)
                    waited[d.semkey] = d.val
                ins = op.fn(e)
                if op.needed:
                    if op.semkey[0] == "cc":
                        ins.then_inc(sems[op.semkey])
                    else:
                        ins.then_inc(sems[op.semkey], 16 if op.dma else 1)
            for k, v in finals.items():
                if waited.get(k, 0) < v:
                    e.wait_ge(sems[k], v)

        with nc.Block() as block:
            @block.tensor
            def _(e):
                run("pe", e)

            @block.scalar
            def _(e):
                run("act", e)

            @block.vector
            def _(e):
                run("dve", e)

            @block.gpsimd
            def _(e):
                run("pool", e)

            @block.sync
            def _(e):
                run("sp", e)


class GC:
    def __init__(self, stack):
        self.stack = stack
        self.lane_sems = {}
        self.lane_vals = {}
        self.uid = 0

    def next_uid(self):
        self.uid += 1
        return self.uid


class KB:
    def __init__(self, nc, stack, gc):
        self.nc = nc
        self.stack = stack
        self.gc = gc
        self.S = Sched(nc, gc)
        self.tag = f"_u{gc.next_uid()}"

    def sb(self, name, shape, dt):
        return self.stack.enter_context(self.nc.sbuf_tensor(name + self.tag, list(shape), dt))

    def ps(self, name, shape, dt):
        return self.stack.enter_context(self.nc.psum_tensor(name + self.tag, list(shape), dt))

    def dram(self, name, shape, dt, kind):
        return self.nc.dram_tensor(name, list(shape), dt, kind=kind).ap()

    def mm(self, out, lhsT, rhs, start, stop, r, w):
        self.S.op("pe", lambda e: e.matmul(out, lhsT, rhs, start=start, stop=stop), r, w)

    def tr(self, out, in_, ident, r, w):
        self.S.op("pe", lambda e: e.transpose(out, in_, ident), r, w)

    def act(self, out, in_, func, r, w, **kw):
        self.S.op("act", lambda e: e.activation(out, in_, func, **kw), r, w)

    def ts(self, eng, out, in0, s1, s2, op0, op1, r, w):
        if op1 is None:
            self.S.op(eng, lambda e: e.tensor_scalar(out, in0, s1, None, op0), r, w)
        else:
            self.S.op(eng, lambda e: e.tensor_scalar(out, in0, s1, s2, op0, op1), r, w)

    def tt(self, eng, out, in0, in1, op, r, w):
        self.S.op(eng, lambda e: e.tensor_tensor(out, in0, in1, op), r, w)

    def stt(self, eng, out, in0, scalar, in1, op0, op1, r, w):
        self.S.op(eng, lambda e: e.scalar_tensor_tensor(out, in0, scalar, in1, op0, op1), r, w)

    def cp(self, eng, out, in_, r, w):
        if eng == "act":
            self.S.op("act", lambda e: e.copy(out, in_), r, w)
        else:
            self.S.op(eng, lambda e: e.tensor_copy(out, in_), r, w)

    def rsum(self, eng, out, in_, r, w):
        self.S.op(eng, lambda e: e.reduce_sum(out, in_, AX.X), r, w)

    def memset(self, eng, ap, v, r, w):
        self.S.op(eng, lambda e: e.memset(ap, v), r, w)

    def dma(self, eng, out, in_, r, w):
        self.S.dma(eng, out, in_, r, w)


G_AV, G_BQ, G_BK, G_CQA, G_CKVA, G_CQN, G_CKN, G_CKR, G_TOT = 0, 256, 640, 1024, 1408, 1664, 2240, 2624, 2656


def v3(ap, g):
    return ap.rearrange("p (g d) -> p g d", g=g)


def bc3(ap2, d):
    p, g = ap2.shape
    return ap2.unsqueeze(2).to_broadcast([p, g, d])


def emit_A(nc, gc, d, NT_OWN, L):
    NT = NT_OWN + 2
    T = NT * 128
    n = NT_OWN * 128
    stack = contextlib.ExitStack()
    with stack:
        K = KB(nc, stack, gc)
        S = K.S
        x_d = d["x"] if L == 0 else d["xs_0"]
        cT_d, rope_d, ident_d = d["cT"], d["rope"], d["ident"]
        wada_d, bada_d, nmix_d, win_d = d[f"w_ada_{L}"], d[f"b_adaT_{L}"], d[f"norm_mixT_{L}"], d[f"w_in_{L}"]
        wsT_d, bs_d, gains_d, wq_d, wkv_d = d[f"w_sT_{L}"], d[f"b_s_{L}"], d[f"gains_{L}"], d[f"wq_{L}"], d[f"wkv_{L}"]
        oa_d, qbT_d, qcT_d, modT_d = d[f"oa_{L}"], d[f"qbT_{L}"], d[f"qcT_{L}"], d[f"modT_{L}"]
        PT = min(4, NT_OWN)
        HT = min(3, NT_OWN)
        kcT_ctx, vc_ctx = d[f"kcT_ctx_{L}"], d[f"vc_ctx_{L}"]
        kvb_own, kvb_ctx, kvb_lo, kvb_hi = d[f"kvb_own_{L}"], d[f"kvb_ctx_{L}"], d[f"kvb_lo_{L}"], d[f"kvb_hi_{L}"]
        ident = K.sb("ident_b", [128, 128], BF16)
        identf = K.sb("identf", [128, 128], F32)
        cT = K.sb("cTs", [128, 16], F32)
        scT = K.sb("scT", [128, 16], F32)
        bada = K.sb("bada", [128, 48], F32)
        nmix = K.sb("nmix", [128, 8], F32)
        modT = K.sb("modTs", [128, 96], F32)
        A1 = K.sb("A1", [128, 16], F32)
        wst = [K.sb(f"wst{i}", [128, 8, 512], F32) for i in range(2)]
        win = K.sb("win", [128, 8, INW], BF16)
        wsT = K.sb("wsT", [128, 4, 128], BF16)
        bs = K.sb("bs", [128, 4], F32)
        gains = K.sb("gainss", [128, G_TOT], F32)
        wq = K.sb("wqs", [128, 3, 576], BF16)
        wkv = K.sb("wkvs", [128, 2, 768], BF16)
        xt = [K.sb(f"xt{i}", [128, D], F32) for i in range(2)]
        ropet = [K.sb(f"ropet{i}", [128, 32], F32) for i in range(2)]
        sqj = K.sb("sqj", [128, D], F32)
        st = K.sb("st", [128, 64], F32)
        xn = K.sb("xn", [128, D], BF16)
        hT = K.sb("hT", [128, 8, 128], BF16)
        z = K.sb("z", [128, INW], F32)
        g1 = K.sb("g1", [128, 512], F32)
        g2 = K.sb("g2", [128, 512], F32)
        gg = K.sb("gg", [128, 512], F32)
        vnb = K.sb("vnb", [128, 256], BF16)
        oa = K.sb("oas", [128, 256], BF16)
        t384 = K.sb("t384", [128, 384], F32)
        u384 = K.sb("u384", [128, 384], F32)
        qnb = K.sb("qnb", [128, 384], BF16)
        qbTs = K.sb("qbTs", [128, 3, 128], BF16)
        kvbs = K.sb("kvbs", [128, 768], BF16)
        qab = K.sb("qab", [128, 384], BF16)
        qaT = K.sb("qaT", [128, 3, 128], BF16)
        qf = K.sb("qf", [128, 576], F32)
        qs = K.sb("qs", [128, 576], F32)
        qc = K.sb("qc", [128, 6, 96], BF16)
        rt = [K.sb(f"rt{i}", [128, 48], F32) for i in range(4)]
        qcTs = K.sb("qcTs", [96, 6, 128], BF16)
        kvab = K.sb("kvab", [128, 256], BF16)
        kvaT = K.sb("kvaT", [128, 2, 128], BF16)
        kvf = K.sb("kvf", [128, 768], F32)
        kc = K.sb("kc", [128, 6, 96], BF16)
        kr = K.sb("kr", [128, 32], F32)
        krr = K.sb("krr", [128, 32], F32)
        vcs = K.sb("vcs", [128, 6, 65], BF16)
        kcTs = K.sb("kcTs", [96, 6, 128], BF16)
        PB = [K.ps(f"pb{i}", [128, 512], F32) for i in range(8)]
        PB0b = PB[0].bitcast(BF16)

        K.dma("sp", identf[:, :], ident_d[:, :], [], ["identf"])
        K.cp("dve", ident[:, :], identf[:, :], ["identf"], ["ident"])
        K.dma("sp", cT[:, :], cT_d[:, :], [], ["cT"])
        K.dma("sp", bada[:, :], bada_d[:, :], [], ["bada"])
        K.dma("sp", nmix[:, :], nmix_d[:, :], [], ["nmix"])
        K.dma("sp", bs[:, :], bs_d[:, :], [], ["bs"])
        K.dma("sp", gains[:, :], gains_d[:, :], [], ["gains"])
        K.dma("pool", win[:, :, :], win_d[:, :, :], [], ["win"])
        K.dma("pool", wsT[:, :, :], wsT_d[:, :, :], [], ["wsT"])
        K.dma("pool", wq[:, :, :], wq_d[:, :, :], [], ["wq"])
        K.dma("pool", wkv[:, :, :], wkv_d[:, :, :], [], ["wkv"])
        K.memset("pool", vcs[:, :, :], 1.0, [], ["vcs"])
        K.act(scT[:, :], cT[:, :], AF.Silu, ["cT"], ["scT"])
        for grp in range(12):
            b = grp % 2
            K.dma("sp", wst[b][:, :, :], wada_d[:, :, grp * 512:(grp + 1) * 512], [], [("wst", b)])
            for jj in range(4):
                j = grp * 4 + jj
                for c in range(8):
                    K.mm(PB[1][:, 2 * j:2 * j + 2], wst[b][:, c, jj * 128:(jj + 1) * 128], scT[:, 2 * c:2 * c + 2],
                         c == 0, c == 7, [("wst", b), "scT"], ["pb1"])
        K.tt("dve", v3(modT[:, :], 48), v3(PB[1][:, 0:96], 48), bc3(bada[:, :], 2), ALU.add, ["pb1", "bada"], ["modT"])
        K.dma("sp", modT_d[:, :], modT[:, :], ["modT"], [])
        K.stt("dve", v3(A1[:, :], 8), v3(modT[:, 16:32], 8), 1.0, bc3(nmix[:, :], 2), ALU.add, ALU.mult,
              ["modT", "nmix"], ["A1"])

        def rstd_of(ss, n, dim, extra=None):
            K.ts("dve", ss, ss, 1.0 / dim, EPS, ALU.mult, ALU.add, ["st"], ["st"])
            K.act(ss, ss, AF.Sqrt, ["st"], ["st"])
            K.S.op("dve", lambda e, a=ss: e.reciprocal(a, a), ["st"], ["st"])
            if extra is not None:
                K.ts("dve", ss, ss, extra, None, ALU.mult, None, ["st"], ["st"])

        def rope(src3, dst3, G, rp, rkeys, wkeys):
            for a in range(2):
                o = 16 * a
                cos = rp[:, 16 * a:16 * a + 8].unsqueeze(1).to_broadcast([128, G, 8])
                sin = rp[:, 16 * a + 8:16 * a + 16].unsqueeze(1).to_broadcast([128, G, 8])
                x1 = src3[:, :, o:o + 8]
                x2 = src3[:, :, o + 8:o + 16]
                t = [v3(rt[i][:, 0:G * 8], G) for i in range(4)]
                K.tt("pool", t[0], x1, cos, ALU.mult, rkeys, ["rt0"])
                K.tt("pool", t[1], x2, sin, ALU.mult, rkeys, ["rt1"])
                K.tt("dve", dst3[:, :, o:o + 8], t[0], t[1], ALU.subtract, ["rt0", "rt1"], wkeys)
                K.tt("pool", t[2], x2, cos, ALU.mult, rkeys, ["rt2"])
                K.tt("pool", t[3], x1, sin, ALU.mult, rkeys, ["rt3"])
                K.tt("dve", dst3[:, :, o + 8:o + 16], t[2], t[3], ALU.add, ["rt2", "rt3"], wkeys)

        for t in range(NT):
            b = t % 2
            wh = 0 if t < NT_OWN else 1
            rows = slice(t * 128, (t + 1) * 128)
            X = xt[b]
            K.dma("sp", X[:, :], x_d[rows, :], [], [("xt", b)])
            K.dma("sp", ropet[b][:, :], rope_d[rows, :], [], [("rope", b)])
            K.tt("dve", sqj[:, :], X[:, :], X[:, :], ALU.mult, [("xt", b)], ["sqj"])
            K.rsum("dve", st[:, 0:1], sqj[:, :], ["sqj"], ["st"])
            rstd_of(st[:, 0:1], 1, D)
            K.act(xn[:, :], X[:, :], AF.Copy, [("xt", b), "st"], ["xn"], scale=st[:, 0:1])
            for c in range(8):
                K.tr(PB0b[:, c * 128:(c + 1) * 128], xn[:, c * 128:(c + 1) * 128], ident[:, :], ["xn", "ident"], ["pb0"])
            for c in range(8):
                K.ts("dve", hT[:, c, :], PB0b[:, c * 128:(c + 1) * 128], A1[:, 2 * c + wh:2 * c + wh + 1],
                     modT[:, 2 * c + wh:2 * c + wh + 1], ALU.mult, ALU.add, ["pb0", "A1", "modT"], ["hT"])
            for k5 in range(5):
                n0 = k5 * 512
                n1 = min(INW, n0 + 512)
                pb = 1 + k5 % 3
                for c in range(8):
                    K.mm(PB[pb][:, 0:n1 - n0], hT[:, c, :], win[:, c, n0:n1], c == 0, c == 7, ["hT", "win"], [f"pb{pb}"])
                K.cp("act", z[:, n0:n1], PB[pb][:, 0:n1 - n0], [f"pb{pb}"], ["z"])
            za = z[:, 0:512]
            K.tt("pool", g1[:, :], za, za, ALU.mult, ["z"], ["g1"])
            K.ts("dve", g1[:, :], g1[:, :], 0.044715, 1.0, ALU.mult, ALU.add, ["g1"], ["g1"])
            K.tt("pool", g1[:, :], g1[:, :], za, ALU.mult, ["g1", "z"], ["g1"])
            K.act(g2[:, :], g1[:, :], AF.Sigmoid, ["g1"], ["g2"], scale=1.5957691216057308)
            K.tt("dve", gg[:, :], g2[:, :], za, ALU.mult, ["g2", "z"], ["gg"])
            K.tt("pool", g1[:, 0:256], gg[:, 256:512], gg[:, 256:512], ALU.mult, ["gg"], ["g1"])
            K.rsum("dve", st[:, 0:1], g1[:, 0:256], ["g1"], ["st"])
            rstd_of(st[:, 0:1], 1, 256)
            K.ts("dve", g1[:, 256:512], gg[:, 256:512], st[:, 0:1], None, ALU.mult, None, ["gg", "st", "g1"], ["g1"])
            K.tt("pool", vnb[:, :], g1[:, 256:512], gains[:, G_AV:G_AV + 256], ALU.mult, ["g1", "gains"], ["vnb"])
            for hd in range(4):
                K.mm(PB[4][:, hd * 64:(hd + 1) * 64], wsT[:, hd, :], vnb[:, hd * 64:(hd + 1) * 64], True, True,
                     ["wsT", "vnb"], ["pb4"])
            for hd in range(4):
                K.stt("dve", oa[:, hd * 64:(hd + 1) * 64], PB[4][:, hd * 64:(hd + 1) * 64], bs[:, hd:hd + 1],
                      gg[:, hd * 64:(hd + 1) * 64], ALU.add, ALU.mult, ["pb4", "bs", "gg"], ["oa"])
            K.dma("sp", oa_d[rows, :], oa[:, :], ["oa"], [])
            for which, (o0, gofs, extra) in enumerate(((512, G_BQ, 0.125), (896, G_BK, None))):
                src = z[:, o0:o0 + 384]
                K.tt("pool", t384[:, :], src, src, ALU.mult, ["z"], ["t384"])
                K.rsum("dve", st[:, 0:6], v3(t384[:, :], 6), ["t384"], ["st"])
                rstd_of(st[:, 0:6], 6, 64, extra)
                K.tt("dve", v3(u384[:, :], 6), v3(src, 6), bc3(st[:, 0:6], 64), ALU.mult, ["z", "st"], ["u384"])
                dst = qnb[:, :] if which == 0 else kvbs[:, 0:384]
                K.tt("pool", dst, u384[:, :], gains[:, gofs:gofs + 384], ALU.mult, ["u384", "gains"],
                     ["qnb" if which == 0 else "kvbs"])
            for pr in range(3):
                K.tr(PB0b[:, pr * 128:(pr + 1) * 128], qnb[:, pr * 128:(pr + 1) * 128], ident[:, :], ["qnb", "ident"], ["pb0"])
            K.cp("dve", qbTs[:, :, :], v3(PB0b[:, 0:384], 3), ["pb0"], ["qbTs"])
            K.dma("sp", qbT_d[:, :, rows].rearrange("a p t -> p a t"), qbTs[:, :, :], ["qbTs"], [])
            K.cp("act", kvbs[:, 384:768], z[:, 1280:1664], ["z"], ["kvbs"])
            if t < NT_OWN:
                K.dma("sp", kvb_own[rows, :], kvbs[:, :], ["kvbs"], ["kvb_x"])
                if t < HT:
                    K.dma("sp", kvb_lo[t * 128:(t + 1) * 128, :], kvbs[:, :], ["kvbs"], ["kvb_x"])
                if t >= NT_OWN - HT:
                    t2 = t - (NT_OWN - HT)
                    K.dma("sp", kvb_hi[t2 * 128:(t2 + 1) * 128, :], kvbs[:, :], ["kvbs"], ["kvb_x"])
            else:
                K.dma("sp", kvb_ctx[(t - NT_OWN) * 128:(t - NT_OWN + 1) * 128, :], kvbs[:, :], ["kvbs"], ["kvb_x"])
            src = z[:, 1664:2048]
            K.tt("pool", t384[:, :], src, src, ALU.mult, ["z"], ["t384"])
            K.rsum("dve", st[:, 0:1], t384[:, :], ["t384"], ["st"])
            rstd_of(st[:, 0:1], 1, 384)
            K.ts("dve", u384[:, :], src, st[:, 0:1], None, ALU.mult, None, ["z", "st"], ["u384"])
            K.tt("pool", qab[:, :], u384[:, :], gains[:, G_CQA:G_CQA + 384], ALU.mult, ["u384", "gains"], ["qab"])
            for c in range(3):
                K.tr(PB0b[:, c * 128:(c + 1) * 128], qab[:, c * 128:(c + 1) * 128], ident[:, :], ["qab", "ident"], ["pb0"])
            K.cp("dve", qaT[:, :, :], v3(PB0b[:, 0:384], 3), ["pb0"], ["qaT"])
            for c in range(3):
                K.mm(PB[5][:, 0:512], qaT[:, c, :], wq[:, c, 0:512], c == 0, c == 2, ["qaT", "wq"], ["pb5"])
            for c in range(3):
                K.mm(PB[7][:, 256:320], qaT[:, c, :], wq[:, c, 512:576], c == 0, c == 2, ["qaT", "wq"], ["pb7b"])
            K.cp("act", qf[:, 0:512], PB[5][:, 0:512], ["pb5"], ["qf"])
            K.cp("act", qf[:, 512:576], PB[7][:, 256:320], ["pb7b"], ["qf"])
            qf3 = v3(qf[:, :], 6)
            qs3 = v3(qs[:, :], 6)
            K.tt("pool", qs[:, :], qf[:, :], qf[:, :], ALU.mult, ["qf"], ["qs"])
            K.rsum("dve", st[:, 0:6], qs3[:, :, 0:64], ["qs"], ["st"])
            K.rsum("dve", st[:, 8:14], qs3[:, :, 64:96], ["qs"], ["st"])
            rstd_of(st[:, 0:6], 6, 64)
            rstd_of(st[:, 8:14], 6, 32)
            K.tt("dve", qs3[:, :, 0:64], qf3[:, :, 0:64], bc3(st[:, 0:6], 64), ALU.mult, ["qf", "st", "qs"], ["qs"])
            K.tt("dve", qs3[:, :, 64:96], qf3[:, :, 64:96], bc3(st[:, 8:14], 32), ALU.mult, ["qf", "st", "qs"], ["qs"])
            K.tt("pool", qf[:, :], qs[:, :], gains[:, G_CQN:G_CQN + 576], ALU.mult, ["qs", "gains"], ["qf"])
            K.cp("act", qc[:, :, 0:64], qf3[:, :, 0:64], ["qf"], ["qc"])
            rope(qf3[:, :, 64:96], qc[:, :, 64:96], 6, ropet[b], ["qf", ("rope", b)], ["qc"])
            for h in range(6):
                K.tr(PB0b[0:96, h * 128:(h + 1) * 128], qc[:, h, :], ident[:, :], ["qc", "ident"], ["pb0"])
            K.cp("dve", qcTs[:, :, :], v3(PB0b[0:96, 0:768], 6), ["pb0"], ["qcTs"])
            K.dma("sp", qcT_d[:, :, rows].rearrange("h p t -> p h t"), qcTs[:, :, :], ["qcTs"], [])
            src = z[:, 2048:2304]
            K.tt("pool", t384[:, 0:256], src, src, ALU.mult, ["z"], ["t384"])
            K.rsum("dve", st[:, 0:1], t384[:, 0:256], ["t384"], ["st"])
            rstd_of(st[:, 0:1], 1, 256)
            K.ts("dve", u384[:, 0:256], src, st[:, 0:1], None, ALU.mult, None, ["z", "st"], ["u384"])
            K.tt("pool", kvab[:, :], u384[:, 0:256], gains[:, G_CKVA:G_CKVA + 256], ALU.mult, ["u384", "gains"], ["kvab"])
            for c in range(2):
                K.tr(PB0b[:, c * 128:(c + 1) * 128], kvab[:, c * 128:(c + 1) * 128], ident[:, :], ["kvab", "ident"], ["pb0"])
            K.cp("dve", kvaT[:, :, :], v3(PB0b[:, 0:256], 2), ["pb0"], ["kvaT"])
            for c in range(2):
                K.mm(PB[6][:, 0:512], kvaT[:, c, :], wkv[:, c, 0:512], c == 0, c == 1, ["kvaT", "wkv"], ["pb6"])
            for c in range(2):
                K.mm(PB[7][:, 0:256], kvaT[:, c, :], wkv[:, c, 512:768], c == 0, c == 1, ["kvaT", "wkv"], ["pb7a"])
            K.cp("act", kvf[:, 0:512], PB[6][:, 0:512], ["pb6"], ["kvf"])
            K.cp("act", kvf[:, 512:768], PB[7][:, 0:256], ["pb7a"], ["kvf"])
            kvf3 = v3(kvf[:, :], 6)
            K.cp("act", vcs[:, :, 0:64], kvf3[:, :, 64:128], ["kvf"], ["vcs"])
            if t < NT_OWN:
                K.dma("sp", d[f"vc_own_{L}_{t // PT}"][(t % PT) * 128:(t % PT + 1) * 128, :],
                      vcs[:, :, :].rearrange("p h d -> p (h d)"), ["vcs"], ["vc_x"])
            else:
                K.dma("sp", vc_ctx[(t - NT_OWN) * 128:(t - NT_OWN + 1) * 128, :], vcs[:, :, :].rearrange("p h d -> p (h d)"), ["vcs"], ["vc_x"])
            t3 = v3(t384[:, :], 6)
            u3 = v3(u384[:, :], 6)
            K.tt("pool", t3, kvf3[:, :, 0:64], kvf3[:, :, 0:64], ALU.mult, ["kvf"], ["t384"])
            K.rsum("dve", st[:, 0:6], t3, ["t384"], ["st"])
            rstd_of(st[:, 0:6], 6, 64)
            K.tt("dve", u3, kvf3[:, :, 0:64], bc3(st[:, 0:6], 64), ALU.mult, ["kvf", "st"], ["u384"])
            K.tt("pool", kc[:, :, 0:64], u3, v3(gains[:, G_CKN:G_CKN + 384], 6), ALU.mult, ["u384", "gains"], ["kc"])
            src = z[:, 2304:2336]
            K.tt("pool", kr[:, :], src, src, ALU.mult, ["z"], ["kr"])
            K.rsum("dve", st[:, 0:1], kr[:, :], ["kr"], ["st"])
            rstd_of(st[:, 0:1], 1, 32)
            K.ts("dve", kr[:, :], src, st[:, 0:1], None, ALU.mult, None, ["z", "st", "kr"], ["kr"])
            K.tt("pool", kr[:, :], kr[:, :], gains[:, G_CKR:G_CKR + 32], ALU.mult, ["kr", "gains"], ["kr"])
            rope(v3(kr[:, :], 1), v3(krr[:, :], 1), 1, ropet[b], ["kr", ("rope", b)], ["krr"])
            K.cp("dve", kc[:, :, 64:96], krr[:, :].unsqueeze(1).to_broadcast([128, 6, 32]), ["krr"], ["kc"])
            for h in range(6):
                K.tr(PB0b[0:96, h * 128:(h + 1) * 128], kc[:, h, :], ident[:, :], ["kc", "ident"], ["pb0"])
            K.cp("dve", kcTs[:, :, :], v3(PB0b[0:96, 0:768], 6), ["pb0"], ["kcTs"])
            if t < NT_OWN:
                K.dma("sp", d[f"kcT_own_{L}_{t // PT}"].rearrange("(h p) t -> p h t", h=6)[:, :, (t % PT) * 128:(t % PT + 1) * 128],
                      kcTs[:, :, :], ["kcTs"], ["kc_x"])
            else:
                K.dma("sp", kcT_ctx.rearrange("(h p) t -> p h t", h=6)[:, :, (t - NT_OWN) * 128:(t - NT_OWN + 1) * 128],
                      kcTs[:, :, :], ["kcTs"], ["kc_x"])
        S.emit()


def _kmaj(w, kc):
    return np.ascontiguousarray(w.reshape(kc, 128, -1).transpose(1, 0, 2))


def _colT(v):
    return np.ascontiguousarray(v.reshape(-1, 128).T)


def _bcrow(v):
    return np.ascontiguousarray(np.broadcast_to(v[None, :], (128, v.shape[0])))


def rope_tables(NT_OWN, rank):
    half = 16
    inv = (np.float32(10000.0) ** (-(np.arange(0, half, 2, dtype=np.float32)) / np.float32(half))).astype(np.float32)
    pos = np.arange(NT_OWN * 128) + rank * NT_OWN * 128
    out = np.zeros(((NT_OWN + 2) * 128, 32), np.float32)
    ar = (pos // GRID_W).astype(np.float32)[:, None] * inv[None, :]
    ac = (pos % GRID_W).astype(np.float32)[:, None] * inv[None, :]
    n = NT_OWN * 128
    out[:n, 0:8] = np.cos(ar)
    out[:n, 8:16] = np.sin(ar)
    out[:n, 16:24] = np.cos(ac)
    out[:n, 24:32] = np.sin(ac)
    out[n:, 0:8] = 1.0
    out[n:, 16:24] = 1.0
    return out


def prep_A(P, i, x_cur, xc_cur, NT_OWN, ranks_per_batch=4):
    B = x_cur.shape[0]
    gains = np.concatenate([
        P["a_v_norm"][i], np.tile(P["b_q_norm"][i], 6), np.tile(P["b_k_norm"][i], 6), P["c_q_a_norm"][i],
        P["c_kv_a_norm"][i], np.tile(P["c_q_norm"][i], 6), np.tile(P["c_k_norm"][i][:64], 6), P["c_k_norm"][i][64:]])
    shared = {
        "w_ada": _kmaj(P["w_ada"][i], 8),
        "b_adaT": _colT(P["b_ada"][i]),
        "norm_mixT": _colT(P["norm_mix"][i]),
        "w_in": _kmaj(P["w_in"][i], 8),
        "w_sT": np.ascontiguousarray(P["a_w_s"][i].transpose(2, 0, 1)),
        "b_s": np.ascontiguousarray(P["a_b_s"][i].T),
        "gains": _bcrow(gains.astype(np.float32)),
        "wq": _kmaj(P["c_w_q_up"][i], 3),
        "wkv": _kmaj(P["c_w_kv_up"][i], 2),
        "ident": np.eye(128, dtype=np.float32),
    }
    maps = []
    n = NT_OWN * 128
    for b in range(B):
        cT = np.stack([_colT(P["c"][b]), _colT(P["c_ctx"])], axis=2).reshape(128, 16)
        for r in range(ranks_per_batch):
            m = dict(shared)
            m["x"] = np.ascontiguousarray(np.concatenate([x_cur[b, r * n:(r + 1) * n], xc_cur[b]], axis=0))
            m["cT"] = np.ascontiguousarray(cT)
            m["rope"] = rope_tables(NT_OWN, r)
            maps.append(m)
    return maps


def _specials(NT_OWN):
    return sorted(set(t for t in (0, 1, NT_OWN - 2, NT_OWN - 1) if 0 <= t < NT_OWN))


def emit_B(nc, gc, d, NT_OWN, L, TB=4, NE=64, NR=4):
    NT = NT_OWN + 2
    T = NT * 128
    n = NT_OWN * 128
    NW = NT_OWN + 8
    NKT = NR * NT_OWN + 2
    QB = min(4, NT_OWN)
    CH = min(4, NT_OWN)
    specials = _specials(NT_OWN)
    BIG = 1.0e30
    stack = contextlib.ExitStack()
    with stack:
        K = KB(nc, stack, gc)
        S = K.S
        x_d = d["x"] if L == 0 else d["xs_0"]
        xo_d = d["xs_0"] if L == 0 else d["xo"]
        oa_d, qbT_d, qcT_d, modT_d, mix_d = d[f"oa_{L}"], d[f"qbT_{L}"], d[f"qcT_{L}"], d[f"modT_{L}"], d[f"mixd_{L}"]
        PCT = min(4, NT_OWN)
        HT = min(3, NT_OWN)
        kcT_ctx, vc_ctx = d[f"kcT_ctx_{L}"], d[f"vc_ctx_{L}"]
        kvb_own, kvb_ctx, kvb_glo, kvb_ghi = d[f"kvb_own_{L}"], d[f"kvb_ctx_{L}"], d[f"kvb_glo_{L}"], d[f"kvb_ghi_{L}"]
        btab_d, wout_d, nffn_d, wgr_d = d[f"btab_{L}"], d[f"w_out_{L}"], d[f"norm_ffnT_{L}"], d[f"wgr_{L}"]
        w1_d, w3_d, w2_d = d[f"w1_{L}"], d[f"w3_{L}"], d[f"w2_{L}"]
        ident_d, widx_d = d["ident"], d["widx"]

        AR = 45056
        arena = K.sb("arena", [128, AR], BF16)
        identb = K.sb("ident_b", [128, 128], BF16)
        identf = K.sb("identf", [128, 128], F32)
        ones = K.sb("ones", [128, 128], F32)
        modT = K.sb("modTs", [128, 96], F32)
        nffn = K.sb("nffn", [128, 8], F32)
        A2 = K.sb("A2", [128, 16], F32)
        G = [K.sb(f"G{i}", [128, D], F32) for i in range(4)]
        gbc = K.sb("gbc", [128, 128], F32)
        wgr = K.sb("wgrs", [128, 8, 72], F32)
        kvt = [K.sb(f"kvt{i}", [128, 768], BF16) for i in range(2)]
        qbt = [K.sb(f"qbt{i}", [128, 3, 128], BF16) for i in range(2)]
        PT = [K.sb(f"PT{i}", [128, 512], BF16) for i in range(3)]
        st = K.sb("st", [128, 64], F32)
        ob = K.sb("ob", [128, 384], BF16)
        oc = K.sb("oc", [128, 4, 64], BF16)
        ocT = K.sb("ocT", [65, 512], F32)
        qT = [K.sb(f"qT{i}", [96, 512], BF16) for i in range(2)]
        xt = [K.sb(f"xt{i}", [128, D], F32) for i in range(2)]
        mixrow = K.sb("mixrow", [128, D], BF16)
        mixT = K.sb("mixT", [128, 8, 128], BF16)
        tmpf = K.sb("tmpf", [128, D], F32)
        h2n = K.sb("h2n", [128, D], F32)
        h2Tf = K.sb("h2Tf", [128, 8, 128], F32)
        lg = K.sb("lg", [128, 72], F32)
        r64 = [K.sb(f"r64_{i}", [128, 64], F32) for i in range(4)]
        sil = [K.sb(f"sil{i}", [128, 256], F32) for i in range(2)]
        hT = K.sb("hTe", [128, 4, 256], BF16)
        PB = [K.ps(f"pb{i}", [128, 512], F32) for i in range(8)]
        PB0b = PB[0].bitcast(BF16)

        K.dma("sp", identf[:, :], ident_d[:, :], [], ["identf"])
        K.cp("dve", identb[:, :], identf[:, :], ["identf"], ["identb"])
        K.memset("dve", ones[:, :], 1.0, [], ["ones"])
        K.dma("sp", modT[:, :], modT_d[:, :], [], ["modT"])
        K.dma("sp", nffn[:, :], nffn_d[:, :], [], ["nffn"])
        K.dma("sp", wgr[:, :, :], wgr_d[:, :, :], [], ["wgr"])
        K.stt("dve", v3(A2[:, :], 8), v3(modT[:, 64:80], 8), 1.0, bc3(nffn[:, :], 2), ALU.add, ALU.mult,
              ["modT", "nffn"], ["A2"])
        for gi, (j0, wh) in enumerate(((16, 0), (16, 1), (40, 0), (40, 1))):
            for c in range(8):
                col = 2 * (j0 + c) + wh
                K.ts("dve", gbc[:, :], ones[:, :], modT[:, col:col + 1], None, ALU.mult, None, ["ones", "modT"], ["gbc"])
                K.mm(PB[7][:, (c % 4) * 128:(c % 4 + 1) * 128], gbc[:, :], identf[:, :], True, True, ["gbc", "identf"], ["pb7"])
                K.cp("act", G[gi][:, c * 128:(c + 1) * 128], PB[7][:, (c % 4) * 128:(c % 4 + 1) * 128], ["pb7"], [("G", gi)])

        o1 = 3 * NW * 128
        o2 = o1 + NW * 390
        KbT = arena[:, 0:o1].rearrange("p (a t) -> p a t", a=3)
        Vb = arena[:, o1:o2].rearrange("p (s h d) -> p s h d", s=NW, h=6)
        bt0 = arena[:, o2:o2 + 6144].rearrange("p (h e) -> p h e", h=6)
        btS = arena[:, o2 + 6144:o2 + 12288].rearrange("p (h e) -> p h e", h=6)
        assert o2 + 12288 <= AR
        K.memset("pool", arena[:, o1:o2], 1.0, [], ["Vb"])
        K.dma("sp", bt0, btab_d[0], [], ["bt0"])
        widx = K.sb("widx", [128, 6], I32)
        K.dma("sp", widx[:, :], widx_d[:, :], [], ["widx"])
        for s in range(NW):
            b = s % 2
            if s < 3 or NT_OWN + 3 <= s < NT_OWN + 6:
                srcg = kvb_ghi if s < 3 else kvb_glo
                col = s if s < 3 else s - NT_OWN
                K.S.dmafn("pool", lambda e, o=kvt[b][:, :], ix=widx[:, col:col + 1], sg=srcg: e.indirect_dma_start(
                    out=o, out_offset=None, in_=sg[:, :], in_offset=bass.IndirectOffsetOnAxis(ap=ix, axis=0)),
                    ["widx"], [("kvt", b)])
            elif s < NT_OWN + 3:
                K.dma("sp", kvt[b][:, :], kvb_own[(s - 3) * 128:(s - 2) * 128, :], [], [("kvt", b)])
            else:
                K.dma("sp", kvt[b][:, :], kvb_ctx[(s - NT_OWN - 6) * 128:(s - NT_OWN - 5) * 128, :], [], [("kvt", b)])
            for pr in range(3):
                K.tr(PB0b[:, pr * 128:(pr + 1) * 128], kvt[b][:, pr * 128:(pr + 1) * 128], identb[:, :], [("kvt", b), "identb"], ["pb0"])
            K.cp("dve", KbT[:, :, s * 128:(s + 1) * 128], v3(PB0b[:, 0:384], 3), ["pb0"], ["KbT"])
            K.cp("pool", Vb[:, s, :, 0:64], v3(kvt[b][:, 384:768], 6), [("kvt", b), "Vb"], ["Vb"])
        DSK = 2
        itemsB = []
        for t in range(NT):
            b = t % 2
            own = t < NT_OWN
            rows = slice(t * 128, (t + 1) * 128)
            special = own and t in specials
            bt, btk = (btS, "btS") if special else (bt0, "bt0")
            if own:
                kts = [(t + j + 3, j) for j in range(-3, 4)] + [(NW - 2, None), (NW - 1, None)]
            else:
                kts = [(NW - 2, None), (NW - 1, None)]
            groups = [kts[i:i + 3] for i in range(0, len(kts), 3)]
            ob_bank = 4 + t % 2
            for h in range(6):
                nk0 = 0
                for gi, grp in enumerate(groups):
                    itemsB.append(dict(t=t, b=b, h=h, grp=grp, nk0=nk0, nkt=len(kts), ob_bank=ob_bank, bt=bt, btk=btk,
                                       first=(h == 0 and gi == 0), last=(h == 5 and gi == len(groups) - 1),
                                       special=special, rows=rows))
                    nk0 += len(grp)

        def qkB(i, it):
            b, h, t = it["b"], it["h"], it["t"]
            if it["first"]:
                K.dma("sp", qbt[b][:, :, :], qbT_d[:, :, it["rows"]].rearrange("a p t -> p a t"), [], [("qbt", b)])
                if it["special"]:
                    K.dma("sp", btS, btab_d[1 + specials.index(t)], [], ["btS"])
            pr, pb = h // 2, (h % 2) * 64
            bank = 1 + i % 3
            pi = i % 3
            for ii, (s_, j) in enumerate(it["grp"]):
                K.mm(PB[bank][:, ii * 128:(ii + 1) * 128], KbT[pb:pb + 64, pr, s_ * 128:(s_ + 1) * 128],
                     qbt[b][pb:pb + 64, pr, :], True, j is None, ["KbT", ("qbt", b)], [f"pb{bank}"])
                if j is not None:
                    e0 = (7 - 2 * j) * 64
                    K.mm(PB[bank][:, ii * 128:(ii + 1) * 128], identb[:, :], it["bt"][:, h, e0:e0 + 128], False, True,
                         ["identb", it["btk"]], [f"pb{bank}"])
            n_ = len(it["grp"]) * 128
            K.act(PT[pi][:, 0:n_], PB[bank][:, 0:n_], AF.Exp, [f"pb{bank}"], [("PT", pi)])

        def pvB(i, it):
            h = it["h"]
            pi = i % 3
            OB = PB[it["ob_bank"]]
            for ii, (s_, j) in enumerate(it["grp"]):
                nk = it["nk0"] + ii
                K.mm(OB[:, h * 65:(h + 1) * 65], PT[pi][:, ii * 128:(ii + 1) * 128], Vb[:, s_, h, :],
                     nk == 0, nk == it["nkt"] - 1, [("PT", pi), "Vb"], [f"pb{it['ob_bank']}"])
            if it["last"]:
                O3 = v3(OB[:, 0:390], 6)
                K.S.op("dve", lambda e, O3=O3: e.reciprocal(st[:, 0:6], O3[:, :, 64]), [f"pb{it['ob_bank']}"], ["st"])
                K.tt("dve", v3(ob[:, :], 6), O3[:, :, 0:64], bc3(st[:, 0:6], 64), ALU.mult, [f"pb{it['ob_bank']}", "st"], ["ob"])
                K.dma("sp", mix_d[it["rows"], 256:640], ob[:, :], ["ob"], [("mixd", it["t"])])

        for i in range(len(itemsB) + DSK):
            if i < len(itemsB):
                qkB(i, itemsB[i])
            if i - DSK >= 0:
                pvB(i - DSK, itemsB[i - DSK])

        S.barrier()
        Kc = [arena[:, i * 2048:(i + 1) * 2048] for i in range(3)]
        Vc = [arena[:, 6144 + i * 1040:6144 + (i + 1) * 1040].rearrange("p (k d) -> p k d", d=65) for i in range(3)]
        qblocks = [(t0, QB, list(range(NKT))) for t0 in range(0, NT_OWN, QB)] + [(NT_OWN, 2, [NKT - 2, NKT - 1])]
        SCALE_C = 96.0 ** -0.5
        itemsC = []
        qh = 0
        cc = 0
        for (t0, ntl, klist) in qblocks:
            nq = ntl * 128
            for h in range(6):
                b2 = qh % 2
                oc_bank = 4 + qh % 2
                qh += 1
                own_k = [k for k in klist if k < NR * NT_OWN]
                ctx_k = [k for k in klist if k >= NR * NT_OWN]
                chunks = [own_k[i:i + CH] for i in range(0, len(own_k), CH)] + ([ctx_k] if ctx_k else [])
                nk = 0
                for chk in chunks:
                    cb = cc % 3
                    cc += 1
                    for kt in range(len(chk)):
                        itemsC.append(dict(t0=t0, ntl=ntl, nq=nq, h=h, b2=b2, oc_bank=oc_bank, cb=cb, chk=chk, kt=kt,
                                           newq=(nk == 0), newchunk=(kt == 0), nk=nk, nkt=len(klist)))
                        nk += 1

        def qkC(i, it):
            h, b2, cb, nq = it["h"], it["b2"], it["cb"], it["nq"]
            if it["newq"]:
                K.dma("sp", qT[b2][:, 0:nq], qcT_d[h, :, it["t0"] * 128:it["t0"] * 128 + nq], [], [("qT", b2)])
            if it["newchunk"]:
                chk = it["chk"]
                k0, n_k = chk[0], len(chk)
                if k0 < NR * NT_OWN:
                    rk, l0 = k0 // NT_OWN, k0 % NT_OWN
                    pj, lt = l0 // PCT, l0 % PCT
                    assert lt + n_k <= PCT
                    ksrc = d[f"kcT_g_{L}_{pj}"].rearrange("(r h p) t -> r h p t", r=NR, h=6)[rk, h, :, lt * 128:(lt + n_k) * 128]
                    v0 = (rk * PCT + lt) * 128
                    vsrc = d[f"vc_g_{L}_{pj}"][v0:v0 + n_k * 128, h * 65:(h + 1) * 65]
                else:
                    c0_ = (k0 - NR * NT_OWN) * 128
                    ksrc = kcT_ctx.rearrange("(h p) t -> h p t", h=6)[h, :, c0_:c0_ + n_k * 128]
                    vsrc = vc_ctx[c0_:c0_ + n_k * 128, h * 65:(h + 1) * 65]
                K.dma("sp", Kc[cb][0:96, 0:n_k * 128], ksrc, [], [("Kc", cb)])
                K.dma("act", Vc[cb][:, 0:n_k, :], vsrc.rearrange("(k p) d -> p k d", p=128), [], [("Vc", cb)])
            bank = 1 + i % 3
            pi = i % 3
            kt = it["kt"]
            K.mm(PB[bank][:, 0:nq], Kc[cb][0:96, kt * 128:(kt + 1) * 128], qT[b2][:, 0:nq], True, True,
                 [("Kc", cb), ("qT", b2)], [f"pb{bank}"])
            K.act(PT[pi][:, 0:nq], PB[bank][:, 0:nq], AF.Exp, [f"pb{bank}"], [("PT", pi)], scale=SCALE_C)

        def pvC(i, it):
            pi = i % 3
            nq, ntl, oc_bank, cb, kt, h = it["nq"], it["ntl"], it["oc_bank"], it["cb"], it["kt"], it["h"]
            OC = PB[oc_bank]
            K.mm(OC[0:65, 0:nq], Vc[cb][:, kt, :], PT[pi][:, 0:nq], it["nk"] == 0, it["nk"] == it["nkt"] - 1,
                 [("PT", pi), ("Vc", cb)], [f"pb{oc_bank}"])
            if it["nk"] == it["nkt"] - 1:
                t0 = it["t0"]
                K.cp("dve", ocT[:, 0:nq], OC[0:65, 0:nq], [f"pb{oc_bank}"], ["ocT"])
                for qi in range(ntl):
                    K.tr(PB[0][:, qi * 65:(qi + 1) * 65], ocT[:, qi * 128:(qi + 1) * 128], identf[0:65, 0:65], ["ocT", "identf"], ["pb0"])
                O3 = v3(PB[0][:, 0:ntl * 65], ntl)
                K.S.op("dve", lambda e, O3=O3, ntl=ntl: e.reciprocal(st[:, 0:ntl], O3[:, :, 64]), ["pb0"], ["st"])
                K.tt("dve", oc[:, 0:ntl, :], O3[:, :, 0:64], bc3(st[:, 0:ntl], 64), ALU.mult, ["pb0", "st"], ["oc"])
                K.dma("sp", mix_d[t0 * 128:t0 * 128 + nq, 640 + h * 64:704 + h * 64].rearrange("(q p) d -> p q d", p=128),
                      oc[:, 0:ntl, :], ["oc"], [("mixd", t0 + i_) for i_ in range(ntl)])

        for i in range(len(itemsC) + DSK):
            if i < len(itemsC):
                qkC(i, itemsC[i])
            if i - DSK >= 0:
                pvC(i - DSK, itemsC[i - DSK])

        S.barrier()
        NB = (2 * T + 255) // 256 + 64
        h2_d, x1_d, xs_d, ys_d = d[f"h2_{L}"], d[f"x1_{L}"], d[f"xsl_{L}"], d[f"ys_{L}"]
        w1r = w1_d.rearrange("e p c n -> (e p) (c n)")
        w3r = w3_d.rearrange("e p c n -> (e p) (c n)")
        w2r = w2_d.rearrange("e p c n -> (e p) (c n)")
        WSZ = 12288
        Wb = [arena[:, i * WSZ:(i + 1) * WSZ] for i in range(2)]
        wout = arena[:, 2 * WSZ:2 * WSZ + 8192].rearrange("p (c n) -> p c n", c=8)
        ohb = arena[:, 2 * WSZ + 8192:2 * WSZ + 8192 + NT * 128].rearrange("p (t e) -> p t e", t=NT)
        assert 2 * WSZ + 8192 + NT * 128 <= AR
        K.dma("pool", wout, wout_d[:, :, :], [], ["wout"])
        ut = K.sb("ut", [128, 128], F32)
        iop = K.sb("iop", [128, 1], F32)
        K.dma("sp", ut[:, :], d["ut"][:, :], [], ["ut"])
        K.dma("sp", iop[:, :], d["iop"][:, :], [], ["iop"])
        base = K.sb("base", [128, 64], F32)
        K.memset("dve", base[:, :], 0.0, [], ["base"])
        rk = K.sb("rk", [128, NT, 2], F32)
        wts = K.sb("wts", [128, NT, 2], F32)
        dstf = K.sb("dstf", [128, NT, 2], F32)
        dsti = K.sb("dsti", [128, NT * 2], I32)
        h2s = [K.sb(f"h2s{i}", [128, D], BF16) for i in range(2)]
        MB = [K.sb(f"MB{i}", [128, D], F32) for i in range(4)]
        for gi, (src, wh) in enumerate((("A2", 0), ("A2", 1), ("B2", 0), ("B2", 1))):
            for c in range(8):
                col = (A2[:, 2 * c + wh:2 * c + wh + 1] if src == "A2"
                       else modT[:, 2 * (24 + c) + wh:2 * (24 + c) + wh + 1])
                K.ts("dve", gbc[:, :], ones[:, :], col, None, ALU.mult, None, ["ones", "modT", "A2"], ["gbc"])
                K.mm(PB[7][:, (c % 4) * 128:(c % 4 + 1) * 128], gbc[:, :], identf[:, :], True, True, ["gbc", "identf"], ["pb7"])
                K.cp("act", MB[gi][:, c * 128:(c + 1) * 128], PB[7][:, (c % 4) * 128:(c % 4 + 1) * 128], ["pb7"], [("MB", gi)])

        def rstd_of(ss, dim):
            K.ts("dve", ss, ss, 1.0 / dim, EPS, ALU.mult, ALU.add, ["st"], ["st"])
            K.act(ss, ss, AF.Sqrt, ["st"], ["st"])
            K.S.op("dve", lambda e, a=ss: e.reciprocal(a, a), ["st"], ["st"])

        x1t = [K.sb(f"x1t{i}", [128, D], F32) for i in range(2)]
        for t in range(NT):
            b = t % 2
            wh = 0 if t < NT_OWN else 1
            rows = slice(t * 128, (t + 1) * 128)
            X1 = x1t[b]
            K.dma("sp", xt[b][:, :], x_d[rows, :], [], [("xt", b)])
            K.dma("sp", mixrow[:, 256:1024], mix_d[rows, 256:1024], [("mixd", t)], ["mixrow"])
            K.dma("sp", mixrow[:, 0:256], oa_d[rows, :], [], ["mixrow"])
            for c in range(8):
                K.tr(PB0b[:, c * 128:(c + 1) * 128], mixrow[:, c * 128:(c + 1) * 128], identb[:, :], ["mixrow", "identb"], ["pb0"])
            K.cp("dve", mixT[:, :, :], v3(PB0b[:, :], 8), ["pb0"], ["mixT"])
            for half in range(2):
                pb = 1 + half
                for c in range(8):
                    K.mm(PB[pb][:, :], mixT[:, c, :], wout[:, c, half * 512:(half + 1) * 512], c == 0, c == 7,
                         ["mixT", "wout"], [f"pb{pb}"])
                hs = slice(half * 512, (half + 1) * 512)
                K.tt("dve", tmpf[:, hs], PB[pb][:, :], G[wh][:, hs], ALU.mult, [f"pb{pb}", ("G", wh)], ["tmpf"])
                K.tt("pool", X1[:, hs], tmpf[:, hs], xt[b][:, hs], ALU.add, ["tmpf", ("xt", b)], [("x1", b)])
            K.dma("sp", x1_d[rows, :], X1[:, :], [("x1", b)], [])
            K.tt("pool", tmpf[:, :], X1[:, :], X1[:, :], ALU.mult, [("x1", b), "tmpf"], ["tmpf"])
            K.rsum("dve", st[:, 0:1], tmpf[:, :], ["tmpf"], ["st"])
            rstd_of(st[:, 0:1], D)
            K.act(h2n[:, :], X1[:, :], AF.Copy, [("x1", b), "st"], ["h2n"], scale=st[:, 0:1])
            K.tt("pool", tmpf[:, :], h2n[:, :], MB[wh][:, :], ALU.mult, ["h2n", ("MB", wh), "tmpf"], ["tmpf"])
            K.tt("pool", h2s[b][:, :], tmpf[:, :], MB[2 + wh][:, :], ALU.add, ["tmpf", ("MB", 2 + wh)], [("h2s", b)])
            K.dma("sp", h2_d[rows, :], h2s[b][:, :], [("h2s", b)], [])
            for c in range(8):
                pb = 6 + c // 4
                K.tr(PB[pb][:, (c % 4) * 128:(c % 4 + 1) * 128], h2n[:, c * 128:(c + 1) * 128], identf[:, :], ["h2n", "identf"], [f"pb{pb}"])
            for c in range(8):
                pb = 6 + c // 4
                K.ts("dve", h2Tf[:, c, :], PB[pb][:, (c % 4) * 128:(c % 4 + 1) * 128], A2[:, 2 * c + wh:2 * c + wh + 1],
                     modT[:, 2 * (24 + c) + wh:2 * (24 + c) + wh + 1], ALU.mult, ALU.add, [f"pb{pb}", "A2", "modT"], ["h2Tf"])
            for c in range(8):
                K.mm(PB[3][:, 0:72], h2Tf[:, c, :], wgr[:, c, :], c == 0, c == 7, ["h2Tf", "wgr"], ["pb3"])
            K.cp("act", lg[:, :], PB[3][:, 0:72], ["pb3"], ["lg"])
            gl = lg[:, 0:8]
            rl3 = v3(lg[:, 8:72], 8)
            s_gmax, s_ngmax, s_gsum, s_m1, s_m2, s_d, s_e2, s_wa, s_wb = [st[:, 16 + i:17 + i] for i in range(9)]
            goh, gex, pen = st[:, 32:40], st[:, 40:48], st[:, 48:56]
            RK = ["lg", "st", "r64"]
            K.S.op("dve", lambda e, a=s_gmax, g=gl: e.reduce_max(a, g, AX.X), ["lg"], ["st"])
            K.ts("dve", goh, gl, s_gmax, None, ALU.is_equal, None, RK, ["st"])
            K.ts("dve", s_ngmax, s_gmax, -1.0, None, ALU.mult, None, RK, ["st"])
            K.act(gex, gl, AF.Exp, RK, ["st"], bias=s_ngmax)
            K.rsum("dve", s_gsum, gex, RK, ["st"])
            K.S.op("dve", lambda e, a=s_gsum: e.reciprocal(a, a), RK, ["st"])
            K.ts("dve", pen, goh, BIG, -BIG, ALU.mult, ALU.add, RK, ["st"])
            rm, oh1, rm2, oh2 = [r[:, :] for r in r64]
            K.tt("dve", v3(rm, 8), rl3, bc3(pen, 8), ALU.add, RK, ["r64"])
            K.S.op("dve", lambda e, a=s_m1, g=rm: e.reduce_max(a, g, AX.X), RK, ["st"])
            K.ts("dve", oh1, rm, s_m1, None, ALU.is_equal, None, RK, ["r64"])
            K.stt("dve", rm2, oh1, -BIG, rm, ALU.mult, ALU.add, RK, ["r64"])
            K.S.op("dve", lambda e, a=s_m2, g=rm2: e.reduce_max(a, g, AX.X), RK, ["st"])
            K.ts("dve", oh2, rm2, s_m2, None, ALU.is_equal, None, RK, ["r64"])
            K.tt("dve", s_d, s_m2, s_m1, ALU.subtract, RK, ["st"])
            K.act(s_e2, s_d, AF.Exp, RK, ["st"])
            K.ts("dve", s_wa, s_e2, 1.0, None, ALU.add, None, RK, ["st"])
            K.S.op("dve", lambda e, a=s_wa: e.reciprocal(a, a), RK, ["st"])
            K.tt("dve", wts[:, t, 0:1], s_wa, s_gsum, ALU.mult, RK, ["wts"])
            K.tt("dve", wts[:, t, 1:2], wts[:, t, 0:1], s_e2, ALU.mult, RK + ["wts"], ["wts"])
            K.cp("dve", ohb[:, t, 0:64], oh1, RK, ["ohb"])
            K.cp("dve", ohb[:, t, 64:128], oh2, RK, ["ohb"])
            K.tt("dve", rm, oh1, oh2, ALU.add, RK, ["r64"])
            K.mm(PB[3][:, 128:192], ut[:, :], rm, True, True, ["ut", "r64"], ["pb3"])
            K.mm(PB[3][:, 192:256], ones[:, :], rm, True, True, ["ones", "r64"], ["pb3"])
            K.tt("dve", rm2, PB[3][:, 128:192], base[:, :], ALU.add, ["pb3", "base", "r64"], ["r64"])
            K.tt("dve", rm, oh1, rm2, ALU.mult, RK, ["r64"])
            K.rsum("dve", rk[:, t, 0:1], rm, RK, ["rk"])
            K.tt("dve", rm, oh2, rm2, ALU.mult, RK, ["r64"])
            K.rsum("dve", rk[:, t, 1:2], rm, RK, ["rk"])
            K.tt("dve", base[:, :], PB[3][:, 192:256], base[:, :], ALU.add, ["pb3", "base", "r64"], ["base"])
        cs = [r64[0][:, :], r64[1][:, :]]
        pc = r64[2][:, :]
        pst = r64[3][:, :]
        KS = ["base", "r64"]
        K.ts("dve", pc, base[:, :], 255.0, None, ALU.add, None, KS, ["r64"])
        pci = K.sb("pci", [128, 64], I32)
        K.cp("dve", pci[:, :], pc, KS, ["pci"])
        K.ts("dve", pci[:, :], pci[:, :], 8, 8, ALU.arith_shift_right, ALU.logical_shift_left, ["pci"], ["pci"])
        K.cp("dve", pc, pci[:, :], ["pci"] + KS, ["r64"])
        K.cp("dve", cs[0], pc, KS, ["r64"])
        cur = 0
        for sh in (1, 2, 4, 8, 16, 32):
            K.cp("dve", cs[1 - cur][:, 0:sh], cs[cur][:, 0:sh], KS, ["r64"])
            K.tt("dve", cs[1 - cur][:, sh:64], cs[cur][:, sh:64], cs[cur][:, 0:64 - sh], ALU.add, KS, ["r64"])
            cur = 1 - cur
        pend = cs[cur]
        K.tt("dve", pst, pend, pc, ALU.subtract, KS, ["r64"])
        other = cs[1 - cur]
        for t in range(NT):
            for k in range(2):
                K.tt("dve", other, ohb[:, t, 64 * k:64 * k + 64], pst, ALU.mult, KS + ["ohb"], ["r64"])
                K.rsum("dve", dstf[:, t, k:k + 1], other, KS, ["dstf"])
        K.tt("dve", dstf[:, :, :], dstf[:, :, :], rk[:, :, :], ALU.add, ["dstf", "rk"], ["dstf"])
        K.cp("dve", dsti[:, :], dstf[:, :, :].rearrange("p t k -> p (t k)"), ["dstf"], ["dsti"])
        zt = K.sb("zt", [128, D], BF16)
        K.memset("pool", zt[:, :], 0.0, [], ["zt"])
        for bk in range(2 * NB):
            K.dma("sp" if bk % 2 == 0 else "act", xs_d[bk * 128:(bk + 1) * 128, :], zt[:, :], ["zt"], ["xs"])
        S.barrier()
        for t in range(NT):
            b = t % 2
            K.dma("sp", h2s[b][:, :], h2_d[t * 128:(t + 1) * 128, :], [], [("h2s", b)])
            for k in range(2):
                K.S.dmafn("pool", lambda e, src=h2s[b][:, :], ix=dsti[:, 2 * t + k:2 * t + k + 1]: e.indirect_dma_start(
                    out=xs_d[:, :], out_offset=bass.IndirectOffsetOnAxis(ap=ix, axis=0), in_=src, in_offset=None),
                    [("h2s", b), "dsti"], ["xs"])
        S.barrier()
        xsb = K.sb("xsb", [128, 2, D], BF16)
        xT = K.sb("xTb", [128, 8, 256], BF16)
        ysbb = K.sb("ysbb", [128, 2, D], F32)
        ysb = [ysbb[:, 0, :], ysbb[:, 1, :]]
        widx2 = K.sb("widx2", [128, 2], I32)
        NROWS_W = NE * 128
        for bk in range(NB):
            wb = bk % 2
            W = Wb[wb]
            w1e = W[:, 0:4096].rearrange("p (c n) -> p c n", c=8)
            w3e = W[:, 4096:8192].rearrange("p (c n) -> p c n", c=8)
            w2e = W[:, 8192:12288].rearrange("p (c n) -> p c n", c=4)
            ef = st[:, 60:61]
            fl = st[:, 61:62]
            K.ts("dve", other, pend, float(256 * bk), None, ALU.is_le, None, ["r64"], ["r64b"])
            K.rsum("dve", ef, other, ["r64b"], ["st"])
            K.ts("dve", ef, ef, float(NE - 1), 128.0, ALU.min, ALU.mult, ["st"], ["st"])
            K.tt("dve", ef, ef, iop[:, :], ALU.add, ["st", "iop"], ["st"])
            K.cp("dve", widx2[:, wb:wb + 1], ef, ["st"], [("widx2", wb)])
            for (dst, srcw) in ((W[:, 0:4096], w1r), (W[:, 4096:8192], w3r), (W[:, 8192:12288], w2r)):
                K.S.dmafn("pool", lambda e, o=dst, sw=srcw, ix=widx2[:, wb:wb + 1]: e.indirect_dma_start(
                    out=o, out_offset=None, in_=sw[:, :], in_offset=bass.IndirectOffsetOnAxis(ap=ix, axis=0)),
                    [("widx2", wb)], [("W", wb)])
            K.dma("sp", xsb[:, :, :], xs_d[bk * 256:(bk + 1) * 256, :].rearrange("(a p) f -> p a f", p=128), [], ["xsb"])
            for a in range(2):
                for c in range(8):
                    K.tr(PB0b[:, c * 128:(c + 1) * 128], xsb[:, a, c * 128:(c + 1) * 128], identb[:, :], ["xsb", "identb"], ["pb0"])
                K.cp("dve", xT[:, :, a * 128:(a + 1) * 128], v3(PB0b[:, :], 8), ["pb0"], ["xT"])
            for m in range(4):
                p1, p3 = 1 + m % 2, 4 + m % 2
                for c in range(8):
                    K.mm(PB[p1][:, 0:256], w1e[:, c, m * 128:(m + 1) * 128], xT[:, c, :], c == 0, c == 7, [("W", wb), "xT"], [f"pb{p1}"])
                for c in range(8):
                    K.mm(PB[p3][:, 0:256], w3e[:, c, m * 128:(m + 1) * 128], xT[:, c, :], c == 0, c == 7, [("W", wb), "xT"], [f"pb{p3}"])
                K.act(sil[m % 2][:, 0:256], PB[p1][:, 0:256], AF.Silu, [f"pb{p1}"], [("sil", m % 2)])
                K.tt("dve", hT[:, m, 0:256], PB[p3][:, 0:256], sil[m % 2][:, 0:256], ALU.mult, [f"pb{p3}", ("sil", m % 2)], ["hTe"])
            for a in range(2):
                for half in range(2):
                    py = 6 + half
                    for kc in range(4):
                        K.mm(PB[py][:, :], hT[:, kc, a * 128:(a + 1) * 128], w2e[:, kc, half * 512:(half + 1) * 512], kc == 0, kc == 3,
                             ["hTe", ("W", wb)], [f"pb{py}"])
                    K.cp("act", ysbb[:, a, half * 512:(half + 1) * 512], PB[py][:, :], [f"pb{py}"], [("ysb", a)])
                K.dma("sp", ys_d[bk * 256 + a * 128:bk * 256 + (a + 1) * 128, :], ysbb[:, a, :], [("ysb", a)], [])
        S.barrier()
        for t in range(NT):
            b = t % 2
            wh = 0 if t < NT_OWN else 1
            rows = slice(t * 128, (t + 1) * 128)
            K.dma("sp", x1t[b][:, :], x1_d[rows, :], [], [("x1", b)])
            for k in range(2):
                K.S.dmafn("pool", lambda e, o=ysb[k][:, :], ix=dsti[:, 2 * t + k:2 * t + k + 1]: e.indirect_dma_start(
                    out=o, out_offset=None, in_=ys_d[:, :], in_offset=bass.IndirectOffsetOnAxis(ap=ix, axis=0)),
                    ["dsti"], [("ysb", k)])
            K.ts("dve", tmpf[:, :], ysb[0][:, :], wts[:, t, 0:1], None, ALU.mult, None, [("ysb", 0), "wts"], ["tmpf"])
            K.stt("dve", tmpf[:, :], ysb[1][:, :], wts[:, t, 1:2], tmpf[:, :], ALU.mult, ALU.add, [("ysb", 1), "wts", "tmpf"], ["tmpf"])
            K.tt("pool", tmpf[:, :], tmpf[:, :], G[2 + wh][:, :], ALU.mult, ["tmpf", ("G", 2 + wh)], ["tmpf"])
            K.tt("pool", h2n[:, :], tmpf[:, :], x1t[b][:, :], ALU.add, ["tmpf", ("x1", b)], ["h2n"])
            K.dma("sp", xo_d[rows, :], h2n[:, :], ["h2n"], [])
        S.emit()


def build_btab(rpb, NT_OWN, rank):
    rows_total = 8 * NT_OWN
    specials = _specials(NT_OWN)
    gts = [rows_total // 4] + [rank * NT_OWN + t for t in specials]
    qc = np.arange(64)
    kc = np.arange(64)
    c0 = np.clip(qc - 8, 0, 48)
    colok = (kc[:, None] >= c0[None, :]) & (kc[:, None] < c0[None, :] + 16)
    dc = np.clip(kc[:, None] - qc[None, :], -15, 15) + 15
    tab = np.full((len(gts), 2, 64, 6, 16, 64), NEG, np.float32)
    for ci, gt in enumerate(gts):
        for e in range(16):
            b = (e + 1) % 2
            j = (7 + b - e) // 2
            qrow = 2 * gt + b
            r0 = min(max(qrow - 4, 0), rows_total - 8)
            for a in range(2):
                krow = 2 * (gt + j) + a
                if krow < 0 or krow >= rows_total or krow < r0 or krow >= r0 + 8:
                    continue
                dr = krow - qrow + 7
                vals = rpb[:, dr, :][:, dc]
                vals = np.where(colok[None], vals, np.float32(NEG))
                tab[ci, a, :, :, e, :] = vals.transpose(1, 0, 2)
    return tab.reshape(len(gts), 128, 6, 1024).astype(NPBF)


def prep_B(P, i, x_cur, xc_cur, NT_OWN, outsA, ranks_per_batch=4, NE=64):
    B = x_cur.shape[0]
    n = NT_OWN * 128
    NTB = ranks_per_batch * NT_OWN
    shared = {
        "w_out": _kmaj(P["w_out"][i], 8),
        "norm_ffnT": _colT(P["norm_ffn"][i]),
        "wgr": _kmaj(np.concatenate([P["moe_w_group"][i], P["moe_w_router"][i]], axis=1), 8),
        "w1": np.ascontiguousarray(P["moe_w1"][i][:NE].reshape(NE, 8, 128, 512).transpose(0, 2, 1, 3)),
        "w3": np.ascontiguousarray(P["moe_w3"][i][:NE].reshape(NE, 8, 128, 512).transpose(0, 2, 1, 3)),
        "w2": np.ascontiguousarray(P["moe_w2"][i][:NE].reshape(NE, 4, 128, 1024).transpose(0, 2, 1, 3)),
        "ident": np.eye(128, dtype=np.float32),
    }
    maps = []
    for b in range(B):
        cores = [outsA[b * ranks_per_batch + r] for r in range(ranks_per_batch)]
        kv_tiles = [c["kvb"][:n].reshape(NT_OWN, 128, 768) for c in cores]
        kv_all = np.concatenate(kv_tiles, axis=0)
        for r in range(ranks_per_batch):
            me = cores[r]
            m = dict(shared)
            m["x"] = np.ascontiguousarray(np.concatenate([x_cur[b, r * n:(r + 1) * n], xc_cur[b]], axis=0))
            m["oa"] = me["oa"]
            m["qbT"] = me["qbT"]
            m["qcT"] = me["qcT"]
            m["modT"] = me["modT"]
            win = np.zeros((NT_OWN + 8, 128, 768), NPBF)
            for s in range(NT_OWN + 6):
                g = r * NT_OWN - 3 + s
                if 0 <= g < NTB:
                    win[s] = kv_all[g]
            win[NT_OWN + 6:] = me["kvb"][n:].reshape(2, 128, 768)
            m["kvbw"] = win.reshape(-1, 768)
            m["kcT_all"] = np.ascontiguousarray(np.concatenate([c["kcT"][:, :, :n] for c in cores] + [me["kcT"][:, :, n:]], axis=2))
            m["vc_all"] = np.ascontiguousarray(np.concatenate([c["vc"][:n] for c in cores] + [me["vc"][n:]], axis=0))
            m["btab"] = build_btab(P["b_rpb"][i], NT_OWN, r)
            maps.append(m)
    return maps


def declare_dram(nc, NT_OWN, NR=4, NE=64):
    NT = NT_OWN + 2
    T = NT * 128
    n = NT_OWN * 128
    NCLS = 1 + len(_specials(NT_OWN))
    d = {}

    def t(name, shape, dt, kind="Internal"):
        d[name] = nc.dram_tensor(name, list(shape), dt, kind=kind).ap()

    EI = "ExternalInput"
    t("x", [T, D], F32, EI)
    t("cT", [128, 16], F32, EI)
    t("rope", [T, 32], F32, EI)
    t("ident", [128, 128], F32, EI)
    t("widx", [128, 6], I32, EI)
    t("ut", [128, 128], F32, EI)
    t("iop", [128, 1], F32, EI)
    t("xo", [T, D], F32, "ExternalOutput")
    t("xs_0", [T, D], F32)
    for L in range(2):
        t(f"w_ada_{L}", [128, 8, 6144], F32, EI)
        t(f"b_adaT_{L}", [128, 48], F32, EI)
        t(f"norm_mixT_{L}", [128, 8], F32, EI)
        t(f"w_in_{L}", [128, 8, INW], F32, EI)
        t(f"w_sT_{L}", [128, 4, 128], F32, EI)
        t(f"b_s_{L}", [128, 4], F32, EI)
        t(f"gains_{L}", [128, G_TOT], F32, EI)
        t(f"wq_{L}", [128, 3, 576], F32, EI)
        t(f"wkv_{L}", [128, 2, 768], F32, EI)
        t(f"btab_{L}", [NCLS, 128, 6, 1024], BF16, EI)
        t(f"w_out_{L}", [128, 8, D], F32, EI)
        t(f"norm_ffnT_{L}", [128, 8], F32, EI)
        t(f"wgr_{L}", [128, 8, 72], F32, EI)
        t(f"w1_{L}", [NE, 128, 8, 512], F32, EI)
        t(f"w3_{L}", [NE, 128, 8, 512], F32, EI)
        t(f"w2_{L}", [NE, 128, 4, D], F32, EI)
        t(f"oa_{L}", [T, 256], BF16)
        t(f"qbT_{L}", [3, 128, T], BF16)
        t(f"qcT_{L}", [6, 96, T], BF16)
        t(f"modT_{L}", [128, 96], F32)
        t(f"mixd_{L}", [T, D], BF16)
        PT = min(4, NT_OWN)
        for j in range(NT_OWN // PT):
            t(f"kcT_own_{L}_{j}", [576, PT * 128], BF16)
            t(f"kcT_g_{L}_{j}", [NR * 576, PT * 128], BF16)
            t(f"vc_own_{L}_{j}", [PT * 128, 390], BF16)
            t(f"vc_g_{L}_{j}", [NR * PT * 128, 390], BF16)
        t(f"kcT_ctx_{L}", [576, 256], BF16)
        t(f"vc_ctx_{L}", [256, 390], BF16)
        t(f"kvb_own_{L}", [n, 768], BF16)
        t(f"kvb_ctx_{L}", [256, 768], BF16)
        HT = min(3, NT_OWN)
        t(f"kvb_lo_{L}", [HT * 128, 768], BF16)
        t(f"kvb_hi_{L}", [HT * 128, 768], BF16)
        t(f"kvb_glo_{L}", [NR * HT * 128, 768], BF16)
        t(f"kvb_ghi_{L}", [NR * HT * 128, 768], BF16)
        t(f"h2_{L}", [T, D], BF16)
        t(f"x1_{L}", [T, D], F32)
        t(f"xsl_{L}", [((2 * T + 255) // 256 + 64) * 256, D], BF16)
        t(f"ys_{L}", [((2 * T + 255) // 256 + 64) * 256, D], F32)
    return d


def emit_X(nc, gc, d, L, groups):
    stack = contextlib.ExitStack()
    with stack:
        K = KB(nc, stack, gc)
        pairs = [(d[f"kvb_lo_{L}"], d[f"kvb_glo_{L}"]), (d[f"kvb_hi_{L}"], d[f"kvb_ghi_{L}"])]
        j = 0
        while f"vc_own_{L}_{j}" in d:
            pairs.append((d[f"vc_own_{L}_{j}"], d[f"vc_g_{L}_{j}"]))
            pairs.append((d[f"kcT_own_{L}_{j}"], d[f"kcT_g_{L}_{j}"]))
            j += 1
        for nm, (src, dst) in enumerate(pairs):
            K.S.cc(lambda e, src=src, dst=dst: e.collective_compute(
                "AllGather", ALU.bypass, replica_groups=groups, ins=[src[:, :]], outs=[dst[:, :]]), [], [nm])
        K.S.emit()


def build_fused(NT_OWN, ngroups=2, NR=4, TB=4, NE=64, do_x=True):
    nc = bass.Bass("TRN2", target_bir_lowering=False)
    gstack = contextlib.ExitStack()
    with gstack:
        gc = GC(gstack)
        d = declare_dram(nc, NT_OWN, NR, NE)
        groups = [list(range(g * NR, (g + 1) * NR)) for g in range(ngroups)]
        for L in range(2):
            emit_A(nc, gc, d, NT_OWN, L)
            if do_x:
                emit_X(nc, gc, d, L, groups)
            emit_B(nc, gc, d, NT_OWN, L, TB=TB, NR=NR, NE=NE)
    return nc


def halo_index(NT_OWN, r, NR):
    HT = min(3, NT_OWN)
    idx = np.zeros((128, 6), np.int32)
    for col in range(6):
        g = r * NT_OWN - 3 + col if col < 3 else (r + 1) * NT_OWN + (col - 3)
        if not (0 <= g < NR * NT_OWN):
            continue
        rk, l = g // NT_OWN, g % NT_OWN
        if col < 3:
            t2 = l - (NT_OWN - HT)
        else:
            t2 = l
        if not (0 <= t2 < HT):
            continue
        idx[:, col] = (rk * HT + t2) * 128 + np.arange(128)
    return idx


def prep_fused(P, NT_OWN, NR=4):
    x = np.asarray(P["x"], np.float32)
    ctx = np.asarray(P["ctx"], np.float32)
    B = x.shape[0]
    n = NT_OWN * 128
    shared = {"ident": np.eye(128, dtype=np.float32),
              "ut": np.triu(np.ones((128, 128), np.float32), 1),
              "iop": np.arange(128, dtype=np.float32).reshape(128, 1)}
    per_rank = [dict() for _ in range(NR)]
    for L in range(2):
        gains = np.concatenate([
            P["a_v_norm"][L], np.tile(P["b_q_norm"][L], 6), np.tile(P["b_k_norm"][L], 6), P["c_q_a_norm"][L],
            P["c_kv_a_norm"][L], np.tile(P["c_q_norm"][L], 6), np.tile(P["c_k_norm"][L][:64], 6), P["c_k_norm"][L][64:]])
        shared.update({
            f"w_ada_{L}": _kmaj(P["w_ada"][L], 8),
            f"b_adaT_{L}": _colT(P["b_ada"][L]),
            f"norm_mixT_{L}": _colT(P["norm_mix"][L]),
            f"w_in_{L}": _kmaj(P["w_in"][L], 8),
            f"w_sT_{L}": np.ascontiguousarray(P["a_w_s"][L].transpose(2, 0, 1)),
            f"b_s_{L}": np.ascontiguousarray(P["a_b_s"][L].T),
            f"gains_{L}": _bcrow(gains.astype(np.float32)),
            f"wq_{L}": _kmaj(P["c_w_q_up"][L], 3),
            f"wkv_{L}": _kmaj(P["c_w_kv_up"][L], 2),
            f"w_out_{L}": _kmaj(P["w_out"][L], 8),
            f"norm_ffnT_{L}": _colT(P["norm_ffn"][L]),
            f"wgr_{L}": _kmaj(np.concatenate([P["moe_w_group"][L], P["moe_w_router"][L]], axis=1), 8),
            f"w1_{L}": np.ascontiguousarray(P["moe_w1"][L].reshape(64, 8, 128, 512).transpose(0, 2, 1, 3)),
            f"w3_{L}": np.ascontiguousarray(P["moe_w3"][L].reshape(64, 8, 128, 512).transpose(0, 2, 1, 3)),
            f"w2_{L}": np.ascontiguousarray(P["moe_w2"][L].reshape(64, 4, 128, 1024).transpose(0, 2, 1, 3)),
        })
        for r in range(NR):
            per_rank[r][f"btab_{L}"] = build_btab(P["b_rpb"][L], NT_OWN, r)
    for r in range(NR):
        per_rank[r]["rope"] = rope_tables(NT_OWN, r)
        per_rank[r]["widx"] = halo_index(NT_OWN, r, NR)
    maps = []
    for b in range(B):
        cT = np.ascontiguousarray(np.stack([_colT(P["c"][b]), _colT(P["c_ctx"])], axis=2).reshape(128, 16))
        for r in range(NR):
            m = dict(shared)
            m.update(per_rank[r])
            m["x"] = np.ascontiguousarray(np.concatenate([x[b, r * n:(r + 1) * n], ctx[b]], axis=0))
            m["cT"] = cT
            maps.append(m)
    return maps


_PROG = {}


def kernel(**inputs):
    P = {k: np.asarray(v) for k, v in inputs.items()}
    NT_OWN = 32
    n = NT_OWN * 128
    if "fused" not in _PROG:
        _PROG["fused"] = build_fused(NT_OWN)
    maps = prep_fused(P, NT_OWN)
    res = run_bass_kernel_spmd(_PROG["fused"], maps, core_ids=list(range(8))).results
    out = np.empty((2, 4 * n, D), np.float32)
    for b in range(2):
        for r in range(4):
            out[b, r * n:(r + 1) * n] = res[b * 4 + r]["xo"][:n]
    return out
```

```python
import contextlib
import numpy as np
import ml_dtypes
import concourse.bass as bass
import concourse.mybir as mybir
from concourse.bass_utils import run_bass_kernel_spmd

F32 = mybir.dt.float32
BF16 = mybir.dt.bfloat16
I32 = mybir.dt.int32
AF = mybir.ActivationFunctionType
ALU = mybir.AluOpType
AX = mybir.AxisListType
NPBF = ml_dtypes.bfloat16

D = 1024
GRID_W = 64
CTX = 256
EPS = 1e-6
INW = 2336
NEG = -30000.0


class _Op:
    __slots__ = ("eng", "fn", "deps", "needed", "semkey", "val", "dma", "idx")

    def __init__(self, eng, fn, dma):
        self.eng = eng
        self.fn = fn
        self.deps = []
        self.needed = False
        self.semkey = None
        self.val = 0
        self.dma = dma


class Sched:
    ENGS = ("pe", "act", "dve", "pool", "sp")
    NLANES = 6

    def __init__(self, nc, gc):
        self.nc = nc
        self.gc = gc
        self.ops = {e: [] for e in self.ENGS}
        self.bufs = {}
        self.phase = 0
        self.lane_ops = {}
        self.lane_n = {e: 0 for e in self.ENGS}
        self.pending = {e: [] for e in self.ENGS}
        self.last = {e: None for e in self.ENGS}

    def _add(self, eng, fn, r, w, dma):
        if getattr(self, "rec", None) is not None:
            self.rec.append((eng, fn, self.keymap(r), self.keymap(w), dma))
            return None
        op = _Op(eng, fn, dma)
        deps = []
        for k in r:
            st = self.bufs.setdefault(k, [None, []])
            if st[0] is not None:
                deps.append(st[0])
        for k in w:
            st = self.bufs.setdefault(k, [None, []])
            if st[0] is not None:
                deps.append(st[0])
            deps.extend(st[1])
        deps.extend(self.pending[eng])
        self.pending[eng] = []
        if dma:
            lane = self.lane_n[eng] % self.NLANES
            self.lane_n[eng] += 1
            key = ("lane", eng, lane)
            prev = self.lane_ops.get(key)
            if prev is not None:
                deps.append(prev)
            self.lane_ops[key] = op
            op.semkey = key
            op.val = (prev.val if prev is not None else self.gc.lane_vals.get(key, 0)) + 16
            op.needed = True
        else:
            op.semkey = ("eng", eng, self.phase)
        seen = set()
        for d in deps:
            if d is op or id(d) in seen:
                continue
            seen.add(id(d))
            if (not d.dma) and d.eng == eng and eng == "pe":
                continue
            d.needed = True
            op.deps.append(d)
        for k in r:
            self.bufs[k][1].append(op)
        for k in w:
            self.bufs[k] = [op, []]
        self.ops[eng].append(op)
        self.last[eng] = op
        return op

    def op(self, eng, fn, r=(), w=()):
        return self._add(eng, fn, r, w, False)

    def dma(self, eng, out, in_, r=(), w=()):
        return self._add(eng, lambda e: e.dma_start(out=out, in_=in_), r, w, True)

    def dmafn(self, eng, fn, r=(), w=()):
        return self._add(eng, fn, r, w, True)

    def cc(self, fn, r=(), w=()):
        op = self._add("pool", fn, r, w, False)
        op.semkey = ("cc", self.gc.next_uid())
        op.needed = True
        op.val = 1
        op.dma = True
        return op

    def barrier(self):
        lasts = [o for o in self.last.values() if o is not None] + list(self.lane_ops.values())
        for e in self.ENGS:
            self.pending[e] = list(lasts)
        self.bufs = {}
        self.phase += 1

    def emit(self):
        nc = self.nc
        gc = self.gc
        for e in self.ENGS:
            if self.ops[e] and not self.ops[e][-1].dma:
                self.ops[e][-1].needed = True
        cnt = {}
        for e in self.ENGS:
            for op in self.ops[e]:
                if not op.dma and op.needed:
                    cnt[op.semkey] = cnt.get(op.semkey, 0) + 1
                    op.val = cnt[op.semkey]
        sems = {}
        finals = {}
        for e in self.ENGS:
            for op in self.ops[e]:
                if not op.needed:
                    continue
                k = op.semkey
                if k not in sems:
                    if k[0] == "lane":
                        if k not in gc.lane_sems:
                            gc.lane_sems[k] = gc.stack.enter_context(nc.semaphore("l_" + "_".join(str(x) for x in k[1:])))
                        sems[k] = gc.lane_sems[k]
                    else:
                        sems[k] = gc.stack.enter_context(nc.semaphore(f"s{gc.next_uid()}_" + "_".join(str(x) for x in k)))
                finals[k] = max(finals.get(k, 0), op.val)
        for k, v in finals.items():
            if k[0] == "lane":
                gc.lane_vals[k] = v

        def run(engname, e):
            waited = {}
            for op in self.ops[engname]:
                for d in op.deps:
                    if waited.get(d.semkey, 0) >= d.val:
                        continue
                    e.wait_ge(sems[d.semkey], d.val)
                    waited[d.semkey] = d.val
                ins = op.fn(e)
                if op.needed:
                    if op.semkey[0] == "cc":
                        ins.then_inc(sems[op.semkey])
                    else:
                        ins.then_inc(sems[op.semkey], 16 if op.dma else 1)
            for k, v in finals.items():
                if waited.get(k, 0) < v:
                    e.wait_ge(sems[k], v)

        with nc.Block() as block:
            @block.tensor
            def _(e):
                run("pe", e)

            @block.scalar
            def _(e):
                run("act", e)

            @block.vector
            def _(e):
                run("dve", e)

            @block.gpsimd
            def _(e):
                run("pool", e)

            @block.sync
            def _(e):
                run("sp", e)


class GC:
    def __init__(self, stack):
        self.stack = stack
        self.lane_sems = {}
        self.lane_vals = {}
        self.uid = 0

    def next_uid(self):
        self.uid += 1
        return self.uid


class KB:
    def __init__(self, nc, stack, gc):
        self.nc = nc
        self.stack = stack
        self.gc = gc
        self.S = Sched(nc, gc)
        self.tag = f"_u{gc.next_uid()}"

    def sb(self, name, shape, dt):
        return self.stack.enter_context(self.nc.sbuf_tensor(name + self.tag, list(shape), dt))

    def ps(self, name, shape, dt):
        return self.stack.enter_context(self.nc.psum_tensor(name + self.tag, list(shape), dt))

    def dram(self, name, shape, dt, kind):
        return self.nc.dram_tensor(name, list(shape), dt, kind=kind).ap()

    def mm(self, out, lhsT, rhs, start, stop, r, w):
        self.S.op("pe", lambda e: e.matmul(out, lhsT, rhs, start=start, stop=stop), r, w)

    def tr(self, out, in_, ident, r, w):
        self.S.op("pe", lambda e: e.transpose(out, in_, ident), r, w)

    def act(self, out, in_, func, r, w, **kw):
        self.S.op("act", lambda e: e.activation(out, in_, func, **kw), r, w)

    def ts(self, eng, out, in0, s1, s2, op0, op1, r, w):
        if op1 is None:
            self.S.op(eng, lambda e: e.tensor_scalar(out, in0, s1, None, op0), r, w)
        else:
            self.S.op(eng, lambda e: e.tensor_scalar(out, in0, s1, s2, op0, op1), r, w)

    def tt(self, eng, out, in0, in1, op, r, w):
        self.S.op(eng, lambda e: e.tensor_tensor(out, in0, in1, op), r, w)

    def stt(self, eng, out, in0, scalar, in1, op0, op1, r, w):
        self.S.op(eng, lambda e: e.scalar_tensor_tensor(out, in0, scalar, in1, op0, op1), r, w)

    def cp(self, eng, out, in_, r, w):
        if eng == "act":
            self.S.op("act", lambda e: e.copy(out, in_), r, w)
        else:
            self.S.op(eng, lambda e: e.tensor_copy(out, in_), r, w)

    def rsum(self, eng, out, in_, r, w):
        self.S.op(eng, lambda e: e.reduce_sum(out, in_, AX.X), r, w)

    def memset(self, eng, ap, v, r, w):
        self.S.op(eng, lambda e: e.memset(ap, v), r, w)

    def dma(self, eng, out, in_, r, w):
        self.S.dma(eng, out, in_, r, w)


G_AV, G_BQ, G_BK, G_CQA, G_CKVA, G_CQN, G_CKN, G_CKR, G_TOT = 0, 256, 640, 1024, 1408, 1664, 2240, 2624, 2656


def v3(ap, g):
    return ap.rearrange("p (g d) -> p g d", g=g)


def bc3(ap2, d):
    p, g = ap2.shape
    return ap2.unsqueeze(2).to_broadcast([p, g, d])


def emit_A(nc, gc, d, NT_OWN, L):
    NT = NT_OWN + 2
    T = NT * 128
    n = NT_OWN * 128
    stack = contextlib.ExitStack()
    with stack:
        K = KB(nc, stack, gc)
        S = K.S
        x_d = d["x"] if L == 0 else d["xs_0"]
        cT_d, rope_d, ident_d = d["cT"], d["rope"], d["ident"]
        wada_d, bada_d, nmix_d, win_d = d[f"w_ada_{L}"], d[f"b_adaT_{L}"], d[f"norm_mixT_{L}"], d[f"w_in_{L}"]
        wsT_d, bs_d, gains_d, wq_d, wkv_d = d[f"w_sT_{L}"], d[f"b_s_{L}"], d[f"gains_{L}"], d[f"wq_{L}"], d[f"wkv_{L}"]
        oa_d, qbT_d, qcT_d, modT_d = d[f"oa_{L}"], d[f"qbT_{L}"], d[f"qcT_{L}"], d[f"modT_{L}"]
        PT = min(4, NT_OWN)
        HT = min(3, NT_OWN)
        kcT_ctx, vc_ctx = d[f"kcT_ctx_{L}"], d[f"vc_ctx_{L}"]
        kvb_own, kvb_ctx, kvb_lo, kvb_hi = d[f"kvb_own_{L}"], d[f"kvb_ctx_{L}"], d[f"kvb_lo_{L}"], d[f"kvb_hi_{L}"]
        ident = K.sb("ident_b", [128, 128], BF16)
        identf = K.sb("identf", [128, 128], F32)
        cT = K.sb("cTs", [128, 16], F32)
        scT = K.sb("scT", [128, 16], F32)
        bada = K.sb("bada", [128, 48], F32)
        nmix = K.sb("nmix", [128, 8], F32)
        modT = K.sb("modTs", [128, 96], F32)
        A1 = K.sb("A1", [128, 16], F32)
        wst = [K.sb(f"wst{i}", [128, 8, 512], F32) for i in range(2)]
        win = K.sb("win", [128, 8, INW], BF16)
        wsT = K.sb("wsT", [128, 4, 128], BF16)
        bs = K.sb("bs", [128, 4], F32)
        gains = K.sb("gainss", [128, G_TOT], F32)
        wq = K.sb("wqs", [128, 3, 576], BF16)
        wkv = K.sb("wkvs", [128, 2, 768], BF16)
        xt = [K.sb(f"xt{i}", [128, D], F32) for i in range(2)]
        ropet = [K.sb(f"ropet{i}", [128, 32], F32) for i in range(2)]
        sqj_2 = [K.sb("sqj%d" % i_, [128, D], F32) for i_ in range(2)]
        st_2 = [K.sb("st%d" % i_, [128, 64], F32) for i_ in range(2)]
        xn_2 = [K.sb("xn%d" % i_, [128, D], BF16) for i_ in range(2)]
        hT_2 = [K.sb("hT%d" % i_, [128, 8, 128], BF16) for i_ in range(2)]
        z_2 = [K.sb("z%d" % i_, [128, INW], F32) for i_ in range(2)]
        g1_2 = [K.sb("g1%d" % i_, [128, 512], F32) for i_ in range(2)]
        g2_2 = [K.sb("g2%d" % i_, [128, 512], F32) for i_ in range(2)]
        gg_2 = [K.sb("gg%d" % i_, [128, 512], F32) for i_ in range(2)]
        vnb_2 = [K.sb("vnb%d" % i_, [128, 256], BF16) for i_ in range(2)]
        oa_2 = [K.sb("oas%d" % i_, [128, 256], BF16) for i_ in range(2)]
        t384_2 = [K.sb("t384%d" % i_, [128, 384], F32) for i_ in range(2)]
        u384_2 = [K.sb("u384%d" % i_, [128, 384], F32) for i_ in range(2)]
        qnb_2 = [K.sb("qnb%d" % i_, [128, 384], BF16) for i_ in range(2)]
        qbTs_2 = [K.sb("qbTs%d" % i_, [128, 3, 128], BF16) for i_ in range(2)]
        kvbs_2 = [K.sb("kvbs%d" % i_, [128, 768], BF16) for i_ in range(2)]
        qab_2 = [K.sb("qab%d" % i_, [128, 384], BF16) for i_ in range(2)]
        qaT_2 = [K.sb("qaT%d" % i_, [128, 3, 128], BF16) for i_ in range(2)]
        qf_2 = [K.sb("qf%d" % i_, [128, 576], F32) for i_ in range(2)]
        qs_2 = [K.sb("qs%d" % i_, [128, 576], F32) for i_ in range(2)]
        qc_2 = [K.sb("qc%d" % i_, [128, 6, 96], BF16) for i_ in range(2)]
        rt_2 = [[K.sb(f"rt{i}_{j_}", [128, 48], F32) for i in range(4)] for j_ in range(2)]
        qcTs_2 = [K.sb("qcTs%d" % i_, [96, 6, 128], BF16) for i_ in range(2)]
        kvab_2 = [K.sb("kvab%d" % i_, [128, 256], BF16) for i_ in range(2)]
        kvaT_2 = [K.sb("kvaT%d" % i_, [128, 2, 128], BF16) for i_ in range(2)]
        kvf_2 = [K.sb("kvf%d" % i_, [128, 768], F32) for i_ in range(2)]
        kc_2 = [K.sb("kc%d" % i_, [128, 6, 96], BF16) for i_ in range(2)]
        kr_2 = [K.sb("kr%d" % i_, [128, 32], F32) for i_ in range(2)]
        krr_2 = [K.sb("krr%d" % i_, [128, 32], F32) for i_ in range(2)]
        vcs_2 = [K.sb("vcs%d" % i_, [128, 6, 65], BF16) for i_ in range(2)]
        kcTs_2 = [K.sb("kcTs%d" % i_, [96, 6, 128], BF16) for i_ in range(2)]
        PB = [K.ps(f"pb{i}", [128, 512], F32) for i in range(8)]
        PB0b = PB[0].bitcast(BF16)

        K.dma("sp", identf[:, :], ident_d[:, :], [], ["identf"])
        K.cp("dve", ident[:, :], identf[:, :], ["identf"], ["ident"])
        K.dma("sp", cT[:, :], cT_d[:, :], [], ["cT"])
        K.dma("sp", bada[:, :], bada_d[:, :], [], ["bada"])
        K.dma("sp", nmix[:, :], nmix_d[:, :], [], ["nmix"])
        K.dma("sp", bs[:, :], bs_d[:, :], [], ["bs"])
        K.dma("sp", gains[:, :], gains_d[:, :], [], ["gains"])
        K.dma("pool", win[:, :, :], win_d[:, :, :], [], ["win"])
        K.dma("pool", wsT[:, :, :], wsT_d[:, :, :], [], ["wsT"])
        K.dma("pool", wq[:, :, :], wq_d[:, :, :], [], ["wq"])
        K.dma("pool", wkv[:, :, :], wkv_d[:, :, :], [], ["wkv"])
        K.memset("pool", vcs_2[0][:, :, :], 1.0, [], [("vcs", 0)])
        K.memset("pool", vcs_2[1][:, :, :], 1.0, [], [("vcs", 1)])
        K.act(scT[:, :], cT[:, :], AF.Silu, ["cT"], ["scT"])
        for grp in range(12):
            b = grp % 2
            K.dma("sp", wst[b][:, :, :], wada_d[:, :, grp * 512:(grp + 1) * 512], [], [("wst", b)])
            for jj in range(4):
                j = grp * 4 + jj
                for c in range(8):
                    K.mm(PB[1][:, 2 * j:2 * j + 2], wst[b][:, c, jj * 128:(jj + 1) * 128], scT[:, 2 * c:2 * c + 2],
                         c == 0, c == 7, [("wst", b), "scT"], ["pb1"])
        K.tt("dve", v3(modT[:, :], 48), v3(PB[1][:, 0:96], 48), bc3(bada[:, :], 2), ALU.add, ["pb1", "bada"], ["modT"])
        K.dma("sp", modT_d[:, :], modT[:, :], ["modT"], [])
        K.stt("dve", v3(A1[:, :], 8), v3(modT[:, 16:32], 8), 1.0, bc3(nmix[:, :], 2), ALU.add, ALU.mult,
              ["modT", "nmix"], ["A1"])

        cur = {}

        def rstd_of(ss, n, dim, extra=None):
            K.ts("dve", ss, ss, 1.0 / dim, EPS, ALU.mult, ALU.add, ["st"], ["st"])
            K.act(ss, ss, AF.Sqrt, ["st"], ["st"])
            K.S.op("dve", lambda e, a=ss: e.reciprocal(a, a), ["st"], ["st"])
            if extra is not None:
                K.ts("dve", ss, ss, extra, None, ALU.mult, None, ["st"], ["st"])

        def rope(src3, dst3, G, rp, rkeys, wkeys):
            for a in range(2):
                o = 16 * a
                cos = rp[:, 16 * a:16 * a + 8].unsqueeze(1).to_broadcast([128, G, 8])
                sin = rp[:, 16 * a + 8:16 * a + 16].unsqueeze(1).to_broadcast([128, G, 8])
                x1 = src3[:, :, o:o + 8]
                x2 = src3[:, :, o + 8:o + 16]
                t = [v3(cur["rt"][i][:, 0:G * 8], G) for i in range(4)]
                K.tt("pool", t[0], x1, cos, ALU.mult, rkeys, ["rt0"])
                K.tt("pool", t[1], x2, sin, ALU.mult, rkeys, ["rt1"])
                K.tt("dve", dst3[:, :, o:o + 8], t[0], t[1], ALU.subtract, ["rt0", "rt1"], wkeys)
                K.tt("pool", t[2], x2, cos, ALU.mult, rkeys, ["rt2"])
                K.tt("pool", t[3], x1, sin, ALU.mult, rkeys, ["rt3"])
                K.tt("dve", dst3[:, :, o + 8:o + 16], t[2], t[3], ALU.add, ["rt2", "rt3"], wkeys)

        SHARED = {"ident", "identf", "cT", "scT", "bada", "nmix", "modT", "A1", "win", "wsT", "bs", "gains", "wq", "wkv",
                  "kvb_x", "vc_x", "kc_x"}
        recs = []
        for t in range(NT):
            b = t % 2
            wh = 0 if t < NT_OWN else 1
            base = 4 * b
            PBT = PB[base].bitcast(BF16)
            kT = f"pb{base}"
            (sqj, st, xn, hT, z, g1, g2, gg, vnb, oa, t384, u384, qnb, qbTs, kvbs, qab, qaT, qf, qs, qc, qcTs, kvab, kvaT,
             kvf, kc, kr, krr, vcs, kcTs) = [lst[b] for lst in (
                sqj_2, st_2, xn_2, hT_2, z_2, g1_2, g2_2, gg_2, vnb_2, oa_2, t384_2, u384_2, qnb_2, qbTs_2, kvbs_2, qab_2,
                qaT_2, qf_2, qs_2, qc_2, qcTs_2, kvab_2, kvaT_2, kvf_2, kc_2, kr_2, krr_2, vcs_2, kcTs_2)]
            cur["rt"] = rt_2[b]
            S.rec = []
            S.keymap = lambda ks, b=b: [k if (isinstance(k, tuple) or k in SHARED or k.startswith("pb")) else (k, b) for k in ks]
            recs.append(S.rec)
            rows = slice(t * 128, (t + 1) * 128)
            X = xt[b]
            K.dma("sp", X[:, :], x_d[rows, :], [], [("xt", b)])
            K.dma("sp", ropet[b][:, :], rope_d[rows, :], [], [("rope", b)])
            K.tt("dve", sqj[:, :], X[:, :], X[:, :], ALU.mult, [("xt", b)], ["sqj"])
            K.rsum("dve", st[:, 0:1], sqj[:, :], ["sqj"], ["st"])
            rstd_of(st[:, 0:1], 1, D)
            K.act(xn[:, :], X[:, :], AF.Copy, [("xt", b), "st"], ["xn"], scale=st[:, 0:1])
            for c in range(8):
                K.tr(PBT[:, c * 128:(c + 1) * 128], xn[:, c * 128:(c + 1) * 128], ident[:, :], ["xn", "ident"], [kT])
            for c in range(8):
                K.ts("dve", hT[:, c, :], PBT[:, c * 128:(c + 1) * 128], A1[:, 2 * c + wh:2 * c + wh + 1],
                     modT[:, 2 * c + wh:2 * c + wh + 1], ALU.mult, ALU.add, [kT, "A1", "modT"], ["hT"])
            for k5 in range(5):
                n0 = k5 * 512
                n1 = min(INW, n0 + 512)
                pb = base + 1 + k5 % 2
                for c in range(8):
                    K.mm(PB[pb][:, 0:n1 - n0], hT[:, c, :], win[:, c, n0:n1], c == 0, c == 7, ["hT", "win"], [f"pb{pb}"])
                K.cp("act", z[:, n0:n1], PB[pb][:, 0:n1 - n0], [f"pb{pb}"], ["z"])
            za = z[:, 0:512]
            K.tt("pool", g1[:, :], za, za, ALU.mult, ["z"], ["g1"])
            K.ts("dve", g1[:, :], g1[:, :], 0.044715, 1.0, ALU.mult, ALU.add, ["g1"], ["g1"])
            K.tt("pool", g1[:, :], g1[:, :], za, ALU.mult, ["g1", "z"], ["g1"])
            K.act(g2[:, :], g1[:, :], AF.Sigmoid, ["g1"], ["g2"], scale=1.5957691216057308)
            K.tt("dve", gg[:, :], g2[:, :], za, ALU.mult, ["g2", "z"], ["gg"])
            K.tt("pool", g1[:, 0:256], gg[:, 256:512], gg[:, 256:512], ALU.mult, ["gg"], ["g1"])
            K.rsum("dve", st[:, 0:1], g1[:, 0:256], ["g1"], ["st"])
            rstd_of(st[:, 0:1], 1, 256)
            K.ts("dve", g1[:, 256:512], gg[:, 256:512], st[:, 0:1], None, ALU.mult, None, ["gg", "st", "g1"], ["g1"])
            K.tt("pool", vnb[:, :], g1[:, 256:512], gains[:, G_AV:G_AV + 256], ALU.mult, ["g1", "gains"], ["vnb"])
            for hd in range(4):
                K.mm(PB[base + 3][:, hd * 64:(hd + 1) * 64], wsT[:, hd, :], vnb[:, hd * 64:(hd + 1) * 64], True, True,
                     ["wsT", "vnb"], [f"pb{base + 3}a"])
            for hd in range(4):
                K.stt("dve", oa[:, hd * 64:(hd + 1) * 64], PB[base + 3][:, hd * 64:(hd + 1) * 64], bs[:, hd:hd + 1],
                      gg[:, hd * 64:(hd + 1) * 64], ALU.add, ALU.mult, [f"pb{base + 3}a", "bs", "gg"], ["oa"])
            K.dma("sp", oa_d[rows, :], oa[:, :], ["oa"], [])
            for which, (o0, gofs, extra) in enumerate(((512, G_BQ, 0.125), (896, G_BK, None))):
                src = z[:, o0:o0 + 384]
                K.tt("pool", t384[:, :], src, src, ALU.mult, ["z"], ["t384"])
                K.rsum("dve", st[:, 0:6], v3(t384[:, :], 6), ["t384"], ["st"])
                rstd_of(st[:, 0:6], 6, 64, extra)
                K.tt("dve", v3(u384[:, :], 6), v3(src, 6), bc3(st[:, 0:6], 64), ALU.mult, ["z", "st"], ["u384"])
                dst = qnb[:, :] if which == 0 else kvbs[:, 0:384]
                K.tt("pool", dst, u384[:, :], gains[:, gofs:gofs + 384], ALU.mult, ["u384", "gains"],
                     ["qnb" if which == 0 else "kvbs"])
            for pr in range(3):
                K.tr(PBT[:, pr * 128:(pr + 1) * 128], qnb[:, pr * 128:(pr + 1) * 128], ident[:, :], ["qnb", "ident"], [kT])
            K.cp("dve", qbTs[:, :, :], v3(PBT[:, 0:384], 3), [kT], ["qbTs"])
            K.dma("sp", qbT_d[:, :, rows].rearrange("a p t -> p a t"), qbTs[:, :, :], ["qbTs"], [])
            K.cp("act", kvbs[:, 384:768], z[:, 1280:1664], ["z"], ["kvbs"])
            if t < NT_OWN:
                K.dma("sp", kvb_own[rows, :], kvbs[:, :], ["kvbs"], ["kvb_x"])
                if t < HT:
                    K.dma("sp", kvb_lo[t * 128:(t + 1) * 128, :], kvbs[:, :], ["kvbs"], ["kvb_x"])
                if t >= NT_OWN - HT:
                    t2 = t - (NT_OWN - HT)
                    K.dma("sp", kvb_hi[t2 * 128:(t2 + 1) * 128, :], kvbs[:, :], ["kvbs"], ["kvb_x"])
            else:
                K.dma("sp", kvb_ctx[(t - NT_OWN) * 128:(t - NT_OWN + 1) * 128, :], kvbs[:, :], ["kvbs"], ["kvb_x"])
            src = z[:, 1664:2048]
            K.tt("pool", t384[:, :], src, src, ALU.mult, ["z"], ["t384"])
            K.rsum("dve", st[:, 0:1], t384[:, :], ["t384"], ["st"])
            rstd_of(st[:, 0:1], 1, 384)
            K.ts("dve", u384[:, :], src, st[:, 0:1], None, ALU.mult, None, ["z", "st"], ["u384"])
            K.tt("pool", qab[:, :], u384[:, :], gains[:, G_CQA:G_CQA + 384], ALU.mult, ["u384", "gains"], ["qab"])
            for c in range(3):
                K.tr(PBT[:, c * 128:(c + 1) * 128], qab[:, c * 128:(c + 1) * 128], ident[:, :], ["qab", "ident"], [kT])
            K.cp("dve", qaT[:, :, :], v3(PBT[:, 0:384], 3), [kT], ["qaT"])
            for c in range(3):
                K.mm(PB[base + 1][:, 0:512], qaT[:, c, :], wq[:, c, 0:512], c == 0, c == 2, ["qaT", "wq"], [f"pb{base + 1}"])
            for c in range(3):
                K.mm(PB[base + 3][:, 256:320], qaT[:, c, :], wq[:, c, 512:576], c == 0, c == 2, ["qaT", "wq"], [f"pb{base + 3}b"])
            K.cp("act", qf[:, 0:512], PB[base + 1][:, 0:512], [f"pb{base + 1}"], ["qf"])
            K.cp("act", qf[:, 512:576], PB[base + 3][:, 256:320], [f"pb{base + 3}b"], ["qf"])
            qf3 = v3(qf[:, :], 6)
            qs3 = v3(qs[:, :], 6)
            K.tt("pool", qs[:, :], qf[:, :], qf[:, :], ALU.mult, ["qf"], ["qs"])
            K.rsum("dve", st[:, 0:6], qs3[:, :, 0:64], ["qs"], ["st"])
            K.rsum("dve", st[:, 8:14], qs3[:, :, 64:96], ["qs"], ["st"])
            rstd_of(st[:, 0:6], 6, 64)
            rstd_of(st[:, 8:14], 6, 32)
            K.tt("dve", qs3[:, :, 0:64], qf3[:, :, 0:64], bc3(st[:, 0:6], 64), ALU.mult, ["qf", "st", "qs"], ["qs"])
            K.tt("dve", qs3[:, :, 64:96], qf3[:, :, 64:96], bc3(st[:, 8:14], 32), ALU.mult, ["qf", "st", "qs"], ["qs"])
            K.tt("pool", qf[:, :], qs[:, :], gains[:, G_CQN:G_CQN + 576], ALU.mult, ["qs", "gains"], ["qf"])
            K.cp("act", qc[:, :, 0:64], qf3[:, :, 0:64], ["qf"], ["qc"])
            rope(qf3[:, :, 64:96], qc[:, :, 64:96], 6, ropet[b], ["qf", ("rope", b)], ["qc"])
            for h in range(6):
                K.tr(PBT[0:96, h * 128:(h + 1) * 128], qc[:, h, :], ident[:, :], ["qc", "ident"], [kT])
            K.cp("dve", qcTs[:, :, :], v3(PBT[0:96, 0:768], 6), [kT], ["qcTs"])
            K.dma("sp", qcT_d[:, :, rows].rearrange("h p t -> p h t"), qcTs[:, :, :], ["qcTs"], [])
            src = z[:, 2048:2304]
            K.tt("pool", t384[:, 0:256], src, src, ALU.mult, ["z"], ["t384"])
            K.rsum("dve", st[:, 0:1], t384[:, 0:256], ["t384"], ["st"])
            rstd_of(st[:, 0:1], 1, 256)
            K.ts("dve", u384[:, 0:256], src, st[:, 0:1], None, ALU.mult, None, ["z", "st"], ["u384"])
            K.tt("pool", kvab[:, :], u384[:, 0:256], gains[:, G_CKVA:G_CKVA + 256], ALU.mult, ["u384", "gains"], ["kvab"])
            for c in range(2):
                K.tr(PBT[:, c * 128:(c + 1) * 128], kvab[:, c * 128:(c + 1) * 128], ident[:, :], ["kvab", "ident"], [kT])
            K.cp("dve", kvaT[:, :, :], v3(PBT[:, 0:256], 2), [kT], ["kvaT"])
            for c in range(2):
                K.mm(PB[base + 2][:, 0:512], kvaT[:, c, :], wkv[:, c, 0:512], c == 0, c == 1, ["kvaT", "wkv"], [f"pb{base + 2}"])
            for c in range(2):
                K.mm(PB[base + 3][:, 0:256], kvaT[:, c, :], wkv[:, c, 512:768], c == 0, c == 1, ["kvaT", "wkv"], [f"pb{base + 3}a"])
            K.cp("act", kvf[:, 0:512], PB[base + 2][:, 0:512], [f"pb{base + 2}"], ["kvf"])
            K.cp("act", kvf[:, 512:768], PB[base + 3][:, 0:256], [f"pb{base + 3}a"], ["kvf"])
            kvf3 = v3(kvf[:, :], 6)
            K.cp("act", vcs[:, :, 0:64], kvf3[:, :, 64:128], ["kvf"], ["vcs"])
            if t < NT_OWN:
                K.dma("sp", d[f"vc_own_{L}_{t // PT}"][(t % PT) * 128:(t % PT + 1) * 128, :],
                      vcs[:, :, :].rearrange("p h d -> p (h d)"), ["vcs"], ["vc_x"])
            else:
                K.dma("sp", vc_ctx[(t - NT_OWN) * 128:(t - NT_OWN + 1) * 128, :], vcs[:, :, :].rearrange("p h d -> p (h d)"), ["vcs"], ["vc_x"])
            t3 = v3(t384[:, :], 6)
            u3 = v3(u384[:, :], 6)
            K.tt("pool", t3, kvf3[:, :, 0:64], kvf3[:, :, 0:64], ALU.mult, ["kvf"], ["t384"])
            K.rsum("dve", st[:, 0:6], t3, ["t384"], ["st"])
            rstd_of(st[:, 0:6], 6, 64)
            K.tt("dve", u3, kvf3[:, :, 0:64], bc3(st[:, 0:6], 64), ALU.mult, ["kvf", "st"], ["u384"])
            K.tt("pool", kc[:, :, 0:64], u3, v3(gains[:, G_CKN:G_CKN + 384], 6), ALU.mult, ["u384", "gains"], ["kc"])
            src = z[:, 2304:2336]
            K.tt("pool", kr[:, :], src, src, ALU.mult, ["z"], ["kr"])
            K.rsum("dve", st[:, 0:1], kr[:, :], ["kr"], ["st"])
            rstd_of(st[:, 0:1], 1, 32)
            K.ts("dve", kr[:, :], src, st[:, 0:1], None, ALU.mult, None, ["z", "st", "kr"], ["kr"])
            K.tt("pool", kr[:, :], kr[:, :], gains[:, G_CKR:G_CKR + 32], ALU.mult, ["kr", "gains"], ["kr"])
            rope(v3(kr[:, :], 1), v3(krr[:, :], 1), 1, ropet[b], ["kr", ("rope", b)], ["krr"])
            K.cp("dve", kc[:, :, 64:96], krr[:, :].unsqueeze(1).to_broadcast([128, 6, 32]), ["krr"], ["kc"])
            for h in range(6):
                K.tr(PBT[0:96, h * 128:(h + 1) * 128], kc[:, h, :], ident[:, :], ["kc", "ident"], [kT])
            K.cp("dve", kcTs[:, :, :], v3(PBT[0:96, 0:768], 6), [kT], ["kcTs"])
            if t < NT_OWN:
                K.dma("sp", d[f"kcT_own_{L}_{t // PT}"].rearrange("(h p) t -> p h t", h=6)[:, :, (t % PT) * 128:(t % PT + 1) * 128],
                      kcTs[:, :, :], ["kcTs"], ["kc_x"])
            else:
                K.dma("sp", kcT_ctx.rearrange("(h p) t -> p h t", h=6)[:, :, (t - NT_OWN) * 128:(t - NT_OWN + 1) * 128],
                      kcTs[:, :, :], ["kcTs"], ["kc_x"])
        S.rec = None
        for p0 in range(0, NT, 2):
            pair = recs[p0:p0 + 2]
            ptr = [0] * len(pair)
            while any(ptr[j_] < len(pair[j_]) for j_ in range(len(pair))):
                for j_, r_ in enumerate(pair):
                    if ptr[j_] < len(r_):
                        S._add(*r_[ptr[j_]])
                        ptr[j_] += 1
                        while ptr[j_] < len(r_) and r_[ptr[j_]][0] == "pe" and r_[ptr[j_] - 1][0] == "pe":
                            S._add(*r_[ptr[j_]])
                            ptr[j_] += 1
        S.emit()


def _kmaj(w, kc):
    return np.ascontiguousarray(w.reshape(kc, 128, -1).transpose(1, 0, 2))


def _colT(v):
    return np.ascontiguousarray(v.reshape(-1, 128).T)


def _bcrow(v):
    return np.ascontiguousarray(np.broadcast_to(v[None, :], (128, v.shape[0])))


def rope_tables(NT_OWN, rank):
    half = 16
    inv = (np.float32(10000.0) ** (-(np.arange(0, half, 2, dtype=np.float32)) / np.float32(half))).astype(np.float32)
    pos = np.arange(NT_OWN * 128) + rank * NT_OWN * 128
    out = np.zeros(((NT_OWN + 2) * 128, 32), np.float32)
    ar = (pos // GRID_W).astype(np.float32)[:, None] * inv[None, :]
    ac = (pos % GRID_W).astype(np.float32)[:, None] * inv[None, :]
    n = NT_OWN * 128
    out[:n, 0:8] = np.cos(ar)
    out[:n, 8:16] = np.sin(ar)
    out[:n, 16:24] = np.cos(ac)
    out[:n, 24:32] = np.sin(ac)
    out[n:, 0:8] = 1.0
    out[n:, 16:24] = 1.0
    return out


def prep_A(P, i, x_cur, xc_cur, NT_OWN, ranks_per_batch=4):
    B = x_cur.shape[0]
    gains = np.concatenate([
        P["a_v_norm"][i], np.tile(P["b_q_norm"][i], 6), np.tile(P["b_k_norm"][i], 6), P["c_q_a_norm"][i],
        P["c_kv_a_norm"][i], np.tile(P["c_q_norm"][i], 6), np.tile(P["c_k_norm"][i][:64], 6), P["c_k_norm"][i][64:]])
    shared = {
        "w_ada": _kmaj(P["w_ada"][i], 8),
        "b_adaT": _colT(P["b_ada"][i]),
        "norm_mixT": _colT(P["norm_mix"][i]),
        "w_in": _kmaj(P["w_in"][i], 8),
        "w_sT": np.ascontiguousarray(P["a_w_s"][i].transpose(2, 0, 1)),
        "b_s": np.ascontiguousarray(P["a_b_s"][i].T),
        "gains": _bcrow(gains.astype(np.float32)),
        "wq": _kmaj(P["c_w_q_up"][i], 3),
        "wkv": _kmaj(P["c_w_kv_up"][i], 2),
        "ident": np.eye(128, dtype=np.float32),
    }
    maps = []
    n = NT_OWN * 128
    for b in range(B):
        cT = np.stack([_colT(P["c"][b]), _colT(P["c_ctx"])], axis=2).reshape(128, 16)
        for r in range(ranks_per_batch):
            m = dict(shared)
            m["x"] = np.ascontiguousarray(np.concatenate([x_cur[b, r * n:(r + 1) * n], xc_cur[b]], axis=0))
            m["cT"] = np.ascontiguousarray(cT)
            m["rope"] = rope_tables(NT_OWN, r)
            maps.append(m)
    return maps


def _specials(NT_OWN):
    return sorted(set(t for t in (0, 1, NT_OWN - 2, NT_OWN - 1) if 0 <= t < NT_OWN))


def emit_B(nc, gc, d, NT_OWN, L, TB=4, NE=64, NR=4):
    NT = NT_OWN + 2
    T = NT * 128
    n = NT_OWN * 128
    NW = NT_OWN + 8
    NKT = NR * NT_OWN + 2
    QB = min(4, NT_OWN)
    CH = min(4, NT_OWN)
    specials = _specials(NT_OWN)
    BIG = 1.0e30
    stack = contextlib.ExitStack()
    with stack:
        K = KB(nc, stack, gc)
        S = K.S
        x_d = d["x"] if L == 0 else d["xs_0"]
        xo_d = d["xs_0"] if L == 0 else d["xo"]
        oa_d, qbT_d, qcT_d, modT_d, mix_d = d[f"oa_{L}"], d[f"qbT_{L}"], d[f"qcT_{L}"], d[f"modT_{L}"], d[f"mixd_{L}"]
        PCT = min(4, NT_OWN)
        HT = min(3, NT_OWN)
        kcT_ctx, vc_ctx = d[f"kcT_ctx_{L}"], d[f"vc_ctx_{L}"]
        kvb_own, kvb_ctx, kvb_glo, kvb_ghi = d[f"kvb_own_{L}"], d[f"kvb_ctx_{L}"], d[f"kvb_glo_{L}"], d[f"kvb_ghi_{L}"]
        btab_d, wout_d, nffn_d, wgr_d = d[f"btab_{L}"], d[f"w_out_{L}"], d[f"norm_ffnT_{L}"], d[f"wgr_{L}"]
        w1_d, w3_d, w2_d = d[f"w1_{L}"], d[f"w3_{L}"], d[f"w2_{L}"]
        ident_d, widx_d = d["ident"], d["widx"]

        AR = 45056
        arena = K.sb("arena", [128, AR], BF16)
        identb = K.sb("ident_b", [128, 128], BF16)
        identf = K.sb("identf", [128, 128], F32)
        ones = K.sb("ones", [128, 128], F32)
        modT = K.sb("modTs", [128, 96], F32)
        nffn = K.sb("nffn", [128, 8], F32)
        A2 = K.sb("A2", [128, 16], F32)
        G = [K.sb(f"G{i}", [128, D], F32) for i in range(4)]
        gbc = K.sb("gbc", [128, 128], F32)
        wgr = K.sb("wgrs", [128, 8, 72], F32)
        kvt = [K.sb(f"kvt{i}", [128, 768], BF16) for i in range(2)]
        qbt = [K.sb(f"qbt{i}", [128, 3, 128], BF16) for i in range(2)]
        PT = [K.sb(f"PT{i}", [128, 512], BF16) for i in range(3)]
        st = K.sb("st", [128, 64], F32)
        ob = K.sb("ob", [128, 384], BF16)
        oc = K.sb("oc", [128, 4, 64], BF16)
        ocT = K.sb("ocT", [65, 512], F32)
        qT = [K.sb(f"qT{i}", [96, 512], BF16) for i in range(2)]
        xt = [K.sb(f"xt{i}", [128, D], F32) for i in range(2)]
        mixrow = K.sb("mixrow", [128, D], BF16)
        mixT = K.sb("mixT", [128, 8, 128], BF16)
        tmpf = K.sb("tmpf", [128, D], F32)
        h2n = K.sb("h2n", [128, D], F32)
        h2Tf = K.sb("h2Tf", [128, 8, 128], F32)
        lg = K.sb("lg", [128, 72], F32)
        r64 = [K.sb(f"r64_{i}", [128, 64], F32) for i in range(4)]
        sil = [K.sb(f"sil{i}", [128, 256], F32) for i in range(2)]
        hT = K.sb("hTe", [128, 4, 256], BF16)
        PB = [K.ps(f"pb{i}", [128, 512], F32) for i in range(8)]
        PB0b = PB[0].bitcast(BF16)

        K.dma("sp", identf[:, :], ident_d[:, :], [], ["identf"])
        K.cp("dve", identb[:, :], identf[:, :], ["identf"], ["identb"])
        K.memset("dve", ones[:, :], 1.0, [], ["ones"])
        K.dma("sp", modT[:, :], modT_d[:, :], [], ["modT"])
        K.dma("sp", nffn[:, :], nffn_d[:, :], [], ["nffn"])
        K.dma("sp", wgr[:, :, :], wgr_d[:, :, :], [], ["wgr"])
        K.stt("dve", v3(A2[:, :], 8), v3(modT[:, 64:80], 8), 1.0, bc3(nffn[:, :], 2), ALU.add, ALU.mult,
              ["modT", "nffn"], ["A2"])
        for gi, (j0, wh) in enumerate(((16, 0), (16, 1), (40, 0), (40, 1))):
            for c in range(8):
                col = 2 * (j0 + c) + wh
                K.ts("dve", gbc[:, :], ones[:, :], modT[:, col:col + 1], None, ALU.mult, None, ["ones", "modT"], ["gbc"])
                K.mm(PB[7][:, (c % 4) * 128:(c % 4 + 1) * 128], gbc[:, :], identf[:, :], True, True, ["gbc", "identf"], ["pb7"])
                K.cp("act", G[gi][:, c * 128:(c + 1) * 128], PB[7][:, (c % 4) * 128:(c % 4 + 1) * 128], ["pb7"], [("G", gi)])

        o1 = 3 * NW * 128
        o2 = o1 + NW * 390
        KbT = arena[:, 0:o1].rearrange("p (a t) -> p a t", a=3)
        Vb = arena[:, o1:o2].rearrange("p (s h d) -> p s h d", s=NW, h=6)
        bt0 = arena[:, o2:o2 + 6144].rearrange("p (h e) -> p h e", h=6)
        btS = arena[:, o2 + 6144:o2 + 12288].rearrange("p (h e) -> p h e", h=6)
        assert o2 + 12288 <= AR
        K.memset("pool", arena[:, o1:o2], 1.0, [], ["Vb"])
        K.dma("sp", bt0, btab_d[0], [], ["bt0"])
        widx = K.sb("widx", [128, 6], I32)
        K.dma("sp", widx[:, :], widx_d[:, :], [], ["widx"])
        for s in range(NW):
            b = s % 2
            if s < 3 or NT_OWN + 3 <= s < NT_OWN + 6:
                srcg = kvb_ghi if s < 3 else kvb_glo
                col = s if s < 3 else s - NT_OWN
                K.S.dmafn("pool", lambda e, o=kvt[b][:, :], ix=widx[:, col:col + 1], sg=srcg: e.indirect_dma_start(
                    out=o, out_offset=None, in_=sg[:, :], in_offset=bass.IndirectOffsetOnAxis(ap=ix, axis=0)),
                    ["widx"], [("kvt", b)])
            elif s < NT_OWN + 3:
                K.dma("sp", kvt[b][:, :], kvb_own[(s - 3) * 128:(s - 2) * 128, :], [], [("kvt", b)])
            else:
                K.dma("sp", kvt[b][:, :], kvb_ctx[(s - NT_OWN - 6) * 128:(s - NT_OWN - 5) * 128, :], [], [("kvt", b)])
            for pr in range(3):
                K.tr(PB0b[:, pr * 128:(pr + 1) * 128], kvt[b][:, pr * 128:(pr + 1) * 128], identb[:, :], [("kvt", b), "identb"], ["pb0"])
            K.cp("dve", KbT[:, :, s * 128:(s + 1) * 128], v3(PB0b[:, 0:384], 3), ["pb0"], ["KbT"])
            K.cp("pool", Vb[:, s, :, 0:64], v3(kvt[b][:, 384:768], 6), [("kvt", b), "Vb"], ["Vb"])
        DSK = 2
        itemsB = []
        for t in range(NT):
            b = t % 2
            own = t < NT_OWN
            rows = slice(t * 128, (t + 1) * 128)
            special = own and t in specials
            bt, btk = (btS, "btS") if special else (bt0, "bt0")
            if own:
                kts = [(t + j + 3, j) for j in range(-3, 4)] + [(NW - 2, None), (NW - 1, None)]
            else:
                kts = [(NW - 2, None), (NW - 1, None)]
            groups = [kts[i:i + 3] for i in range(0, len(kts), 3)]
            ob_bank = 4 + t % 2
            for h in range(6):
                nk0 = 0
                for gi, grp in enumerate(groups):
                    itemsB.append(dict(t=t, b=b, h=h, grp=grp, nk0=nk0, nkt=len(kts), ob_bank=ob_bank, bt=bt, btk=btk,
                                       first=(h == 0 and gi == 0), last=(h == 5 and gi == len(groups) - 1),
                                       special=special, rows=rows))
                    nk0 += len(grp)

        def qkB(i, it):
            b, h, t = it["b"], it["h"], it["t"]
            if it["first"]:
                K.dma("sp", qbt[b][:, :, :], qbT_d[:, :, it["rows"]].rearrange("a p t -> p a t"), [], [("qbt", b)])
                if it["special"]:
                    K.dma("sp", btS, btab_d[1 + specials.index(t)], [], ["btS"])
            pr, pb = h // 2, (h % 2) * 64
            bank = 1 + i % 3
            pi = i % 3
            for ii, (s_, j) in enumerate(it["grp"]):
                K.mm(PB[bank][:, ii * 128:(ii + 1) * 128], KbT[pb:pb + 64, pr, s_ * 128:(s_ + 1) * 128],
                     qbt[b][pb:pb + 64, pr, :], True, j is None, ["KbT", ("qbt", b)], [f"pb{bank}"])
                if j is not None:
                    e0 = (7 - 2 * j) * 64
                    K.mm(PB[bank][:, ii * 128:(ii + 1) * 128], identb[:, :], it["bt"][:, h, e0:e0 + 128], False, True,
                         ["identb", it["btk"]], [f"pb{bank}"])
            n_ = len(it["grp"]) * 128
            K.act(PT[pi][:, 0:n_], PB[bank][:, 0:n_], AF.Exp, [f"pb{bank}"], [("PT", pi)])

        def pvB(i, it):
            h = it["h"]
            pi = i % 3
            OB = PB[it["ob_bank"]]
            for ii, (s_, j) in enumerate(it["grp"]):
                nk = it["nk0"] + ii
                K.mm(OB[:, h * 65:(h + 1) * 65], PT[pi][:, ii * 128:(ii + 1) * 128], Vb[:, s_, h, :],
                     nk == 0, nk == it["nkt"] - 1, [("PT", pi), "Vb"], [f"pb{it['ob_bank']}"])
            if it["last"]:
                O3 = v3(OB[:, 0:390], 6)
                K.S.op("dve", lambda e, O3=O3: e.reciprocal(st[:, 0:6], O3[:, :, 64]), [f"pb{it['ob_bank']}"], ["st"])
                K.tt("dve", v3(ob[:, :], 6), O3[:, :, 0:64], bc3(st[:, 0:6], 64), ALU.mult, [f"pb{it['ob_bank']}", "st"], ["ob"])
                K.dma("sp", mix_d[it["rows"], 256:640], ob[:, :], ["ob"], [("mixd", it["t"])])

        for i in range(len(itemsB) + DSK):
            if i < len(itemsB):
                qkB(i, itemsB[i])
            if i - DSK >= 0:
                pvB(i - DSK, itemsB[i - DSK])

        S.barrier()
        Kc = [arena[:, i * 2048:(i + 1) * 2048] for i in range(3)]
        Vc = [arena[:, 6144 + i * 1040:6144 + (i + 1) * 1040].rearrange("p (k d) -> p k d", d=65) for i in range(3)]
        qblocks = [(t0, QB, list(range(NKT))) for t0 in range(0, NT_OWN, QB)] + [(NT_OWN, 2, [NKT - 2, NKT - 1])]
        SCALE_C = 96.0 ** -0.5
        itemsC = []
        qh = 0
        cc = 0
        for (t0, ntl, klist) in qblocks:
            nq = ntl * 128
            for h in range(6):
                b2 = qh % 2
                oc_bank = 4 + qh % 2
                qh += 1
                own_k = [k for k in klist if k < NR * NT_OWN]
                ctx_k = [k for k in klist if k >= NR * NT_OWN]
                chunks = [own_k[i:i + CH] for i in range(0, len(own_k), CH)] + ([ctx_k] if ctx_k else [])
                nk = 0
                for chk in chunks:
                    cb = cc % 3
                    cc += 1
                    for kt in range(len(chk)):
                        itemsC.append(dict(t0=t0, ntl=ntl, nq=nq, h=h, b2=b2, oc_bank=oc_bank, cb=cb, chk=chk, kt=kt,
                                           newq=(nk == 0), newchunk=(kt == 0), nk=nk, nkt=len(klist)))
                        nk += 1

        def qkC(i, it):
            h, b2, cb, nq = it["h"], it["b2"], it["cb"], it["nq"]
            if it["newq"]:
                K.dma("sp", qT[b2][:, 0:nq], qcT_d[h, :, it["t0"] * 128:it["t0"] * 128 + nq], [], [("qT", b2)])
            if it["newchunk"]:
                chk = it["chk"]
                k0, n_k = chk[0], len(chk)
                if k0 < NR * NT_OWN:
                    rk, l0 = k0 // NT_OWN, k0 % NT_OWN
                    pj, lt = l0 // PCT, l0 % PCT
                    assert lt + n_k <= PCT
                    ksrc = d[f"kcT_g_{L}_{pj}"].rearrange("(r h p) t -> r h p t", r=NR, h=6)[rk, h, :, lt * 128:(lt + n_k) * 128]
                    v0 = (rk * PCT + lt) * 128
                    vsrc = d[f"vc_g_{L}_{pj}"][v0:v0 + n_k * 128, h * 65:(h + 1) * 65]
                else:
                    c0_ = (k0 - NR * NT_OWN) * 128
                    ksrc = kcT_ctx.rearrange("(h p) t -> h p t", h=6)[h, :, c0_:c0_ + n_k * 128]
                    vsrc = vc_ctx[c0_:c0_ + n_k * 128, h * 65:(h + 1) * 65]
                K.dma("sp", Kc[cb][0:96, 0:n_k * 128], ksrc, [], [("Kc", cb)])
                K.dma("act", Vc[cb][:, 0:n_k, :], vsrc.rearrange("(k p) d -> p k d", p=128), [], [("Vc", cb)])
            bank = 1 + i % 3
            pi = i % 3
            kt = it["kt"]
            K.mm(PB[bank][:, 0:nq], Kc[cb][0:96, kt * 128:(kt + 1) * 128], qT[b2][:, 0:nq], True, True,
                 [("Kc", cb), ("qT", b2)], [f"pb{bank}"])
            K.act(PT[pi][:, 0:nq], PB[bank][:, 0:nq], AF.Exp, [f"pb{bank}"], [("PT", pi)], scale=SCALE_C)

        def pvC(i, it):
            pi = i % 3
            nq, ntl, oc_bank, cb, kt, h = it["nq"], it["ntl"], it["oc_bank"], it["cb"], it["kt"], it["h"]
            OC = PB[oc_bank]
            K.mm(OC[0:65, 0:nq], Vc[cb][:, kt, :], PT[pi][:, 0:nq], it["nk"] == 0, it["nk"] == it["nkt"] - 1,
                 [("PT", pi), ("Vc", cb)], [f"pb{oc_bank}"])
            if it["nk"] == it["nkt"] - 1:
                t0 = it["t0"]
                K.cp("dve", ocT[:, 0:nq], OC[0:65, 0:nq], [f"pb{oc_bank}"], ["ocT"])
                for qi in range(ntl):
                    K.tr(PB[0][:, qi * 65:(qi + 1) * 65], ocT[:, qi * 128:(qi + 1) * 128], identf[0:65, 0:65], ["ocT", "identf"], ["pb0"])
                O3 = v3(PB[0][:, 0:ntl * 65], ntl)
                K.S.op("dve", lambda e, O3=O3, ntl=ntl: e.reciprocal(st[:, 0:ntl], O3[:, :, 64]), ["pb0"], ["st"])
                K.tt("dve", oc[:, 0:ntl, :], O3[:, :, 0:64], bc3(st[:, 0:ntl], 64), ALU.mult, ["pb0", "st"], ["oc"])
                K.dma("sp", mix_d[t0 * 128:t0 * 128 + nq, 640 + h * 64:704 + h * 64].rearrange("(q p) d -> p q d", p=128),
                      oc[:, 0:ntl, :], ["oc"], [("mixd", t0 + i_) for i_ in range(ntl)])

        for i in range(len(itemsC) + DSK):
            if i < len(itemsC):
                qkC(i, itemsC[i])
            if i - DSK >= 0:
                pvC(i - DSK, itemsC[i - DSK])

        S.barrier()
        NB = (2 * T + 255) // 256 + 64
        h2_d, x1_d, xs_d, ys_d = d[f"h2_{L}"], d[f"x1_{L}"], d[f"xsl_{L}"], d[f"ys_{L}"]
        w1r = w1_d.rearrange("e p c n -> (e p) (c n)")
        w3r = w3_d.rearrange("e p c n -> (e p) (c n)")
        w2r = w2_d.rearrange("e p c n -> (e p) (c n)")
        WSZ = 12288
        Wb = [arena[:, i * WSZ:(i + 1) * WSZ] for i in range(2)]
        wout = arena[:, 2 * WSZ:2 * WSZ + 8192].rearrange("p (c n) -> p c n", c=8)
        ohb = arena[:, 2 * WSZ + 8192:2 * WSZ + 8192 + NT * 128].rearrange("p (t e) -> p t e", t=NT)
        assert 2 * WSZ + 8192 + NT * 128 <= AR
        K.dma("pool", wout, wout_d[:, :, :], [], ["wout"])
        ut = K.sb("ut", [128, 128], F32)
        iop = K.sb("iop", [128, 1], F32)
        K.dma("sp", ut[:, :], d["ut"][:, :], [], ["ut"])
        K.dma("sp", iop[:, :], d["iop"][:, :], [], ["iop"])
        base = K.sb("base", [128, 64], F32)
        K.memset("dve", base[:, :], 0.0, [], ["base"])
        rk = K.sb("rk", [128, NT, 2], F32)
        wts = K.sb("wts", [128, NT, 2], F32)
        dstf = K.sb("dstf", [128, NT, 2], F32)
        dsti = K.sb("dsti", [128, NT * 2], I32)
        h2s = [K.sb(f"h2s{i}", [128, D], BF16) for i in range(2)]
        MB = [K.sb(f"MB{i}", [128, D], F32) for i in range(4)]
        for gi, (src, wh) in enumerate((("A2", 0), ("A2", 1), ("B2", 0), ("B2", 1))):
            for c in range(8):
                col = (A2[:, 2 * c + wh:2 * c + wh + 1] if src == "A2"
                       else modT[:, 2 * (24 + c) + wh:2 * (24 + c) + wh + 1])
                K.ts("dve", gbc[:, :], ones[:, :], col, None, ALU.mult, None, ["ones", "modT", "A2"], ["gbc"])
                K.mm(PB[7][:, (c % 4) * 128:(c % 4 + 1) * 128], gbc[:, :], identf[:, :], True, True, ["gbc", "identf"], ["pb7"])
                K.cp("act", MB[gi][:, c * 128:(c + 1) * 128], PB[7][:, (c % 4) * 128:(c % 4 + 1) * 128], ["pb7"], [("MB", gi)])

        def rstd_of(ss, dim):
            K.ts("dve", ss, ss, 1.0 / dim, EPS, ALU.mult, ALU.add, ["st"], ["st"])
            K.act(ss, ss, AF.Sqrt, ["st"], ["st"])
            K.S.op("dve", lambda e, a=ss: e.reciprocal(a, a), ["st"], ["st"])

        x1t = [K.sb(f"x1t{i}", [128, D], F32) for i in range(2)]
        for t in range(NT):
            b = t % 2
            wh = 0 if t < NT_OWN else 1
            rows = slice(t * 128, (t + 1) * 128)
            X1 = x1t[b]
            K.dma("sp", xt[b][:, :], x_d[rows, :], [], [("xt", b)])
            K.dma("sp", mixrow[:, 256:1024], mix_d[rows, 256:1024], [("mixd", t)], ["mixrow"])
            K.dma("sp", mixrow[:, 0:256], oa_d[rows, :], [], ["mixrow"])
            for c in range(8):
                K.tr(PB0b[:, c * 128:(c + 1) * 128], mixrow[:, c * 128:(c + 1) * 128], identb[:, :], ["mixrow", "identb"], ["pb0"])
            K.cp("dve", mixT[:, :, :], v3(PB0b[:, :], 8), ["pb0"], ["mixT"])
            for half in range(2):
                pb = 1 + half
                for c in range(8):
                    K.mm(PB[pb][:, :], mixT[:, c, :], wout[:, c, half * 512:(half + 1) * 512], c == 0, c == 7,
                         ["mixT", "wout"], [f"pb{pb}"])
                hs = slice(half * 512, (half + 1) * 512)
                K.tt("dve", tmpf[:, hs], PB[pb][:, :], G[wh][:, hs], ALU.mult, [f"pb{pb}", ("G", wh)], ["tmpf"])
                K.tt("pool", X1[:, hs], tmpf[:, hs], xt[b][:, hs], ALU.add, ["tmpf", ("xt", b)], [("x1", b)])
            K.dma("sp", x1_d[rows, :], X1[:, :], [("x1", b)], [])
            K.tt("pool", tmpf[:, :], X1[:, :], X1[:, :], ALU.mult, [("x1", b), "tmpf"], ["tmpf"])
            K.rsum("dve", st[:, 0:1], tmpf[:, :], ["tmpf"], ["st"])
            rstd_of(st[:, 0:1], D)
            K.act(h2n[:, :], X1[:, :], AF.Copy, [("x1", b), "st"], ["h2n"], scale=st[:, 0:1])
            K.tt("pool", tmpf[:, :], h2n[:, :], MB[wh][:, :], ALU.mult, ["h2n", ("MB", wh), "tmpf"], ["tmpf"])
            K.tt("pool", h2s[b][:, :], tmpf[:, :], MB[2 + wh][:, :], ALU.add, ["tmpf", ("MB", 2 + wh)], [("h2s", b)])
            K.dma("sp", h2_d[rows, :], h2s[b][:, :], [("h2s", b)], [])
            for c in range(8):
                pb = 6 + c // 4
                K.tr(PB[pb][:, (c % 4) * 128:(c % 4 + 1) * 128], h2n[:, c * 128:(c + 1) * 128], identf[:, :], ["h2n", "identf"], [f"pb{pb}"])
            for c in range(8):
                pb = 6 + c // 4
                K.ts("dve", h2Tf[:, c, :], PB[pb][:, (c % 4) * 128:(c % 4 + 1) * 128], A2[:, 2 * c + wh:2 * c + wh + 1],
                     modT[:, 2 * (24 + c) + wh:2 * (24 + c) + wh + 1], ALU.mult, ALU.add, [f"pb{pb}", "A2", "modT"], ["h2Tf"])
            for c in range(8):
                K.mm(PB[3][:, 0:72], h2Tf[:, c, :], wgr[:, c, :], c == 0, c == 7, ["h2Tf", "wgr"], ["pb3"])
            K.cp("act", lg[:, :], PB[3][:, 0:72], ["pb3"], ["lg"])
            gl = lg[:, 0:8]
            rl3 = v3(lg[:, 8:72], 8)
            s_gmax, s_ngmax, s_gsum, s_m1, s_m2, s_d, s_e2, s_wa, s_wb = [st[:, 16 + i:17 + i] for i in range(9)]
            goh, gex, pen = st[:, 32:40], st[:, 40:48], st[:, 48:56]
            RK = ["lg", "st", "r64"]
            K.S.op("dve", lambda e, a=s_gmax, g=gl: e.reduce_max(a, g, AX.X), ["lg"], ["st"])
            K.ts("dve", goh, gl, s_gmax, None, ALU.is_equal, None, RK, ["st"])
            K.ts("dve", s_ngmax, s_gmax, -1.0, None, ALU.mult, None, RK, ["st"])
            K.act(gex, gl, AF.Exp, RK, ["st"], bias=s_ngmax)
            K.rsum("dve", s_gsum, gex, RK, ["st"])
            K.S.op("dve", lambda e, a=s_gsum: e.reciprocal(a, a), RK, ["st"])
            K.ts("dve", pen, goh, BIG, -BIG, ALU.mult, ALU.add, RK, ["st"])
            rm, oh1, rm2, oh2 = [r[:, :] for r in r64]
            K.tt("dve", v3(rm, 8), rl3, bc3(pen, 8), ALU.add, RK, ["r64"])
            K.S.op("dve", lambda e, a=s_m1, g=rm: e.reduce_max(a, g, AX.X), RK, ["st"])
            K.ts("dve", oh1, rm, s_m1, None, ALU.is_equal, None, RK, ["r64"])
            K.stt("dve", rm2, oh1, -BIG, rm, ALU.mult, ALU.add, RK, ["r64"])
            K.S.op("dve", lambda e, a=s_m2, g=rm2: e.reduce_max(a, g, AX.X), RK, ["st"])
            K.ts("dve", oh2, rm2, s_m2, None, ALU.is_equal, None, RK, ["r64"])
            K.tt("dve", s_d, s_m2, s_m1, ALU.subtract, RK, ["st"])
            K.act(s_e2, s_d, AF.Exp, RK, ["st"])
            K.ts("dve", s_wa, s_e2, 1.0, None, ALU.add, None, RK, ["st"])
            K.S.op("dve", lambda e, a=s_wa: e.reciprocal(a, a), RK, ["st"])
            K.tt("dve", wts[:, t, 0:1], s_wa, s_gsum, ALU.mult, RK, ["wts"])
            K.tt("dve", wts[:, t, 1:2], wts[:, t, 0:1], s_e2, ALU.mult, RK + ["wts"], ["wts"])
            K.cp("dve", ohb[:, t, 0:64], oh1, RK, ["ohb"])
            K.cp("dve", ohb[:, t, 64:128], oh2, RK, ["ohb"])
            K.tt("dve", rm, oh1, oh2, ALU.add, RK, ["r64"])
            K.mm(PB[3][:, 128:192], ut[:, :], rm, True, True, ["ut", "r64"], ["pb3"])
            K.mm(PB[3][:, 192:256], ones[:, :], rm, True, True, ["ones", "r64"], ["pb3"])
            K.tt("dve", rm2, PB[3][:, 128:192], base[:, :], ALU.add, ["pb3", "base", "r64"], ["r64"])
            K.tt("dve", rm, oh1, rm2, ALU.mult, RK, ["r64"])
            K.rsum("dve", rk[:, t, 0:1], rm, RK, ["rk"])
            K.tt("dve", rm, oh2, rm2, ALU.mult, RK, ["r64"])
            K.rsum("dve", rk[:, t, 1:2], rm, RK, ["rk"])
            K.tt("dve", base[:, :], PB[3][:, 192:256], base[:, :], ALU.add, ["pb3", "base", "r64"], ["base"])
        cs = [r64[0][:, :], r64[1][:, :]]
        pc = r64[2][:, :]
        pst = r64[3][:, :]
        KS = ["base", "r64"]
        K.ts("dve", pc, base[:, :], 255.0, None, ALU.add, None, KS, ["r64"])
        pci = K.sb("pci", [128, 64], I32)
        K.cp("dve", pci[:, :], pc, KS, ["pci"])
        K.ts("dve", pci[:, :], pci[:, :], 8, 8, ALU.arith_shift_right, ALU.logical_shift_left, ["pci"], ["pci"])
        K.cp("dve", pc, pci[:, :], ["pci"] + KS, ["r64"])
        K.cp("dve", cs[0], pc, KS, ["r64"])
        cur = 0
        for sh in (1, 2, 4, 8, 16, 32):
            K.cp("dve", cs[1 - cur][:, 0:sh], cs[cur][:, 0:sh], KS, ["r64"])
            K.tt("dve", cs[1 - cur][:, sh:64], cs[cur][:, sh:64], cs[cur][:, 0:64 - sh], ALU.add, KS, ["r64"])
            cur = 1 - cur
        pend = cs[cur]
        K.tt("dve", pst, pend, pc, ALU.subtract, KS, ["r64"])
        other = cs[1 - cur]
        for t in range(NT):
            for k in range(2):
                K.tt("dve", other, ohb[:, t, 64 * k:64 * k + 64], pst, ALU.mult, KS + ["ohb"], ["r64"])
                K.rsum("dve", dstf[:, t, k:k + 1], other, KS, ["dstf"])
        K.tt("dve", dstf[:, :, :], dstf[:, :, :], rk[:, :, :], ALU.add, ["dstf", "rk"], ["dstf"])
        K.cp("dve", dsti[:, :], dstf[:, :, :].rearrange("p t k -> p (t k)"), ["dstf"], ["dsti"])
        zt = K.sb("zt", [128, D], BF16)
        K.memset("pool", zt[:, :], 0.0, [], ["zt"])
        for bk in range(2 * NB):
            K.dma("sp" if bk % 2 == 0 else "act", xs_d[bk * 128:(bk + 1) * 128, :], zt[:, :], ["zt"], ["xs"])
        S.barrier()
        for t in range(NT):
            b = t % 2
            K.dma("sp", h2s[b][:, :], h2_d[t * 128:(t + 1) * 128, :], [], [("h2s", b)])
            for k in range(2):
                K.S.dmafn("pool", lambda e, src=h2s[b][:, :], ix=dsti[:, 2 * t + k:2 * t + k + 1]: e.indirect_dma_start(
                    out=xs_d[:, :], out_offset=bass.IndirectOffsetOnAxis(ap=ix, axis=0), in_=src, in_offset=None),
                    [("h2s", b), "dsti"], ["xs"])
        S.barrier()
        xsb = K.sb("xsb", [128, 2, D], BF16)
        xT = K.sb("xTb", [128, 8, 256], BF16)
        ysbb = K.sb("ysbb", [128, 2, D], F32)
        ysb = [ysbb[:, 0, :], ysbb[:, 1, :]]
        widx2 = K.sb("widx2", [128, 2], I32)
        NROWS_W = NE * 128
        for bk in range(NB):
            wb = bk % 2
            W = Wb[wb]
            w1e = W[:, 0:4096].rearrange("p (c n) -> p c n", c=8)
            w3e = W[:, 4096:8192].rearrange("p (c n) -> p c n", c=8)
            w2e = W[:, 8192:12288].rearrange("p (c n) -> p c n", c=4)
            ef = st[:, 60:61]
            fl = st[:, 61:62]
            K.ts("dve", other, pend, float(256 * bk), None, ALU.is_le, None, ["r64"], ["r64b"])
            K.rsum("dve", ef, other, ["r64b"], ["st"])
            K.ts("dve", ef, ef, float(NE - 1), 128.0, ALU.min, ALU.mult, ["st"], ["st"])
            K.tt("dve", ef, ef, iop[:, :], ALU.add, ["st", "iop"], ["st"])
            K.cp("dve", widx2[:, wb:wb + 1], ef, ["st"], [("widx2", wb)])
            for (dst, srcw, wk) in ((W[:, 0:4096], w1r, "W1"), (W[:, 4096:8192], w3r, "W3"), (W[:, 8192:12288], w2r, "W2")):
                K.S.dmafn("pool", lambda e, o=dst, sw=srcw, ix=widx2[:, wb:wb + 1]: e.indirect_dma_start(
                    out=o, out_offset=None, in_=sw[:, :], in_offset=bass.IndirectOffsetOnAxis(ap=ix, axis=0)),
                    [("widx2", wb)], [(wk, wb)])
            K.dma("sp", xsb[:, :, :], xs_d[bk * 256:(bk + 1) * 256, :].rearrange("(a p) f -> p a f", p=128), [], ["xsb"])
            for a in range(2):
                for c in range(8):
                    K.tr(PB0b[:, c * 128:(c + 1) * 128], xsb[:, a, c * 128:(c + 1) * 128], identb[:, :], ["xsb", "identb"], ["pb0"])
                K.cp("dve", xT[:, :, a * 128:(a + 1) * 128], v3(PB0b[:, :], 8), ["pb0"], ["xT"])
            for m in range(4):
                p1, p3 = 1 + m % 2, 4 + m % 2
                for c in range(8):
                    K.mm(PB[p1][:, 0:256], w1e[:, c, m * 128:(m + 1) * 128], xT[:, c, :], c == 0, c == 7, [("W1", wb), "xT"], [f"pb{p1}"])
                for c in range(8):
                    K.mm(PB[p3][:, 0:256], w3e[:, c, m * 128:(m + 1) * 128], xT[:, c, :], c == 0, c == 7, [("W3", wb), "xT"], [f"pb{p3}"])
                K.act(sil[m % 2][:, 0:256], PB[p1][:, 0:256], AF.Silu, [f"pb{p1}"], [("sil", m % 2)])
                K.tt("dve", hT[:, m, 0:256], PB[p3][:, 0:256], sil[m % 2][:, 0:256], ALU.mult, [f"pb{p3}", ("sil", m % 2)], ["hTe"])
            for a in range(2):
                for half in range(2):
                    py = 6 + half
                    for kc in range(4):
                        K.mm(PB[py][:, :], hT[:, kc, a * 128:(a + 1) * 128], w2e[:, kc, half * 512:(half + 1) * 512], kc == 0, kc == 3,
                             ["hTe", ("W2", wb)], [f"pb{py}"])
                    K.cp("act", ysbb[:, a, half * 512:(half + 1) * 512], PB[py][:, :], [f"pb{py}"], [("ysb", a)])
                K.dma("sp", ys_d[bk * 256 + a * 128:bk * 256 + (a + 1) * 128, :], ysbb[:, a, :], [("ysb", a)], [])
        S.barrier()
        for t in range(NT):
            b = t % 2
            wh = 0 if t < NT_OWN else 1
            rows = slice(t * 128, (t + 1) * 128)
            K.dma("sp", x1t[b][:, :], x1_d[rows, :], [], [("x1", b)])
            for k in range(2):
                K.S.dmafn("pool", lambda e, o=ysb[k][:, :], ix=dsti[:, 2 * t + k:2 * t + k + 1]: e.indirect_dma_start(
                    out=o, out_offset=None, in_=ys_d[:, :], in_offset=bass.IndirectOffsetOnAxis(ap=ix, axis=0)),
                    ["dsti"], [("ysb", k)])
            K.ts("dve", tmpf[:, :], ysb[0][:, :], wts[:, t, 0:1], None, ALU.mult, None, [("ysb", 0), "wts"], ["tmpf"])
            K.stt("dve", tmpf[:, :], ysb[1][:, :], wts[:, t, 1:2], tmpf[:, :], ALU.mult, ALU.add, [("ysb", 1), "wts", "tmpf"], ["tmpf"])
            K.tt("pool", tmpf[:, :], tmpf[:, :], G[2 + wh][:, :], ALU.mult, ["tmpf", ("G", 2 + wh)], ["tmpf"])
            K.tt("pool", h2n[:, :], tmpf[:, :], x1t[b][:, :], ALU.add, ["tmpf", ("x1", b)], ["h2n"])
            K.dma("sp", xo_d[rows, :], h2n[:, :], ["h2n"], [])
        S.emit()


def build_btab(rpb, NT_OWN, rank):
    rows_total = 8 * NT_OWN
    specials = _specials(NT_OWN)
    gts = [rows_total // 4] + [rank * NT_OWN + t for t in specials]
    qc = np.arange(64)
    kc = np.arange(64)
    c0 = np.clip(qc - 8, 0, 48)
    colok = (kc[:, None] >= c0[None, :]) & (kc[:, None] < c0[None, :] + 16)
    dc = np.clip(kc[:, None] - qc[None, :], -15, 15) + 15
    tab = np.full((len(gts), 2, 64, 6, 16, 64), NEG, np.float32)
    for ci, gt in enumerate(gts):
        for e in range(16):
            b = (e + 1) % 2
            j = (7 + b - e) // 2
            qrow = 2 * gt + b
            r0 = min(max(qrow - 4, 0), rows_total - 8)
            for a in range(2):
                krow = 2 * (gt + j) + a
                if krow < 0 or krow >= rows_total or krow < r0 or krow >= r0 + 8:
                    continue
                dr = krow - qrow + 7
                vals = rpb[:, dr, :][:, dc]
                vals = np.where(colok[None], vals, np.float32(NEG))
                tab[ci, a, :, :, e, :] = vals.transpose(1, 0, 2)
    return tab.reshape(len(gts), 128, 6, 1024).astype(NPBF)


def prep_B(P, i, x_cur, xc_cur, NT_OWN, outsA, ranks_per_batch=4, NE=64):
    B = x_cur.shape[0]
    n = NT_OWN * 128
    NTB = ranks_per_batch * NT_OWN
    shared = {
        "w_out": _kmaj(P["w_out"][i], 8),
        "norm_ffnT": _colT(P["norm_ffn"][i]),
        "wgr": _kmaj(np.concatenate([P["moe_w_group"][i], P["moe_w_router"][i]], axis=1), 8),
        "w1": np.ascontiguousarray(P["moe_w1"][i][:NE].reshape(NE, 8, 128, 512).transpose(0, 2, 1, 3)),
        "w3": np.ascontiguousarray(P["moe_w3"][i][:NE].reshape(NE, 8, 128, 512).transpose(0, 2, 1, 3)),
        "w2": np.ascontiguousarray(P["moe_w2"][i][:NE].reshape(NE, 4, 128, 1024).transpose(0, 2, 1, 3)),
        "ident": np.eye(128, dtype=np.float32),
    }
    maps = []
    for b in range(B):
        cores = [outsA[b * ranks_per_batch + r] for r in range(ranks_per_batch)]
        kv_tiles = [c["kvb"][:n].reshape(NT_OWN, 128, 768) for c in cores]
        kv_all = np.concatenate(kv_tiles, axis=0)
        for r in range(ranks_per_batch):
            me = cores[r]
            m = dict(shared)
            m["x"] = np.ascontiguousarray(np.concatenate([x_cur[b, r * n:(r + 1) * n], xc_cur[b]], axis=0))
            m["oa"] = me["oa"]
            m["qbT"] = me["qbT"]
            m["qcT"] = me["qcT"]
            m["modT"] = me["modT"]
            win = np.zeros((NT_OWN + 8, 128, 768), NPBF)
            for s in range(NT_OWN + 6):
                g = r * NT_OWN - 3 + s
                if 0 <= g < NTB:
                    win[s] = kv_all[g]
            win[NT_OWN + 6:] = me["kvb"][n:].reshape(2, 128, 768)
            m["kvbw"] = win.reshape(-1, 768)
            m["kcT_all"] = np.ascontiguousarray(np.concatenate([c["kcT"][:, :, :n] for c in cores] + [me["kcT"][:, :, n:]], axis=2))
            m["vc_all"] = np.ascontiguousarray(np.concatenate([c["vc"][:n] for c in cores] + [me["vc"][n:]], axis=0))
            m["btab"] = build_btab(P["b_rpb"][i], NT_OWN, r)
            maps.append(m)
    return maps


def declare_dram(nc, NT_OWN, NR=4, NE=64):
    NT = NT_OWN + 2
    T = NT * 128
    n = NT_OWN * 128
    NCLS = 1 + len(_specials(NT_OWN))
    d = {}

    def t(name, shape, dt, kind="Internal"):
        d[name] = nc.dram_tensor(name, list(shape), dt, kind=kind).ap()

    EI = "ExternalInput"
    t("x", [T, D], F32, EI)
    t("cT", [128, 16], F32, EI)
    t("rope", [T, 32], F32, EI)
    t("ident", [128, 128], F32, EI)
    t("widx", [128, 6], I32, EI)
    t("ut", [128, 128], F32, EI)
    t("iop", [128, 1], F32, EI)
    t("xo", [T, D], F32, "ExternalOutput")
    t("xs_0", [T, D], F32)
    for L in range(2):
        t(f"w_ada_{L}", [128, 8, 6144], F32, EI)
        t(f"b_adaT_{L}", [128, 48], F32, EI)
        t(f"norm_mixT_{L}", [128, 8], F32, EI)
        t(f"w_in_{L}", [128, 8, INW], F32, EI)
        t(f"w_sT_{L}", [128, 4, 128], F32, EI)
        t(f"b_s_{L}", [128, 4], F32, EI)
        t(f"gains_{L}", [128, G_TOT], F32, EI)
        t(f"wq_{L}", [128, 3, 576], F32, EI)
        t(f"wkv_{L}", [128, 2, 768], F32, EI)
        t(f"btab_{L}", [NCLS, 128, 6, 1024], BF16, EI)
        t(f"w_out_{L}", [128, 8, D], F32, EI)
        t(f"norm_ffnT_{L}", [128, 8], F32, EI)
        t(f"wgr_{L}", [128, 8, 72], F32, EI)
        t(f"w1_{L}", [NE, 128, 8, 512], F32, EI)
        t(f"w3_{L}", [NE, 128, 8, 512], F32, EI)
        t(f"w2_{L}", [NE, 128, 4, D], F32, EI)
        t(f"oa_{L}", [T, 256], BF16)
        t(f"qbT_{L}", [3, 128, T], BF16)
        t(f"qcT_{L}", [6, 96, T], BF16)
        t(f"modT_{L}", [128, 96], F32)
        t(f"mixd_{L}", [T, D], BF16)
        PT = min(4, NT_OWN)
        for j in range(NT_OWN // PT):
            t(f"kcT_own_{L}_{j}", [576, PT * 128], BF16)
            t(f"kcT_g_{L}_{j}", [NR * 576, PT * 128], BF16)
            t(f"vc_own_{L}_{j}", [PT * 128, 390], BF16)
            t(f"vc_g_{L}_{j}", [NR * PT * 128, 390], BF16)
        t(f"kcT_ctx_{L}", [576, 256], BF16)
        t(f"vc_ctx_{L}", [256, 390], BF16)
        t(f"kvb_own_{L}", [n, 768], BF16)
        t(f"kvb_ctx_{L}", [256, 768], BF16)
        HT = min(3, NT_OWN)
        t(f"kvb_lo_{L}", [HT * 128, 768], BF16)
        t(f"kvb_hi_{L}", [HT * 128, 768], BF16)
        t(f"kvb_glo_{L}", [NR * HT * 128, 768], BF16)
        t(f"kvb_ghi_{L}", [NR * HT * 128, 768], BF16)
        t(f"h2_{L}", [T, D], BF16)
        t(f"x1_{L}", [T, D], F32)
        t(f"xsl_{L}", [((2 * T + 255) // 256 + 64) * 256, D], BF16)
        t(f"ys_{L}", [((2 * T + 255) // 256 + 64) * 256, D], F32)
    return d


def emit_X(nc, gc, d, L, groups):
    stack = contextlib.ExitStack()
    with stack:
        K = KB(nc, stack, gc)
        pairs = [(d[f"kvb_lo_{L}"], d[f"kvb_glo_{L}"]), (d[f"kvb_hi_{L}"], d[f"kvb_ghi_{L}"])]
        j = 0
        while f"vc_own_{L}_{j}" in d:
            pairs.append((d[f"vc_own_{L}_{j}"], d[f"vc_g_{L}_{j}"]))
            pairs.append((d[f"kcT_own_{L}_{j}"], d[f"kcT_g_{L}_{j}"]))
            j += 1
        for nm, (src, dst) in enumerate(pairs):
            K.S.cc(lambda e, src=src, dst=dst: e.collective_compute(
                "AllGather", ALU.bypass, replica_groups=groups, ins=[src[:, :]], outs=[dst[:, :]]), [], [nm])
        K.S.emit()


def build_fused(NT_OWN, ngroups=2, NR=4, TB=4, NE=64, do_x=True):
    nc = bass.Bass("TRN2", target_bir_lowering=False)
    gstack = contextlib.ExitStack()
    with gstack:
        gc = GC(gstack)
        d = declare_dram(nc, NT_OWN, NR, NE)
        groups = [list(range(g * NR, (g + 1) * NR)) for g in range(ngroups)]
        for L in range(2):
            emit_A(nc, gc, d, NT_OWN, L)
            if do_x:
                emit_X(nc, gc, d, L, groups)
            emit_B(nc, gc, d, NT_OWN, L, TB=TB, NR=NR, NE=NE)
    return nc


def halo_index(NT_OWN, r, NR):
    HT = min(3, NT_OWN)
    idx = np.zeros((128, 6), np.int32)
    for col in range(6):
        g = r * NT_OWN - 3 + col if col < 3 else (r + 1) * NT_OWN + (col - 3)
        if not (0 <= g < NR * NT_OWN):
            continue
        rk, l = g // NT_OWN, g % NT_OWN
        if col < 3:
            t2 = l - (NT_OWN - HT)
        else:
            t2 = l
        if not (0 <= t2 < HT):
            continue
        idx[:, col] = (rk * HT + t2) * 128 + np.arange(128)
    return idx


def prep_fused(P, NT_OWN, NR=4):
    x = np.asarray(P["x"], np.float32)
    ctx = np.asarray(P["ctx"], np.float32)
    B = x.shape[0]
    n = NT_OWN * 128
    shared = {"ident": np.eye(128, dtype=np.float32),
              "ut": np.triu(np.ones((128, 128), np.float32), 1),
              "iop": np.arange(128, dtype=np.float32).reshape(128, 1)}
    per_rank = [dict() for _ in range(NR)]
    for L in range(2):
        gains = np.concatenate([
            P["a_v_norm"][L], np.tile(P["b_q_norm"][L], 6), np.tile(P["b_k_norm"][L], 6), P["c_q_a_norm"][L],
            P["c_kv_a_norm"][L], np.tile(P["c_q_norm"][L], 6), np.tile(P["c_k_norm"][L][:64], 6), P["c_k_norm"][L][64:]])
        shared.update({
            f"w_ada_{L}": _kmaj(P["w_ada"][L], 8),
            f"b_adaT_{L}": _colT(P["b_ada"][L]),
            f"norm_mixT_{L}": _colT(P["norm_mix"][L]),
            f"w_in_{L}": _kmaj(P["w_in"][L], 8),
            f"w_sT_{L}": np.ascontiguousarray(P["a_w_s"][L].transpose(2, 0, 1)),
            f"b_s_{L}": np.ascontiguousarray(P["a_b_s"][L].T),
            f"gains_{L}": _bcrow(gains.astype(np.float32)),
            f"wq_{L}": _kmaj(P["c_w_q_up"][L], 3),
            f"wkv_{L}": _kmaj(P["c_w_kv_up"][L], 2),
            f"w_out_{L}": _kmaj(P["w_out"][L], 8),
            f"norm_ffnT_{L}": _colT(P["norm_ffn"][L]),
            f"wgr_{L}": _kmaj(np.concatenate([P["moe_w_group"][L], P["moe_w_router"][L]], axis=1), 8),
            f"w1_{L}": np.ascontiguousarray(P["moe_w1"][L].reshape(64, 8, 128, 512).transpose(0, 2, 1, 3)),
            f"w3_{L}": np.ascontiguousarray(P["moe_w3"][L].reshape(64, 8, 128, 512).transpose(0, 2, 1, 3)),
            f"w2_{L}": np.ascontiguousarray(P["moe_w2"][L].reshape(64, 4, 128, 1024).transpose(0, 2, 1, 3)),
        })
        for r in range(NR):
            per_rank[r][f"btab_{L}"] = build_btab(P["b_rpb"][L], NT_OWN, r)
    for r in range(NR):
        per_rank[r]["rope"] = rope_tables(NT_OWN, r)
        per_rank[r]["widx"] = halo_index(NT_OWN, r, NR)
    maps = []
    for b in range(B):
        cT = np.ascontiguousarray(np.stack([_colT(P["c"][b]), _colT(P["c_ctx"])], axis=2).reshape(128, 16))
        for r in range(NR):
            m = dict(shared)
            m.update(per_rank[r])
            m["x"] = np.ascontiguousarray(np.concatenate([x[b, r * n:(r + 1) * n], ctx[b]], axis=0))
            m["cT"] = cT
            maps.append(m)
    return maps


_PROG = {}


def kernel(**inputs):
    P = {k: np.asarray(v) for k, v in inputs.items()}
    NT_OWN = 32
    n = NT_OWN * 128
    if "fused" not in _PROG:
        _PROG["fused"] = build_fused(NT_OWN)
    maps = prep_fused(P, NT_OWN)
    res = run_bass_kernel_spmd(_PROG["fused"], maps, core_ids=list(range(8))).results
    out = np.empty((2, 4 * n, D), np.float32)
    for b in range(2):
        for r in range(4):
            out[b, r * n:(r + 1) * n] = res[b * 4 + r]["xo"][:n]
    return out
```

```python
import contextlib
import numpy as np
import ml_dtypes
import concourse.bass as bass
import concourse.mybir as mybir
from concourse.bass_utils import run_bass_kernel_spmd

F32 = mybir.dt.float32
BF16 = mybir.dt.bfloat16
I32 = mybir.dt.int32
AF = mybir.ActivationFunctionType
ALU = mybir.AluOpType
AX = mybir.AxisListType
NPBF = ml_dtypes.bfloat16

D = 1024
GRID_W = 64
CTX = 256
EPS = 1e-6
INW = 2336
NEG = -30000.0


class _Op:
    __slots__ = ("eng", "fn", "deps", "needed", "semkey", "val", "dma", "idx")

    def __init__(self, eng, fn, dma):
        self.eng = eng
        self.fn = fn
        self.deps = []
        self.needed = False
        self.semkey = None
        self.val = 0
        self.dma = dma


class Sched:
    ENGS = ("pe", "act", "dve", "pool", "sp")
    NLANES = 6

    def __init__(self, nc, gc):
        self.nc = nc
        self.gc = gc
        self.ops = {e: [] for e in self.ENGS}
        self.bufs = {}
        self.phase = 0
        self.lane_ops = {}
        self.lane_n = {e: 0 for e in self.ENGS}
        self.pending = {e: [] for e in self.ENGS}
        self.last = {e: None for e in self.ENGS}

    def _add(self, eng, fn, r, w, dma):
        if getattr(self, "rec", None) is not None:
            self.rec.append((eng, fn, self.keymap(r), self.keymap(w), dma))
            return None
        op = _Op(eng, fn, dma)
        deps = []
        for k in r:
            st = self.bufs.setdefault(k, [None, []])
            if st[0] is not None:
                deps.append(st[0])
        for k in w:
            st = self.bufs.setdefault(k, [None, []])
            if st[0] is not None:
                deps.append(st[0])
            deps.extend(st[1])
        deps.extend(self.pending[eng])
        self.pending[eng] = []
        if dma:
            lane = self.lane_n[eng] % self.NLANES
            self.lane_n[eng] += 1
            key = ("lane", eng, lane)
            prev = self.lane_ops.get(key)
            if prev is not None:
                deps.append(prev)
            self.lane_ops[key] = op
            op.semkey = key
            op.val = (prev.val if prev is not None else self.gc.lane_vals.get(key, 0)) + 16
            op.needed = True
        else:
            op.semkey = ("eng", eng, self.phase)
        seen = set()
        for d in deps:
            if d is op or id(d) in seen:
                continue
            seen.add(id(d))
            if (not d.dma) and d.eng == eng and eng == "pe":
                continue
            d.needed = True
            op.deps.append(d)
        for k in r:
            self.bufs[k][1].append(op)
        for k in w:
            self.bufs[k] = [op, []]
        self.ops[eng].append(op)
        self.last[eng] = op
        return op

    def op(self, eng, fn, r=(), w=()):
        return self._add(eng, fn, r, w, False)

    def dma(self, eng, out, in_, r=(), w=()):
        return self._add(eng, lambda e: e.dma_start(out=out, in_=in_), r, w, True)

    def dmafn(self, eng, fn, r=(), w=()):
        return self._add(eng, fn, r, w, True)

    def cc(self, fn, r=(), w=()):
        op = self._add("pool", fn, r, w, False)
        op.semkey = ("cc", self.gc.next_uid())
        op.needed = True
        op.val = 1
        op.dma = True
        return op

    def barrier(self):
        lasts = [o for o in self.last.values() if o is not None] + list(self.lane_ops.values())
        for e in self.ENGS:
            self.pending[e] = list(lasts)
        self.bufs = {}
        self.phase += 1

    def emit(self):
        nc = self.nc
        gc = self.gc
        for e in self.ENGS:
            if self.ops[e] and not self.ops[e][-1].dma:
                self.ops[e][-1].needed = True
        cnt = {}
        for e in self.ENGS:
            for op in self.ops[e]:
                if not op.dma and op.needed:
                    cnt[op.semkey] = cnt.get(op.semkey, 0) + 1
                    op.val = cnt[op.semkey]
        sems = {}
        finals = {}
        for e in self.ENGS:
            for op in self.ops[e]:
                if not op.needed:
                    continue
                k = op.semkey
                if k not in sems:
                    if k[0] == "lane":
                        if k not in gc.lane_sems:
                            gc.lane_sems[k] = gc.stack.enter_context(nc.semaphore("l_" + "_".join(str(x) for x in k[1:])))
                        sems[k] = gc.lane_sems[k]
                    else:
                        sems[k] = gc.stack.enter_context(nc.semaphore(f"s{gc.next_uid()}_" + "_".join(str(x) for x in k)))
                finals[k] = max(finals.get(k, 0), op.val)
        for k, v in finals.items():
            if k[0] == "lane":
                gc.lane_vals[k] = v

        def run(engname, e):
            waited = {}
            for op in self.ops[engname]:
                for d in op.deps:
                    if waited.get(d.semkey, 0) >= d.val:
                        continue
                    e.wait_ge(sems[d.semkey], d.val)
                    waited[d.semkey] = d.val
                ins = op.fn(e)
                if op.needed:
                    if op.semkey[0] == "cc":
                        ins.then_inc(sems[op.semkey])
                    else:
                        ins.then_inc(sems[op.semkey], 16 if op.dma else 1)
            for k, v in finals.items():
                if waited.get(k, 0) < v:
                    e.wait_ge(sems[k], v)

        with nc.Block() as block:
            @block.tensor
            def _(e):
                run("pe", e)

            @block.scalar
            def _(e):
                run("act", e)

            @block.vector
            def _(e):
                run("dve", e)

            @block.gpsimd
            def _(e):
                run("pool", e)

            @block.sync
            def _(e):
                run("sp", e)


class GC:
    def __init__(self, stack):
        self.stack = stack
        self.lane_sems = {}
        self.lane_vals = {}
        self.uid = 0

    def next_uid(self):
        self.uid += 1
        return self.uid


class KB:
    def __init__(self, nc, stack, gc):
        self.nc = nc
        self.stack = stack
        self.gc = gc
        self.S = Sched(nc, gc)
        self.tag = f"_u{gc.next_uid()}"

    def sb(self, name, shape, dt):
        return self.stack.enter_context(self.nc.sbuf_tensor(name + self.tag, list(shape), dt))

    def ps(self, name, shape, dt):
        return self.stack.enter_context(self.nc.psum_tensor(name + self.tag, list(shape), dt))

    def dram(self, name, shape, dt, kind):
        return self.nc.dram_tensor(name, list(shape), dt, kind=kind).ap()

    def mm(self, out, lhsT, rhs, start, stop, r, w):
        self.S.op("pe", lambda e: e.matmul(out, lhsT, rhs, start=start, stop=stop), r, w)

    def tr(self, out, in_, ident, r, w):
        self.S.op("pe", lambda e: e.transpose(out, in_, ident), r, w)

    def act(self, out, in_, func, r, w, **kw):
        self.S.op("act", lambda e: e.activation(out, in_, func, **kw), r, w)

    def ts(self, eng, out, in0, s1, s2, op0, op1, r, w):
        if op1 is None:
            self.S.op(eng, lambda e: e.tensor_scalar(out, in0, s1, None, op0), r, w)
        else:
            self.S.op(eng, lambda e: e.tensor_scalar(out, in0, s1, s2, op0, op1), r, w)

    def tt(self, eng, out, in0, in1, op, r, w):
        self.S.op(eng, lambda e: e.tensor_tensor(out, in0, in1, op), r, w)

    def stt(self, eng, out, in0, scalar, in1, op0, op1, r, w):
        self.S.op(eng, lambda e: e.scalar_tensor_tensor(out, in0, scalar, in1, op0, op1), r, w)

    def cp(self, eng, out, in_, r, w):
        if eng == "act":
            self.S.op("act", lambda e: e.copy(out, in_), r, w)
        else:
            self.S.op(eng, lambda e: e.tensor_copy(out, in_), r, w)

    def rsum(self, eng, out, in_, r, w):
        self.S.op(eng, lambda e: e.reduce_sum(out, in_, AX.X), r, w)

    def memset(self, eng, ap, v, r, w):
        self.S.op(eng, lambda e: e.memset(ap, v), r, w)

    def dma(self, eng, out, in_, r, w):
        self.S.dma(eng, out, in_, r, w)


G_AV, G_BQ, G_BK, G_CQA, G_CKVA, G_CQN, G_CKN, G_CKR, G_TOT = 0, 256, 640, 1024, 1408, 1664, 2240, 2624, 2656


def v3(ap, g):
    return ap.rearrange("p (g d) -> p g d", g=g)


def bc3(ap2, d):
    p, g = ap2.shape
    return ap2.unsqueeze(2).to_broadcast([p, g, d])


def emit_A(nc, gc, d, NT_OWN, L):
    NT = NT_OWN + 2
    T = NT * 128
    n = NT_OWN * 128
    stack = contextlib.ExitStack()
    with stack:
        K = KB(nc, stack, gc)
        S = K.S
        x_d = d["x"] if L == 0 else d["xs_0"]
        cT_d, rope_d, ident_d = d["cT"], d["rope"], d["ident"]
        wada_d, bada_d, nmix_d, win_d = d[f"w_ada_{L}"], d[f"b_adaT_{L}"], d[f"norm_mixT_{L}"], d[f"w_in_{L}"]
        wsT_d, bs_d, gains_d, wq_d, wkv_d = d[f"w_sT_{L}"], d[f"b_s_{L}"], d[f"gains_{L}"], d[f"wq_{L}"], d[f"wkv_{L}"]
        oa_d, qbT_d, qcT_d, modT_d = d[f"oa_{L}"], d[f"qbT_{L}"], d[f"qcT_{L}"], d[f"modT_{L}"]
        PT = min(4, NT_OWN)
        HT = min(3, NT_OWN)
        kcT_ctx, vc_ctx = d[f"kcT_ctx_{L}"], d[f"vc_ctx_{L}"]
        kvb_own, kvb_ctx, kvb_lo, kvb_hi = d[f"kvb_own_{L}"], d[f"kvb_ctx_{L}"], d[f"kvb_lo_{L}"], d[f"kvb_hi_{L}"]
        ident = K.sb("ident_b", [128, 128], BF16)
        identf = K.sb("identf", [128, 128], F32)
        cT = K.sb("cTs", [128, 16], F32)
        scT = K.sb("scT", [128, 16], F32)
        bada = K.sb("bada", [128, 48], F32)
        nmix = K.sb("nmix", [128, 8], F32)
        modT = K.sb("modTs", [128, 96], F32)
        A1 = K.sb("A1", [128, 16], F32)
        wst = [K.sb(f"wst{i}", [128, 8, 512], F32) for i in range(2)]
        win = K.sb("win", [128, 8, INW], BF16)
        wsT = K.sb("wsT", [128, 4, 128], BF16)
        bs = K.sb("bs", [128, 4], F32)
        gains = K.sb("gainss", [128, G_TOT], F32)
        wq = K.sb("wqs", [128, 3, 576], BF16)
        wkv = K.sb("wkvs", [128, 2, 768], BF16)
        xt = [K.sb(f"xt{i}", [128, D], F32) for i in range(2)]
        ropet = [K.sb(f"ropet{i}", [128, 32], F32) for i in range(2)]
        sqj_2 = [K.sb("sqj%d" % i_, [128, D], F32) for i_ in range(2)]
        st_2 = [K.sb("st%d" % i_, [128, 64], F32) for i_ in range(2)]
        xn_2 = [K.sb("xn%d" % i_, [128, D], BF16) for i_ in range(2)]
        hT_2 = [K.sb("hT%d" % i_, [128, 8, 128], BF16) for i_ in range(2)]
        z_2 = [K.sb("z%d" % i_, [128, INW], F32) for i_ in range(2)]
        g1_2 = [K.sb("g1%d" % i_, [128, 512], F32) for i_ in range(2)]
        g2_2 = [K.sb("g2%d" % i_, [128, 512], F32) for i_ in range(2)]
        gg_2 = [K.sb("gg%d" % i_, [128, 512], F32) for i_ in range(2)]
        vnb_2 = [K.sb("vnb%d" % i_, [128, 256], BF16) for i_ in range(2)]
        oa_2 = [K.sb("oas%d" % i_, [128, 256], BF16) for i_ in range(2)]
        t384_2 = [K.sb("t384%d" % i_, [128, 384], F32) for i_ in range(2)]
        u384_2 = [K.sb("u384%d" % i_, [128, 384], F32) for i_ in range(2)]
        qnb_2 = [K.sb("qnb%d" % i_, [128, 384], BF16) for i_ in range(2)]
        qbTs_2 = [K.sb("qbTs%d" % i_, [128, 3, 128], BF16) for i_ in range(2)]
        kvbs_2 = [K.sb("kvbs%d" % i_, [128, 768], BF16) for i_ in range(2)]
        qab_2 = [K.sb("qab%d" % i_, [128, 384], BF16) for i_ in range(2)]
        qaT_2 = [K.sb("qaT%d" % i_, [128, 3, 128], BF16) for i_ in range(2)]
        qf_2 = [K.sb("qf%d" % i_, [128, 576], F32) for i_ in range(2)]
        qs_2 = [K.sb("qs%d" % i_, [128, 576], F32) for i_ in range(2)]
        qc_2 = [K.sb("qc%d" % i_, [128, 6, 96], BF16) for i_ in range(2)]
        rt_2 = [[K.sb(f"rt{i}_{j_}", [128, 48], F32) for i in range(4)] for j_ in range(2)]
        qcTs_2 = [K.sb("qcTs%d" % i_, [96, 6, 128], BF16) for i_ in range(2)]
        kvab_2 = [K.sb("kvab%d" % i_, [128, 256], BF16) for i_ in range(2)]
        kvaT_2 = [K.sb("kvaT%d" % i_, [128, 2, 128], BF16) for i_ in range(2)]
        kvf_2 = [K.sb("kvf%d" % i_, [128, 768], F32) for i_ in range(2)]
        kc_2 = [K.sb("kc%d" % i_, [128, 6, 96], BF16) for i_ in range(2)]
        kr_2 = [K.sb("kr%d" % i_, [128, 32], F32) for i_ in range(2)]
        krr_2 = [K.sb("krr%d" % i_, [128, 32], F32) for i_ in range(2)]
        vcs_2 = [K.sb("vcs%d" % i_, [128, 6, 65], BF16) for i_ in range(2)]
        kcTs_2 = [K.sb("kcTs%d" % i_, [96, 6, 128], BF16) for i_ in range(2)]
        PB = [K.ps(f"pb{i}", [128, 512], F32) for i in range(8)]
        PB0b = PB[0].bitcast(BF16)

        K.dma("sp", identf[:, :], ident_d[:, :], [], ["identf"])
        K.cp("dve", ident[:, :], identf[:, :], ["identf"], ["ident"])
        K.dma("sp", cT[:, :], cT_d[:, :], [], ["cT"])
        K.dma("sp", bada[:, :], bada_d[:, :], [], ["bada"])
        K.dma("sp", nmix[:, :], nmix_d[:, :], [], ["nmix"])
        K.dma("sp", bs[:, :], bs_d[:, :], [], ["bs"])
        K.dma("sp", gains[:, :], gains_d[:, :], [], ["gains"])
        K.dma("pool", win[:, :, :], win_d[:, :, :], [], ["win"])
        K.dma("pool", wsT[:, :, :], wsT_d[:, :, :], [], ["wsT"])
        K.dma("pool", wq[:, :, :], wq_d[:, :, :], [], ["wq"])
        K.dma("pool", wkv[:, :, :], wkv_d[:, :, :], [], ["wkv"])
        K.memset("pool", vcs_2[0][:, :, :], 1.0, [], [("vcs", 0)])
        K.memset("pool", vcs_2[1][:, :, :], 1.0, [], [("vcs", 1)])
        K.act(scT[:, :], cT[:, :], AF.Silu, ["cT"], ["scT"])
        for grp in range(12):
            b = grp % 2
            K.dma("sp", wst[b][:, :, :], wada_d[:, :, grp * 512:(grp + 1) * 512], [], [("wst", b)])
            for jj in range(4):
                j = grp * 4 + jj
                for c in range(8):
                    K.mm(PB[1][:, 2 * j:2 * j + 2], wst[b][:, c, jj * 128:(jj + 1) * 128], scT[:, 2 * c:2 * c + 2],
                         c == 0, c == 7, [("wst", b), "scT"], ["pb1"])
        K.tt("dve", v3(modT[:, :], 48), v3(PB[1][:, 0:96], 48), bc3(bada[:, :], 2), ALU.add, ["pb1", "bada"], ["modT"])
        K.dma("sp", modT_d[:, :], modT[:, :], ["modT"], [])
        K.stt("dve", v3(A1[:, :], 8), v3(modT[:, 16:32], 8), 1.0, bc3(nmix[:, :], 2), ALU.add, ALU.mult,
              ["modT", "nmix"], ["A1"])

        cur = {}

        def rstd_of(ss, n, dim, extra=None):
            K.ts("dve", ss, ss, 1.0 / dim, EPS, ALU.mult, ALU.add, ["st"], ["st"])
            K.act(ss, ss, AF.Sqrt, ["st"], ["st"])
            K.S.op("dve", lambda e, a=ss: e.reciprocal(a, a), ["st"], ["st"])
            if extra is not None:
                K.ts("dve", ss, ss, extra, None, ALU.mult, None, ["st"], ["st"])

        def rope(src3, dst3, G, rp, rkeys, wkeys):
            for a in range(2):
                o = 16 * a
                cos = rp[:, 16 * a:16 * a + 8].unsqueeze(1).to_broadcast([128, G, 8])
                sin = rp[:, 16 * a + 8:16 * a + 16].unsqueeze(1).to_broadcast([128, G, 8])
                x1 = src3[:, :, o:o + 8]
                x2 = src3[:, :, o + 8:o + 16]
                t = [v3(cur["rt"][i][:, 0:G * 8], G) for i in range(4)]
                K.tt("pool", t[0], x1, cos, ALU.mult, rkeys, ["rt0"])
                K.tt("pool", t[1], x2, sin, ALU.mult, rkeys, ["rt1"])
                K.tt("dve", dst3[:, :, o:o + 8], t[0], t[1], ALU.subtract, ["rt0", "rt1"], wkeys)
                K.tt("pool", t[2], x2, cos, ALU.mult, rkeys, ["rt2"])
                K.tt("pool", t[3], x1, sin, ALU.mult, rkeys, ["rt3"])
                K.tt("dve", dst3[:, :, o + 8:o + 16], t[2], t[3], ALU.add, ["rt2", "rt3"], wkeys)

        SHARED = {"ident", "identf", "cT", "scT", "bada", "nmix", "modT", "A1", "win", "wsT", "bs", "gains", "wq", "wkv",
                  "kvb_x", "vc_x", "kc_x"}
        recs = []
        for t in range(NT):
            b = t % 2
            wh = 0 if t < NT_OWN else 1
            base = 4 * b
            PBT = PB[base].bitcast(BF16)
            kT = f"pb{base}"
            (sqj, st, xn, hT, z, g1, g2, gg, vnb, oa, t384, u384, qnb, qbTs, kvbs, qab, qaT, qf, qs, qc, qcTs, kvab, kvaT,
             kvf, kc, kr, krr, vcs, kcTs) = [lst[b] for lst in (
                sqj_2, st_2, xn_2, hT_2, z_2, g1_2, g2_2, gg_2, vnb_2, oa_2, t384_2, u384_2, qnb_2, qbTs_2, kvbs_2, qab_2,
                qaT_2, qf_2, qs_2, qc_2, qcTs_2, kvab_2, kvaT_2, kvf_2, kc_2, kr_2, krr_2, vcs_2, kcTs_2)]
            cur["rt"] = rt_2[b]
            S.rec = []
            S.keymap = lambda ks, b=b: [k if (isinstance(k, tuple) or k in SHARED or k.startswith("pb")) else (k, b) for k in ks]
            recs.append(S.rec)
            rows = slice(t * 128, (t + 1) * 128)
            X = xt[b]
            K.dma("sp", X[:, :], x_d[rows, :], [], [("xt", b)])
            K.dma("sp", ropet[b][:, :], rope_d[rows, :], [], [("rope", b)])
            K.tt("dve", sqj[:, :], X[:, :], X[:, :], ALU.mult, [("xt", b)], ["sqj"])
            K.rsum("dve", st[:, 0:1], sqj[:, :], ["sqj"], ["st"])
            rstd_of(st[:, 0:1], 1, D)
            K.act(xn[:, :], X[:, :], AF.Copy, [("xt", b), "st"], ["xn"], scale=st[:, 0:1])
            for c in range(8):
                K.tr(PBT[:, c * 128:(c + 1) * 128], xn[:, c * 128:(c + 1) * 128], ident[:, :], ["xn", "ident"], [kT])
            for c in range(8):
                K.ts("dve", hT[:, c, :], PBT[:, c * 128:(c + 1) * 128], A1[:, 2 * c + wh:2 * c + wh + 1],
                     modT[:, 2 * c + wh:2 * c + wh + 1], ALU.mult, ALU.add, [kT, "A1", "modT"], ["hT"])
            for k5 in range(5):
                n0 = k5 * 512
                n1 = min(INW, n0 + 512)
                pb = base + 1 + k5 % 2
                for c in range(8):
                    K.mm(PB[pb][:, 0:n1 - n0], hT[:, c, :], win[:, c, n0:n1], c == 0, c == 7, ["hT", "win"], [f"pb{pb}"])
                K.cp("act", z[:, n0:n1], PB[pb][:, 0:n1 - n0], [f"pb{pb}"], ["z"])
            za = z[:, 0:512]
            K.tt("pool", g1[:, :], za, za, ALU.mult, ["z"], ["g1"])
            K.ts("dve", g1[:, :], g1[:, :], 0.044715, 1.0, ALU.mult, ALU.add, ["g1"], ["g1"])
            K.tt("pool", g1[:, :], g1[:, :], za, ALU.mult, ["g1", "z"], ["g1"])
            K.act(g2[:, :], g1[:, :], AF.Sigmoid, ["g1"], ["g2"], scale=1.5957691216057308)
            K.tt("dve", gg[:, :], g2[:, :], za, ALU.mult, ["g2", "z"], ["gg"])
            K.tt("pool", g1[:, 0:256], gg[:, 256:512], gg[:, 256:512], ALU.mult, ["gg"], ["g1"])
            K.rsum("dve", st[:, 0:1], g1[:, 0:256], ["g1"], ["st"])
            rstd_of(st[:, 0:1], 1, 256)
            K.ts("dve", g1[:, 256:512], gg[:, 256:512], st[:, 0:1], None, ALU.mult, None, ["gg", "st", "g1"], ["g1"])
            K.tt("pool", vnb[:, :], g1[:, 256:512], gains[:, G_AV:G_AV + 256], ALU.mult, ["g1", "gains"], ["vnb"])
            for hd in range(4):
                K.mm(PB[base + 3][:, hd * 64:(hd + 1) * 64], wsT[:, hd, :], vnb[:, hd * 64:(hd + 1) * 64], True, True,
                     ["wsT", "vnb"], [f"pb{base + 3}a"])
            for hd in range(4):
                K.stt("dve", oa[:, hd * 64:(hd + 1) * 64], PB[base + 3][:, hd * 64:(hd + 1) * 64], bs[:, hd:hd + 1],
                      gg[:, hd * 64:(hd + 1) * 64], ALU.add, ALU.mult, [f"pb{base + 3}a", "bs", "gg"], ["oa"])
            K.dma("sp", oa_d[rows, :], oa[:, :], ["oa"], [])
            for which, (o0, gofs, extra) in enumerate(((512, G_BQ, 0.125), (896, G_BK, None))):
                src = z[:, o0:o0 + 384]
                K.tt("pool", t384[:, :], src, src, ALU.mult, ["z"], ["t384"])
                K.rsum("dve", st[:, 0:6], v3(t384[:, :], 6), ["t384"], ["st"])
                rstd_of(st[:, 0:6], 6, 64, extra)
                K.tt("dve", v3(u384[:, :], 6), v3(src, 6), bc3(st[:, 0:6], 64), ALU.mult, ["z", "st"], ["u384"])
                dst = qnb[:, :] if which == 0 else kvbs[:, 0:384]
                K.tt("pool", dst, u384[:, :], gains[:, gofs:gofs + 384], ALU.mult, ["u384", "gains"],
                     ["qnb" if which == 0 else "kvbs"])
            for pr in range(3):
                K.tr(PBT[:, pr * 128:(pr + 1) * 128], qnb[:, pr * 128:(pr + 1) * 128], ident[:, :], ["qnb", "ident"], [kT])
            K.cp("dve", qbTs[:, :, :], v3(PBT[:, 0:384], 3), [kT], ["qbTs"])
            K.dma("sp", qbT_d[:, :, rows].rearrange("a p t -> p a t"), qbTs[:, :, :], ["qbTs"], [])
            K.cp("act", kvbs[:, 384:768], z[:, 1280:1664], ["z"], ["kvbs"])
            if t < NT_OWN:
                K.dma("sp", kvb_own[rows, :], kvbs[:, :], ["kvbs"], ["kvb_x"])
                if t < HT:
                    K.dma("sp", kvb_lo[t * 128:(t + 1) * 128, :], kvbs[:, :], ["kvbs"], ["kvb_x"])
                if t >= NT_OWN - HT:
                    t2 = t - (NT_OWN - HT)
                    K.dma("sp", kvb_hi[t2 * 128:(t2 + 1) * 128, :], kvbs[:, :], ["kvbs"], ["kvb_x"])
            else:
                K.dma("sp", kvb_ctx[(t - NT_OWN) * 128:(t - NT_OWN + 1) * 128, :], kvbs[:, :], ["kvbs"], ["kvb_x"])
            src = z[:, 1664:2048]
            K.tt("pool", t384[:, :], src, src, ALU.mult, ["z"], ["t384"])
            K.rsum("dve", st[:, 0:1], t384[:, :], ["t384"], ["st"])
            rstd_of(st[:, 0:1], 1, 384)
            K.ts("dve", u384[:, :], src, st[:, 0:1], None, ALU.mult, None, ["z", "st"], ["u384"])
            K.tt("pool", qab[:, :], u384[:, :], gains[:, G_CQA:G_CQA + 384], ALU.mult, ["u384", "gains"], ["qab"])
            for c in range(3):
                K.tr(PBT[:, c * 128:(c + 1) * 128], qab[:, c * 128:(c + 1) * 128], ident[:, :], ["qab", "ident"], [kT])
            K.cp("dve", qaT[:, :, :], v3(PBT[:, 0:384], 3), [kT], ["qaT"])
            for c in range(3):
                K.mm(PB[base + 1][:, 0:512], qaT[:, c, :], wq[:, c, 0:512], c == 0, c == 2, ["qaT", "wq"], [f"pb{base + 1}"])
            for c in range(3):
                K.mm(PB[base + 3][:, 256:320], qaT[:, c, :], wq[:, c, 512:576], c == 0, c == 2, ["qaT", "wq"], [f"pb{base + 3}b"])
            K.cp("act", qf[:, 0:512], PB[base + 1][:, 0:512], [f"pb{base + 1}"], ["qf"])
            K.cp("act", qf[:, 512:576], PB[base + 3][:, 256:320], [f"pb{base + 3}b"], ["qf"])
            qf3 = v3(qf[:, :], 6)
            qs3 = v3(qs[:, :], 6)
            K.tt("pool", qs[:, :], qf[:, :], qf[:, :], ALU.mult, ["qf"], ["qs"])
            K.rsum("dve", st[:, 0:6], qs3[:, :, 0:64], ["qs"], ["st"])
            K.rsum("dve", st[:, 8:14], qs3[:, :, 64:96], ["qs"], ["st"])
            rstd_of(st[:, 0:6], 6, 64)
            rstd_of(st[:, 8:14], 6, 32)
            K.tt("dve", qs3[:, :, 0:64], qf3[:, :, 0:64], bc3(st[:, 0:6], 64), ALU.mult, ["qf", "st", "qs"], ["qs"])
            K.tt("dve", qs3[:, :, 64:96], qf3[:, :, 64:96], bc3(st[:, 8:14], 32), ALU.mult, ["qf", "st", "qs"], ["qs"])
            K.tt("pool", qf[:, :], qs[:, :], gains[:, G_CQN:G_CQN + 576], ALU.mult, ["qs", "gains"], ["qf"])
            K.cp("act", qc[:, :, 0:64], qf3[:, :, 0:64], ["qf"], ["qc"])
            rope(qf3[:, :, 64:96], qc[:, :, 64:96], 6, ropet[b], ["qf", ("rope", b)], ["qc"])
            for h in range(6):
                K.tr(PBT[0:96, h * 128:(h + 1) * 128], qc[:, h, :], ident[:, :], ["qc", "ident"], [kT])
            K.cp("dve", qcTs[:, :, :], v3(PBT[0:96, 0:768], 6), [kT], ["qcTs"])
            K.dma("sp", qcT_d[:, :, rows].rearrange("h p t -> p h t"), qcTs[:, :, :], ["qcTs"], [])
            src = z[:, 2048:2304]
            K.tt("pool", t384[:, 0:256], src, src, ALU.mult, ["z"], ["t384"])
            K.rsum("dve", st[:, 0:1], t384[:, 0:256], ["t384"], ["st"])
            rstd_of(st[:, 0:1], 1, 256)
            K.ts("dve", u384[:, 0:256], src, st[:, 0:1], None, ALU.mult, None, ["z", "st"], ["u384"])
            K.tt("pool", kvab[:, :], u384[:, 0:256], gains[:, G_CKVA:G_CKVA + 256], ALU.mult, ["u384", "gains"], ["kvab"])
            for c in range(2):
                K.tr(PBT[:, c * 128:(c + 1) * 128], kvab[:, c * 128:(c + 1) * 128], ident[:, :], ["kvab", "ident"], [kT])
            K.cp("dve", kvaT[:, :, :], v3(PBT[:, 0:256], 2), [kT], ["kvaT"])
            for c in range(2):
                K.mm(PB[base + 2][:, 0:512], kvaT[:, c, :], wkv[:, c, 0:512], c == 0, c == 1, ["kvaT", "wkv"], [f"pb{base + 2}"])
            for c in range(2):
                K.mm(PB[base + 3][:, 0:256], kvaT[:, c, :], wkv[:, c, 512:768], c == 0, c == 1, ["kvaT", "wkv"], [f"pb{base + 3}a"])
            K.cp("act", kvf[:, 0:512], PB[base + 2][:, 0:512], [f"pb{base + 2}"], ["kvf"])
            K.cp("act", kvf[:, 512:768], PB[base + 3][:, 0:256], [f"pb{base + 3}a"], ["kvf"])
            kvf3 = v3(kvf[:, :], 6)
            K.cp("act", vcs[:, :, 0:64], kvf3[:, :, 64:128], ["kvf"], ["vcs"])
            if t < NT_OWN:
                K.dma("sp", d[f"vc_own_{L}_{t // PT}"][(t % PT) * 128:(t % PT + 1) * 128, :],
                      vcs[:, :, :].rearrange("p h d -> p (h d)"), ["vcs"], ["vc_x"])
            else:
                K.dma("sp", vc_ctx[(t - NT_OWN) * 128:(t - NT_OWN + 1) * 128, :], vcs[:, :, :].rearrange("p h d -> p (h d)"), ["vcs"], ["vc_x"])
            t3 = v3(t384[:, :], 6)
            u3 = v3(u384[:, :], 6)
            K.tt("pool", t3, kvf3[:, :, 0:64], kvf3[:, :, 0:64], ALU.mult, ["kvf"], ["t384"])
            K.rsum("dve", st[:, 0:6], t3, ["t384"], ["st"])
            rstd_of(st[:, 0:6], 6, 64)
            K.tt("dve", u3, kvf3[:, :, 0:64], bc3(st[:, 0:6], 64), ALU.mult, ["kvf", "st"], ["u384"])
            K.tt("pool", kc[:, :, 0:64], u3, v3(gains[:, G_CKN:G_CKN + 384], 6), ALU.mult, ["u384", "gains"], ["kc"])
            src = z[:, 2304:2336]
            K.tt("pool", kr[:, :], src, src, ALU.mult, ["z"], ["kr"])
            K.rsum("dve", st[:, 0:1], kr[:, :], ["kr"], ["st"])
            rstd_of(st[:, 0:1], 1, 32)
            K.ts("dve", kr[:, :], src, st[:, 0:1], None, ALU.mult, None, ["z", "st", "kr"], ["kr"])
            K.tt("pool", kr[:, :], kr[:, :], gains[:, G_CKR:G_CKR + 32], ALU.mult, ["kr", "gains"], ["kr"])
            rope(v3(kr[:, :], 1), v3(krr[:, :], 1), 1, ropet[b], ["kr", ("rope", b)], ["krr"])
            K.cp("dve", kc[:, :, 64:96], krr[:, :].unsqueeze(1).to_broadcast([128, 6, 32]), ["krr"], ["kc"])
            for h in range(6):
                K.tr(PBT[0:96, h * 128:(h + 1) * 128], kc[:, h, :], ident[:, :], ["kc", "ident"], [kT])
            K.cp("dve", kcTs[:, :, :], v3(PBT[0:96, 0:768], 6), [kT], ["kcTs"])
            if t < NT_OWN:
                K.dma("sp", d[f"kcT_own_{L}_{t // PT}"].rearrange("(h p) t -> p h t", h=6)[:, :, (t % PT) * 128:(t % PT + 1) * 128],
                      kcTs[:, :, :], ["kcTs"], ["kc_x"])
            else:
                K.dma("sp", kcT_ctx.rearrange("(h p) t -> p h t", h=6)[:, :, (t - NT_OWN) * 128:(t - NT_OWN + 1) * 128],
                      kcTs[:, :, :], ["kcTs"], ["kc_x"])
        S.rec = None
        for p0 in range(0, NT, 2):
            pair = recs[p0:p0 + 2]
            ptr = [0] * len(pair)
            while any(ptr[j_] < len(pair[j_]) for j_ in range(len(pair))):
                for j_, r_ in enumerate(pair):
                    if ptr[j_] < len(r_):
                        S._add(*r_[ptr[j_]])
                        ptr[j_] += 1
                        while ptr[j_] < len(r_) and r_[ptr[j_]][0] == "pe" and r_[ptr[j_] - 1][0] == "pe":
                            S._add(*r_[ptr[j_]])
                            ptr[j_] += 1
        S.emit()


def _kmaj(w, kc):
    return np.ascontiguousarray(w.reshape(kc, 128, -1).transpose(1, 0, 2))


def _colT(v):
    return np.ascontiguousarray(v.reshape(-1, 128).T)


def _bcrow(v):
    return np.ascontiguousarray(np.broadcast_to(v[None, :], (128, v.shape[0])))


def rope_tables(NT_OWN, rank):
    half = 16
    inv = (np.float32(10000.0) ** (-(np.arange(0, half, 2, dtype=np.float32)) / np.float32(half))).astype(np.float32)
    pos = np.arange(NT_OWN * 128) + rank * NT_OWN * 128
    out = np.zeros(((NT_OWN + 2) * 128, 32), np.float32)
    ar = (pos // GRID_W).astype(np.float32)[:, None] * inv[None, :]
    ac = (pos % GRID_W).astype(np.float32)[:, None] * inv[None, :]
    n = NT_OWN * 128
    out[:n, 0:8] = np.cos(ar)
    out[:n, 8:16] = np.sin(ar)
    out[:n, 16:24] = np.cos(ac)
    out[:n, 24:32] = np.sin(ac)
    out[n:, 0:8] = 1.0
    out[n:, 16:24] = 1.0
    return out


def prep_A(P, i, x_cur, xc_cur, NT_OWN, ranks_per_batch=4):
    B = x_cur.shape[0]
    gains = np.concatenate([
        P["a_v_norm"][i], np.tile(P["b_q_norm"][i], 6), np.tile(P["b_k_norm"][i], 6), P["c_q_a_norm"][i],
        P["c_kv_a_norm"][i], np.tile(P["c_q_norm"][i], 6), np.tile(P["c_k_norm"][i][:64], 6), P["c_k_norm"][i][64:]])
    shared = {
        "w_ada": _kmaj(P["w_ada"][i], 8),
        "b_adaT": _colT(P["b_ada"][i]),
        "norm_mixT": _colT(P["norm_mix"][i]),
        "w_in": _kmaj(P["w_in"][i], 8),
        "w_sT": np.ascontiguousarray(P["a_w_s"][i].transpose(2, 0, 1)),
        "b_s": np.ascontiguousarray(P["a_b_s"][i].T),
        "gains": _bcrow(gains.astype(np.float32)),
        "wq": _kmaj(P["c_w_q_up"][i], 3),
        "wkv": _kmaj(P["c_w_kv_up"][i], 2),
        "ident": np.eye(128, dtype=np.float32),
    }
    maps = []
    n = NT_OWN * 128
    for b in range(B):
        cT = np.stack([_colT(P["c"][b]), _colT(P["c_ctx"])], axis=2).reshape(128, 16)
        for r in range(ranks_per_batch):
            m = dict(shared)
            m["x"] = np.ascontiguousarray(np.concatenate([x_cur[b, r * n:(r + 1) * n], xc_cur[b]], axis=0))
            m["cT"] = np.ascontiguousarray(cT)
            m["rope"] = rope_tables(NT_OWN, r)
            maps.append(m)
    return maps


def _specials(NT_OWN):
    return sorted(set(t for t in (0, 1, NT_OWN - 2, NT_OWN - 1) if 0 <= t < NT_OWN))


def emit_B(nc, gc, d, NT_OWN, L, TB=4, NE=64, NR=4):
    NT = NT_OWN + 2
    T = NT * 128
    n = NT_OWN * 128
    NW = NT_OWN + 8
    NKT = NR * NT_OWN + 2
    QB = min(4, NT_OWN)
    CH = min(4, NT_OWN)
    specials = _specials(NT_OWN)
    BIG = 1.0e30
    stack = contextlib.ExitStack()
    with stack:
        K = KB(nc, stack, gc)
        S = K.S
        x_d = d["x"] if L == 0 else d["xs_0"]
        xo_d = d["xs_0"] if L == 0 else d["xo"]
        oa_d, qbT_d, qcT_d, modT_d, mix_d = d[f"oa_{L}"], d[f"qbT_{L}"], d[f"qcT_{L}"], d[f"modT_{L}"], d[f"mixd_{L}"]
        PCT = min(4, NT_OWN)
        HT = min(3, NT_OWN)
        kcT_ctx, vc_ctx = d[f"kcT_ctx_{L}"], d[f"vc_ctx_{L}"]
        kvb_own, kvb_ctx, kvb_glo, kvb_ghi = d[f"kvb_own_{L}"], d[f"kvb_ctx_{L}"], d[f"kvb_glo_{L}"], d[f"kvb_ghi_{L}"]
        btab_d, wout_d, nffn_d, wgr_d = d[f"btab_{L}"], d[f"w_out_{L}"], d[f"norm_ffnT_{L}"], d[f"wgr_{L}"]
        w1_d, w3_d, w2_d = d[f"w1_{L}"], d[f"w3_{L}"], d[f"w2_{L}"]
        ident_d, widx_d = d["ident"], d["widx"]

        AR = 45056
        arena = K.sb("arena", [128, AR], BF16)
        identb = K.sb("ident_b", [128, 128], BF16)
        identf = K.sb("identf", [128, 128], F32)
        ones = K.sb("ones", [128, 128], F32)
        modT = K.sb("modTs", [128, 96], F32)
        nffn = K.sb("nffn", [128, 8], F32)
        A2 = K.sb("A2", [128, 16], F32)
        G = [K.sb(f"G{i}", [128, D], F32) for i in range(4)]
        gbc = K.sb("gbc", [128, 128], F32)
        wgr = K.sb("wgrs", [128, 8, 72], F32)
        kvt = [K.sb(f"kvt{i}", [128, 768], BF16) for i in range(2)]
        qbt = [K.sb(f"qbt{i}", [128, 3, 128], BF16) for i in range(2)]
        PT = [K.sb(f"PT{i}", [128, 512], BF16) for i in range(3)]
        st = K.sb("st", [128, 64], F32)
        ob = K.sb("ob", [128, 384], BF16)
        oc = K.sb("oc", [128, 4, 64], BF16)
        ocT = K.sb("ocT", [65, 512], F32)
        qT = [K.sb(f"qT{i}", [96, 512], BF16) for i in range(2)]
        xt = [K.sb(f"xt{i}", [128, D], F32) for i in range(2)]
        mixrow = K.sb("mixrow", [128, D], BF16)
        mixT = K.sb("mixT", [128, 8, 128], BF16)
        tmpf = K.sb("tmpf", [128, D], F32)
        h2n = K.sb("h2n", [128, D], F32)
        h2Tf = K.sb("h2Tf", [128, 8, 128], F32)
        lg = K.sb("lg", [128, 72], F32)
        r64 = [K.sb(f"r64_{i}", [128, 64], F32) for i in range(4)]
        sil = [K.sb(f"sil{i}", [128, 256], F32) for i in range(2)]
        hT = K.sb("hTe", [128, 4, 256], BF16)
        PB = [K.ps(f"pb{i}", [128, 512], F32) for i in range(8)]
        PB0b = PB[0].bitcast(BF16)

        K.dma("sp", identf[:, :], ident_d[:, :], [], ["identf"])
        K.cp("dve", identb[:, :], identf[:, :], ["identf"], ["identb"])
        K.memset("dve", ones[:, :], 1.0, [], ["ones"])
        K.dma("sp", modT[:, :], modT_d[:, :], [], ["modT"])
        K.dma("sp", nffn[:, :], nffn_d[:, :], [], ["nffn"])
        K.dma("sp", wgr[:, :, :], wgr_d[:, :, :], [], ["wgr"])
        K.stt("dve", v3(A2[:, :], 8), v3(modT[:, 64:80], 8), 1.0, bc3(nffn[:, :], 2), ALU.add, ALU.mult,
              ["modT", "nffn"], ["A2"])
        for gi, (j0, wh) in enumerate(((16, 0), (16, 1), (40, 0), (40, 1))):
            for c in range(8):
                col = 2 * (j0 + c) + wh
                K.ts("dve", gbc[:, :], ones[:, :], modT[:, col:col + 1], None, ALU.mult, None, ["ones", "modT"], ["gbc"])
                K.mm(PB[7][:, (c % 4) * 128:(c % 4 + 1) * 128], gbc[:, :], identf[:, :], True, True, ["gbc", "identf"], ["pb7"])
                K.cp("act", G[gi][:, c * 128:(c + 1) * 128], PB[7][:, (c % 4) * 128:(c % 4 + 1) * 128], ["pb7"], [("G", gi)])

        o1 = 3 * NW * 128
        o2 = o1 + NW * 390
        KbT = arena[:, 0:o1].rearrange("p (a t) -> p a t", a=3)
        Vb = arena[:, o1:o2].rearrange("p (s h d) -> p s h d", s=NW, h=6)
        bt0 = arena[:, o2:o2 + 6144].rearrange("p (h e) -> p h e", h=6)
        btS = arena[:, o2 + 6144:o2 + 12288].rearrange("p (h e) -> p h e", h=6)
        assert o2 + 12288 <= AR
        K.memset("pool", arena[:, o1:o2], 1.0, [], ["Vb"])
        K.dma("sp", bt0, btab_d[0], [], ["bt0"])
        widx = K.sb("widx", [128, 6], I32)
        K.dma("sp", widx[:, :], widx_d[:, :], [], ["widx"])
        for s in range(NW):
            b = s % 2
            if s < 3 or NT_OWN + 3 <= s < NT_OWN + 6:
                srcg = kvb_ghi if s < 3 else kvb_glo
                col = s if s < 3 else s - NT_OWN
                K.S.dmafn("pool", lambda e, o=kvt[b][:, :], ix=widx[:, col:col + 1], sg=srcg: e.indirect_dma_start(
                    out=o, out_offset=None, in_=sg[:, :], in_offset=bass.IndirectOffsetOnAxis(ap=ix, axis=0)),
                    ["widx"], [("kvt", b)])
            elif s < NT_OWN + 3:
                K.dma("sp", kvt[b][:, :], kvb_own[(s - 3) * 128:(s - 2) * 128, :], [], [("kvt", b)])
            else:
                K.dma("sp", kvt[b][:, :], kvb_ctx[(s - NT_OWN - 6) * 128:(s - NT_OWN - 5) * 128, :], [], [("kvt", b)])
            for pr in range(3):
                K.tr(PB0b[:, pr * 128:(pr + 1) * 128], kvt[b][:, pr * 128:(pr + 1) * 128], identb[:, :], [("kvt", b), "identb"], ["pb0"])
            K.cp("dve", KbT[:, :, s * 128:(s + 1) * 128], v3(PB0b[:, 0:384], 3), ["pb0"], ["KbT"])
            K.cp("pool", Vb[:, s, :, 0:64], v3(kvt[b][:, 384:768], 6), [("kvt", b), "Vb"], ["Vb"])
        DSK = 2
        itemsB = []
        for t in range(NT):
            b = t % 2
            own = t < NT_OWN
            rows = slice(t * 128, (t + 1) * 128)
            special = own and t in specials
            bt, btk = (btS, "btS") if special else (bt0, "bt0")
            if own:
                kts = [(t + j + 3, j) for j in range(-3, 4)] + [(NW - 2, None), (NW - 1, None)]
            else:
                kts = [(NW - 2, None), (NW - 1, None)]
            groups = [kts[i:i + 3] for i in range(0, len(kts), 3)]
            ob_bank = 4 + t % 2
            for h in range(6):
                nk0 = 0
                for gi, grp in enumerate(groups):
                    itemsB.append(dict(t=t, b=b, h=h, grp=grp, nk0=nk0, nkt=len(kts), ob_bank=ob_bank, bt=bt, btk=btk,
                                       first=(h == 0 and gi == 0), last=(h == 5 and gi == len(groups) - 1),
                                       special=special, rows=rows))
                    nk0 += len(grp)

        def qkB(i, it):
            b, h, t = it["b"], it["h"], it["t"]
            if it["first"]:
                K.dma("sp", qbt[b][:, :, :], qbT_d[:, :, it["rows"]].rearrange("a p t -> p a t"), [], [("qbt", b)])
                if it["special"]:
                    K.dma("sp", btS, btab_d[1 + specials.index(t)], [], ["btS"])
            pr, pb = h // 2, (h % 2) * 64
            bank = 1 + i % 3
            pi = i % 3
            for ii, (s_, j) in enumerate(it["grp"]):
                K.mm(PB[bank][:, ii * 128:(ii + 1) * 128], KbT[pb:pb + 64, pr, s_ * 128:(s_ + 1) * 128],
                     qbt[b][pb:pb + 64, pr, :], True, j is None, ["KbT", ("qbt", b)], [f"pb{bank}"])
                if j is not None:
                    e0 = (7 - 2 * j) * 64
                    K.mm(PB[bank][:, ii * 128:(ii + 1) * 128], identb[:, :], it["bt"][:, h, e0:e0 + 128], False, True,
                         ["identb", it["btk"]], [f"pb{bank}"])
            n_ = len(it["grp"]) * 128
            op_ = K.S.op("act", lambda e, o=PT[pi][:, 0:n_], i_=PB[bank][:, 0:n_]: e.activation(o, i_, AF.Exp),
                         [f"pb{bank}"], [("PT", pi)])
            if i >= 3:
                op_.deps = [d_ for d_ in op_.deps if d_.dma or d_.eng != "act"]

        def pvB(i, it):
            h = it["h"]
            pi = i % 3
            OB = PB[it["ob_bank"]]
            for ii, (s_, j) in enumerate(it["grp"]):
                nk = it["nk0"] + ii
                K.mm(OB[:, h * 65:(h + 1) * 65], PT[pi][:, ii * 128:(ii + 1) * 128], Vb[:, s_, h, :],
                     nk == 0, nk == it["nkt"] - 1, [("PT", pi), "Vb"], [f"pb{it['ob_bank']}"])
            if it["last"]:
                O3 = v3(OB[:, 0:390], 6)
                K.S.op("dve", lambda e, O3=O3: e.reciprocal(st[:, 0:6], O3[:, :, 64]), [f"pb{it['ob_bank']}"], ["st"])
                K.tt("dve", v3(ob[:, :], 6), O3[:, :, 0:64], bc3(st[:, 0:6], 64), ALU.mult, [f"pb{it['ob_bank']}", "st"], ["ob"])
                K.dma("sp", mix_d[it["rows"], 256:640], ob[:, :], ["ob"], [("mixd", it["t"])])

        for i in range(len(itemsB) + DSK):
            if i < len(itemsB):
                qkB(i, itemsB[i])
            if i - DSK >= 0:
                pvB(i - DSK, itemsB[i - DSK])

        S.barrier()
        Kc = [arena[:, i * 2048:(i + 1) * 2048] for i in range(3)]
        Vc = [arena[:, 6144 + i * 1040:6144 + (i + 1) * 1040].rearrange("p (k d) -> p k d", d=65) for i in range(3)]
        qblocks = [(t0, QB, list(range(NKT))) for t0 in range(0, NT_OWN, QB)] + [(NT_OWN, 2, [NKT - 2, NKT - 1])]
        SCALE_C = 96.0 ** -0.5
        itemsC = []
        qh = 0
        cc = 0
        for (t0, ntl, klist) in qblocks:
            nq = ntl * 128
            for h in range(6):
                b2 = qh % 2
                oc_bank = 4 + qh % 2
                qh += 1
                own_k = [k for k in klist if k < NR * NT_OWN]
                ctx_k = [k for k in klist if k >= NR * NT_OWN]
                chunks = [own_k[i:i + CH] for i in range(0, len(own_k), CH)] + ([ctx_k] if ctx_k else [])
                nk = 0
                for chk in chunks:
                    cb = cc % 3
                    cc += 1
                    for kt in range(len(chk)):
                        itemsC.append(dict(t0=t0, ntl=ntl, nq=nq, h=h, b2=b2, oc_bank=oc_bank, cb=cb, chk=chk, kt=kt,
                                           newq=(nk == 0), newchunk=(kt == 0), nk=nk, nkt=len(klist)))
                        nk += 1

        def qkC(i, it):
            h, b2, cb, nq = it["h"], it["b2"], it["cb"], it["nq"]
            if it["newq"]:
                K.dma("sp", qT[b2][:, 0:nq], qcT_d[h, :, it["t0"] * 128:it["t0"] * 128 + nq], [], [("qT", b2)])
            if it["newchunk"]:
                chk = it["chk"]
                k0, n_k = chk[0], len(chk)
                if k0 < NR * NT_OWN:
                    rk, l0 = k0 // NT_OWN, k0 % NT_OWN
                    pj, lt = l0 // PCT, l0 % PCT
                    assert lt + n_k <= PCT
                    ksrc = d[f"kcT_g_{L}_{pj}"].rearrange("(r h p) t -> r h p t", r=NR, h=6)[rk, h, :, lt * 128:(lt + n_k) * 128]
                    v0 = (rk * PCT + lt) * 128
                    vsrc = d[f"vc_g_{L}_{pj}"][v0:v0 + n_k * 128, h * 65:(h + 1) * 65]
                else:
                    c0_ = (k0 - NR * NT_OWN) * 128
                    ksrc = kcT_ctx.rearrange("(h p) t -> h p t", h=6)[h, :, c0_:c0_ + n_k * 128]
                    vsrc = vc_ctx[c0_:c0_ + n_k * 128, h * 65:(h + 1) * 65]
                K.dma("sp", Kc[cb][0:96, 0:n_k * 128], ksrc, [], [("Kc", cb)])
                K.dma("sp", Vc[cb][:, 0:n_k, :], vsrc.rearrange("(k p) d -> p k d", p=128), [], [("Vc", cb)])
            bank = 1 + i % 3
            pi = i % 3
            kt = it["kt"]
            K.mm(PB[bank][:, 0:nq], Kc[cb][0:96, kt * 128:(kt + 1) * 128], qT[b2][:, 0:nq], True, True,
                 [("Kc", cb), ("qT", b2)], [f"pb{bank}"])
            op_ = K.S.op("act", lambda e, o=PT[pi][:, 0:nq], i_=PB[bank][:, 0:nq]: e.activation(o, i_, AF.Exp, scale=SCALE_C),
                         [f"pb{bank}"], [("PT", pi)])
            if i >= 3:
                op_.deps = [d_ for d_ in op_.deps if d_.dma or d_.eng != "act"]

        def pvC(i, it):
            pi = i % 3
            nq, ntl, oc_bank, cb, kt, h = it["nq"], it["ntl"], it["oc_bank"], it["cb"], it["kt"], it["h"]
            OC = PB[oc_bank]
            K.mm(OC[0:65, 0:nq], Vc[cb][:, kt, :], PT[pi][:, 0:nq], it["nk"] == 0, it["nk"] == it["nkt"] - 1,
                 [("PT", pi), ("Vc", cb)], [f"pb{oc_bank}"])
            if it["nk"] == it["nkt"] - 1:
                t0 = it["t0"]
                K.cp("dve", ocT[:, 0:nq], OC[0:65, 0:nq], [f"pb{oc_bank}"], ["ocT"])
                for qi in range(ntl):
                    K.tr(PB[0][:, qi * 65:(qi + 1) * 65], ocT[:, qi * 128:(qi + 1) * 128], identf[0:65, 0:65], ["ocT", "identf"], ["pb0"])
                O3 = v3(PB[0][:, 0:ntl * 65], ntl)
                K.S.op("dve", lambda e, O3=O3, ntl=ntl: e.reciprocal(st[:, 0:ntl], O3[:, :, 64]), ["pb0"], ["st"])
                K.tt("dve", oc[:, 0:ntl, :], O3[:, :, 0:64], bc3(st[:, 0:ntl], 64), ALU.mult, ["pb0", "st"], ["oc"])
                K.dma("sp", mix_d[t0 * 128:t0 * 128 + nq, 640 + h * 64:704 + h * 64].rearrange("(q p) d -> p q d", p=128),
                      oc[:, 0:ntl, :], ["oc"], [("mixd", t0 + i_) for i_ in range(ntl)])

        for i in range(len(itemsC) + DSK):
            if i < len(itemsC):
                qkC(i, itemsC[i])
            if i - DSK >= 0:
                pvC(i - DSK, itemsC[i - DSK])

        S.barrier()
        NB = (2 * T + 255) // 256 + 64
        h2_d, x1_d, xs_d, ys_d = d[f"h2_{L}"], d[f"x1_{L}"], d[f"xsl_{L}"], d[f"ys_{L}"]
        w1r = w1_d.rearrange("e p c n -> (e p) (c n)")
        w3r = w3_d.rearrange("e p c n -> (e p) (c n)")
        w2r = w2_d.rearrange("e p c n -> (e p) (c n)")
        WSZ = 12288
        Wb = [arena[:, i * WSZ:(i + 1) * WSZ] for i in range(2)]
        wout = arena[:, 2 * WSZ:2 * WSZ + 8192].rearrange("p (c n) -> p c n", c=8)
        ohb = arena[:, 2 * WSZ + 8192:2 * WSZ + 8192 + NT * 128].rearrange("p (t e) -> p t e", t=NT)
        assert 2 * WSZ + 8192 + NT * 128 <= AR
        K.dma("pool", wout, wout_d[:, :, :], [], ["wout"])
        ut = K.sb("ut", [128, 128], F32)
        iop = K.sb("iop", [128, 1], F32)
        K.dma("sp", ut[:, :], d["ut"][:, :], [], ["ut"])
        K.dma("sp", iop[:, :], d["iop"][:, :], [], ["iop"])
        base = K.sb("base", [128, 64], F32)
        K.memset("dve", base[:, :], 0.0, [], ["base"])
        rk = K.sb("rk", [128, NT, 2], F32)
        wts = K.sb("wts", [128, NT, 2], F32)
        dstf = K.sb("dstf", [128, NT, 2], F32)
        dsti = K.sb("dsti", [128, NT * 2], I32)
        h2s = [K.sb(f"h2s{i}", [128, D], BF16) for i in range(2)]
        MB = [K.sb(f"MB{i}", [128, D], F32) for i in range(4)]
        for gi, (src, wh) in enumerate((("A2", 0), ("A2", 1), ("B2", 0), ("B2", 1))):
            for c in range(8):
                col = (A2[:, 2 * c + wh:2 * c + wh + 1] if src == "A2"
                       else modT[:, 2 * (24 + c) + wh:2 * (24 + c) + wh + 1])
                K.ts("dve", gbc[:, :], ones[:, :], col, None, ALU.mult, None, ["ones", "modT", "A2"], ["gbc"])
                K.mm(PB[7][:, (c % 4) * 128:(c % 4 + 1) * 128], gbc[:, :], identf[:, :], True, True, ["gbc", "identf"], ["pb7"])
                K.cp("act", MB[gi][:, c * 128:(c + 1) * 128], PB[7][:, (c % 4) * 128:(c % 4 + 1) * 128], ["pb7"], [("MB", gi)])

        def rstd_of(ss, dim):
            K.ts("dve", ss, ss, 1.0 / dim, EPS, ALU.mult, ALU.add, ["st"], ["st"])
            K.act(ss, ss, AF.Sqrt, ["st"], ["st"])
            K.S.op("dve", lambda e, a=ss: e.reciprocal(a, a), ["st"], ["st"])

        x1t = [K.sb(f"x1t{i}", [128, D], F32) for i in range(2)]
        for t in range(NT):
            b = t % 2
            wh = 0 if t < NT_OWN else 1
            rows = slice(t * 128, (t + 1) * 128)
            X1 = x1t[b]
            K.dma("sp", xt[b][:, :], x_d[rows, :], [], [("xt", b)])
            K.dma("sp", mixrow[:, 256:1024], mix_d[rows, 256:1024], [("mixd", t)], ["mixrow"])
            K.dma("sp", mixrow[:, 0:256], oa_d[rows, :], [], ["mixrow"])
            for c in range(8):
                K.tr(PB0b[:, c * 128:(c + 1) * 128], mixrow[:, c * 128:(c + 1) * 128], identb[:, :], ["mixrow", "identb"], ["pb0"])
            K.cp("dve", mixT[:, :, :], v3(PB0b[:, :], 8), ["pb0"], ["mixT"])
            for half in range(2):
                pb = 1 + half
                for c in range(8):
                    K.mm(PB[pb][:, :], mixT[:, c, :], wout[:, c, half * 512:(half + 1) * 512], c == 0, c == 7,
                         ["mixT", "wout"], [f"pb{pb}"])
                hs = slice(half * 512, (half + 1) * 512)
                K.tt("dve", tmpf[:, hs], PB[pb][:, :], G[wh][:, hs], ALU.mult, [f"pb{pb}", ("G", wh)], ["tmpf"])
                K.tt("pool", X1[:, hs], tmpf[:, hs], xt[b][:, hs], ALU.add, ["tmpf", ("xt", b)], [("x1", b)])
            K.dma("sp", x1_d[rows, :], X1[:, :], [("x1", b)], [])
            K.tt("pool", tmpf[:, :], X1[:, :], X1[:, :], ALU.mult, [("x1", b), "tmpf"], ["tmpf"])
            K.rsum("dve", st[:, 0:1], tmpf[:, :], ["tmpf"], ["st"])
            rstd_of(st[:, 0:1], D)
            K.act(h2n[:, :], X1[:, :], AF.Copy, [("x1", b), "st"], ["h2n"], scale=st[:, 0:1])
            K.tt("pool", tmpf[:, :], h2n[:, :], MB[wh][:, :], ALU.mult, ["h2n", ("MB", wh), "tmpf"], ["tmpf"])
            K.tt("pool", h2s[b][:, :], tmpf[:, :], MB[2 + wh][:, :], ALU.add, ["tmpf", ("MB", 2 + wh)], [("h2s", b)])
            K.dma("sp", h2_d[rows, :], h2s[b][:, :], [("h2s", b)], [])
            for c in range(8):
                pb = 6 + c // 4
                K.tr(PB[pb][:, (c % 4) * 128:(c % 4 + 1) * 128], h2n[:, c * 128:(c + 1) * 128], identf[:, :], ["h2n", "identf"], [f"pb{pb}"])
            for c in range(8):
                pb = 6 + c // 4
                K.ts("dve", h2Tf[:, c, :], PB[pb][:, (c % 4) * 128:(c % 4 + 1) * 128], A2[:, 2 * c + wh:2 * c + wh + 1],
                     modT[:, 2 * (24 + c) + wh:2 * (24 + c) + wh + 1], ALU.mult, ALU.add, [f"pb{pb}", "A2", "modT"], ["h2Tf"])
            for c in range(8):
                K.mm(PB[3][:, 0:72], h2Tf[:, c, :], wgr[:, c, :], c == 0, c == 7, ["h2Tf", "wgr"], ["pb3"])
            K.cp("act", lg[:, :], PB[3][:, 0:72], ["pb3"], ["lg"])
            gl = lg[:, 0:8]
            rl3 = v3(lg[:, 8:72], 8)
            s_gmax, s_ngmax, s_gsum, s_m1, s_m2, s_d, s_e2, s_wa, s_wb = [st[:, 16 + i:17 + i] for i in range(9)]
            goh, gex, pen = st[:, 32:40], st[:, 40:48], st[:, 48:56]
            RK = ["lg", "st", "r64"]
            K.S.op("dve", lambda e, a=s_gmax, g=gl: e.reduce_max(a, g, AX.X), ["lg"], ["st"])
            K.ts("dve", goh, gl, s_gmax, None, ALU.is_equal, None, RK, ["st"])
            K.ts("dve", s_ngmax, s_gmax, -1.0, None, ALU.mult, None, RK, ["st"])
            K.act(gex, gl, AF.Exp, RK, ["st"], bias=s_ngmax)
            K.rsum("dve", s_gsum, gex, RK, ["st"])
            K.S.op("dve", lambda e, a=s_gsum: e.reciprocal(a, a), RK, ["st"])
            K.ts("dve", pen, goh, BIG, -BIG, ALU.mult, ALU.add, RK, ["st"])
            rm, oh1, rm2, oh2 = [r[:, :] for r in r64]
            K.tt("dve", v3(rm, 8), rl3, bc3(pen, 8), ALU.add, RK, ["r64"])
            K.S.op("dve", lambda e, a=s_m1, g=rm: e.reduce_max(a, g, AX.X), RK, ["st"])
            K.ts("dve", oh1, rm, s_m1, None, ALU.is_equal, None, RK, ["r64"])
            K.stt("dve", rm2, oh1, -BIG, rm, ALU.mult, ALU.add, RK, ["r64"])
            K.S.op("dve", lambda e, a=s_m2, g=rm2: e.reduce_max(a, g, AX.X), RK, ["st"])
            K.ts("dve", oh2, rm2, s_m2, None, ALU.is_equal, None, RK, ["r64"])
            K.tt("dve", s_d, s_m2, s_m1, ALU.subtract, RK, ["st"])
            K.act(s_e2, s_d, AF.Exp, RK, ["st"])
            K.ts("dve", s_wa, s_e2, 1.0, None, ALU.add, None, RK, ["st"])
            K.S.op("dve", lambda e, a=s_wa: e.reciprocal(a, a), RK, ["st"])
            K.tt("dve", wts[:, t, 0:1], s_wa, s_gsum, ALU.mult, RK, ["wts"])
            K.tt("dve", wts[:, t, 1:2], wts[:, t, 0:1], s_e2, ALU.mult, RK + ["wts"], ["wts"])
            K.cp("dve", ohb[:, t, 0:64], oh1, RK, ["ohb"])
            K.cp("dve", ohb[:, t, 64:128], oh2, RK, ["ohb"])
            K.tt("dve", rm, oh1, oh2, ALU.add, RK, ["r64"])
            K.mm(PB[3][:, 128:192], ut[:, :], rm, True, True, ["ut", "r64"], ["pb3"])
            K.mm(PB[3][:, 192:256], ones[:, :], rm, True, True, ["ones", "r64"], ["pb3"])
            K.tt("dve", rm2, PB[3][:, 128:192], base[:, :], ALU.add, ["pb3", "base", "r64"], ["r64"])
            K.tt("dve", rm, oh1, rm2, ALU.mult, RK, ["r64"])
            K.rsum("dve", rk[:, t, 0:1], rm, RK, ["rk"])
            K.tt("dve", rm, oh2, rm2, ALU.mult, RK, ["r64"])
            K.rsum("dve", rk[:, t, 1:2], rm, RK, ["rk"])
            K.tt("dve", base[:, :], PB[3][:, 192:256], base[:, :], ALU.add, ["pb3", "base", "r64"], ["base"])
        cs = [r64[0][:, :], r64[1][:, :]]
        pc = r64[2][:, :]
        pst = r64[3][:, :]
        KS = ["base", "r64"]
        K.ts("dve", pc, base[:, :], 255.0, None, ALU.add, None, KS, ["r64"])
        pci = K.sb("pci", [128, 64], I32)
        K.cp("dve", pci[:, :], pc, KS, ["pci"])
        K.ts("dve", pci[:, :], pci[:, :], 8, 8, ALU.arith_shift_right, ALU.logical_shift_left, ["pci"], ["pci"])
        K.cp("dve", pc, pci[:, :], ["pci"] + KS, ["r64"])
        K.cp("dve", cs[0], pc, KS, ["r64"])
        cur = 0
        for sh in (1, 2, 4, 8, 16, 32):
            K.cp("dve", cs[1 - cur][:, 0:sh], cs[cur][:, 0:sh], KS, ["r64"])
            K.tt("dve", cs[1 - cur][:, sh:64], cs[cur][:, sh:64], cs[cur][:, 0:64 - sh], ALU.add, KS, ["r64"])
            cur = 1 - cur
        pend = cs[cur]
        K.tt("dve", pst, pend, pc, ALU.subtract, KS, ["r64"])
        other = cs[1 - cur]
        for t in range(NT):
            for k in range(2):
                K.tt("dve", other, ohb[:, t, 64 * k:64 * k + 64], pst, ALU.mult, KS + ["ohb"], ["r64"])
                K.rsum("dve", dstf[:, t, k:k + 1], other, KS, ["dstf"])
        K.tt("dve", dstf[:, :, :], dstf[:, :, :], rk[:, :, :], ALU.add, ["dstf", "rk"], ["dstf"])
        K.cp("dve", dsti[:, :], dstf[:, :, :].rearrange("p t k -> p (t k)"), ["dstf"], ["dsti"])
        zt = K.sb("zt", [128, D], BF16)
        K.memset("pool", zt[:, :], 0.0, [], ["zt"])
        for bk in range(2 * NB):
            K.dma("sp" if bk % 2 == 0 else "act", xs_d[bk * 128:(bk + 1) * 128, :], zt[:, :], ["zt"], ["xs"])
        S.barrier()
        for t in range(NT):
            b = t % 2
            K.dma("sp", h2s[b][:, :], h2_d[t * 128:(t + 1) * 128, :], [], [("h2s", b)])
            for k in range(2):
                K.S.dmafn("pool", lambda e, src=h2s[b][:, :], ix=dsti[:, 2 * t + k:2 * t + k + 1]: e.indirect_dma_start(
                    out=xs_d[:, :], out_offset=bass.IndirectOffsetOnAxis(ap=ix, axis=0), in_=src, in_offset=None),
                    [("h2s", b), "dsti"], ["xs"])
        S.barrier()
        xsb = K.sb("xsb", [128, 2, D], BF16)
        xT = K.sb("xTb", [128, 8, 256], BF16)
        ysbb = K.sb("ysbb", [128, 2, D], F32)
        ysb = [ysbb[:, 0, :], ysbb[:, 1, :]]
        widx2 = K.sb("widx2", [128, 2], I32)
        NROWS_W = NE * 128
        for bk in range(NB):
            wb = bk % 2
            W = Wb[wb]
            w1e = W[:, 0:4096].rearrange("p (c n) -> p c n", c=8)
            w3e = W[:, 4096:8192].rearrange("p (c n) -> p c n", c=8)
            w2e = W[:, 8192:12288].rearrange("p (c n) -> p c n", c=4)
            ef = st[:, 60:61]
            fl = st[:, 61:62]
            K.ts("dve", other, pend, float(256 * bk), None, ALU.is_le, None, ["r64"], ["r64b"])
            K.rsum("dve", ef, other, ["r64b"], ["st"])
            K.ts("dve", ef, ef, float(NE - 1), 128.0, ALU.min, ALU.mult, ["st"], ["st"])
            K.tt("dve", ef, ef, iop[:, :], ALU.add, ["st", "iop"], ["st"])
            K.cp("dve", widx2[:, wb:wb + 1], ef, ["st"], [("widx2", wb)])
            for (dst, srcw, wk) in ((W[:, 0:4096], w1r, "W1"), (W[:, 4096:8192], w3r, "W3"), (W[:, 8192:12288], w2r, "W2")):
                K.S.dmafn("pool", lambda e, o=dst, sw=srcw, ix=widx2[:, wb:wb + 1]: e.indirect_dma_start(
                    out=o, out_offset=None, in_=sw[:, :], in_offset=bass.IndirectOffsetOnAxis(ap=ix, axis=0)),
                    [("widx2", wb)], [(wk, wb)])
            K.dma("sp", xsb[:, :, :], xs_d[bk * 256:(bk + 1) * 256, :].rearrange("(a p) f -> p a f", p=128), [], ["xsb"])
            for a in range(2):
                for c in range(8):
                    K.tr(PB0b[:, c * 128:(c + 1) * 128], xsb[:, a, c * 128:(c + 1) * 128], identb[:, :], ["xsb", "identb"], ["pb0"])
                K.cp("dve", xT[:, :, a * 128:(a + 1) * 128], v3(PB0b[:, :], 8), ["pb0"], ["xT"])
            for m in range(4):
                p1, p3 = 1 + m % 2, 4 + m % 2
                for c in range(8):
                    K.mm(PB[p1][:, 0:256], w1e[:, c, m * 128:(m + 1) * 128], xT[:, c, :], c == 0, c == 7, [("W1", wb), "xT"], [f"pb{p1}"])
                for c in range(8):
                    K.mm(PB[p3][:, 0:256], w3e[:, c, m * 128:(m + 1) * 128], xT[:, c, :], c == 0, c == 7, [("W3", wb), "xT"], [f"pb{p3}"])
                K.act(sil[m % 2][:, 0:256], PB[p1][:, 0:256], AF.Silu, [f"pb{p1}"], [("sil", m % 2)])
                K.tt("dve", hT[:, m, 0:256], PB[p3][:, 0:256], sil[m % 2][:, 0:256], ALU.mult, [f"pb{p3}", ("sil", m % 2)], ["hTe"])
            for a in range(2):
                for half in range(2):
                    py = 6 + half
                    for kc in range(4):
                        K.mm(PB[py][:, :], hT[:, kc, a * 128:(a + 1) * 128], w2e[:, kc, half * 512:(half + 1) * 512], kc == 0, kc == 3,
                             ["hTe", ("W2", wb)], [f"pb{py}"])
                    K.cp("act", ysbb[:, a, half * 512:(half + 1) * 512], PB[py][:, :], [f"pb{py}"], [("ysb", a)])
                K.dma("sp", ys_d[bk * 256 + a * 128:bk * 256 + (a + 1) * 128, :], ysbb[:, a, :], [("ysb", a)], [])
        S.barrier()
        for t in range(NT):
            b = t % 2
            wh = 0 if t < NT_OWN else 1
            rows = slice(t * 128, (t + 1) * 128)
            K.dma("sp", x1t[b][:, :], x1_d[rows, :], [], [("x1", b)])
            for k in range(2):
                K.S.dmafn("pool", lambda e, o=ysb[k][:, :], ix=dsti[:, 2 * t + k:2 * t + k + 1]: e.indirect_dma_start(
                    out=o, out_offset=None, in_=ys_d[:, :], in_offset=bass.IndirectOffsetOnAxis(ap=ix, axis=0)),
                    ["dsti"], [("ysb", k)])
            K.ts("dve", tmpf[:, :], ysb[0][:, :], wts[:, t, 0:1], None, ALU.mult, None, [("ysb", 0), "wts"], ["tmpf"])
            K.stt("dve", tmpf[:, :], ysb[1][:, :], wts[:, t, 1:2], tmpf[:, :], ALU.mult, ALU.add, [("ysb", 1), "wts", "tmpf"], ["tmpf"])
            K.tt("pool", tmpf[:, :], tmpf[:, :], G[2 + wh][:, :], ALU.mult, ["tmpf", ("G", 2 + wh)], ["tmpf"])
            K.tt("pool", h2n[:, :], tmpf[:, :], x1t[b][:, :], ALU.add, ["tmpf", ("x1", b)], ["h2n"])
            K.dma("sp", xo_d[rows, :], h2n[:, :], ["h2n"], [])
        S.emit()


def build_btab(rpb, NT_OWN, rank):
    rows_total = 8 * NT_OWN
    specials = _specials(NT_OWN)
    gts = [rows_total // 4] + [rank * NT_OWN + t for t in specials]
    qc = np.arange(64)
    kc = np.arange(64)
    c0 = np.clip(qc - 8, 0, 48)
    colok = (kc[:, None] >= c0[None, :]) & (kc[:, None] < c0[None, :] + 16)
    dc = np.clip(kc[:, None] - qc[None, :], -15, 15) + 15
    tab = np.full((len(gts), 2, 64, 6, 16, 64), NEG, np.float32)
    for ci, gt in enumerate(gts):
        for e in range(16):
            b = (e + 1) % 2
            j = (7 + b - e) // 2
            qrow = 2 * gt + b
            r0 = min(max(qrow - 4, 0), rows_total - 8)
            for a in range(2):
                krow = 2 * (gt + j) + a
                if krow < 0 or krow >= rows_total or krow < r0 or krow >= r0 + 8:
                    continue
                dr = krow - qrow + 7
                vals = rpb[:, dr, :][:, dc]
                vals = np.where(colok[None], vals, np.float32(NEG))
                tab[ci, a, :, :, e, :] = vals.transpose(1, 0, 2)
    return tab.reshape(len(gts), 128, 6, 1024).astype(NPBF)


def prep_B(P, i, x_cur, xc_cur, NT_OWN, outsA, ranks_per_batch=4, NE=64):
    B = x_cur.shape[0]
    n = NT_OWN * 128
    NTB = ranks_per_batch * NT_OWN
    shared = {
        "w_out": _kmaj(P["w_out"][i], 8),
        "norm_ffnT": _colT(P["norm_ffn"][i]),
        "wgr": _kmaj(np.concatenate([P["moe_w_group"][i], P["moe_w_router"][i]], axis=1), 8),
        "w1": np.ascontiguousarray(P["moe_w1"][i][:NE].reshape(NE, 8, 128, 512).transpose(0, 2, 1, 3)),
        "w3": np.ascontiguousarray(P["moe_w3"][i][:NE].reshape(NE, 8, 128, 512).transpose(0, 2, 1, 3)),
        "w2": np.ascontiguousarray(P["moe_w2"][i][:NE].reshape(NE, 4, 128, 1024).transpose(0, 2, 1, 3)),
        "ident": np.eye(128, dtype=np.float32),
    }
    maps = []
    for b in range(B):
        cores = [outsA[b * ranks_per_batch + r] for r in range(ranks_per_batch)]
        kv_tiles = [c["kvb"][:n].reshape(NT_OWN, 128, 768) for c in cores]
        kv_all = np.concatenate(kv_tiles, axis=0)
        for r in range(ranks_per_batch):
            me = cores[r]
            m = dict(shared)
            m["x"] = np.ascontiguousarray(np.concatenate([x_cur[b, r * n:(r + 1) * n], xc_cur[b]], axis=0))
            m["oa"] = me["oa"]
            m["qbT"] = me["qbT"]
            m["qcT"] = me["qcT"]
            m["modT"] = me["modT"]
            win = np.zeros((NT_OWN + 8, 128, 768), NPBF)
            for s in range(NT_OWN + 6):
                g = r * NT_OWN - 3 + s
                if 0 <= g < NTB:
                    win[s] = kv_all[g]
            win[NT_OWN + 6:] = me["kvb"][n:].reshape(2, 128, 768)
            m["kvbw"] = win.reshape(-1, 768)
            m["kcT_all"] = np.ascontiguousarray(np.concatenate([c["kcT"][:, :, :n] for c in cores] + [me["kcT"][:, :, n:]], axis=2))
            m["vc_all"] = np.ascontiguousarray(np.concatenate([c["vc"][:n] for c in cores] + [me["vc"][n:]], axis=0))
            m["btab"] = build_btab(P["b_rpb"][i], NT_OWN, r)
            maps.append(m)
    return maps


def declare_dram(nc, NT_OWN, NR=4, NE=64):
    NT = NT_OWN + 2
    T = NT * 128
    n = NT_OWN * 128
    NCLS = 1 + len(_specials(NT_OWN))
    d = {}

    def t(name, shape, dt, kind="Internal"):
        d[name] = nc.dram_tensor(name, list(shape), dt, kind=kind).ap()

    EI = "ExternalInput"
    t("x", [T, D], F32, EI)
    t("cT", [128, 16], F32, EI)
    t("rope", [T, 32], F32, EI)
    t("ident", [128, 128], F32, EI)
    t("widx", [128, 6], I32, EI)
    t("ut", [128, 128], F32, EI)
    t("iop", [128, 1], F32, EI)
    t("xo", [T, D], F32, "ExternalOutput")
    t("xs_0", [T, D], F32)
    for L in range(2):
        t(f"w_ada_{L}", [128, 8, 6144], F32, EI)
        t(f"b_adaT_{L}", [128, 48], F32, EI)
        t(f"norm_mixT_{L}", [128, 8], F32, EI)
        t(f"w_in_{L}", [128, 8, INW], F32, EI)
        t(f"w_sT_{L}", [128, 4, 128], F32, EI)
        t(f"b_s_{L}", [128, 4], F32, EI)
        t(f"gains_{L}", [128, G_TOT], F32, EI)
        t(f"wq_{L}", [128, 3, 576], F32, EI)
        t(f"wkv_{L}", [128, 2, 768], F32, EI)
        t(f"btab_{L}", [NCLS, 128, 6, 1024], BF16, EI)
        t(f"w_out_{L}", [128, 8, D], F32, EI)
        t(f"norm_ffnT_{L}", [128, 8], F32, EI)
        t(f"wgr_{L}", [128, 8, 72], F32, EI)
        t(f"w1_{L}", [NE, 128, 8, 512], F32, EI)
        t(f"w3_{L}", [NE, 128, 8, 512], F32, EI)
        t(f"w2_{L}", [NE, 128, 4, D], F32, EI)
        t(f"oa_{L}", [T, 256], BF16)
        t(f"qbT_{L}", [3, 128, T], BF16)
        t(f"qcT_{L}", [6, 96, T], BF16)
        t(f"modT_{L}", [128, 96], F32)
        t(f"mixd_{L}", [T, D], BF16)
        PT = min(4, NT_OWN)
        for j in range(NT_OWN // PT):
            t(f"kcT_own_{L}_{j}", [576, PT * 128], BF16)
            t(f"kcT_g_{L}_{j}", [NR * 576, PT * 128], BF16)
            t(f"vc_own_{L}_{j}", [PT * 128, 390], BF16)
            t(f"vc_g_{L}_{j}", [NR * PT * 128, 390], BF16)
        t(f"kcT_ctx_{L}", [576, 256], BF16)
        t(f"vc_ctx_{L}", [256, 390], BF16)
        t(f"kvb_own_{L}", [n, 768], BF16)
        t(f"kvb_ctx_{L}", [256, 768], BF16)
        HT = min(3, NT_OWN)
        t(f"kvb_lo_{L}", [HT * 128, 768], BF16)
        t(f"kvb_hi_{L}", [HT * 128, 768], BF16)
        t(f"kvb_glo_{L}", [NR * HT * 128, 768], BF16)
        t(f"kvb_ghi_{L}", [NR * HT * 128, 768], BF16)
        t(f"h2_{L}", [T, D], BF16)
        t(f"x1_{L}", [T, D], F32)
        t(f"xsl_{L}", [((2 * T + 255) // 256 + 64) * 256, D], BF16)
        t(f"ys_{L}", [((2 * T + 255) // 256 + 64) * 256, D], F32)
    return d


def emit_X(nc, gc, d, L, groups):
    stack = contextlib.ExitStack()
    with stack:
        K = KB(nc, stack, gc)
        pairs = [(d[f"kvb_lo_{L}"], d[f"kvb_glo_{L}"]), (d[f"kvb_hi_{L}"], d[f"kvb_ghi_{L}"])]
        j = 0
        while f"vc_own_{L}_{j}" in d:
            pairs.append((d[f"vc_own_{L}_{j}"], d[f"vc_g_{L}_{j}"]))
            pairs.append((d[f"kcT_own_{L}_{j}"], d[f"kcT_g_{L}_{j}"]))
            j += 1
        for nm, (src, dst) in enumerate(pairs):
            K.S.cc(lambda e, src=src, dst=dst: e.collective_compute(
                "AllGather", ALU.bypass, replica_groups=groups, ins=[src[:, :]], outs=[dst[:, :]]), [], [nm])
        K.S.emit()


def build_fused(NT_OWN, ngroups=2, NR=4, TB=4, NE=64, do_x=True):
    nc = bass.Bass("TRN2", target_bir_lowering=False)
    gstack = contextlib.ExitStack()
    with gstack:
        gc = GC(gstack)
        d = declare_dram(nc, NT_OWN, NR, NE)
        groups = [list(range(g * NR, (g + 1) * NR)) for g in range(ngroups)]
        for L in range(2):
            emit_A(nc, gc, d, NT_OWN, L)
            if do_x:
                emit_X(nc, gc, d, L, groups)
            emit_B(nc, gc, d, NT_OWN, L, TB=TB, NR=NR, NE=NE)
    return nc


def halo_index(NT_OWN, r, NR):
    HT = min(3, NT_OWN)
    idx = np.zeros((128, 6), np.int32)
    for col in range(6):
        g = r * NT_OWN - 3 + col if col < 3 else (r + 1) * NT_OWN + (col - 3)
        if not (0 <= g < NR * NT_OWN):
            continue
        rk, l = g // NT_OWN, g % NT_OWN
        if col < 3:
            t2 = l - (NT_OWN - HT)
        else:
            t2 = l
        if not (0 <= t2 < HT):
            continue
        idx[:, col] = (rk * HT + t2) * 128 + np.arange(128)
    return idx


def prep_fused(P, NT_OWN, NR=4):
    x = np.asarray(P["x"], np.float32)
    ctx = np.asarray(P["ctx"], np.float32)
    B = x.shape[0]
    n = NT_OWN * 128
    shared = {"ident": np.eye(128, dtype=np.float32),
              "ut": np.triu(np.ones((128, 128), np.float32), 1),
              "iop": np.arange(128, dtype=np.float32).reshape(128, 1)}
    per_rank = [dict() for _ in range(NR)]
    for L in range(2):
        gains = np.concatenate([
            P["a_v_norm"][L], np.tile(P["b_q_norm"][L], 6), np.tile(P["b_k_norm"][L], 6), P["c_q_a_norm"][L],
            P["c_kv_a_norm"][L], np.tile(P["c_q_norm"][L], 6), np.tile(P["c_k_norm"][L][:64], 6), P["c_k_norm"][L][64:]])
        shared.update({
            f"w_ada_{L}": _kmaj(P["w_ada"][L], 8),
            f"b_adaT_{L}": _colT(P["b_ada"][L]),
            f"norm_mixT_{L}": _colT(P["norm_mix"][L]),
            f"w_in_{L}": _kmaj(P["w_in"][L], 8),
            f"w_sT_{L}": np.ascontiguousarray(P["a_w_s"][L].transpose(2, 0, 1)),
            f"b_s_{L}": np.ascontiguousarray(P["a_b_s"][L].T),
            f"gains_{L}": _bcrow(gains.astype(np.float32)),
            f"wq_{L}": _kmaj(P["c_w_q_up"][L], 3),
            f"wkv_{L}": _kmaj(P["c_w_kv_up"][L], 2),
            f"w_out_{L}": _kmaj(P["w_out"][L], 8),
            f"norm_ffnT_{L}": _colT(P["norm_ffn"][L]),
            f"wgr_{L}": _kmaj(np.concatenate([P["moe_w_group"][L], P["moe_w_router"][L]], axis=1), 8),
            f"w1_{L}": np.ascontiguousarray(P["moe_w1"][L].reshape(64, 8, 128, 512).transpose(0, 2, 1, 3)),
            f"w3_{L}": np.ascontiguousarray(P["moe_w3"][L].reshape(64, 8, 128, 512).transpose(0, 2, 1, 3)),
            f"w2_{L}": np.ascontiguousarray(P["moe_w2"][L].reshape(64, 4, 128, 1024).transpose(0, 2, 1, 3)),
        })
        for r in range(NR):
            per_rank[r][f"btab_{L}"] = build_btab(P["b_rpb"][L], NT_OWN, r)
    for r in range(NR):
        per_rank[r]["rope"] = rope_tables(NT_OWN, r)
        per_rank[r]["widx"] = halo_index(NT_OWN, r, NR)
    maps = []
    for b in range(B):
        cT = np.ascontiguousarray(np.stack([_colT(P["c"][b]), _colT(P["c_ctx"])], axis=2).reshape(128, 16))
        for r in range(NR):
            m = dict(shared)
            m.update(per_rank[r])
            m["x"] = np.ascontiguousarray(np.concatenate([x[b, r * n:(r + 1) * n], ctx[b]], axis=0))
            m["cT"] = cT
            maps.append(m)
    return maps


_PROG = {}


def kernel(**inputs):
    P = {k: np.asarray(v) for k, v in inputs.items()}
    NT_OWN = 32
    n = NT_OWN * 128
    if "fused" not in _PROG:
        _PROG["fused"] = build_fused(NT_OWN)
    maps = prep_fused(P, NT_OWN)
    res = run_bass_kernel_spmd(_PROG["fused"], maps, core_ids=list(range(8))).results
    out = np.empty((2, 4 * n, D), np.float32)
    for b in range(2):
        for r in range(4):
            out[b, r * n:(r + 1) * n] = res[b * 4 + r]["xo"][:n]
    return out
```

```python
import contextlib
import numpy as np
import ml_dtypes
import concourse.bass as bass
import concourse.mybir as mybir
from concourse.bass_utils import run_bass_kernel_spmd

F32 = mybir.dt.float32
BF16 = mybir.dt.bfloat16
I32 = mybir.dt.int32
AF = mybir.ActivationFunctionType
ALU = mybir.AluOpType
AX = mybir.AxisListType
NPBF = ml_dtypes.bfloat16

D = 1024
GRID_W = 64
CTX = 256
EPS = 1e-6
INW = 2336
NEG = -30000.0


class _Op:
    __slots__ = ("eng", "fn", "deps", "needed", "semkey", "val", "dma", "idx")

    def __init__(self, eng, fn, dma):
        self.eng = eng
        self.fn = fn
        self.deps = []
        self.needed = False
        self.semkey = None
        self.val = 0
        self.dma = dma


class Sched:
    ENGS = ("pe", "act", "dve", "pool", "sp")
    NLANES = 6

    def __init__(self, nc, gc):
        self.nc = nc
        self.gc = gc
        self.ops = {e: [] for e in self.ENGS}
        self.bufs = {}
        self.phase = 0
        self.lane_ops = {}
        self.lane_n = {e: 0 for e in self.ENGS}
        self.pending = {e: [] for e in self.ENGS}
        self.last = {e: None for e in self.ENGS}
        self.cc_ops = []

    def _add(self, eng, fn, r, w, dma):
        if getattr(self, "rec", None) is not None:
            self.rec.append((eng, fn, self.keymap(r), self.keymap(w), dma))
            return None
        op = _Op(eng, fn, dma)
        deps = []
        for k in r:
            st = self.bufs.setdefault(k, [None, []])
            if st[0] is not None:
                deps.append(st[0])
        for k in w:
            st = self.bufs.setdefault(k, [None, []])
            if st[0] is not None:
                deps.append(st[0])
            deps.extend(st[1])
        deps.extend(self.pending[eng])
        self.pending[eng] = []
        if dma:
            lane = self.lane_n[eng] % self.NLANES
            self.lane_n[eng] += 1
            key = ("lane", eng, lane)
            prev = self.lane_ops.get(key)
            if prev is not None:
                deps.append(prev)
            self.lane_ops[key] = op
            op.semkey = key
            op.val = (prev.val if prev is not None else self.gc.lane_vals.get(key, 0)) + 16
            op.needed = True
        else:
            op.semkey = ("eng", eng, self.phase)
        seen = set()
        for d in deps:
            if d is op or id(d) in seen:
                continue
            seen.add(id(d))
            if (not d.dma) and d.eng == eng and eng == "pe":
                continue
            d.needed = True
            op.deps.append(d)
        for k in r:
            self.bufs[k][1].append(op)
        for k in w:
            self.bufs[k] = [op, []]
        self.ops[eng].append(op)
        self.last[eng] = op
        return op

    def op(self, eng, fn, r=(), w=()):
        return self._add(eng, fn, r, w, False)

    def dma(self, eng, out, in_, r=(), w=()):
        return self._add(eng, lambda e: e.dma_start(out=out, in_=in_), r, w, True)

    def dmafn(self, eng, fn, r=(), w=()):
        return self._add(eng, fn, r, w, True)

    def cc(self, fn, r=(), w=()):
        op = self._add("pool", fn, r, w, False)
        op.semkey = ("cc", self.gc.next_uid())
        op.needed = True
        op.val = 1
        op.dma = True
        self.cc_ops.append(op)
        return op

    def barrier(self):
        lasts = [o for o in self.last.values() if o is not None] + list(self.lane_ops.values()) + list(self.cc_ops)
        for e in self.ENGS:
            self.pending[e] = list(lasts)
        self.bufs = {}
        self.phase += 1

    def emit(self):
        nc = self.nc
        gc = self.gc
        for e in self.ENGS:
            if self.ops[e] and not self.ops[e][-1].dma:
                self.ops[e][-1].needed = True
        cnt = {}
        for e in self.ENGS:
            for op in self.ops[e]:
                if not op.dma and op.needed:
                    cnt[op.semkey] = cnt.get(op.semkey, 0) + 1
                    op.val = cnt[op.semkey]
        sems = {}
        finals = {}
        for e in self.ENGS:
            for op in self.ops[e]:
                if not op.needed:
                    continue
                k = op.semkey
                if k not in sems:
                    if k[0] == "lane":
                        if k not in gc.lane_sems:
                            gc.lane_sems[k] = gc.stack.enter_context(nc.semaphore("l_" + "_".join(str(x) for x in k[1:])))
                        sems[k] = gc.lane_sems[k]
                    else:
                        sems[k] = gc.stack.enter_context(nc.semaphore(f"s{gc.next_uid()}_" + "_".join(str(x) for x in k)))
                finals[k] = max(finals.get(k, 0), op.val)
        for k, v in finals.items():
            if k[0] == "lane":
                gc.lane_vals[k] = v

        def run(engname, e):
            waited = {}
            for op in self.ops[engname]:
                for d in op.deps:
                    if waited.get(d.semkey, 0) >= d.val:
                        continue
                    e.wait_ge(sems[d.semkey], d.val)
                    waited[d.semkey] = d.val
                ins = op.fn(e)
                if op.needed:
                    if op.semkey[0] == "cc":
                        ins.then_inc(sems[op.semkey])
                    else:
                        ins.then_inc(sems[op.semkey], 16 if op.dma else 1)
            for k, v in finals.items():
                if waited.get(k, 0) < v:
                    e.wait_ge(sems[k], v)

        with nc.Block() as block:
            @block.tensor
            def _(e):
                run("pe", e)

            @block.scalar
            def _(e):
                run("act", e)

            @block.vector
            def _(e):
                run("dve", e)

            @block.gpsimd
            def _(e):
                run("pool", e)

            @block.sync
            def _(e):
                run("sp", e)


class GC:
    def __init__(self, stack):
        self.stack = stack
        self.lane_sems = {}
        self.lane_vals = {}
        self.uid = 0

    def next_uid(self):
        self.uid += 1
        return self.uid


class KB:
    def __init__(self, nc, stack, gc):
        self.nc = nc
        self.stack = stack
        self.gc = gc
        self.S = Sched(nc, gc)
        self.tag = f"_u{gc.next_uid()}"

    def sb(self, name, shape, dt):
        return self.stack.enter_context(self.nc.sbuf_tensor(name + self.tag, list(shape), dt))

    def ps(self, name, shape, dt):
        return self.stack.enter_context(self.nc.psum_tensor(name + self.tag, list(shape), dt))

    def dram(self, name, shape, dt, kind):
        return self.nc.dram_tensor(name, list(shape), dt, kind=kind).ap()

    def mm(self, out, lhsT, rhs, start, stop, r, w):
        self.S.op("pe", lambda e: e.matmul(out, lhsT, rhs, start=start, stop=stop), r, w)

    def tr(self, out, in_, ident, r, w):
        self.S.op("pe", lambda e: e.transpose(out, in_, ident), r, w)

    def act(self, out, in_, func, r, w, **kw):
        self.S.op("act", lambda e: e.activation(out, in_, func, **kw), r, w)

    def ts(self, eng, out, in0, s1, s2, op0, op1, r, w):
        if op1 is None:
            self.S.op(eng, lambda e: e.tensor_scalar(out, in0, s1, None, op0), r, w)
        else:
            self.S.op(eng, lambda e: e.tensor_scalar(out, in0, s1, s2, op0, op1), r, w)

    def tt(self, eng, out, in0, in1, op, r, w):
        self.S.op(eng, lambda e: e.tensor_tensor(out, in0, in1, op), r, w)

    def stt(self, eng, out, in0, scalar, in1, op0, op1, r, w):
        self.S.op(eng, lambda e: e.scalar_tensor_tensor(out, in0, scalar, in1, op0, op1), r, w)

    def cp(self, eng, out, in_, r, w):
        if eng == "act":
            self.S.op("act", lambda e: e.copy(out, in_), r, w)
        else:
            self.S.op(eng, lambda e: e.tensor_copy(out, in_), r, w)

    def rsum(self, eng, out, in_, r, w):
        self.S.op(eng, lambda e: e.reduce_sum(out, in_, AX.X), r, w)

    def memset(self, eng, ap, v, r, w):
        self.S.op(eng, lambda e: e.memset(ap, v), r, w)

    def dma(self, eng, out, in_, r, w):
        self.S.dma(eng, out, in_, r, w)


G_AV, G_BQ, G_BK, G_CQA, G_CKVA, G_CQN, G_CKN, G_CKR, G_TOT = 0, 256, 640, 1024, 1408, 1664, 2240, 2624, 2656


def v3(ap, g):
    return ap.rearrange("p (g d) -> p g d", g=g)


def bc3(ap2, d):
    p, g = ap2.shape
    return ap2.unsqueeze(2).to_broadcast([p, g, d])


def emit_A(nc, gc, d, NT_OWN, L):
    NT = NT_OWN + 2
    T = NT * 128
    n = NT_OWN * 128
    stack = contextlib.ExitStack()
    with stack:
        K = KB(nc, stack, gc)
        S = K.S
        x_d = d["x"] if L == 0 else d["xs_0"]
        cT_d, rope_d, ident_d = d["cT"], d["rope"], d["ident"]
        wada_d, bada_d, nmix_d, win_d = d[f"w_ada_{L}"], d[f"b_adaT_{L}"], d[f"norm_mixT_{L}"], d[f"w_in_{L}"]
        wsT_d, bs_d, gains_d, wq_d, wkv_d = d[f"w_sT_{L}"], d[f"b_s_{L}"], d[f"gains_{L}"], d[f"wq_{L}"], d[f"wkv_{L}"]
        oa_d, qbT_d, qcT_d, modT_d = d[f"oa_{L}"], d[f"qbT_{L}"], d[f"qcT_{L}"], d[f"modT_{L}"]
        PT = min(4, NT_OWN)
        HT = min(3, NT_OWN)
        kcT_ctx, vc_ctx = d[f"kcT_ctx_{L}"], d[f"vc_ctx_{L}"]
        kvb_own, kvb_ctx, kvb_lo, kvb_hi = d[f"kvb_own_{L}"], d[f"kvb_ctx_{L}"], d[f"kvb_lo_{L}"], d[f"kvb_hi_{L}"]
        ident = K.sb("ident_b", [128, 128], BF16)
        identf = K.sb("identf", [128, 128], F32)
        cT = K.sb("cTs", [128, 16], F32)
        scT = K.sb("scT", [128, 16], F32)
        bada = K.sb("bada", [128, 48], F32)
        nmix = K.sb("nmix", [128, 8], F32)
        modT = K.sb("modTs", [128, 96], F32)
        A1 = K.sb("A1", [128, 16], F32)
        wst = [K.sb(f"wst{i}", [128, 8, 512], F32) for i in range(2)]
        win = K.sb("win", [128, 8, INW], BF16)
        wsT = K.sb("wsT", [128, 4, 128], BF16)
        bs = K.sb("bs", [128, 4], F32)
        gains = K.sb("gainss", [128, G_TOT], F32)
        wq = K.sb("wqs", [128, 3, 576], BF16)
        wkv = K.sb("wkvs", [128, 2, 768], BF16)
        xt = [K.sb(f"xt{i}", [128, D], F32) for i in range(2)]
        ropet = [K.sb(f"ropet{i}", [128, 32], F32) for i in range(2)]
        sqj_2 = [K.sb("sqj%d" % i_, [128, D], F32) for i_ in range(2)]
        st_2 = [K.sb("st%d" % i_, [128, 64], F32) for i_ in range(2)]
        xn_2 = [K.sb("xn%d" % i_, [128, D], BF16) for i_ in range(2)]
        hT_2 = [K.sb("hT%d" % i_, [128, 8, 128], BF16) for i_ in range(2)]
        z_2 = [K.sb("z%d" % i_, [128, INW], F32) for i_ in range(2)]
        g1_2 = [K.sb("g1%d" % i_, [128, 512], F32) for i_ in range(2)]
        g2_2 = [K.sb("g2%d" % i_, [128, 512], F32) for i_ in range(2)]
        gg_2 = [K.sb("gg%d" % i_, [128, 512], F32) for i_ in range(2)]
        vnb_2 = [K.sb("vnb%d" % i_, [128, 256], BF16) for i_ in range(2)]
        oa_2 = [K.sb("oas%d" % i_, [128, 256], BF16) for i_ in range(2)]
        t384_2 = [K.sb("t384%d" % i_, [128, 384], F32) for i_ in range(2)]
        u384_2 = [K.sb("u384%d" % i_, [128, 384], F32) for i_ in range(2)]
        qnb_2 = [K.sb("qnb%d" % i_, [128, 384], BF16) for i_ in range(2)]
        qbTs_2 = [K.sb("qbTs%d" % i_, [128, 3, 128], BF16) for i_ in range(2)]
        kvbs_2 = [K.sb("kvbs%d" % i_, [128, 768], BF16) for i_ in range(2)]
        qab_2 = [K.sb("qab%d" % i_, [128, 384], BF16) for i_ in range(2)]
        qaT_2 = [K.sb("qaT%d" % i_, [128, 3, 128], BF16) for i_ in range(2)]
        qf_2 = [K.sb("qf%d" % i_, [128, 576], F32) for i_ in range(2)]
        qs_2 = [K.sb("qs%d" % i_, [128, 576], F32) for i_ in range(2)]
        qc_2 = [K.sb("qc%d" % i_, [128, 6, 96], BF16) for i_ in range(2)]
        rt_2 = [[K.sb(f"rt{i}_{j_}", [128, 48], F32) for i in range(4)] for j_ in range(2)]
        qcTs_2 = [K.sb("qcTs%d" % i_, [96, 6, 128], BF16) for i_ in range(2)]
        kvab_2 = [K.sb("kvab%d" % i_, [128, 256], BF16) for i_ in range(2)]
        kvaT_2 = [K.sb("kvaT%d" % i_, [128, 2, 128], BF16) for i_ in range(2)]
        kvf_2 = [K.sb("kvf%d" % i_, [128, 768], F32) for i_ in range(2)]
        kc_2 = [K.sb("kc%d" % i_, [128, 6, 96], BF16) for i_ in range(2)]
        kr_2 = [K.sb("kr%d" % i_, [128, 32], F32) for i_ in range(2)]
        krr_2 = [K.sb("krr%d" % i_, [128, 32], F32) for i_ in range(2)]
        vcs_2 = [K.sb("vcs%d" % i_, [128, 6, 65], BF16) for i_ in range(2)]
        kcTs_2 = [K.sb("kcTs%d" % i_, [96, 6, 128], BF16) for i_ in range(2)]
        PB = [K.ps(f"pb{i}", [128, 512], F32) for i in range(8)]
        PB0b = PB[0].bitcast(BF16)

        K.dma("sp", identf[:, :], ident_d[:, :], [], ["identf"])
        K.cp("dve", ident[:, :], identf[:, :], ["identf"], ["ident"])
        K.dma("sp", cT[:, :], cT_d[:, :], [], ["cT"])
        K.dma("sp", bada[:, :], bada_d[:, :], [], ["bada"])
        K.dma("sp", nmix[:, :], nmix_d[:, :], [], ["nmix"])
        K.dma("sp", bs[:, :], bs_d[:, :], [], ["bs"])
        K.dma("sp", gains[:, :], gains_d[:, :], [], ["gains"])
        K.dma("pool", win[:, :, :], win_d[:, :, :], [], ["win"])
        K.dma("pool", wsT[:, :, :], wsT_d[:, :, :], [], ["wsT"])
        K.dma("pool", wq[:, :, :], wq_d[:, :, :], [], ["wq"])
        K.dma("pool", wkv[:, :, :], wkv_d[:, :, :], [], ["wkv"])
        K.memset("pool", vcs_2[0][:, :, :], 1.0, [], [("vcs", 0)])
        K.memset("pool", vcs_2[1][:, :, :], 1.0, [], [("vcs", 1)])
        K.act(scT[:, :], cT[:, :], AF.Silu, ["cT"], ["scT"])
        for grp in range(12):
            b = grp % 2
            K.dma("sp", wst[b][:, :, :], wada_d[:, :, grp * 512:(grp + 1) * 512], [], [("wst", b)])
            for jj in range(4):
                j = grp * 4 + jj
                for c in range(8):
                    K.mm(PB[1][:, 2 * j:2 * j + 2], wst[b][:, c, jj * 128:(jj + 1) * 128], scT[:, 2 * c:2 * c + 2],
                         c == 0, c == 7, [("wst", b), "scT"], ["pb1"])
        K.tt("dve", v3(modT[:, :], 48), v3(PB[1][:, 0:96], 48), bc3(bada[:, :], 2), ALU.add, ["pb1", "bada"], ["modT"])
        K.dma("sp", modT_d[:, :], modT[:, :], ["modT"], [])
        K.stt("dve", v3(A1[:, :], 8), v3(modT[:, 16:32], 8), 1.0, bc3(nmix[:, :], 2), ALU.add, ALU.mult,
              ["modT", "nmix"], ["A1"])

        cur = {}

        def rstd_of(ss, n, dim, extra=None):
            K.ts("dve", ss, ss, 1.0 / dim, EPS, ALU.mult, ALU.add, ["st"], ["st"])
            K.act(ss, ss, AF.Sqrt, ["st"], ["st"])
            K.S.op("dve", lambda e, a=ss: e.reciprocal(a, a), ["st"], ["st"])
            if extra is not None:
                K.ts("dve", ss, ss, extra, None, ALU.mult, None, ["st"], ["st"])

        def rope(src3, dst3, G, rp, rkeys, wkeys):
            for a in range(2):
                o = 16 * a
                cos = rp[:, 16 * a:16 * a + 8].unsqueeze(1).to_broadcast([128, G, 8])
                sin = rp[:, 16 * a + 8:16 * a + 16].unsqueeze(1).to_broadcast([128, G, 8])
                x1 = src3[:, :, o:o + 8]
                x2 = src3[:, :, o + 8:o + 16]
                t = [v3(cur["rt"][i][:, 0:G * 8], G) for i in range(4)]
                K.tt("pool", t[0], x1, cos, ALU.mult, rkeys, ["rt0"])
                K.tt("pool", t[1], x2, sin, ALU.mult, rkeys, ["rt1"])
                K.tt("dve", dst3[:, :, o:o + 8], t[0], t[1], ALU.subtract, ["rt0", "rt1"], wkeys)
                K.tt("pool", t[2], x2, cos, ALU.mult, rkeys, ["rt2"])
                K.tt("pool", t[3], x1, sin, ALU.mult, rkeys, ["rt3"])
                K.tt("dve", dst3[:, :, o + 8:o + 16], t[2], t[3], ALU.add, ["rt2", "rt3"], wkeys)

        SHARED = {"ident", "identf", "cT", "scT", "bada", "nmix", "modT", "A1", "win", "wsT", "bs", "gains", "wq", "wkv",
                  "kvb_x", "vc_x", "kc_x"}
        recs = []
        for t in range(NT):
            b = t % 2
            wh = 0 if t < NT_OWN else 1
            base = 4 * b
            PBT = PB[base].bitcast(BF16)
            kT = f"pb{base}"
            (sqj, st, xn, hT, z, g1, g2, gg, vnb, oa, t384, u384, qnb, qbTs, kvbs, qab, qaT, qf, qs, qc, qcTs, kvab, kvaT,
             kvf, kc, kr, krr, vcs, kcTs) = [lst[b] for lst in (
                sqj_2, st_2, xn_2, hT_2, z_2, g1_2, g2_2, gg_2, vnb_2, oa_2, t384_2, u384_2, qnb_2, qbTs_2, kvbs_2, qab_2,
                qaT_2, qf_2, qs_2, qc_2, qcTs_2, kvab_2, kvaT_2, kvf_2, kc_2, kr_2, krr_2, vcs_2, kcTs_2)]
            cur["rt"] = rt_2[b]
            S.rec = []
            S.keymap = lambda ks, b=b: [k if (isinstance(k, tuple) or k in SHARED or k.startswith("pb")) else (k, b) for k in ks]
            recs.append(S.rec)
            rows = slice(t * 128, (t + 1) * 128)
            X = xt[b]
            K.dma("sp", X[:, :], x_d[rows, :], [], [("xt", b)])
            K.dma("sp", ropet[b][:, :], rope_d[rows, :], [], [("rope", b)])
            K.tt("dve", sqj[:, :], X[:, :], X[:, :], ALU.mult, [("xt", b)], ["sqj"])
            K.rsum("dve", st[:, 0:1], sqj[:, :], ["sqj"], ["st"])
            rstd_of(st[:, 0:1], 1, D)
            K.act(xn[:, :], X[:, :], AF.Copy, [("xt", b), "st"], ["xn"], scale=st[:, 0:1])
            for c in range(8):
                K.tr(PBT[:, c * 128:(c + 1) * 128], xn[:, c * 128:(c + 1) * 128], ident[:, :], ["xn", "ident"], [kT])
            for c in range(8):
                K.ts("dve", hT[:, c, :], PBT[:, c * 128:(c + 1) * 128], A1[:, 2 * c + wh:2 * c + wh + 1],
                     modT[:, 2 * c + wh:2 * c + wh + 1], ALU.mult, ALU.add, [kT, "A1", "modT"], ["hT"])
            for k5 in range(5):
                n0 = k5 * 512
                n1 = min(INW, n0 + 512)
                pb = base + 1 + k5 % 2
                for c in range(8):
                    K.mm(PB[pb][:, 0:n1 - n0], hT[:, c, :], win[:, c, n0:n1], c == 0, c == 7, ["hT", "win"], [f"pb{pb}"])
                K.cp("act", z[:, n0:n1], PB[pb][:, 0:n1 - n0], [f"pb{pb}"], ["z"])
            za = z[:, 0:512]
            K.tt("pool", g1[:, :], za, za, ALU.mult, ["z"], ["g1"])
            K.ts("dve", g1[:, :], g1[:, :], 0.044715, 1.0, ALU.mult, ALU.add, ["g1"], ["g1"])
            K.tt("pool", g1[:, :], g1[:, :], za, ALU.mult, ["g1", "z"], ["g1"])
            K.act(g2[:, :], g1[:, :], AF.Sigmoid, ["g1"], ["g2"], scale=1.5957691216057308)
            K.tt("dve", gg[:, :], g2[:, :], za, ALU.mult, ["g2", "z"], ["gg"])
            K.tt("pool", g1[:, 0:256], gg[:, 256:512], gg[:, 256:512], ALU.mult, ["gg"], ["g1"])
            K.rsum("dve", st[:, 0:1], g1[:, 0:256], ["g1"], ["st"])
            rstd_of(st[:, 0:1], 1, 256)
            K.ts("dve", g1[:, 256:512], gg[:, 256:512], st[:, 0:1], None, ALU.mult, None, ["gg", "st", "g1"], ["g1"])
            K.tt("pool", vnb[:, :], g1[:, 256:512], gains[:, G_AV:G_AV + 256], ALU.mult, ["g1", "gains"], ["vnb"])
            for hd in range(4):
                K.mm(PB[base + 3][:, hd * 64:(hd + 1) * 64], wsT[:, hd, :], vnb[:, hd * 64:(hd + 1) * 64], True, True,
                     ["wsT", "vnb"], [f"pb{base + 3}a"])
            for hd in range(4):
                K.stt("dve", oa[:, hd * 64:(hd + 1) * 64], PB[base + 3][:, hd * 64:(hd + 1) * 64], bs[:, hd:hd + 1],
                      gg[:, hd * 64:(hd + 1) * 64], ALU.add, ALU.mult, [f"pb{base + 3}a", "bs", "gg"], ["oa"])
            K.dma("sp", oa_d[rows, :], oa[:, :], ["oa"], [])
            for which, (o0, gofs, extra) in enumerate(((512, G_BQ, 0.125), (896, G_BK, None))):
                src = z[:, o0:o0 + 384]
                K.tt("pool", t384[:, :], src, src, ALU.mult, ["z"], ["t384"])
                K.rsum("dve", st[:, 0:6], v3(t384[:, :], 6), ["t384"], ["st"])
                rstd_of(st[:, 0:6], 6, 64, extra)
                K.tt("dve", v3(u384[:, :], 6), v3(src, 6), bc3(st[:, 0:6], 64), ALU.mult, ["z", "st"], ["u384"])
                dst = qnb[:, :] if which == 0 else kvbs[:, 0:384]
                K.tt("pool", dst, u384[:, :], gains[:, gofs:gofs + 384], ALU.mult, ["u384", "gains"],
                     ["qnb" if which == 0 else "kvbs"])
            for pr in range(3):
                K.tr(PBT[:, pr * 128:(pr + 1) * 128], qnb[:, pr * 128:(pr + 1) * 128], ident[:, :], ["qnb", "ident"], [kT])
            K.cp("dve", qbTs[:, :, :], v3(PBT[:, 0:384], 3), [kT], ["qbTs"])
            K.dma("sp", qbT_d[:, :, rows].rearrange("a p t -> p a t"), qbTs[:, :, :], ["qbTs"], [])
            K.cp("act", kvbs[:, 384:768], z[:, 1280:1664], ["z"], ["kvbs"])
            if t < NT_OWN:
                K.dma("sp", kvb_own[rows, :], kvbs[:, :], ["kvbs"], ["kvb_x"])
                if t < HT:
                    K.dma("sp", kvb_lo[t * 128:(t + 1) * 128, :], kvbs[:, :], ["kvbs"], ["kvb_x"])
                if t >= NT_OWN - HT:
                    t2 = t - (NT_OWN - HT)
                    K.dma("sp", kvb_hi[t2 * 128:(t2 + 1) * 128, :], kvbs[:, :], ["kvbs"], ["kvb_x"])
            else:
                K.dma("sp", kvb_ctx[(t - NT_OWN) * 128:(t - NT_OWN + 1) * 128, :], kvbs[:, :], ["kvbs"], ["kvb_x"])
            src = z[:, 1664:2048]
            K.tt("pool", t384[:, :], src, src, ALU.mult, ["z"], ["t384"])
            K.rsum("dve", st[:, 0:1], t384[:, :], ["t384"], ["st"])
            rstd_of(st[:, 0:1], 1, 384)
            K.ts("dve", u384[:, :], src, st[:, 0:1], None, ALU.mult, None, ["z", "st"], ["u384"])
            K.tt("pool", qab[:, :], u384[:, :], gains[:, G_CQA:G_CQA + 384], ALU.mult, ["u384", "gains"], ["qab"])
            for c in range(3):
                K.tr(PBT[:, c * 128:(c + 1) * 128], qab[:, c * 128:(c + 1) * 128], ident[:, :], ["qab", "ident"], [kT])
            K.cp("dve", qaT[:, :, :], v3(PBT[:, 0:384], 3), [kT], ["qaT"])
            for c in range(3):
                K.mm(PB[base + 1][:, 0:512], qaT[:, c, :], wq[:, c, 0:512], c == 0, c == 2, ["qaT", "wq"], [f"pb{base + 1}"])
            for c in range(3):
                K.mm(PB[base + 3][:, 256:320], qaT[:, c, :], wq[:, c, 512:576], c == 0, c == 2, ["qaT", "wq"], [f"pb{base + 3}b"])
            K.cp("act", qf[:, 0:512], PB[base + 1][:, 0:512], [f"pb{base + 1}"], ["qf"])
            K.cp("act", qf[:, 512:576], PB[base + 3][:, 256:320], [f"pb{base + 3}b"], ["qf"])
            qf3 = v3(qf[:, :], 6)
            qs3 = v3(qs[:, :], 6)
            K.tt("pool", qs[:, :], qf[:, :], qf[:, :], ALU.mult, ["qf"], ["qs"])
            K.rsum("dve", st[:, 0:6], qs3[:, :, 0:64], ["qs"], ["st"])
            K.rsum("dve", st[:, 8:14], qs3[:, :, 64:96], ["qs"], ["st"])
            rstd_of(st[:, 0:6], 6, 64)
            rstd_of(st[:, 8:14], 6, 32)
            K.tt("dve", qs3[:, :, 0:64], qf3[:, :, 0:64], bc3(st[:, 0:6], 64), ALU.mult, ["qf", "st", "qs"], ["qs"])
            K.tt("dve", qs3[:, :, 64:96], qf3[:, :, 64:96], bc3(st[:, 8:14], 32), ALU.mult, ["qf", "st", "qs"], ["qs"])
            K.tt("pool", qf[:, :], qs[:, :], gains[:, G_CQN:G_CQN + 576], ALU.mult, ["qs", "gains"], ["qf"])
            K.cp("act", qc[:, :, 0:64], qf3[:, :, 0:64], ["qf"], ["qc"])
            rope(qf3[:, :, 64:96], qc[:, :, 64:96], 6, ropet[b], ["qf", ("rope", b)], ["qc"])
            for h in range(6):
                K.tr(PBT[0:96, h * 128:(h + 1) * 128], qc[:, h, :], ident[:, :], ["qc", "ident"], [kT])
            K.cp("dve", qcTs[:, :, :], v3(PBT[0:96, 0:768], 6), [kT], ["qcTs"])
            K.dma("sp", qcT_d[:, :, rows].rearrange("h p t -> p h t"), qcTs[:, :, :], ["qcTs"], [])
            src = z[:, 2048:2304]
            K.tt("pool", t384[:, 0:256], src, src, ALU.mult, ["z"], ["t384"])
            K.rsum("dve", st[:, 0:1], t384[:, 0:256], ["t384"], ["st"])
            rstd_of(st[:, 0:1], 1, 256)
            K.ts("dve", u384[:, 0:256], src, st[:, 0:1], None, ALU.mult, None, ["z", "st"], ["u384"])
            K.tt("pool", kvab[:, :], u384[:, 0:256], gains[:, G_CKVA:G_CKVA + 256], ALU.mult, ["u384", "gains"], ["kvab"])
            for c in range(2):
                K.tr(PBT[:, c * 128:(c + 1) * 128], kvab[:, c * 128:(c + 1) * 128], ident[:, :], ["kvab", "ident"], [kT])
            K.cp("dve", kvaT[:, :, :], v3(PBT[:, 0:256], 2), [kT], ["kvaT"])
            for c in range(2):
                K.mm(PB[base + 2][:, 0:512], kvaT[:, c, :], wkv[:, c, 0:512], c == 0, c == 1, ["kvaT", "wkv"], [f"pb{base + 2}"])
            for c in range(2):
                K.mm(PB[base + 3][:, 0:256], kvaT[:, c, :], wkv[:, c, 512:768], c == 0, c == 1, ["kvaT", "wkv"], [f"pb{base + 3}a"])
            K.cp("act", kvf[:, 0:512], PB[base + 2][:, 0:512], [f"pb{base + 2}"], ["kvf"])
            K.cp("act", kvf[:, 512:768], PB[base + 3][:, 0:256], [f"pb{base + 3}a"], ["kvf"])
            kvf3 = v3(kvf[:, :], 6)
            K.cp("act", vcs[:, :, 0:64], kvf3[:, :, 64:128], ["kvf"], ["vcs"])
            if t < NT_OWN:
                K.dma("sp", d[f"vc_own_{L}_{t // PT}"][(t % PT) * 128:(t % PT + 1) * 128, :],
                      vcs[:, :, :].rearrange("p h d -> p (h d)"), ["vcs"], ["vc_x"])
            else:
                K.dma("sp", vc_ctx[(t - NT_OWN) * 128:(t - NT_OWN + 1) * 128, :], vcs[:, :, :].rearrange("p h d -> p (h d)"), ["vcs"], ["vc_x"])
            t3 = v3(t384[:, :], 6)
            u3 = v3(u384[:, :], 6)
            K.tt("pool", t3, kvf3[:, :, 0:64], kvf3[:, :, 0:64], ALU.mult, ["kvf"], ["t384"])
            K.rsum("dve", st[:, 0:6], t3, ["t384"], ["st"])
            rstd_of(st[:, 0:6], 6, 64)
            K.tt("dve", u3, kvf3[:, :, 0:64], bc3(st[:, 0:6], 64), ALU.mult, ["kvf", "st"], ["u384"])
            K.tt("pool", kc[:, :, 0:64], u3, v3(gains[:, G_CKN:G_CKN + 384], 6), ALU.mult, ["u384", "gains"], ["kc"])
            src = z[:, 2304:2336]
            K.tt("pool", kr[:, :], src, src, ALU.mult, ["z"], ["kr"])
            K.rsum("dve", st[:, 0:1], kr[:, :], ["kr"], ["st"])
            rstd_of(st[:, 0:1], 1, 32)
            K.ts("dve", kr[:, :], src, st[:, 0:1], None, ALU.mult, None, ["z", "st", "kr"], ["kr"])
            K.tt("pool", kr[:, :], kr[:, :], gains[:, G_CKR:G_CKR + 32], ALU.mult, ["kr", "gains"], ["kr"])
            rope(v3(kr[:, :], 1), v3(krr[:, :], 1), 1, ropet[b], ["kr", ("rope", b)], ["krr"])
            K.cp("dve", kc[:, :, 64:96], krr[:, :].unsqueeze(1).to_broadcast([128, 6, 32]), ["krr"], ["kc"])
            for h in range(6):
                K.tr(PBT[0:96, h * 128:(h + 1) * 128], kc[:, h, :], ident[:, :], ["kc", "ident"], [kT])
            K.cp("dve", kcTs[:, :, :], v3(PBT[0:96, 0:768], 6), [kT], ["kcTs"])
            if t < NT_OWN:
                K.dma("sp", d[f"kcT_own_{L}_{t // PT}"].rearrange("(h p) t -> p h t", h=6)[:, :, (t % PT) * 128:(t % PT + 1) * 128],
                      kcTs[:, :, :], ["kcTs"], ["kc_x"])
            else:
                K.dma("sp", kcT_ctx.rearrange("(h p) t -> p h t", h=6)[:, :, (t - NT_OWN) * 128:(t - NT_OWN + 1) * 128],
                      kcTs[:, :, :], ["kcTs"], ["kc_x"])
        S.rec = None
        for p0 in range(0, NT, 2):
            pair = recs[p0:p0 + 2]
            ptr = [0] * len(pair)
            while any(ptr[j_] < len(pair[j_]) for j_ in range(len(pair))):
                for j_, r_ in enumerate(pair):
                    if ptr[j_] < len(r_):
                        S._add(*r_[ptr[j_]])
                        ptr[j_] += 1
                        while ptr[j_] < len(r_) and r_[ptr[j_]][0] == "pe" and r_[ptr[j_] - 1][0] == "pe":
                            S._add(*r_[ptr[j_]])
                            ptr[j_] += 1
        S.emit()


def _kmaj(w, kc):
    return np.ascontiguousarray(w.reshape(kc, 128, -1).transpose(1, 0, 2))


def _colT(v):
    return np.ascontiguousarray(v.reshape(-1, 128).T)


def _bcrow(v):
    return np.ascontiguousarray(np.broadcast_to(v[None, :], (128, v.shape[0])))


def rope_tables(NT_OWN, rank):
    half = 16
    inv = (np.float32(10000.0) ** (-(np.arange(0, half, 2, dtype=np.float32)) / np.float32(half))).astype(np.float32)
    pos = np.arange(NT_OWN * 128) + rank * NT_OWN * 128
    out = np.zeros(((NT_OWN + 2) * 128, 32), np.float32)
    ar = (pos // GRID_W).astype(np.float32)[:, None] * inv[None, :]
    ac = (pos % GRID_W).astype(np.float32)[:, None] * inv[None, :]
    n = NT_OWN * 128
    out[:n, 0:8] = np.cos(ar)
    out[:n, 8:16] = np.sin(ar)
    out[:n, 16:24] = np.cos(ac)
    out[:n, 24:32] = np.sin(ac)
    out[n:, 0:8] = 1.0
    out[n:, 16:24] = 1.0
    return out


def prep_A(P, i, x_cur, xc_cur, NT_OWN, ranks_per_batch=4):
    B = x_cur.shape[0]
    gains = np.concatenate([
        P["a_v_norm"][i], np.tile(P["b_q_norm"][i], 6), np.tile(P["b_k_norm"][i], 6), P["c_q_a_norm"][i],
        P["c_kv_a_norm"][i], np.tile(P["c_q_norm"][i], 6), np.tile(P["c_k_norm"][i][:64], 6), P["c_k_norm"][i][64:]])
    shared = {
        "w_ada": _kmaj(P["w_ada"][i], 8),
        "b_adaT": _colT(P["b_ada"][i]),
        "norm_mixT": _colT(P["norm_mix"][i]),
        "w_in": _kmaj(P["w_in"][i], 8),
        "w_sT": np.ascontiguousarray(P["a_w_s"][i].transpose(2, 0, 1)),
        "b_s": np.ascontiguousarray(P["a_b_s"][i].T),
        "gains": _bcrow(gains.astype(np.float32)),
        "wq": _kmaj(P["c_w_q_up"][i], 3),
        "wkv": _kmaj(P["c_w_kv_up"][i], 2),
        "ident": np.eye(128, dtype=np.float32),
    }
    maps = []
    n = NT_OWN * 128
    for b in range(B):
        cT = np.stack([_colT(P["c"][b]), _colT(P["c_ctx"])], axis=2).reshape(128, 16)
        for r in range(ranks_per_batch):
            m = dict(shared)
            m["x"] = np.ascontiguousarray(np.concatenate([x_cur[b, r * n:(r + 1) * n], xc_cur[b]], axis=0))
            m["cT"] = np.ascontiguousarray(cT)
            m["rope"] = rope_tables(NT_OWN, r)
            maps.append(m)
    return maps


def _specials(NT_OWN):
    return sorted(set(t for t in (0, 1, NT_OWN - 2, NT_OWN - 1) if 0 <= t < NT_OWN))


def emit_B(nc, gc, d, NT_OWN, L, TB=4, NE=64, NR=4, groups=None):
    NT = NT_OWN + 2
    T = NT * 128
    n = NT_OWN * 128
    NW = NT_OWN + 8
    NKT = NR * NT_OWN + 2
    QB = min(4, NT_OWN)
    CH = min(4, NT_OWN)
    specials = _specials(NT_OWN)
    BIG = 1.0e30
    stack = contextlib.ExitStack()
    with stack:
        K = KB(nc, stack, gc)
        S = K.S
        x_d = d["x"] if L == 0 else d["xs_0"]
        xo_d = d["xs_0"] if L == 0 else d["xo"]
        oa_d, qbT_d, qcT_d, modT_d, mix_d = d[f"oa_{L}"], d[f"qbT_{L}"], d[f"qcT_{L}"], d[f"modT_{L}"], d[f"mixd_{L}"]
        PCT = min(4, NT_OWN)
        HT = min(3, NT_OWN)
        kcT_ctx, vc_ctx = d[f"kcT_ctx_{L}"], d[f"vc_ctx_{L}"]
        kvb_own, kvb_ctx, kvb_glo, kvb_ghi = d[f"kvb_own_{L}"], d[f"kvb_ctx_{L}"], d[f"kvb_glo_{L}"], d[f"kvb_ghi_{L}"]
        btab_d, wout_d, nffn_d, wgr_d = d[f"btab_{L}"], d[f"w_out_{L}"], d[f"norm_ffnT_{L}"], d[f"wgr_{L}"]
        w1_d, w3_d, w2_d = d[f"w1_{L}"], d[f"w3_{L}"], d[f"w2_{L}"]
        ident_d, widx_d = d["ident"], d["widx"]
        if groups is not None:
            pairs = [(d[f"kvb_lo_{L}"], d[f"kvb_glo_{L}"]), (d[f"kvb_hi_{L}"], d[f"kvb_ghi_{L}"])]
            j_ = 0
            while f"vc_own_{L}_{j_}" in d:
                pairs.append((d[f"vc_own_{L}_{j_}"], d[f"vc_g_{L}_{j_}"]))
                pairs.append((d[f"kcT_own_{L}_{j_}"], d[f"kcT_g_{L}_{j_}"]))
                j_ += 1
            for nm_, (src_, dst_) in enumerate(pairs):
                K.S.cc(lambda e, src=src_, dst=dst_, rg=[list(g_) for g_ in groups]: e.collective_compute(
                    "AllGather", ALU.bypass, replica_groups=rg, ins=[src[:, :]], outs=[dst[:, :]]), [], [f"cc{nm_}"])

        AR = 45056
        arena = K.sb("arena", [128, AR], BF16)
        identb = K.sb("ident_b", [128, 128], BF16)
        identf = K.sb("identf", [128, 128], F32)
        ones = K.sb("ones", [128, 128], F32)
        modT = K.sb("modTs", [128, 96], F32)
        nffn = K.sb("nffn", [128, 8], F32)
        A2 = K.sb("A2", [128, 16], F32)
        G = [K.sb(f"G{i}", [128, D], F32) for i in range(4)]
        gbc = K.sb("gbc", [128, 128], F32)
        wgr = K.sb("wgrs", [128, 8, 72], F32)
        kvt = [K.sb(f"kvt{i}", [128, 768], BF16) for i in range(2)]
        qbt = [K.sb(f"qbt{i}", [128, 3, 128], BF16) for i in range(2)]
        PT = [K.sb(f"PT{i}", [128, 512], BF16) for i in range(3)]
        st = K.sb("st", [128, 64], F32)
        ob = K.sb("ob", [128, 384], BF16)
        oc = K.sb("oc", [128, 4, 64], BF16)
        ocT = K.sb("ocT", [65, 512], F32)
        qT = [K.sb(f"qT{i}", [96, 512], BF16) for i in range(2)]
        xt = [K.sb(f"xt{i}", [128, D], F32) for i in range(2)]
        mixrow = K.sb("mixrow", [128, D], BF16)
        mixT = K.sb("mixT", [128, 8, 128], BF16)
        tmpf = K.sb("tmpf", [128, D], F32)
        h2n = K.sb("h2n", [128, D], F32)
        h2Tf = K.sb("h2Tf", [128, 8, 128], F32)
        lg = K.sb("lg", [128, 72], F32)
        r64 = [K.sb(f"r64_{i}", [128, 64], F32) for i in range(4)]
        sil = [K.sb(f"sil{i}", [128, 256], F32) for i in range(2)]
        hT = K.sb("hTe", [128, 4, 256], BF16)
        PB = [K.ps(f"pb{i}", [128, 512], F32) for i in range(8)]
        PB0b = PB[0].bitcast(BF16)

        K.dma("sp", identf[:, :], ident_d[:, :], [], ["identf"])
        K.cp("dve", identb[:, :], identf[:, :], ["identf"], ["identb"])
        K.memset("dve", ones[:, :], 1.0, [], ["ones"])
        K.dma("sp", modT[:, :], modT_d[:, :], [], ["modT"])
        K.dma("sp", nffn[:, :], nffn_d[:, :], [], ["nffn"])
        K.dma("sp", wgr[:, :, :], wgr_d[:, :, :], [], ["wgr"])
        K.stt("dve", v3(A2[:, :], 8), v3(modT[:, 64:80], 8), 1.0, bc3(nffn[:, :], 2), ALU.add, ALU.mult,
              ["modT", "nffn"], ["A2"])
        for gi, (j0, wh) in enumerate(((16, 0), (16, 1), (40, 0), (40, 1))):
            for c in range(8):
                col = 2 * (j0 + c) + wh
                K.ts("dve", gbc[:, :], ones[:, :], modT[:, col:col + 1], None, ALU.mult, None, ["ones", "modT"], ["gbc"])
                K.mm(PB[7][:, (c % 4) * 128:(c % 4 + 1) * 128], gbc[:, :], identf[:, :], True, True, ["gbc", "identf"], ["pb7"])
                K.cp("act", G[gi][:, c * 128:(c + 1) * 128], PB[7][:, (c % 4) * 128:(c % 4 + 1) * 128], ["pb7"], [("G", gi)])

        o1 = 3 * NW * 128
        o2 = o1 + NW * 390
        KbT = arena[:, 0:o1].rearrange("p (a t) -> p a t", a=3)
        Vb = arena[:, o1:o2].rearrange("p (s h d) -> p s h d", s=NW, h=6)
        bt0 = arena[:, o2:o2 + 6144].rearrange("p (h e) -> p h e", h=6)
        btS = arena[:, o2 + 6144:o2 + 12288].rearrange("p (h e) -> p h e", h=6)
        assert o2 + 12288 <= AR
        K.memset("pool", arena[:, o1:o2], 1.0, [], ["Vb"])
        K.dma("sp", bt0, btab_d[0], [], ["bt0"])
        widx = K.sb("widx", [128, 6], I32)
        K.dma("sp", widx[:, :], widx_d[:, :], [], ["widx"])
        for s in range(NW):
            b = s % 2
            if s < 3 or NT_OWN + 3 <= s < NT_OWN + 6:
                srcg = kvb_ghi if s < 3 else kvb_glo
                col = s if s < 3 else s - NT_OWN
                K.S.dmafn("pool", lambda e, o=kvt[b][:, :], ix=widx[:, col:col + 1], sg=srcg: e.indirect_dma_start(
                    out=o, out_offset=None, in_=sg[:, :], in_offset=bass.IndirectOffsetOnAxis(ap=ix, axis=0)),
                    ["widx", "cc0", "cc1"], [("kvt", b)])
            elif s < NT_OWN + 3:
                K.dma("sp", kvt[b][:, :], kvb_own[(s - 3) * 128:(s - 2) * 128, :], [], [("kvt", b)])
            else:
                K.dma("sp", kvt[b][:, :], kvb_ctx[(s - NT_OWN - 6) * 128:(s - NT_OWN - 5) * 128, :], [], [("kvt", b)])
            for pr in range(3):
                K.tr(PB0b[:, pr * 128:(pr + 1) * 128], kvt[b][:, pr * 128:(pr + 1) * 128], identb[:, :], [("kvt", b), "identb"], ["pb0"])
            K.cp("dve", KbT[:, :, s * 128:(s + 1) * 128], v3(PB0b[:, 0:384], 3), ["pb0"], ["KbT"])
            K.cp("pool", Vb[:, s, :, 0:64], v3(kvt[b][:, 384:768], 6), [("kvt", b), "Vb"], ["Vb"])
        DSK = 2
        itemsB = []
        for t in range(NT):
            b = t % 2
            own = t < NT_OWN
            rows = slice(t * 128, (t + 1) * 128)
            special = own and t in specials
            bt, btk = (btS, "btS") if special else (bt0, "bt0")
            if own:
                kts = [(t + j + 3, j) for j in range(-3, 4)] + [(NW - 2, None), (NW - 1, None)]
            else:
                kts = [(NW - 2, None), (NW - 1, None)]
            groups = [kts[i:i + 3] for i in range(0, len(kts), 3)]
            ob_bank = 4 + t % 2
            for h in range(6):
                nk0 = 0
                for gi, grp in enumerate(groups):
                    itemsB.append(dict(t=t, b=b, h=h, grp=grp, nk0=nk0, nkt=len(kts), ob_bank=ob_bank, bt=bt, btk=btk,
                                       first=(h == 0 and gi == 0), last=(h == 5 and gi == len(groups) - 1),
                                       special=special, rows=rows))
                    nk0 += len(grp)

        def qkB(i, it):
            b, h, t = it["b"], it["h"], it["t"]
            if it["first"]:
                K.dma("sp", qbt[b][:, :, :], qbT_d[:, :, it["rows"]].rearrange("a p t -> p a t"), [], [("qbt", b)])
                if it["special"]:
                    K.dma("sp", btS, btab_d[1 + specials.index(t)], [], ["btS"])
            pr, pb = h // 2, (h % 2) * 64
            bank = 1 + i % 3
            pi = i % 3
            for ii, (s_, j) in enumerate(it["grp"]):
                K.mm(PB[bank][:, ii * 128:(ii + 1) * 128], KbT[pb:pb + 64, pr, s_ * 128:(s_ + 1) * 128],
                     qbt[b][pb:pb + 64, pr, :], True, j is None, ["KbT", ("qbt", b)], [f"pb{bank}"])
                if j is not None:
                    e0 = (7 - 2 * j) * 64
                    K.mm(PB[bank][:, ii * 128:(ii + 1) * 128], identb[:, :], it["bt"][:, h, e0:e0 + 128], False, True,
                         ["identb", it["btk"]], [f"pb{bank}"])
            n_ = len(it["grp"]) * 128
            op_ = K.S.op("act", lambda e, o=PT[pi][:, 0:n_], i_=PB[bank][:, 0:n_]: e.activation(o, i_, AF.Exp),
                         [f"pb{bank}"], [("PT", pi)])
            if i >= 3:
                op_.deps = [d_ for d_ in op_.deps if d_.dma or d_.eng != "act"]

        def pvB(i, it):
            h = it["h"]
            pi = i % 3
            OB = PB[it["ob_bank"]]
            for ii, (s_, j) in enumerate(it["grp"]):
                nk = it["nk0"] + ii
                K.mm(OB[:, h * 65:(h + 1) * 65], PT[pi][:, ii * 128:(ii + 1) * 128], Vb[:, s_, h, :],
                     nk == 0, nk == it["nkt"] - 1, [("PT", pi), "Vb"], [f"pb{it['ob_bank']}"])
            if it["last"]:
                O3 = v3(OB[:, 0:390], 6)
                K.S.op("dve", lambda e, O3=O3: e.reciprocal(st[:, 0:6], O3[:, :, 64]), [f"pb{it['ob_bank']}"], ["st"])
                K.tt("dve", v3(ob[:, :], 6), O3[:, :, 0:64], bc3(st[:, 0:6], 64), ALU.mult, [f"pb{it['ob_bank']}", "st"], ["ob"])
                K.dma("sp", mix_d[it["rows"], 256:640], ob[:, :], ["ob"], [("mixd", it["t"])])

        for i in range(len(itemsB) + DSK):
            if i < len(itemsB):
                qkB(i, itemsB[i])
            if i - DSK >= 0:
                pvB(i - DSK, itemsB[i - DSK])

        S.barrier()
        Kc = [arena[:, i * 2048:(i + 1) * 2048] for i in range(3)]
        Vc = [arena[:, 6144 + i * 1040:6144 + (i + 1) * 1040].rearrange("p (k d) -> p k d", d=65) for i in range(3)]
        qblocks = [(t0, QB, list(range(NKT))) for t0 in range(0, NT_OWN, QB)] + [(NT_OWN, 2, [NKT - 2, NKT - 1])]
        SCALE_C = 96.0 ** -0.5
        itemsC = []
        qh = 0
        cc = 0
        for (t0, ntl, klist) in qblocks:
            nq = ntl * 128
            for h in range(6):
                b2 = qh % 2
                oc_bank = 4 + qh % 2
                qh += 1
                own_k = [k for k in klist if k < NR * NT_OWN]
                ctx_k = [k for k in klist if k >= NR * NT_OWN]
                chunks = [own_k[i:i + CH] for i in range(0, len(own_k), CH)] + ([ctx_k] if ctx_k else [])
                nk = 0
                for chk in chunks:
                    cb = cc % 3
                    cc += 1
                    for kt in range(len(chk)):
                        itemsC.append(dict(t0=t0, ntl=ntl, nq=nq, h=h, b2=b2, oc_bank=oc_bank, cb=cb, chk=chk, kt=kt,
                                           newq=(nk == 0), newchunk=(kt == 0), nk=nk, nkt=len(klist)))
                        nk += 1

        def qkC(i, it):
            h, b2, cb, nq = it["h"], it["b2"], it["cb"], it["nq"]
            if it["newq"]:
                K.dma("sp", qT[b2][:, 0:nq], qcT_d[h, :, it["t0"] * 128:it["t0"] * 128 + nq], [], [("qT", b2)])
            if it["newchunk"]:
                chk = it["chk"]
                k0, n_k = chk[0], len(chk)
                if k0 < NR * NT_OWN:
                    rk, l0 = k0 // NT_OWN, k0 % NT_OWN
                    pj, lt = l0 // PCT, l0 % PCT
                    assert lt + n_k <= PCT
                    ksrc = d[f"kcT_g_{L}_{pj}"].rearrange("(r h p) t -> r h p t", r=NR, h=6)[rk, h, :, lt * 128:(lt + n_k) * 128]
                    v0 = (rk * PCT + lt) * 128
                    vsrc = d[f"vc_g_{L}_{pj}"][v0:v0 + n_k * 128, h * 65:(h + 1) * 65]
                else:
                    c0_ = (k0 - NR * NT_OWN) * 128
                    ksrc = kcT_ctx.rearrange("(h p) t -> h p t", h=6)[h, :, c0_:c0_ + n_k * 128]
                    vsrc = vc_ctx[c0_:c0_ + n_k * 128, h * 65:(h + 1) * 65]
                K.dma("sp", Kc[cb][0:96, 0:n_k * 128], ksrc, [], [("Kc", cb)])
                K.dma("sp", Vc[cb][:, 0:n_k, :], vsrc.rearrange("(k p) d -> p k d", p=128), [], [("Vc", cb)])
            bank = 1 + i % 3
            pi = i % 3
            kt = it["kt"]
            K.mm(PB[bank][:, 0:nq], Kc[cb][0:96, kt * 128:(kt + 1) * 128], qT[b2][:, 0:nq], True, True,
                 [("Kc", cb), ("qT", b2)], [f"pb{bank}"])
            op_ = K.S.op("act", lambda e, o=PT[pi][:, 0:nq], i_=PB[bank][:, 0:nq]: e.activation(o, i_, AF.Exp, scale=SCALE_C),
                         [f"pb{bank}"], [("PT", pi)])
            if i >= 3:
                op_.deps = [d_ for d_ in op_.deps if d_.dma or d_.eng != "act"]

        def pvC(i, it):
            pi = i % 3
            nq, ntl, oc_bank, cb, kt, h = it["nq"], it["ntl"], it["oc_bank"], it["cb"], it["kt"], it["h"]
            OC = PB[oc_bank]
            K.mm(OC[0:65, 0:nq], Vc[cb][:, kt, :], PT[pi][:, 0:nq], it["nk"] == 0, it["nk"] == it["nkt"] - 1,
                 [("PT", pi), ("Vc", cb)], [f"pb{oc_bank}"])
            if it["nk"] == it["nkt"] - 1:
                t0 = it["t0"]
                K.cp("dve", ocT[:, 0:nq], OC[0:65, 0:nq], [f"pb{oc_bank}"], ["ocT"])
                for qi in range(ntl):
                    K.tr(PB[0][:, qi * 65:(qi + 1) * 65], ocT[:, qi * 128:(qi + 1) * 128], identf[0:65, 0:65], ["ocT", "identf"], ["pb0"])
                O3 = v3(PB[0][:, 0:ntl * 65], ntl)
                K.S.op("dve", lambda e, O3=O3, ntl=ntl: e.reciprocal(st[:, 0:ntl], O3[:, :, 64]), ["pb0"], ["st"])
                K.tt("dve", oc[:, 0:ntl, :], O3[:, :, 0:64], bc3(st[:, 0:ntl], 64), ALU.mult, ["pb0", "st"], ["oc"])
                K.dma("sp", mix_d[t0 * 128:t0 * 128 + nq, 640 + h * 64:704 + h * 64].rearrange("(q p) d -> p q d", p=128),
                      oc[:, 0:ntl, :], ["oc"], [("mixd", t0 + i_) for i_ in range(ntl)])

        for i in range(len(itemsC) + DSK):
            if i < len(itemsC):
                qkC(i, itemsC[i])
            if i - DSK >= 0:
                pvC(i - DSK, itemsC[i - DSK])

        S.barrier()
        NB = (2 * T + 255) // 256 + 64
        h2_d, x1_d, xs_d, ys_d = d[f"h2_{L}"], d[f"x1_{L}"], d[f"xsl_{L}"], d[f"ys_{L}"]
        w1r = w1_d.rearrange("e p c n -> (e p) (c n)")
        w3r = w3_d.rearrange("e p c n -> (e p) (c n)")
        w2r = w2_d.rearrange("e p c n -> (e p) (c n)")
        WSZ = 12288
        Wb = [arena[:, i * WSZ:(i + 1) * WSZ] for i in range(2)]
        wout = arena[:, 2 * WSZ:2 * WSZ + 8192].rearrange("p (c n) -> p c n", c=8)
        ohb = arena[:, 2 * WSZ + 8192:2 * WSZ + 8192 + NT * 128].rearrange("p (t e) -> p t e", t=NT)
        assert 2 * WSZ + 8192 + NT * 128 <= AR
        K.dma("pool", wout, wout_d[:, :, :], [], ["wout"])
        ut = K.sb("ut", [128, 128], F32)
        iop = K.sb("iop", [128, 1], F32)
        K.dma("sp", ut[:, :], d["ut"][:, :], [], ["ut"])
        K.dma("sp", iop[:, :], d["iop"][:, :], [], ["iop"])
        base = K.sb("base", [128, 64], F32)
        K.memset("dve", base[:, :], 0.0, [], ["base"])
        rk = K.sb("rk", [128, NT, 2], F32)
        wts = K.sb("wts", [128, NT, 2], F32)
        dstf = K.sb("dstf", [128, NT, 2], F32)
        dsti = K.sb("dsti", [128, NT * 2], I32)
        h2s = [K.sb(f"h2s{i}", [128, D], BF16) for i in range(2)]
        MB = [K.sb(f"MB{i}", [128, D], F32) for i in range(4)]
        for gi, (src, wh) in enumerate((("A2", 0), ("A2", 1), ("B2", 0), ("B2", 1))):
            for c in range(8):
                col = (A2[:, 2 * c + wh:2 * c + wh + 1] if src == "A2"
                       else modT[:, 2 * (24 + c) + wh:2 * (24 + c) + wh + 1])
                K.ts("dve", gbc[:, :], ones[:, :], col, None, ALU.mult, None, ["ones", "modT", "A2"], ["gbc"])
                K.mm(PB[7][:, (c % 4) * 128:(c % 4 + 1) * 128], gbc[:, :], identf[:, :], True, True, ["gbc", "identf"], ["pb7"])
                K.cp("act", MB[gi][:, c * 128:(c + 1) * 128], PB[7][:, (c % 4) * 128:(c % 4 + 1) * 128], ["pb7"], [("MB", gi)])

        def rstd_of(ss, dim):
            K.ts("dve", ss, ss, 1.0 / dim, EPS, ALU.mult, ALU.add, ["st"], ["st"])
            K.act(ss, ss, AF.Sqrt, ["st"], ["st"])
            K.S.op("dve", lambda e, a=ss: e.reciprocal(a, a), ["st"], ["st"])

        x1t = [K.sb(f"x1t{i}", [128, D], F32) for i in range(2)]
        for t in range(NT):
            b = t % 2
            wh = 0 if t < NT_OWN else 1
            rows = slice(t * 128, (t + 1) * 128)
            X1 = x1t[b]
            K.dma("sp", xt[b][:, :], x_d[rows, :], [], [("xt", b)])
            K.dma("sp", mixrow[:, 256:1024], mix_d[rows, 256:1024], [("mixd", t)], ["mixrow"])
            K.dma("sp", mixrow[:, 0:256], oa_d[rows, :], [], ["mixrow"])
            for c in range(8):
                K.tr(PB0b[:, c * 128:(c + 1) * 128], mixrow[:, c * 128:(c + 1) * 128], identb[:, :], ["mixrow", "identb"], ["pb0"])
            K.cp("dve", mixT[:, :, :], v3(PB0b[:, :], 8), ["pb0"], ["mixT"])
            for half in range(2):
                pb = 1 + half
                for c in range(8):
                    K.mm(PB[pb][:, :], mixT[:, c, :], wout[:, c, half * 512:(half + 1) * 512], c == 0, c == 7,
                         ["mixT", "wout"], [f"pb{pb}"])
                hs = slice(half * 512, (half + 1) * 512)
                K.tt("dve", tmpf[:, hs], PB[pb][:, :], G[wh][:, hs], ALU.mult, [f"pb{pb}", ("G", wh)], ["tmpf"])
                K.tt("pool", X1[:, hs], tmpf[:, hs], xt[b][:, hs], ALU.add, ["tmpf", ("xt", b)], [("x1", b)])
            K.dma("sp", x1_d[rows, :], X1[:, :], [("x1", b)], [])
            K.tt("pool", tmpf[:, :], X1[:, :], X1[:, :], ALU.mult, [("x1", b), "tmpf"], ["tmpf"])
            K.rsum("dve", st[:, 0:1], tmpf[:, :], ["tmpf"], ["st"])
            rstd_of(st[:, 0:1], D)
            K.act(h2n[:, :], X1[:, :], AF.Copy, [("x1", b), "st"], ["h2n"], scale=st[:, 0:1])
            K.tt("pool", tmpf[:, :], h2n[:, :], MB[wh][:, :], ALU.mult, ["h2n", ("MB", wh), "tmpf"], ["tmpf"])
            K.tt("pool", h2s[b][:, :], tmpf[:, :], MB[2 + wh][:, :], ALU.add, ["tmpf", ("MB", 2 + wh)], [("h2s", b)])
            K.dma("sp", h2_d[rows, :], h2s[b][:, :], [("h2s", b)], [])
            for c in range(8):
                pb = 6 + c // 4
                K.tr(PB[pb][:, (c % 4) * 128:(c % 4 + 1) * 128], h2n[:, c * 128:(c + 1) * 128], identf[:, :], ["h2n", "identf"], [f"pb{pb}"])
            for c in range(8):
                pb = 6 + c // 4
                K.ts("dve", h2Tf[:, c, :], PB[pb][:, (c % 4) * 128:(c % 4 + 1) * 128], A2[:, 2 * c + wh:2 * c + wh + 1],
                     modT[:, 2 * (24 + c) + wh:2 * (24 + c) + wh + 1], ALU.mult, ALU.add, [f"pb{pb}", "A2", "modT"], ["h2Tf"])
            for c in range(8):
                K.mm(PB[3][:, 0:72], h2Tf[:, c, :], wgr[:, c, :], c == 0, c == 7, ["h2Tf", "wgr"], ["pb3"])
            K.cp("act", lg[:, :], PB[3][:, 0:72], ["pb3"], ["lg"])
            gl = lg[:, 0:8]
            rl3 = v3(lg[:, 8:72], 8)
            s_gmax, s_ngmax, s_gsum, s_m1, s_m2, s_d, s_e2, s_wa, s_wb = [st[:, 16 + i:17 + i] for i in range(9)]
            goh, gex, pen = st[:, 32:40], st[:, 40:48], st[:, 48:56]
            RK = ["lg", "st", "r64"]
            K.S.op("dve", lambda e, a=s_gmax, g=gl: e.reduce_max(a, g, AX.X), ["lg"], ["st"])
            K.ts("dve", goh, gl, s_gmax, None, ALU.is_equal, None, RK, ["st"])
            K.ts("dve", s_ngmax, s_gmax, -1.0, None, ALU.mult, None, RK, ["st"])
            K.act(gex, gl, AF.Exp, RK, ["st"], bias=s_ngmax)
            K.rsum("dve", s_gsum, gex, RK, ["st"])
            K.S.op("dve", lambda e, a=s_gsum: e.reciprocal(a, a), RK, ["st"])
            K.ts("dve", pen, goh, BIG, -BIG, ALU.mult, ALU.add, RK, ["st"])
            rm, oh1, rm2, oh2 = [r[:, :] for r in r64]
            K.tt("dve", v3(rm, 8), rl3, bc3(pen, 8), ALU.add, RK, ["r64"])
            K.S.op("dve", lambda e, a=s_m1, g=rm: e.reduce_max(a, g, AX.X), RK, ["st"])
            K.ts("dve", oh1, rm, s_m1, None, ALU.is_equal, None, RK, ["r64"])
            K.stt("dve", rm2, oh1, -BIG, rm, ALU.mult, ALU.add, RK, ["r64"])
            K.S.op("dve", lambda e, a=s_m2, g=rm2: e.reduce_max(a, g, AX.X), RK, ["st"])
            K.ts("dve", oh2, rm2, s_m2, None, ALU.is_equal, None, RK, ["r64"])
            K.tt("dve", s_d, s_m2, s_m1, ALU.subtract, RK, ["st"])
            K.act(s_e2, s_d, AF.Exp, RK, ["st"])
            K.ts("dve", s_wa, s_e2, 1.0, None, ALU.add, None, RK, ["st"])
            K.S.op("dve", lambda e, a=s_wa: e.reciprocal(a, a), RK, ["st"])
            K.tt("dve", wts[:, t, 0:1], s_wa, s_gsum, ALU.mult, RK, ["wts"])
            K.tt("dve", wts[:, t, 1:2], wts[:, t, 0:1], s_e2, ALU.mult, RK + ["wts"], ["wts"])
            K.cp("dve", ohb[:, t, 0:64], oh1, RK, ["ohb"])
            K.cp("dve", ohb[:, t, 64:128], oh2, RK, ["ohb"])
            K.tt("dve", rm, oh1, oh2, ALU.add, RK, ["r64"])
            K.mm(PB[3][:, 128:192], ut[:, :], rm, True, True, ["ut", "r64"], ["pb3"])
            K.mm(PB[3][:, 192:256], ones[:, :], rm, True, True, ["ones", "r64"], ["pb3"])
            K.tt("dve", rm2, PB[3][:, 128:192], base[:, :], ALU.add, ["pb3", "base", "r64"], ["r64"])
            K.tt("dve", rm, oh1, rm2, ALU.mult, RK, ["r64"])
            K.rsum("dve", rk[:, t, 0:1], rm, RK, ["rk"])
            K.tt("dve", rm, oh2, rm2, ALU.mult, RK, ["r64"])
            K.rsum("dve", rk[:, t, 1:2], rm, RK, ["rk"])
            K.tt("dve", base[:, :], PB[3][:, 192:256], base[:, :], ALU.add, ["pb3", "base", "r64"], ["base"])
        cs = [r64[0][:, :], r64[1][:, :]]
        pc = r64[2][:, :]
        pst = r64[3][:, :]
        KS = ["base", "r64"]
        K.ts("dve", pc, base[:, :], 255.0, None, ALU.add, None, KS, ["r64"])
        pci = K.sb("pci", [128, 64], I32)
        K.cp("dve", pci[:, :], pc, KS, ["pci"])
        K.ts("dve", pci[:, :], pci[:, :], 8, 8, ALU.arith_shift_right, ALU.logical_shift_left, ["pci"], ["pci"])
        K.cp("dve", pc, pci[:, :], ["pci"] + KS, ["r64"])
        K.cp("dve", cs[0], pc, KS, ["r64"])
        cur = 0
        for sh in (1, 2, 4, 8, 16, 32):
            K.cp("dve", cs[1 - cur][:, 0:sh], cs[cur][:, 0:sh], KS, ["r64"])
            K.tt("dve", cs[1 - cur][:, sh:64], cs[cur][:, sh:64], cs[cur][:, 0:64 - sh], ALU.add, KS, ["r64"])
            cur = 1 - cur
        pend = cs[cur]
        K.tt("dve", pst, pend, pc, ALU.subtract, KS, ["r64"])
        other = cs[1 - cur]
        for t in range(NT):
            for k in range(2):
                K.tt("dve", other, ohb[:, t, 64 * k:64 * k + 64], pst, ALU.mult, KS + ["ohb"], ["r64"])
                K.rsum("dve", dstf[:, t, k:k + 1], other, KS, ["dstf"])
        K.tt("dve", dstf[:, :, :], dstf[:, :, :], rk[:, :, :], ALU.add, ["dstf", "rk"], ["dstf"])
        K.cp("dve", dsti[:, :], dstf[:, :, :].rearrange("p t k -> p (t k)"), ["dstf"], ["dsti"])
        zt = K.sb("zt", [128, D], BF16)
        K.memset("pool", zt[:, :], 0.0, [], ["zt"])
        for bk in range(2 * NB):
            K.dma("sp" if bk % 2 == 0 else "act", xs_d[bk * 128:(bk + 1) * 128, :], zt[:, :], ["zt"], ["xs"])
        S.barrier()
        for t in range(NT):
            b = t % 2
            K.dma("sp", h2s[b][:, :], h2_d[t * 128:(t + 1) * 128, :], [], [("h2s", b)])
            for k in range(2):
                K.S.dmafn("pool", lambda e, src=h2s[b][:, :], ix=dsti[:, 2 * t + k:2 * t + k + 1]: e.indirect_dma_start(
                    out=xs_d[:, :], out_offset=bass.IndirectOffsetOnAxis(ap=ix, axis=0), in_=src, in_offset=None),
                    [("h2s", b), "dsti"], ["xs"])
        S.barrier()
        xsb = K.sb("xsb", [128, 2, D], BF16)
        xT = K.sb("xTb", [128, 8, 256], BF16)
        ysbb = K.sb("ysbb", [128, 2, D], F32)
        ysb = [ysbb[:, 0, :], ysbb[:, 1, :]]
        widx2 = K.sb("widx2", [128, 2], I32)
        NROWS_W = NE * 128
        for bk in range(NB):
            wb = bk % 2
            W = Wb[wb]
            w1e = W[:, 0:4096].rearrange("p (c n) -> p c n", c=8)
            w3e = W[:, 4096:8192].rearrange("p (c n) -> p c n", c=8)
            w2e = W[:, 8192:12288].rearrange("p (c n) -> p c n", c=4)
            ef = st[:, 60:61]
            fl = st[:, 61:62]
            K.ts("dve", other, pend, float(256 * bk), None, ALU.is_le, None, ["r64"], ["r64b"])
            K.rsum("dve", ef, other, ["r64b"], ["st"])
            K.ts("dve", ef, ef, float(NE - 1), 128.0, ALU.min, ALU.mult, ["st"], ["st"])
            K.tt("dve", ef, ef, iop[:, :], ALU.add, ["st", "iop"], ["st"])
            K.cp("dve", widx2[:, wb:wb + 1], ef, ["st"], [("widx2", wb)])
            for (dst, srcw, wk) in ((W[:, 0:4096], w1r, "W1"), (W[:, 4096:8192], w3r, "W3"), (W[:, 8192:12288], w2r, "W2")):
                K.S.dmafn("pool", lambda e, o=dst, sw=srcw, ix=widx2[:, wb:wb + 1]: e.indirect_dma_start(
                    out=o, out_offset=None, in_=sw[:, :], in_offset=bass.IndirectOffsetOnAxis(ap=ix, axis=0)),
                    [("widx2", wb)], [(wk, wb)])
            K.dma("sp", xsb[:, :, :], xs_d[bk * 256:(bk + 1) * 256, :].rearrange("(a p) f -> p a f", p=128), [], ["xsb"])
            for a in range(2):
                for c in range(8):
                    K.tr(PB0b[:, c * 128:(c + 1) * 128], xsb[:, a, c * 128:(c + 1) * 128], identb[:, :], ["xsb", "identb"], ["pb0"])
                K.cp("dve", xT[:, :, a * 128:(a + 1) * 128], v3(PB0b[:, :], 8), ["pb0"], ["xT"])
            for m in range(4):
                p1, p3 = 1 + m % 2, 4 + m % 2
                for c in range(8):
                    K.mm(PB[p1][:, 0:256], w1e[:, c, m * 128:(m + 1) * 128], xT[:, c, :], c == 0, c == 7, [("W1", wb), "xT"], [f"pb{p1}"])
                for c in range(8):
                    K.mm(PB[p3][:, 0:256], w3e[:, c, m * 128:(m + 1) * 128], xT[:, c, :], c == 0, c == 7, [("W3", wb), "xT"], [f"pb{p3}"])
                K.act(sil[m % 2][:, 0:256], PB[p1][:, 0:256], AF.Silu, [f"pb{p1}"], [("sil", m % 2)])
                K.tt("dve", hT[:, m, 0:256], PB[p3][:, 0:256], sil[m % 2][:, 0:256], ALU.mult, [f"pb{p3}", ("sil", m % 2)], ["hTe"])
            for a in range(2):
                for half in range(2):
                    py = 6 + half
                    for kc in range(4):
                        K.mm(PB[py][:, :], hT[:, kc, a * 128:(a + 1) * 128], w2e[:, kc, half * 512:(half + 1) * 512], kc == 0, kc == 3,
                             ["hTe", ("W2", wb)], [f"pb{py}"])
                    K.cp("act", ysbb[:, a, half * 512:(half + 1) * 512], PB[py][:, :], [f"pb{py}"], [("ysb", a)])
                K.dma("sp", ys_d[bk * 256 + a * 128:bk * 256 + (a + 1) * 128, :], ysbb[:, a, :], [("ysb", a)], [])
        S.barrier()
        for t in range(NT):
            b = t % 2
            wh = 0 if t < NT_OWN else 1
            rows = slice(t * 128, (t + 1) * 128)
            K.dma("sp", x1t[b][:, :], x1_d[rows, :], [], [("x1", b)])
            for k in range(2):
                K.S.dmafn("pool", lambda e, o=ysb[k][:, :], ix=dsti[:, 2 * t + k:2 * t + k + 1]: e.indirect_dma_start(
                    out=o, out_offset=None, in_=ys_d[:, :], in_offset=bass.IndirectOffsetOnAxis(ap=ix, axis=0)),
                    ["dsti"], [("ysb", k)])
            K.ts("dve", tmpf[:, :], ysb[0][:, :], wts[:, t, 0:1], None, ALU.mult, None, [("ysb", 0), "wts"], ["tmpf"])
            K.stt("dve", tmpf[:, :], ysb[1][:, :], wts[:, t, 1:2], tmpf[:, :], ALU.mult, ALU.add, [("ysb", 1), "wts", "tmpf"], ["tmpf"])
            K.tt("pool", tmpf[:, :], tmpf[:, :], G[2 + wh][:, :], ALU.mult, ["tmpf", ("G", 2 + wh)], ["tmpf"])
            K.tt("pool", h2n[:, :], tmpf[:, :], x1t[b][:, :], ALU.add, ["tmpf", ("x1", b)], ["h2n"])
            K.dma("sp", xo_d[rows, :], h2n[:, :], ["h2n"], [])
        S.emit()


def build_btab(rpb, NT_OWN, rank):
    rows_total = 8 * NT_OWN
    specials = _specials(NT_OWN)
    gts = [rows_total // 4] + [rank * NT_OWN + t for t in specials]
    qc = np.arange(64)
    kc = np.arange(64)
    c0 = np.clip(qc - 8, 0, 48)
    colok = (kc[:, None] >= c0[None, :]) & (kc[:, None] < c0[None, :] + 16)
    dc = np.clip(kc[:, None] - qc[None, :], -15, 15) + 15
    tab = np.full((len(gts), 2, 64, 6, 16, 64), NEG, np.float32)
    for ci, gt in enumerate(gts):
        for e in range(16):
            b = (e + 1) % 2
            j = (7 + b - e) // 2
            qrow = 2 * gt + b
            r0 = min(max(qrow - 4, 0), rows_total - 8)
            for a in range(2):
                krow = 2 * (gt + j) + a
                if krow < 0 or krow >= rows_total or krow < r0 or krow >= r0 + 8:
                    continue
                dr = krow - qrow + 7
                vals = rpb[:, dr, :][:, dc]
                vals = np.where(colok[None], vals, np.float32(NEG))
                tab[ci, a, :, :, e, :] = vals.transpose(1, 0, 2)
    return tab.reshape(len(gts), 128, 6, 1024).astype(NPBF)


def prep_B(P, i, x_cur, xc_cur, NT_OWN, outsA, ranks_per_batch=4, NE=64):
    B = x_cur.shape[0]
    n = NT_OWN * 128
    NTB = ranks_per_batch * NT_OWN
    shared = {
        "w_out": _kmaj(P["w_out"][i], 8),
        "norm_ffnT": _colT(P["norm_ffn"][i]),
        "wgr": _kmaj(np.concatenate([P["moe_w_group"][i], P["moe_w_router"][i]], axis=1), 8),
        "w1": np.ascontiguousarray(P["moe_w1"][i][:NE].reshape(NE, 8, 128, 512).transpose(0, 2, 1, 3)),
        "w3": np.ascontiguousarray(P["moe_w3"][i][:NE].reshape(NE, 8, 128, 512).transpose(0, 2, 1, 3)),
        "w2": np.ascontiguousarray(P["moe_w2"][i][:NE].reshape(NE, 4, 128, 1024).transpose(0, 2, 1, 3)),
        "ident": np.eye(128, dtype=np.float32),
    }
    maps = []
    for b in range(B):
        cores = [outsA[b * ranks_per_batch + r] for r in range(ranks_per_batch)]
        kv_tiles = [c["kvb"][:n].reshape(NT_OWN, 128, 768) for c in cores]
        kv_all = np.concatenate(kv_tiles, axis=0)
        for r in range(ranks_per_batch):
            me = cores[r]
            m = dict(shared)
            m["x"] = np.ascontiguousarray(np.concatenate([x_cur[b, r * n:(r + 1) * n], xc_cur[b]], axis=0))
            m["oa"] = me["oa"]
            m["qbT"] = me["qbT"]
            m["qcT"] = me["qcT"]
            m["modT"] = me["modT"]
            win = np.zeros((NT_OWN + 8, 128, 768), NPBF)
            for s in range(NT_OWN + 6):
                g = r * NT_OWN - 3 + s
                if 0 <= g < NTB:
                    win[s] = kv_all[g]
            win[NT_OWN + 6:] = me["kvb"][n:].reshape(2, 128, 768)
            m["kvbw"] = win.reshape(-1, 768)
            m["kcT_all"] = np.ascontiguousarray(np.concatenate([c["kcT"][:, :, :n] for c in cores] + [me["kcT"][:, :, n:]], axis=2))
            m["vc_all"] = np.ascontiguousarray(np.concatenate([c["vc"][:n] for c in cores] + [me["vc"][n:]], axis=0))
            m["btab"] = build_btab(P["b_rpb"][i], NT_OWN, r)
            maps.append(m)
    return maps


def declare_dram(nc, NT_OWN, NR=4, NE=64):
    NT = NT_OWN + 2
    T = NT * 128
    n = NT_OWN * 128
    NCLS = 1 + len(_specials(NT_OWN))
    d = {}

    def t(name, shape, dt, kind="Internal"):
        d[name] = nc.dram_tensor(name, list(shape), dt, kind=kind).ap()

    EI = "ExternalInput"
    t("x", [T, D], F32, EI)
    t("cT", [128, 16], F32, EI)
    t("rope", [T, 32], F32, EI)
    t("ident", [128, 128], F32, EI)
    t("widx", [128, 6], I32, EI)
    t("ut", [128, 128], F32, EI)
    t("iop", [128, 1], F32, EI)
    t("xo", [T, D], F32, "ExternalOutput")
    t("xs_0", [T, D], F32)
    for L in range(2):
        t(f"w_ada_{L}", [128, 8, 6144], F32, EI)
        t(f"b_adaT_{L}", [128, 48], F32, EI)
        t(f"norm_mixT_{L}", [128, 8], F32, EI)
        t(f"w_in_{L}", [128, 8, INW], F32, EI)
        t(f"w_sT_{L}", [128, 4, 128], F32, EI)
        t(f"b_s_{L}", [128, 4], F32, EI)
        t(f"gains_{L}", [128, G_TOT], F32, EI)
        t(f"wq_{L}", [128, 3, 576], F32, EI)
        t(f"wkv_{L}", [128, 2, 768], F32, EI)
        t(f"btab_{L}", [NCLS, 128, 6, 1024], BF16, EI)
        t(f"w_out_{L}", [128, 8, D], F32, EI)
        t(f"norm_ffnT_{L}", [128, 8], F32, EI)
        t(f"wgr_{L}", [128, 8, 72], F32, EI)
        t(f"w1_{L}", [NE, 128, 8, 512], F32, EI)
        t(f"w3_{L}", [NE, 128, 8, 512], F32, EI)
        t(f"w2_{L}", [NE, 128, 4, D], F32, EI)
        t(f"oa_{L}", [T, 256], BF16)
        t(f"qbT_{L}", [3, 128, T], BF16)
        t(f"qcT_{L}", [6, 96, T], BF16)
        t(f"modT_{L}", [128, 96], F32)
        t(f"mixd_{L}", [T, D], BF16)
        PT = min(4, NT_OWN)
        for j in range(NT_OWN // PT):
            t(f"kcT_own_{L}_{j}", [576, PT * 128], BF16)
            t(f"kcT_g_{L}_{j}", [NR * 576, PT * 128], BF16)
            t(f"vc_own_{L}_{j}", [PT * 128, 390], BF16)
            t(f"vc_g_{L}_{j}", [NR * PT * 128, 390], BF16)
        t(f"kcT_ctx_{L}", [576, 256], BF16)
        t(f"vc_ctx_{L}", [256, 390], BF16)
        t(f"kvb_own_{L}", [n, 768], BF16)
        t(f"kvb_ctx_{L}", [256, 768], BF16)
        HT = min(3, NT_OWN)
        t(f"kvb_lo_{L}", [HT * 128, 768], BF16)
        t(f"kvb_hi_{L}", [HT * 128, 768], BF16)
        t(f"kvb_glo_{L}", [NR * HT * 128, 768], BF16)
        t(f"kvb_ghi_{L}", [NR * HT * 128, 768], BF16)
        t(f"h2_{L}", [T, D], BF16)
        t(f"x1_{L}", [T, D], F32)
        t(f"xsl_{L}", [((2 * T + 255) // 256 + 64) * 256, D], BF16)
        t(f"ys_{L}", [((2 * T + 255) // 256 + 64) * 256, D], F32)
    return d


def emit_X(nc, gc, d, L, groups):
    stack = contextlib.ExitStack()
    with stack:
        K = KB(nc, stack, gc)
        pairs = [(d[f"kvb_lo_{L}"], d[f"kvb_glo_{L}"]), (d[f"kvb_hi_{L}"], d[f"kvb_ghi_{L}"])]
        j = 0
        while f"vc_own_{L}_{j}" in d:
            pairs.append((d[f"vc_own_{L}_{j}"], d[f"vc_g_{L}_{j}"]))
            pairs.append((d[f"kcT_own_{L}_{j}"], d[f"kcT_g_{L}_{j}"]))
            j += 1
        for nm, (src, dst) in enumerate(pairs):
            K.S.cc(lambda e, src=src, dst=dst: e.collective_compute(
                "AllGather", ALU.bypass, replica_groups=groups, ins=[src[:, :]], outs=[dst[:, :]]), [], [nm])
        K.S.emit()


def build_fused(NT_OWN, ngroups=2, NR=4, TB=4, NE=64, do_x=True):
    nc = bass.Bass("TRN2", target_bir_lowering=False)
    gstack = contextlib.ExitStack()
    with gstack:
        gc = GC(gstack)
        d = declare_dram(nc, NT_OWN, NR, NE)
        groups = [list(range(g * NR, (g + 1) * NR)) for g in range(ngroups)]
        for L in range(2):
            emit_A(nc, gc, d, NT_OWN, L)
            emit_B(nc, gc, d, NT_OWN, L, TB=TB, NR=NR, NE=NE, groups=groups if do_x else None)
    return nc


def halo_index(NT_OWN, r, NR):
    HT = min(3, NT_OWN)
    idx = np.zeros((128, 6), np.int32)
    for col in range(6):
        g = r * NT_OWN - 3 + col if col < 3 else (r + 1) * NT_OWN + (col - 3)
        if not (0 <= g < NR * NT_OWN):
            continue
        rk, l = g // NT_OWN, g % NT_OWN
        if col < 3:
            t2 = l - (NT_OWN - HT)
        else:
            t2 = l
        if not (0 <= t2 < HT):
            continue
        idx[:, col] = (rk * HT + t2) * 128 + np.arange(128)
    return idx


def prep_fused(P, NT_OWN, NR=4):
    x = np.asarray(P["x"], np.float32)
    ctx = np.asarray(P["ctx"], np.float32)
    B = x.shape[0]
    n = NT_OWN * 128
    shared = {"ident": np.eye(128, dtype=np.float32),
              "ut": np.triu(np.ones((128, 128), np.float32), 1),
              "iop": np.arange(128, dtype=np.float32).reshape(128, 1)}
    per_rank = [dict() for _ in range(NR)]
    for L in range(2):
        gains = np.concatenate([
            P["a_v_norm"][L], np.tile(P["b_q_norm"][L], 6), np.tile(P["b_k_norm"][L], 6), P["c_q_a_norm"][L],
            P["c_kv_a_norm"][L], np.tile(P["c_q_norm"][L], 6), np.tile(P["c_k_norm"][L][:64], 6), P["c_k_norm"][L][64:]])
        shared.update({
            f"w_ada_{L}": _kmaj(P["w_ada"][L], 8),
            f"b_adaT_{L}": _colT(P["b_ada"][L]),
            f"norm_mixT_{L}": _colT(P["norm_mix"][L]),
            f"w_in_{L}": _kmaj(P["w_in"][L], 8),
            f"w_sT_{L}": np.ascontiguousarray(P["a_w_s"][L].transpose(2, 0, 1)),
            f"b_s_{L}": np.ascontiguousarray(P["a_b_s"][L].T),
            f"gains_{L}": _bcrow(gains.astype(np.float32)),
            f"wq_{L}": _kmaj(P["c_w_q_up"][L], 3),
            f"wkv_{L}": _kmaj(P["c_w_kv_up"][L], 2),
            f"w_out_{L}": _kmaj(P["w_out"][L], 8),
            f"norm_ffnT_{L}": _colT(P["norm_ffn"][L]),
            f"wgr_{L}": _kmaj(np.concatenate([P["moe_w_group"][L], P["moe_w_router"][L]], axis=1), 8),
            f"w1_{L}": np.ascontiguousarray(P["moe_w1"][L].reshape(64, 8, 128, 512).transpose(0, 2, 1, 3)),
            f"w3_{L}": np.ascontiguousarray(P["moe_w3"][L].reshape(64, 8, 128, 512).transpose(0, 2, 1, 3)),
            f"w2_{L}": np.ascontiguousarray(P["moe_w2"][L].reshape(64, 4, 128, 1024).transpose(0, 2, 1, 3)),
        })
        for r in range(NR):
            per_rank[r][f"btab_{L}"] = build_btab(P["b_rpb"][L], NT_OWN, r)
    for r in range(NR):
        per_rank[r]["rope"] = rope_tables(NT_OWN, r)
        per_rank[r]["widx"] = halo_index(NT_OWN, r, NR)
    maps = []
    for b in range(B):
        cT = np.ascontiguousarray(np.stack([_colT(P["c"][b]), _colT(P["c_ctx"])], axis=2).reshape(128, 16))
        for r in range(NR):
            m = dict(shared)
            m.update(per_rank[r])
            m["x"] = np.ascontiguousarray(np.concatenate([x[b, r * n:(r + 1) * n], ctx[b]], axis=0))
            m["cT"] = cT
            maps.append(m)
    return maps


_PROG = {}


def kernel(**inputs):
    P = {k: np.asarray(v) for k, v in inputs.items()}
    NT_OWN = 32
    n = NT_OWN * 128
    if "fused" not in _PROG:
        _PROG["fused"] = build_fused(NT_OWN)
    maps = prep_fused(P, NT_OWN)
    res = run_bass_kernel_spmd(_PROG["fused"], maps, core_ids=list(range(8))).results
    out = np.empty((2, 4 * n, D), np.float32)
    for b in range(2):
        for r in range(4):
            out[b, r * n:(r + 1) * n] = res[b * 4 + r]["xo"][:n]
    return out
```
